# Optimizing a Trainium2 kernel written in Bass

```python
import jax, jax.numpy as jnp
from jax import lax
import numpy as np

D_MODEL = 1024
BATCH = 16
SEQ = 2048
DEPTH = 4

N_BRANCH = 3
MIX_W = D_MODEL // 2

RWKV_HEAD = 64
RWKV_HEADS = MIX_W // RWKV_HEAD
RWKV_DECAY_RANK = 64
RWKV_A_RANK = 64
RWKV_G_RANK = 128
RWKV_GN_EPS = RWKV_HEAD * 1e-5

S5_GROUP = 16
S5_GROUPS = MIX_W // S5_GROUP
S5_STATE = 64

GLA_HEADS = 4
GLA_DV = MIX_W // GLA_HEADS
GLA_DK = GLA_DV // 2
GLA_GATE_RANK = 16
GLA_GATE_NORM = 16.0
GLA_CHUNK = 64

N_MEM = 256
XA_HEADS = 4
XA_HEAD = D_MODEL // XA_HEADS

D_FF = 2816
CONV_W = 3
NORM_EPS = 1e-6

RWKV_SIZES = (MIX_W, MIX_W, MIX_W, RWKV_DECAY_RANK, RWKV_A_RANK, RWKV_G_RANK)
RWKV_COLS = 3 * MIX_W + RWKV_DECAY_RANK + RWKV_A_RANK + RWKV_G_RANK
S5_COLS = MIX_W
GLA_SIZES = (GLA_HEADS * GLA_DK, GLA_HEADS * GLA_DK, MIX_W, GLA_GATE_RANK, MIX_W)
GLA_COLS = 2 * GLA_HEADS * GLA_DK + 2 * MIX_W + GLA_GATE_RANK
GATE_COLS = N_BRANCH * D_MODEL
IN_SIZES = (RWKV_COLS, S5_COLS, GLA_COLS, GATE_COLS)
N_IN = RWKV_COLS + S5_COLS + GLA_COLS + GATE_COLS

kernel_name = "hybrid_rwkv7_s5_gla_gated_trunk"

F32 = jnp.float32


def _split(t, sizes):
    idx = np.cumsum(np.array(sizes))[:-1].tolist()
    return jnp.split(t, idx, axis=-1)


def rms_norm(x, g):
    xf = x.astype(F32)
    y = xf * lax.rsqrt(jnp.mean(xf * xf, axis=-1, keepdims=True) + NORM_EPS)
    return (y * g).astype(x.dtype)


def _token_shift(p):
    return jnp.pad(p, ((0, 0), (1, 0), (0, 0)))[:, :-1]


def rwkv7_mix(p, mu, w0, w_up, a0, a_up, g_up, k_k, k_a, r_k, ln_w, ln_b):
    B, L, _ = p.shape
    p = p + (_token_shift(p) - p) * mu
    r, k, v, wd, ad, gd = _split(p, RWKV_SIZES)
    w_log = -jax.nn.softplus(-(w0 + jnp.tanh(wd) @ w_up)) - 0.5
    a = jax.nn.sigmoid(a0 + ad @ a_up)
    g = jax.nn.sigmoid(gd) @ g_up

    def heads(t):
        return t.reshape(B, L, RWKV_HEADS, RWKV_HEAD).astype(F32)

    kk = heads(k * k_k)
    kk = kk * lax.rsqrt(jnp.sum(kk * kk, axis=-1, keepdims=True) + 1e-12)
    k = k * (1 + (a - 1) * k_a)
    r_h, k_h, v_h, a_h = heads(r), heads(k), heads(v), heads(a)
    decay = jnp.exp(-jnp.exp(heads(w_log)))

    def step(S, inp):
        r_t, w_t, k_t, v_t, kk_t, a_t = inp
        sa = jnp.einsum('bhvk,bhk->bhv', S, -kk_t)
        S = (S * w_t[:, :, None, :]
             + sa[..., None] * (kk_t * a_t)[:, :, None, :]
             + v_t[..., None] * k_t[:, :, None, :])
        return S, jnp.einsum('bhvk,bhk->bhv', S, r_t)

    seq = tuple(jnp.moveaxis(t, 1, 0) for t in (r_h, decay, k_h, v_h, kk, a_h))
    S0 = jnp.zeros((B, RWKV_HEADS, RWKV_HEAD, RWKV_HEAD), F32)
    _, y = lax.scan(step, S0, seq)
    y = jnp.moveaxis(y, 0, 1)
    mean = jnp.mean(y, axis=-1, keepdims=True)
    var = jnp.mean(jnp.square(y - mean), axis=-1, keepdims=True)
    y = ((y - mean) * lax.rsqrt(var + RWKV_GN_EPS)).reshape(B, L, MIX_W) * ln_w + ln_b
    bonus = jnp.sum(r_h * k_h * r_k, axis=-1, keepdims=True) * v_h
    y = (y + bonus.reshape(B, L, MIX_W)) * g
    return y.astype(p.dtype)


def _complex_affine_combine(e1, e2):
    a1r, a1i, b1r, b1i = e1
    a2r, a2i, b2r, b2i = e2
    return (a2r * a1r - a2i * a1i,
            a2r * a1i + a2i * a1r,
            a2r * b1r - a2i * b1i + b2r,
            a2r * b1i + a2i * b1r + b2i)


def s5_mix(u, a_re, a_im, b_re, b_im, c_re, c_im, d, log_step, w_glu, b_glu):
    B, L, _ = u.shape
    uf = u.astype(F32).reshape(B, L, S5_GROUPS, S5_GROUP)
    ar, ai = a_re.astype(F32), a_im.astype(F32)
    dt = jnp.exp(log_step.astype(F32))[:, None]
    mag = jnp.exp(dt * ar)
    ang = dt * ai
    abar_re, abar_im = mag * jnp.cos(ang), mag * jnp.sin(ang)
    den = ar * ar + ai * ai
    nr, ni = abar_re - 1.0, abar_im
    f_re = ((nr * ar + ni * ai) / den)[..., None]
    f_im = ((ni * ar - nr * ai) / den)[..., None]
    br, bi = b_re.astype(F32), b_im.astype(F32)
    bbar_re = f_re * br - f_im * bi
    bbar_im = f_re * bi + f_im * br
    bu_re = jnp.einsum('blgc,gnc->blgn', uf, bbar_re)
    bu_im = jnp.einsum('blgc,gnc->blgn', uf, bbar_im)
    shape = (1, L, S5_GROUPS, S5_STATE)
    elems = (jnp.broadcast_to(abar_re, shape), jnp.broadcast_to(abar_im, shape), bu_re, bu_im)
    _, _, s_re, s_im = lax.associative_scan(_complex_affine_combine, elems, axis=1)
    y = (jnp.einsum('blgn,gcn->blgc', s_re, c_re.astype(F32))
         - jnp.einsum('blgn,gcn->blgc', s_im, c_im.astype(F32))
         + d.astype(F32) * uf)
    y = jax.nn.gelu(y.reshape(B, L, MIX_W))
    y = y * jax.nn.sigmoid(y @ w_glu.astype(F32) + b_glu.astype(F32))
    return y.astype(u.dtype)


def gla_mix(p, gk_up, gk_b, norm_g):
    B, L, _ = p.shape
    nc = L // GLA_CHUNK
    q, k, v, gkd, go = _split(p, GLA_SIZES)
    gk = jax.nn.log_sigmoid((gkd @ gk_up + gk_b).astype(F32)) / GLA_GATE_NORM

    def chunks(t, dh):
        return t.astype(F32).reshape(B, nc, GLA_CHUNK, GLA_HEADS, dh).transpose(0, 3, 1, 2, 4)

    q = chunks(q, GLA_DK) * (GLA_DK ** -0.5)
    k = chunks(k, GLA_DK)
    v = chunks(v, GLA_DV)
    b = jnp.cumsum(chunks(gk, GLA_DK), axis=3)
    b_last = b[:, :, :, -1:, :]
    q_dec = q * jnp.exp(b)
    k_inv = k * jnp.exp(-b)
    mask = jnp.tril(jnp.ones((GLA_CHUNK, GLA_CHUNK), dtype=bool))
    att = jnp.where(mask, jnp.einsum('bhncd,bhnsd->bhncs', q_dec, k_inv), 0.0)
    o = jnp.einsum('bhncs,bhnse->bhnce', att, v)
    u = jnp.einsum('bhncd,bhnce->bhnde', k * jnp.exp(b_last - b), v)

    def step(S, inp):
        u_c, dec_c = inp
        return jnp.exp(dec_c)[..., None] * S + u_c, S

    S0 = jnp.zeros((B, GLA_HEADS, GLA_DK, GLA_DV), F32)
    _, S_prev = lax.scan(step, S0, (jnp.moveaxis(u, 2, 0), jnp.moveaxis(b_last[:, :, :, 0], 2, 0)))
    S_prev = jnp.moveaxis(S_prev, 0, 2)
    o = o + jnp.einsum('bhncd,bhnde->bhnce', q_dec, S_prev)
    o = o.transpose(0, 2, 3, 1, 4).reshape(B, L, GLA_HEADS, GLA_DV)
    o = o * lax.rsqrt(jnp.mean(o * o, axis=-1, keepdims=True) + 1e-5) * norm_g
    o = o.reshape(B, L, MIX_W) * jax.nn.silu(go.astype(F32))
    return o.astype(p.dtype)


def cross_attention(h, mem_n, wq, wkv, wo):
    B, L, _ = h.shape
    M = mem_n.shape[1]
    q = (h @ wq).reshape(B, L, XA_HEADS, XA_HEAD)
    kv = (mem_n @ wkv).reshape(B, M, 2, XA_HEADS, XA_HEAD)
    k, v = kv[:, :, 0], kv[:, :, 1]
    s = jnp.einsum('bqhd,bkhd->bhqk', q, k).astype(F32) * (XA_HEAD ** -0.5)
    pr = jax.nn.softmax(s, axis=-1).astype(h.dtype)
    o = jnp.einsum('bhqk,bkhd->bqhd', pr, v).reshape(B, L, D_MODEL)
    return o @ wo


def conv_ffn(h, w_up, conv_w, conv_b, w_down):
    u = h @ w_up
    u = lax.conv_general_dilated(
        u, conv_w[:, None, :], window_strides=(1,), padding=[(CONV_W - 1, 0)],
        dimension_numbers=('NWC', 'WIO', 'NWC'), feature_group_count=2 * D_FF) + conv_b
    gate, val = jnp.split(u, 2, axis=-1)
    return (jax.nn.silu(gate) * val) @ w_down


def setup_inputs(seed: int = 0) -> dict:
    key = jax.random.key(seed)
    keys = iter(jax.random.split(key, 64))

    def nrm(shape, scale):
        return jax.random.normal(next(keys), shape, F32) * scale

    def unif(shape, lo, hi):
        return jax.random.uniform(next(keys), shape, F32, lo, hi)

    def gain(shape):
        return 1.0 + nrm(shape, 0.02)

    Ly = DEPTH
    n_idx = jnp.arange(S5_STATE, dtype=F32)
    return {
        "x": nrm((BATCH, SEQ, D_MODEL), 1.0),
        "mem": nrm((BATCH, N_MEM, D_MODEL), 1.0),
        "norm_mix": gain((Ly, D_MODEL)),
        "w_in": nrm((Ly, D_MODEL, N_IN), D_MODEL ** -0.5),
        "rw_mu": unif((Ly, RWKV_COLS), 0.0, 1.0),
        "rw_w0": unif((Ly, MIX_W), -6.0, -1.0),
        "rw_w_up": nrm((Ly, RWKV_DECAY_RANK, MIX_W), RWKV_DECAY_RANK ** -0.5),
        "rw_a0": nrm((Ly, MIX_W), 0.1),
        "rw_a_up": nrm((Ly, RWKV_A_RANK, MIX_W), RWKV_A_RANK ** -0.5),
        "rw_g_up": nrm((Ly, RWKV_G_RANK, MIX_W), RWKV_G_RANK ** -0.5),
        "rw_k_k": 0.85 + nrm((Ly, MIX_W), 0.02),
        "rw_k_a": 1.0 + nrm((Ly, MIX_W), 0.02),
        "rw_r_k": nrm((Ly, RWKV_HEADS, RWKV_HEAD), 0.1),
        "rw_ln_w": gain((Ly, MIX_W)),
        "rw_ln_b": nrm((Ly, MIX_W), 0.02),
        "s5_a_re": -0.5 * jnp.exp(nrm((Ly, S5_GROUPS, S5_STATE), 0.02)),
        "s5_a_im": jnp.pi * n_idx + nrm((Ly, S5_GROUPS, S5_STATE), 0.02),
        "s5_b_re": nrm((Ly, S5_GROUPS, S5_STATE, S5_GROUP), (2 * S5_GROUP) ** -0.5),
        "s5_b_im": nrm((Ly, S5_GROUPS, S5_STATE, S5_GROUP), (2 * S5_GROUP) ** -0.5),
        "s5_c_re": nrm((Ly, S5_GROUPS, S5_GROUP, S5_STATE), (2 * S5_STATE) ** -0.5),
        "s5_c_im": nrm((Ly, S5_GROUPS, S5_GROUP, S5_STATE), (2 * S5_STATE) ** -0.5),
        "s5_d": nrm((Ly, S5_GROUPS, S5_GROUP), 1.0),
        "s5_log_step": unif((Ly, S5_GROUPS), float(np.log(1e-3)), float(np.log(1e-1))),
        "s5_w_glu": nrm((Ly, MIX_W, MIX_W), MIX_W ** -0.5),
        "s5_b_glu": nrm((Ly, MIX_W), 0.02),
        "gla_gk_up": nrm((Ly, GLA_GATE_RANK, GLA_HEADS * GLA_DK), GLA_GATE_RANK ** -0.5),
        "gla_gk_b": unif((Ly, GLA_HEADS * GLA_DK), 0.0, 3.0),
        "gla_norm": gain((Ly, GLA_DV)),
        "w_branch": nrm((Ly, N_BRANCH, MIX_W, D_MODEL), MIX_W ** -0.5),
        "w_out": nrm((Ly, D_MODEL, D_MODEL), D_MODEL ** -0.5),
        "norm_mem": gain((D_MODEL,)),
        "norm_xattn": gain((Ly, D_MODEL)),
        "xa_wq": nrm((Ly, D_MODEL, D_MODEL), D_MODEL ** -0.5),
        "xa_wkv": nrm((Ly, D_MODEL, 2 * D_MODEL), D_MODEL ** -0.5),
        "xa_wo": nrm((Ly, D_MODEL, D_MODEL), D_MODEL ** -0.5),
        "norm_ffn": gain((Ly, D_MODEL)),
        "ffn_w_up": nrm((Ly, D_MODEL, 2 * D_FF), D_MODEL ** -0.5),
        "ffn_conv": nrm((Ly, CONV_W, 2 * D_FF), CONV_W ** -0.5),
        "ffn_conv_b": nrm((Ly, 2 * D_FF), 0.02),
        "ffn_w_down": nrm((Ly, D_FF, D_MODEL), D_FF ** -0.5),
        "norm_final": gain((D_MODEL,)),
    }


def reference(x, mem, norm_mix, w_in, rw_mu, rw_w0, rw_w_up, rw_a0, rw_a_up, rw_g_up,
              rw_k_k, rw_k_a, rw_r_k, rw_ln_w, rw_ln_b,
              s5_a_re, s5_a_im, s5_b_re, s5_b_im, s5_c_re, s5_c_im, s5_d, s5_log_step,
              s5_w_glu, s5_b_glu, gla_gk_up, gla_gk_b, gla_norm, w_branch, w_out,
              norm_mem, norm_xattn, xa_wq, xa_wkv, xa_wo,
              norm_ffn, ffn_w_up, ffn_conv, ffn_conv_b, ffn_w_down, norm_final):
    B, L, _ = x.shape
    mem_n = rms_norm(mem, norm_mem)
    for l in range(DEPTH):
        h = rms_norm(x, norm_mix[l])
        p_rw, p_s5, p_gla, p_gate = _split(h @ w_in[l], IN_SIZES)
        y_rw = rwkv7_mix(p_rw, rw_mu[l], rw_w0[l], rw_w_up[l], rw_a0[l], rw_a_up[l], rw_g_up[l],
                         rw_k_k[l], rw_k_a[l], rw_r_k[l], rw_ln_w[l], rw_ln_b[l])
        y_s5 = s5_mix(p_s5, s5_a_re[l], s5_a_im[l], s5_b_re[l], s5_b_im[l], s5_c_re[l], s5_c_im[l],
                      s5_d[l], s5_log_step[l], s5_w_glu[l], s5_b_glu[l])
        y_gla = gla_mix(p_gla, gla_gk_up[l], gla_gk_b[l], gla_norm[l])
        gates = jax.nn.sigmoid(p_gate).reshape(B, L, N_BRANCH, D_MODEL)
        merged = (gates[:, :, 0] * (y_rw @ w_branch[l, 0])
                  + gates[:, :, 1] * (y_s5 @ w_branch[l, 1])
                  + gates[:, :, 2] * (y_gla @ w_branch[l, 2]))
        x = x + merged @ w_out[l]
        x = x + cross_attention(rms_norm(x, norm_xattn[l]), mem_n, xa_wq[l], xa_wkv[l], xa_wo[l])
        x = x + conv_ffn(rms_norm(x, norm_ffn[l]), ffn_w_up[l], ffn_conv[l], ffn_conv_b[l], ffn_w_down[l])
    return rms_norm(x, norm_final)
```

```python
import numpy as np
from contextlib import ExitStack
import concourse.bass as bass
import concourse.mybir as mybir
from concourse.bass_utils import run_bass_kernel_spmd

F32 = mybir.dt.float32
BF16 = mybir.dt.bfloat16
ALU = mybir.AluOpType
AF = mybir.ActivationFunctionType
AX = mybir.AxisListType

NCORES = 8
DEPTH = 4
D = 1024
SEQ = 2048
NSEQ = 2
T = NSEQ * SEQ
NMEM = 256
N_IN = 6928
RW0, S50, GL0, GT0 = 0, 1792, 2304, 3856
DFF = 2816
EPOCH = 30000


class Buf:
    __slots__ = ("name", "lw", "rd")

    def __init__(self, name=""):
        self.name = name
        self.lw = None
        self.rd = {}


class Sched:
    def __init__(self, nc):
        self.nc = nc
        self.E = {"pe": nc.tensor, "dve": nc.vector, "act": nc.scalar, "pool": nc.gpsimd, "sp": nc.sync}
        self.sems = {}
        self.cur = {}
        self.nsem = 0
        for e in self.E:
            self._new_epoch(e)
        self.seen = {e: {} for e in self.E}
        self.dma_pools = {}
        self.dma_rr = {}
        nsd = 0
        for q, n in (("sp", 36), ("pool", 16), ("act", 8)):
            pl = []
            for i in range(n):
                k = "d%s%d" % (q, i)
                self.sems[k] = nc.alloc_semaphore("sd%d" % nsd)
                nsd += 1
                pl.append([k, 0])
            self.dma_pools[q] = pl
            self.dma_rr[q] = 0
        self.n_dma_sems = nsd
        self.n_ops = 0
        self.n_waits = 0

    def _new_epoch(self, e):
        k = "%s_%d" % (e, self.nsem)
        self.nsem += 1
        self.sems[k] = self.nc.alloc_semaphore("s" + k)
        self.cur[e] = [k, 0]

    def _wait(self, eng, deps):
        best = {}
        for d in deps:
            if d is None:
                continue
            k, v, de = d
            if de == eng and eng == "pe":
                continue
            if best.get(k, 0) < v:
                best[k] = v
        for k, v in best.items():
            if self.seen[eng].get(k, 0) >= v:
                continue
            self.E[eng].wait_ge(self.sems[k], v)
            self.seen[eng][k] = v
            self.n_waits += 1

    @staticmethod
    def _deps(reads, writes):
        deps = []
        for b in reads:
            deps.append(b.lw)
        for b in writes:
            deps.append(b.lw)
            deps.extend(b.rd.values())
        return deps

    def op(self, eng, fn, reads=(), writes=()):
        self._wait(eng, self._deps(reads, writes))
        ins = fn()
        c = self.cur[eng]
        c[1] += 1
        ins.then_inc(self.sems[c[0]], 1)
        t = (c[0], c[1], eng)
        for b in reads:
            b.rd[eng] = t
        for b in writes:
            b.lw = t
            b.rd = {}
        if c[1] >= EPOCH:
            self._new_epoch(eng)
        self.n_ops += 1
        return t

    def dma(self, q, out, in_, reads=(), writes=(), **kw):
        pl = self.dma_pools[q]
        slot = self.dma_rr[q]
        self.dma_rr[q] = (slot + 1) % len(pl)
        s = pl[slot]
        deps = self._deps(reads, writes)
        if s[1] > 0:
            deps.append((s[0], s[1], "dma"))
        self._wait(q, deps)
        s[1] += 16
        self.E[q].dma_start(out=out, in_=in_, **kw).then_inc(self.sems[s[0]], 16)
        t = (s[0], s[1], "dma")
        for b in reads:
            b.rd["dma_%s%d" % (q, slot)] = t
        for b in writes:
            b.lw = t
            b.rd = {}
        self.n_ops += 1
        return t

    def barrier(self):
        deps = []
        for e, c in self.cur.items():
            if c[1] > 0:
                deps.append((c[0], c[1], "x"))
        for pl in self.dma_pools.values():
            for k, v in pl:
                if v > 0:
                    deps.append((k, v, "x"))
        for e in self.E:
            self._wait(e, deps)


VEC_SPECS = [
    ("norm_mix", 1024), ("norm_xattn", 1024), ("norm_ffn", 1024),
    ("rw_mu", 1792), ("rw_w0", 512), ("rw_a0", 512), ("rw_k_k", 512), ("rw_k_a", 512), ("rw_r_k", 512),
    ("rw_ln_w", 512), ("rw_ln_b", 512),
    ("s5_b_glu", 512), ("s5_d", 512),
    ("gla_gk_b", 256), ("gla_norm", 128),
    ("ffn_conv0", 5632), ("ffn_conv1", 5632), ("ffn_conv2", 5632), ("ffn_conv_b", 5632),
]
VEC_OFF = {}
_o = 0
for _n, _l in VEC_SPECS:
    VEC_OFF[_n] = _o
    _o += _l // 128
NVEC = _o
GVEC_OFF = {"norm_mem": 0, "norm_final": 8}
NGVEC = 24


def pack_vecs(inp):
    v = np.zeros((DEPTH, 128, NVEC), np.float32)
    for l in range(DEPTH):
        for n, ln in VEC_SPECS:
            if n.startswith("ffn_conv") and n != "ffn_conv_b":
                a = inp["ffn_conv"][l, int(n[-1])]
            else:
                a = inp[n][l]
            a = np.asarray(a, np.float32).reshape(-1)
            v[l, :, VEC_OFF[n]:VEC_OFF[n] + ln // 128] = a.reshape(ln // 128, 128).T
    g = np.zeros((128, NGVEC), np.float32)
    g[:, 0:8] = np.asarray(inp["norm_mem"], np.float32).reshape(8, 128).T
    g[:, 8:16] = np.asarray(inp["norm_final"], np.float32).reshape(8, 128).T
    pp = np.arange(128)
    g[:, 16] = ((pp // 16) % 2 == 0)
    g[:, 17] = ((pp // 16) % 2 == 1)
    g[:, 18] = np.pi / 2
    g[:, 19] = (pp >= 96)
    return np.ascontiguousarray(v.transpose(1, 0, 2)), g


S5X = 3124
oAC, oBC, oLC, oAP, oLP, oBP, oCP = 0, 512, 1024, 1028, 1060, 1076, 2100


def pack_s5(inp):
    out = np.zeros((128, DEPTH, S5X), np.float32)
    for l in range(DEPTH):
        a = np.stack([inp["s5_a_re"][l], inp["s5_a_im"][l]]).astype(np.float32)
        b = np.stack([inp["s5_b_re"][l], inp["s5_b_im"][l]]).astype(np.float32)
        c = np.stack([inp["s5_c_re"][l], inp["s5_c_im"][l]]).astype(np.float32)
        ls = np.asarray(inp["s5_log_step"][l], np.float32)
        aC = np.broadcast_to(a.reshape(2, 4, 8, 64).transpose(2, 1, 0, 3)[:, None], (8, 16, 4, 2, 64)).reshape(128, 512)
        bC = b.reshape(2, 4, 8, 64, 16).transpose(2, 4, 1, 0, 3).reshape(128, 512)
        lC = np.broadcast_to(ls.reshape(4, 8).T[:, None, :], (8, 16, 4)).reshape(128, 4)
        aP = a.reshape(2, 16, 2, 64).transpose(2, 3, 1, 0).reshape(128, 32)
        lP = np.broadcast_to(ls.reshape(16, 2).T[:, None, :], (2, 64, 16)).reshape(128, 16)
        bP = np.zeros((2, 64, 16, 2, 2, 16), np.float32)
        cP = np.zeros((2, 64, 16, 2, 2, 16), np.float32)
        b5 = b.reshape(2, 16, 2, 64, 16)
        c5 = c.reshape(2, 16, 2, 16, 64)
        for bb in range(2):
            bP[bb, :, :, :, bb, :] = b5[:, :, bb].transpose(2, 1, 0, 3)
            cP[bb, :, :, :, bb, :] = c5[:, :, bb].transpose(3, 1, 0, 2)
        out[:, l, oAC:oAC + 512] = aC
        out[:, l, oBC:oBC + 512] = bC
        out[:, l, oLC:oLC + 4] = lC
        out[:, l, oAP:oAP + 32] = aP
        out[:, l, oLP:oLP + 16] = lP
        out[:, l, oBP:oBP + 1024] = bP.reshape(128, 1024)
        out[:, l, oCP:oCP + 1024] = cP.reshape(128, 1024)
    return out


class K:
    def __init__(self, debug=None, n_layers=DEPTH):
        self.debug = debug or {}
        self.n_layers = n_layers
        nc = self.nc = bass.Bass("TRN2", target_bir_lowering=False)
        self.S = Sched(nc)
        self.uid = 0
        self.dram = {}
        self.dbuf = {}
        di = self.dram_in
        di("x", [T, D]); di("mem", [NSEQ * NMEM, D]); di("vecs", [128, DEPTH, NVEC]); di("gvec", [128, NGVEC])
        di("w_in", [DEPTH, D, N_IN])
        di("gla_gk_up", [DEPTH, 16, 256])
        di("s5w", [128, DEPTH, S5X]); di("s5_w_glu", [DEPTH, 512, 512])
        di("w_branch", [DEPTH, 3, 512, D]); di("w_out", [DEPTH, D, D])
        di("xa_wq", [DEPTH, D, D]); di("xa_wkv", [DEPTH, D, 2 * D]); di("xa_wo", [DEPTH, D, D])
        di("ffn_w_up", [DEPTH, D, 2 * DFF]); di("ffn_w_down", [DEPTH, DFF, D]); di("norm_final_bc", [128, D])
        di("rw_w_up", [DEPTH, 64, 512]); di("rw_a_up", [DEPTH, 64, 512]); di("rw_g_up", [DEPTH, 128, 512])
        self.out = nc.dram_tensor("out", [T, D], F32, kind="ExternalOutput").ap()
        self.Bout = Buf("out")
        self.scr("xres", [T, D], F32)
        self.scr("pT", [N_IN, T], BF16)
        self.scr("ymix", [3, 512, T], BF16)
        self.scr("actT", [DFF, T], BF16)
        self.ps = []
        for i in range(8):
            t = nc.alloc_psum_tensor("ps%d" % i, [128, 512], F32)
            self.ps.append((t, Buf("ps%d" % i)))
        self.ps_rr = 0
        self.ev_rr = 0
        self.consts()

    def dram_in(self, name, shape, dt=F32):
        self.dram[name] = self.nc.dram_tensor(name, shape, dt, kind="ExternalInput").ap()
        self.dbuf[name] = Buf(name)

    def scr(self, name, shape, dt):
        kind = "ExternalOutput" if name in self.debug else "Internal"
        self.dram[name] = self.nc.dram_tensor(name, shape, dt, kind=kind).ap()
        self.dbuf[name] = Buf(name)

    def psum(self):
        t, b = self.ps[self.ps_rr]
        self.ps_rr = (self.ps_rr + 1) % 8
        return t, b

    def ev_eng(self):
        self.ev_rr ^= 1
        return "act" if self.ev_rr else "dve"

    def consts(self):
        nc, S = self.nc, self.S
        self.vecs = nc.alloc_sbuf_tensor("vecs_sb", [128, DEPTH, NVEC], F32)
        self.Bvecs = Buf("vecs")
        if "delay" in self.debug:
            dj = nc.alloc_sbuf_tensor("dly", [128, 512], F32)
            Bd = Buf()
            for i in range(60):
                S.op("pool", lambda: nc.gpsimd.memset(dj[:], 0.0), writes=[Bd])
            S.barrier()
        S.dma("sp", self.vecs[:], self.dram["vecs"], writes=[self.Bvecs])
        self.gvec = nc.alloc_sbuf_tensor("gvec_sb", [128, NGVEC], F32)
        S.dma("sp", self.gvec[:], self.dram["gvec"], writes=[self.Bvecs])
        self.ident_f = nc.alloc_sbuf_tensor("ident_f", [128, 128], F32)
        self.ident_b = nc.alloc_sbuf_tensor("ident_b", [128, 128], BF16)
        self.Bconst = Buf("const")
        self.eps6 = nc.alloc_sbuf_tensor("eps6", [128, 1], F32)
        S.op("pool", lambda: nc.gpsimd.memset(self.ident_f[:], 0.0), writes=[self.Bconst])
        S.op("pool", lambda: nc.gpsimd.affine_select(out=self.ident_f[:], in_=self.ident_f[:], pattern=[[-1, 128]],
                                                     compare_op=ALU.not_equal, fill=1.0, base=0, channel_multiplier=1),
             reads=[self.Bconst], writes=[self.Bconst])
        S.op("dve", lambda: nc.vector.tensor_copy(out=self.ident_b[:], in_=self.ident_f[:]), reads=[self.Bconst], writes=[self.Bconst])
        S.op("dve", lambda: nc.vector.memset(self.eps6[:], 1e-6), writes=[self.Bconst])
        self.cst = nc.alloc_sbuf_tensor("cst", [128, 8], F32)
        for i, v in enumerate([1.0, 1e-5, 1e-12, 64e-5, 0.0]):
            S.op("dve", lambda: nc.vector.memset(self.cst[:, i:i + 1], v), writes=[self.Bconst])
        self.ones_f = nc.alloc_sbuf_tensor("ones_f", [128, 128], F32)
        S.op("dve", lambda: nc.vector.memset(self.ones_f[:], 1.0), writes=[self.Bconst])
        self.ones_bd = nc.alloc_sbuf_tensor("ones_bd", [128, 128], F32)
        S.op("dve", lambda: nc.vector.memset(self.ones_bd[:], 0.0), writes=[self.Bconst])
        S.op("dve", lambda: nc.vector.memset(self.ones_bd[0:64, 0:64], 1.0), writes=[self.Bconst])
        S.op("dve", lambda: nc.vector.memset(self.ones_bd[64:128, 64:128], 1.0), writes=[self.Bconst])
        self.mbd = nc.alloc_sbuf_tensor("mbd", [128, 3, 128], F32)
        S.op("pool", lambda: nc.gpsimd.memset(self.mbd[:], 1.0), writes=[self.Bconst])
        for i, (cm, st, op) in enumerate([(1, -1, ALU.is_gt), (-1, 1, ALU.is_gt), (-1, 1, ALU.is_ge)]):
            S.op("pool", lambda: nc.gpsimd.affine_select(out=self.mbd[:, i, :], in_=self.mbd[:, i, :], pattern=[[st, 128]],
                                                         compare_op=op, fill=0.0, base=0, channel_multiplier=cm),
                 reads=[self.Bconst], writes=[self.Bconst])
        S.op("pool", lambda: nc.gpsimd.memset(self.mbd[0:64, :, 64:128], 0.0), reads=[self.Bconst], writes=[self.Bconst])
        S.op("pool", lambda: nc.gpsimd.memset(self.mbd[64:128, :, 0:64], 0.0), reads=[self.Bconst], writes=[self.Bconst])
        self.mask128 = nc.alloc_sbuf_tensor("mask128", [128, 128], F32)
        S.op("pool", lambda: nc.gpsimd.memset(self.mask128[:], 1.0), writes=[self.Bconst])
        S.op("pool", lambda: nc.gpsimd.affine_select(out=self.mask128[:], in_=self.mask128[:], pattern=[[1, 128]],
                                                     compare_op=ALU.is_ge, fill=0.0, base=0, channel_multiplier=-1),
             reads=[self.Bconst], writes=[self.Bconst])

    def sbuf(self, name, shape, dt):
        self.uid += 1
        return self.nc.sbuf_tensor("%s_u%d" % (name, self.uid), shape, dt)

    def tiles(self, es, name, shape, dt, n):
        return [(es.enter_context(self.sbuf("%s%d" % (name, i), shape, dt)), Buf(name)) for i in range(n)]

    def V(self, fn, r=(), w=()):
        return self.S.op("dve", fn, r, w)

    def A(self, fn, r=(), w=()):
        return self.S.op("act", fn, r, w)

    def P(self, fn, r=(), w=()):
        return self.S.op("pe", fn, r, w)

    def G(self, fn, r=(), w=()):
        return self.S.op("pool", fn, r, w)

    def vcol(self, l, name, j=0, n=1):
        o = VEC_OFF[name] + j
        return self.vecs[:, l, o:o + n]

    def rmsnorm_fm(self, es, src_ap, Bsrc, ntok, gain_ap, hT, BhT, eps_ap):
        nc, S = self.nc, self.S
        TB = 4
        xts = [(es.enter_context(self.sbuf("nx%d" % i, [128, TB, D], F32)), [Buf() for _ in range(TB)]) for i in range(2)]
        xss = [(es.enter_context(self.sbuf("nxs%d" % i, [128, D], BF16)), Buf()) for i in range(2)]
        junk = es.enter_context(self.sbuf("njunk", [128, D], BF16)); Bjunk = Buf()
        st = [(es.enter_context(self.sbuf("nst%d" % i, [128, 4], F32)), Buf()) for i in range(2)]
        nblk = ntok // (128 * TB)
        cnt = 0
        for blk in range(nblk):
            xt, Bxs_ = xts[blk % 2]
            for j in range(TB):
                r0 = (blk * TB + j) * 128
                S.dma("sp", xt[:, j, :], src_ap[r0:r0 + 128, :], reads=[Bsrc], writes=[Bxs_[j]])
            for j in range(TB):
                Bx = Bxs_[j]
                s_, Bs = st[cnt % 2]
                xs, Bxs = xss[cnt % 2]
                cnt += 1
                S.op("act", lambda: nc.scalar.activation(out=junk[:], in_=xt[:, j, :], func=AF.Square, accum_out=s_[:, 0:1]),
                     reads=[Bx], writes=[Bjunk, Bs])
                S.op("act", lambda: nc.scalar.activation(out=s_[:, 1:2], in_=s_[:, 0:1], func=AF.Sqrt, scale=1.0 / D, bias=eps_ap),
                     reads=[Bs, self.Bconst], writes=[Bs])
                S.op("dve", lambda: nc.vector.reciprocal(out=s_[:, 2:3], in_=s_[:, 1:2]), reads=[Bs], writes=[Bs])
                S.op("dve", lambda: nc.vector.tensor_scalar(out=xs[:], in0=xt[:, j, :], scalar1=s_[:, 2:3], scalar2=None, op0=ALU.mult),
                     reads=[Bx, Bs], writes=[Bxs])
                pt, Bp = self.psum()
                pb = pt[:].bitcast(BF16)
                for kc in range(8):
                    S.op("pe", lambda: nc.tensor.transpose(out=pb[:, kc * 128:(kc + 1) * 128], in_=xs[:, kc * 128:(kc + 1) * 128], identity=self.ident_b[:]),
                         reads=[Bxs, self.Bconst], writes=[Bp])
                t0 = (blk * TB + j) * 128
                S.op("dve", lambda: nc.vector.tensor_tensor(out=hT[:, :, t0:t0 + 128], in0=pb.rearrange("p (k t) -> p k t", k=8),
                                                            in1=gain_ap.unsqueeze(2).to_broadcast([128, 8, 128]), op=ALU.mult),
                     reads=[Bp, self.Bvecs], writes=[BhT])

    def proj_fm(self, es, w_ap, Bw, segs, hT, BhT, ntok, dst_ap, Bdst, kc_n=8):
        nc, S = self.nc, self.S
        wts = [(es.enter_context(self.sbuf("pw%d" % i, [128, kc_n, 512], BF16)), [Buf() for _ in range((kc_n + 3) // 4)]) for i in range(2)]
        ots = [(es.enter_context(self.sbuf("po%d" % i, [128, 512], BF16)), Buf()) for i in range(4)]
        wi = 0
        oi = 0
        for (c0, ncol, func, r0) in segs:
            for g0 in range(0, ncol, 512):
                gn = min(512, ncol - g0)
                wt, Bwt = wts[wi % 2]; wi += 1
                src = w_ap[:, c0 + g0:c0 + g0 + gn].rearrange("(k p) c -> p k c", p=128)
                Bwt_l = Bwt
                for k0 in range(0, kc_n, 4):
                    S.dma("pool", wt[:, k0:k0 + 4, 0:gn], src[:, k0:k0 + 4, :], reads=[Bw], writes=[Bwt_l[k0 // 4]])
                for m0 in range(0, gn, 128):
                    mn = min(128, gn - m0)
                    for tb in range(ntok // 512):
                        pt, Bp = self.psum()
                        for kc in range(kc_n):
                            S.op("pe", lambda: nc.tensor.matmul(pt[0:mn, :], lhsT=wt[:, kc, m0:m0 + mn], rhs=hT[:, kc, tb * 512:(tb + 1) * 512],
                                                                start=(kc == 0), stop=(kc == kc_n - 1)),
                                 reads=[Bwt[kc // 4], BhT], writes=[Bp])
                        ot, Bo = ots[oi % 4]; oi += 1
                        if func is not None:
                            S.op("act", lambda: nc.scalar.activation(out=ot[0:mn, :], in_=pt[0:mn, :], func=func), reads=[Bp], writes=[Bo])
                        else:
                            e = self.ev_eng()
                            if e == "act":
                                S.op("act", lambda: nc.scalar.copy(out=ot[0:mn, :], in_=pt[0:mn, :]), reads=[Bp], writes=[Bo])
                            else:
                                S.op("dve", lambda: nc.vector.tensor_copy(out=ot[0:mn, :], in_=pt[0:mn, :]), reads=[Bp], writes=[Bo])
                        rr = r0 + g0 + m0
                        S.dma("sp", dst_ap[rr:rr + mn, tb * 512:(tb + 1) * 512], ot[0:mn, :], reads=[Bo], writes=[Bdst])

    def stage_mixproj(self, l):
        nc, S = self.nc, self.S
        with ExitStack() as es:
            hT = es.enter_context(self.sbuf("hT", [128, 8, T], BF16)); BhT = Buf("hT")
            xsrc, Bx = (self.dram["x"], self.dbuf["x"]) if l == 0 else (self.dram["xres"], self.dbuf["xres"])
            with ExitStack() as es2:
                self.rmsnorm_fm(es2, xsrc, Bx, T, self.vcol(l, "norm_mix", 0, 8), hT, BhT, self.eps6[:, 0:1])
                S.barrier()
            if "hT" in self.debug:
                self.dump("hT_dbg", hT[:], [BhT])
            segs = [(RW0, 1792, None, RW0), (S50, 512, None, S50), (GL0, 1552, None, GL0), (GT0, 3072, AF.Sigmoid, GT0)]
            if "segs" in self.debug:
                segs = self.debug["segs"]
            with ExitStack() as es2:
                self.proj_fm(es2, self.dram["w_in"][l], self.dbuf["w_in"], segs, hT, BhT, T, self.dram["pT"], self.dbuf["pT"])
                S.barrier()
        S.barrier()

    def dump(self, name, ap, bufs, dt=None):
        d = self.nc.dram_tensor(name, list(ap.shape), dt or ap.dtype, kind="ExternalOutput").ap()
        self.S.dma("sp", d, ap, reads=bufs, writes=[Buf()])

    def stage_gla(self, l, sq):
        nc, S = self.nc, self.S
        V, A, P = self.V, self.A, self.P
        pT, BpT = self.dram["pT"], self.dbuf["pT"]
        ym, Bym = self.dram["ymix"], self.dbuf["ymix"]
        t0 = sq * SEQ
        L = SEQ
        NCH = L // 128
        qr, kr, vr, gr, gor = GL0, GL0 + 256, GL0 + 512, GL0 + 1024, GL0 + 1040
        Bc = self.Bconst
        with ExitStack() as es:
            gkup = es.enter_context(self.sbuf("gkup", [16, 256], BF16)); Bgk = Buf()
            S.dma("pool", gkup[:], self.dram["gla_gk_up"][l], reads=[self.dbuf["gla_gk_up"]], writes=[Bgk])
            gkd = es.enter_context(self.sbuf("gkd", [16, L], BF16)); Bgkd = Buf()
            S.dma("sp", gkd[:], pT[gr:gr + 16, t0:t0 + L], reads=[BpT], writes=[Bgkd])
            negb = es.enter_context(self.sbuf("negb", [128, 2], F32)); Bnb = Buf()
            V(lambda: nc.vector.tensor_scalar(out=negb[:], in0=self.vcol(l, "gla_gk_b", 0, 2), scalar1=-1.0, scalar2=None, op0=ALU.mult),
              [self.Bvecs], [Bnb])
            qk = self.tiles(es, "gqk", [128, L], BF16, 2)
            qd = es.enter_context(self.sbuf("gqd", [128, L], BF16)); Bqd = Buf()
            ki = es.enter_context(self.sbuf("gki", [128, L], BF16)); Bki = Buf()
            csz = es.enter_context(self.sbuf("gcsz", [128, L + 1], F32)); Bcs = Buf()
            lt = es.enter_context(self.sbuf("glt", [128, L], F32)); Blt = Buf()
            csl = es.enter_context(self.sbuf("gcsl", [128, L], F32)); Bcsl = Buf()
            Eq = es.enter_context(self.sbuf("gEq", [128, L], F32)); BEq = Buf()
            Ek = es.enter_context(self.sbuf("gEk", [128, L], F32)); BEk = Buf()
            vts = self.tiles(es, "gv", [128, L], BF16, 2)
            ofm = self.tiles(es, "gofm", [128, L], F32, 2)
            S32 = self.tiles(es, "gS32", [128, 128], F32, 2)
            S16 = self.tiles(es, "gS16", [128, 128], BF16, 2)
            vtm = self.tiles(es, "gvtm", [128, 128], BF16, 4)
            ktm = self.tiles(es, "gktm", [128, 128], BF16, 2)
            att = self.tiles(es, "gatt", [128, 128], BF16, 4)
            gos = self.tiles(es, "ggo", [128, 512], BF16, 2)
            tmpa = self.tiles(es, "gta", [128, 512], F32, 2)
            tmpb = self.tiles(es, "gtb", [128, 512], F32, 2)
            tmpc = self.tiles(es, "gtc", [128, 512], F32, 2)
            yo = self.tiles(es, "gyo", [128, 512], BF16, 2)
            V(lambda: nc.vector.memset(csz[:, 0:1], 0.0), [], [Bcs])
            for j in range(2):
                qt, Bq = qk[0]; kt, Bk = qk[1]
                S.dma("sp", qt[:], pT[qr + 128 * j:qr + 128 * j + 128, t0:t0 + L], reads=[BpT], writes=[Bq])
                S.dma("sp", kt[:], pT[kr + 128 * j:kr + 128 * j + 128, t0:t0 + L], reads=[BpT], writes=[Bk])
                for tb in range(L // 512):
                    pt, Bp = self.psum()
                    P(lambda: nc.tensor.matmul(pt[:], lhsT=gkup[0:16, 128 * j:128 * j + 128], rhs=gkd[0:16, tb * 512:(tb + 1) * 512], start=True, stop=True),
                      [Bgk, Bgkd], [Bp])
                    A(lambda: nc.scalar.activation(out=lt[:, tb * 512:(tb + 1) * 512], in_=pt[:], func=AF.Exp, scale=-1.0, bias=negb[:, j:j + 1]),
                      [Bp, Bnb], [Blt])
                A(lambda: nc.scalar.activation(out=lt[:], in_=lt[:], func=AF.Ln, scale=1.0, bias=self.cst[:, 0:1]), [Blt, Bc], [Blt])
                V(lambda: nc.vector.tensor_tensor_scan(out=csz[:, 1:L + 1], data0=self.cst[:, 0:1].to_broadcast([128, L]), data1=lt[:],
                                                       initial=0.0, op0=ALU.mult, op1=ALU.add), [Blt, Bc], [Bcs])
                V(lambda: nc.vector.tensor_tensor(out=csl[:].rearrange("p (c t) -> p c t", t=128),
                                                  in0=csz[:, 1:L + 1].rearrange("p (c t) -> p c t", t=128),
                                                  in1=csz[:, 0:L].rearrange("p (c t) -> p c t", t=128)[:, :, 0:1].to_broadcast([128, NCH, 128]),
                                                  op=ALU.subtract), [Bcs], [Bcsl])
                A(lambda: nc.scalar.activation(out=Eq[:], in_=csl[:], func=AF.Exp, scale=-1.0 / 16), [Bcsl], [BEq])
                A(lambda: nc.scalar.activation(out=Ek[:], in_=csl[:], func=AF.Exp, scale=1.0 / 16), [Bcsl], [BEk])
                V(lambda: nc.vector.scalar_tensor_tensor(out=qd[:], in0=qt[:], scalar=0.125, in1=Eq[:], op0=ALU.mult, op1=ALU.mult), [Bq, BEq], [Bqd])
                V(lambda: nc.vector.tensor_tensor(out=ki[:], in0=kt[:], in1=Ek[:], op=ALU.mult), [Bk, BEk], [Bki])
                Eq3 = Eq[:].rearrange("p (c t) -> p c t", t=128)
                heads = [2 * j, 2 * j + 1]
                for hh in range(2):
                    h = heads[hh]
                    vt, Bv = vts[hh]
                    S.dma("sp", vt[:], pT[vr + 128 * h:vr + 128 * h + 128, t0:t0 + L], reads=[BpT], writes=[Bv])
                    V(lambda: nc.vector.memset(S32[hh][0][:], 0.0), [], [S32[hh][1]])
                    V(lambda: nc.vector.memset(S16[hh][0][:], 0.0), [], [S16[hh][1]])
                cnt = 0
                for ci in range(NCH):
                    ts = slice(ci * 128, ci * 128 + 128)
                    ktmt, Bktm = ktm[ci % 2]
                    pt, Bp = self.psum()
                    pb = pt[:].bitcast(BF16)
                    P(lambda: nc.tensor.transpose(out=pb[:, 0:128], in_=ki[:, ts], identity=self.ident_b[:]), [Bki, Bc], [Bp])
                    A(lambda: nc.scalar.copy(out=ktmt[:], in_=pb[:, 0:128]), [Bp], [Bktm])
                    for hh in range(2):
                        h = heads[hh]
                        hp = slice(64 * hh, 64 * hh + 64)
                        vt, Bv = vts[hh]
                        vtmt, Bvtm = vtm[cnt % 4]
                        attt, Batt = att[cnt % 4]
                        cnt += 1
                        pt, Bp = self.psum()
                        pb = pt[:].bitcast(BF16)
                        P(lambda: nc.tensor.transpose(out=pb[:, 0:128], in_=vt[:, ts], identity=self.ident_b[:]), [Bv, Bc], [Bp])
                        A(lambda: nc.scalar.copy(out=vtmt[:], in_=pb[:, 0:128]), [Bp], [Bvtm])
                        pt2, Bp2 = self.psum()
                        P(lambda: nc.tensor.matmul(pt2[:, 0:128], lhsT=ki[hp, ts], rhs=qd[hp, ts], start=True, stop=True), [Bki, Bqd], [Bp2])
                        V(lambda: nc.vector.tensor_tensor(out=attt[:], in0=pt2[:, 0:128], in1=self.mask128[:], op=ALU.mult), [Bp2, Bc], [Batt])
                        pt3, Bp3 = self.psum()
                        P(lambda: nc.tensor.matmul(pt3[:, 0:128], lhsT=vtmt[:], rhs=attt[:], start=True, stop=False), [Bvtm, Batt], [Bp3])
                        P(lambda: nc.tensor.matmul(pt3[:, 0:128], lhsT=S16[hh][0][:], rhs=qd[:, ts], start=False, stop=True), [S16[hh][1], Bqd], [Bp3])
                        A(lambda: nc.scalar.copy(out=ofm[hh][0][:, ts], in_=pt3[:, 0:128]), [Bp3], [ofm[hh][1]])
                        if ci < NCH - 1:
                            pt4, Bp4 = self.psum()
                            P(lambda: nc.tensor.matmul(pt4[:, 0:128], lhsT=ktmt[:], rhs=vtmt[:], start=True, stop=True), [Bktm, Bvtm], [Bp4])
                            s32, Bs32 = S32[hh]
                            V(lambda: nc.vector.tensor_tensor(out=s32[hp, :], in0=s32[hp, :], in1=pt4[hp, 0:128], op=ALU.add), [Bs32, Bp4], [Bs32])
                            V(lambda: nc.vector.tensor_scalar(out=s32[hp, :], in0=s32[hp, :], scalar1=Eq3[hp, ci, 127:128], scalar2=None, op0=ALU.mult),
                              [Bs32, BEq], [Bs32])
                            V(lambda: nc.vector.tensor_copy(out=S16[hh][0][hp, :], in_=s32[hp, :]), [Bs32], [S16[hh][1]])
                for hh in range(2):
                    h = heads[hh]
                    o, Bo = ofm[hh]
                    for tb in range(L // 512):
                        cs = slice(tb * 512, tb * 512 + 512)
                        got, Bgo = gos[tb % 2]
                        S.dma("sp", got[:], pT[gor + 128 * h:gor + 128 * h + 128, t0 + tb * 512:t0 + tb * 512 + 512], reads=[BpT], writes=[Bgo])
                        ta, Bta = tmpa[tb % 2]; tb_, Btb = tmpb[tb % 2]; tc, Btc = tmpc[tb % 2]
                        A(lambda: nc.scalar.activation(out=ta[:], in_=o[:, cs], func=AF.Square), [Bo], [Bta])
                        pt, Bp = self.psum()
                        P(lambda: nc.tensor.matmul(pt[:], lhsT=self.ones_f[:], rhs=ta[:], start=True, stop=True), [Bc, Bta], [Bp])
                        A(lambda: nc.scalar.activation(out=tb_[:], in_=pt[:], func=AF.Sqrt, scale=1.0 / 128, bias=self.cst[:, 1:2]), [Bp, Bc], [Btb])
                        V(lambda: nc.vector.reciprocal(out=tb_[:], in_=tb_[:]), [Btb], [Btb])
                        V(lambda: nc.vector.scalar_tensor_tensor(out=tb_[:], in0=o[:, cs], scalar=self.vcol(l, "gla_norm", 0, 1), in1=tb_[:],
                                                                 op0=ALU.mult, op1=ALU.mult), [Bo, Btb, self.Bvecs], [Btb])
                        A(lambda: nc.scalar.activation(out=tc[:], in_=got[:], func=AF.Sigmoid), [Bgo], [Btc])
                        V(lambda: nc.vector.tensor_tensor(out=tc[:], in0=tc[:], in1=got[:], op=ALU.mult), [Btc, Bgo], [Btc])
                        yt, By = yo[tb % 2]
                        V(lambda: nc.vector.tensor_tensor(out=yt[:], in0=tb_[:], in1=tc[:], op=ALU.mult), [Btb, Btc], [By])
                        S.dma("sp", ym[2, 128 * h:128 * h + 128, t0 + tb * 512:t0 + tb * 512 + 512], yt[:], reads=[By], writes=[Bym])
            S.barrier()

    def s5_lambda(self, es, tag, are, aim, ls_bc, shp):
        nc = self.nc
        V, A = self.V, self.A
        F = int(np.prod(shp[1:]))
        def new(n):
            return es.enter_context(self.sbuf(tag + n, [128, F], F32))
        def vw(t):
            return t[:] if len(shp) == 2 else (t[:].rearrange("p (a b) -> p a b", b=shp[2]) if len(shp) == 3 else t[:])
        B = Buf()
        Bin = self.Bs5w
        dt, mag, ang, c, sn, t1, t2, lre, lim, fre, fim = [new(n) for n in ["dt", "mag", "ang", "c", "s", "t1", "t2", "lre", "lim", "fre", "fim"]]
        A(lambda: nc.scalar.activation(out=vw(dt), in_=ls_bc, func=AF.Exp), [Bin], [B])
        V(lambda: nc.vector.tensor_tensor(out=vw(mag), in0=vw(dt), in1=are, op=ALU.mult), [B, Bin], [B])
        A(lambda: nc.scalar.activation(out=mag[:], in_=mag[:], func=AF.Exp), [B], [B])
        V(lambda: nc.vector.tensor_tensor(out=vw(ang), in0=vw(dt), in1=aim, op=ALU.mult), [B, Bin], [B])
        A(lambda: nc.scalar.activation(out=sn[:], in_=ang[:], func=AF.Sin, scale=1.0 / 16), [B], [B])
        A(lambda: nc.scalar.activation(out=c[:], in_=ang[:], func=AF.Sin, scale=-1.0 / 16, bias=self.gvec[:, 18:19]), [B, self.Bvecs], [B])
        for _ in range(4):
            V(lambda: nc.vector.tensor_tensor(out=t1[:], in0=c[:], in1=sn[:], op=ALU.mult), [B], [B])
            V(lambda: nc.vector.tensor_tensor(out=c[:], in0=c[:], in1=c[:], op=ALU.mult), [B], [B])
            V(lambda: nc.vector.tensor_tensor(out=t2[:], in0=sn[:], in1=sn[:], op=ALU.mult), [B], [B])
            V(lambda: nc.vector.tensor_tensor(out=c[:], in0=c[:], in1=t2[:], op=ALU.subtract), [B], [B])
            V(lambda: nc.vector.tensor_scalar(out=sn[:], in0=t1[:], scalar1=2.0, scalar2=None, op0=ALU.mult), [B], [B])
        V(lambda: nc.vector.tensor_tensor(out=lre[:], in0=mag[:], in1=c[:], op=ALU.mult), [B], [B])
        V(lambda: nc.vector.tensor_tensor(out=lim[:], in0=mag[:], in1=sn[:], op=ALU.mult), [B], [B])
        V(lambda: nc.vector.tensor_tensor(out=vw(t1), in0=are, in1=are, op=ALU.mult), [Bin, B], [B])
        V(lambda: nc.vector.tensor_tensor(out=vw(t2), in0=aim, in1=aim, op=ALU.mult), [Bin, B], [B])
        V(lambda: nc.vector.tensor_tensor(out=t1[:], in0=t1[:], in1=t2[:], op=ALU.add), [B], [B])
        V(lambda: nc.vector.reciprocal(out=t1[:], in_=t1[:]), [B], [B])
        V(lambda: nc.vector.tensor_scalar(out=c[:], in0=lre[:], scalar1=-1.0, scalar2=None, op0=ALU.add), [B], [B])
        V(lambda: nc.vector.tensor_tensor(out=vw(fre), in0=vw(c), in1=are, op=ALU.mult), [B, Bin], [B])
        V(lambda: nc.vector.tensor_tensor(out=vw(t2), in0=vw(lim), in1=aim, op=ALU.mult), [B, Bin], [B])
        V(lambda: nc.vector.tensor_tensor(out=fre[:], in0=fre[:], in1=t2[:], op=ALU.add), [B], [B])
        V(lambda: nc.vector.tensor_tensor(out=fre[:], in0=fre[:], in1=t1[:], op=ALU.mult), [B], [B])
        V(lambda: nc.vector.tensor_tensor(out=vw(fim), in0=vw(lim), in1=are, op=ALU.mult), [B, Bin], [B])
        V(lambda: nc.vector.tensor_tensor(out=vw(t2), in0=vw(c), in1=aim, op=ALU.mult), [B, Bin], [B])
        V(lambda: nc.vector.tensor_tensor(out=fim[:], in0=fim[:], in1=t2[:], op=ALU.subtract), [B], [B])
        V(lambda: nc.vector.tensor_tensor(out=fim[:], in0=fim[:], in1=t1[:], op=ALU.mult), [B], [B])
        return lre, lim, fre, fim, B

    def cmul(self, ore, oim, are, aim, bre, bim, t1, r, w):
        nc, V = self.nc, self.V
        V(lambda: nc.vector.tensor_tensor(out=ore, in0=are, in1=bre, op=ALU.mult), r, w)
        V(lambda: nc.vector.tensor_tensor(out=t1, in0=aim, in1=bim, op=ALU.mult), r, w)
        V(lambda: nc.vector.tensor_tensor(out=ore, in0=ore, in1=t1, op=ALU.subtract), r, w)
        V(lambda: nc.vector.tensor_tensor(out=oim, in0=are, in1=bim, op=ALU.mult), r, w)
        V(lambda: nc.vector.tensor_tensor(out=t1, in0=aim, in1=bre, op=ALU.mult), r, w)
        V(lambda: nc.vector.tensor_tensor(out=oim, in0=oim, in1=t1, op=ALU.add), r, w)

    def stage_s5(self, l):
        nc, S = self.nc, self.S
        V, A, P = self.V, self.A, self.P
        pT, BpT = self.dram["pT"], self.dbuf["pT"]
        ym, Bym = self.dram["ymix"], self.dbuf["ymix"]
        Bc = self.Bconst
        with ExitStack() as es:
            M1re = es.enter_context(self.sbuf("M1re", [128, 4, 8, 128], BF16))
            M1im = es.enter_context(self.sbuf("M1im", [128, 4, 8, 128], BF16))
            M1hr = es.enter_context(self.sbuf("M1hr", [128, 4, 8, 128], BF16))
            M1hi = es.enter_context(self.sbuf("M1hi", [128, 4, 8, 128], BF16))
            M2hr = es.enter_context(self.sbuf("M2hr", [128, 4, 8, 64], BF16))
            M2hi = es.enter_context(self.sbuf("M2hi", [128, 4, 8, 64], BF16))
            M2re = es.enter_context(self.sbuf("M2re", [128, 16, 8, 32], BF16))
            M2im = es.enter_context(self.sbuf("M2im", [128, 16, 8, 32], BF16))
            Kbd = es.enter_context(self.sbuf("Kbd", [128, 4, 8, 128], BF16))
            LK = es.enter_context(self.sbuf("LK", [128, 16, 8, 3], F32))
            wglu = es.enter_context(self.sbuf("wglu", [128, 4, 512], BF16))
            BW = Buf("s5w")
            Bwg = Buf()
            S.dma("pool", wglu[:], self.dram["s5_w_glu"][l].rearrange("(k p) c -> p k c", p=128), reads=[self.dbuf["s5_w_glu"]], writes=[Bwg])
            with ExitStack() as e2:
                w = e2.enter_context(self.sbuf("s5w_sb", [128, S5X], F32)); self.Bs5w = Buf()
                S.dma("sp", w[:], self.dram["s5w"][:, l, :], reads=[self.dbuf["s5w"]], writes=[self.Bs5w])
                aC = w[:, oAC:oAC + 512].rearrange("p (g r n) -> p g r n", g=4, r=2)
                bC = w[:, oBC:oBC + 512].rearrange("p (g r n) -> p g r n", g=4, r=2)
                lC = w[:, oLC:oLC + 4].unsqueeze(2).to_broadcast([128, 4, 64])
                lre, lim, fre, fim, B1 = self.s5_lambda(e2, "c", aC[:, :, 0, :], aC[:, :, 1, :], lC, [128, 4, 64])
                v3 = lambda t: t[:].rearrange("p (a b) -> p a b", b=64)
                Bre = e2.enter_context(self.sbuf("cBre", [128, 256], F32)); Bim = e2.enter_context(self.sbuf("cBim", [128, 256], F32))
                tq = e2.enter_context(self.sbuf("ctq", [128, 256], F32))
                self.cmul(v3(Bre), v3(Bim), v3(fre), v3(fim), bC[:, :, 0, :], bC[:, :, 1, :], v3(tq), [B1, self.Bs5w], [B1])
                Pw = e2.enter_context(self.sbuf("cPw", [128, 8, 2, 256], F32))
                V(lambda: nc.vector.memset(Pw[:, 0, 0, :], 1.0), [], [B1])
                V(lambda: nc.vector.memset(Pw[:, 0, 1, :], 0.0), [], [B1])
                for m in range(1, 8):
                    self.cmul(Pw[:, m, 0, :], Pw[:, m, 1, :], Pw[:, m - 1, 0, :], Pw[:, m - 1, 1, :], lre[:], lim[:], tq[:], [B1], [B1])
                vre = e2.enter_context(self.sbuf("cvre", [128, 256], F32)); vim = e2.enter_context(self.sbuf("cvim", [128, 256], F32))
                for j in range(8):
                    m = 7 - j
                    self.cmul(vre[:], vim[:], Pw[:, m, 0, :], Pw[:, m, 1, :], Bre[:], Bim[:], tq[:], [B1], [B1])
                    for bb in range(2):
                        V(lambda: nc.vector.tensor_scalar(out=M1re[:, :, j, bb * 64:(bb + 1) * 64], in0=v3(vre), scalar1=self.gvec[:, 16 + bb:17 + bb], scalar2=None, op0=ALU.mult),
                          [B1, self.Bvecs], [BW])
                        V(lambda: nc.vector.tensor_scalar(out=M1im[:, :, j, bb * 64:(bb + 1) * 64], in0=v3(vim), scalar1=self.gvec[:, 16 + bb:17 + bb], scalar2=None, op0=ALU.mult),
                          [B1, self.Bvecs], [BW])
                        V(lambda: nc.vector.tensor_scalar(out=M1hr[:, :, j, bb * 64:(bb + 1) * 64], in0=v3(vre), scalar1=self.gvec[:, 16 + bb:17 + bb], scalar2=self.gvec[:, 19:20], op0=ALU.mult, op1=ALU.mult),
                          [B1, self.Bvecs], [BW])
                        V(lambda: nc.vector.tensor_scalar(out=M1hi[:, :, j, bb * 64:(bb + 1) * 64], in0=v3(vim), scalar1=self.gvec[:, 16 + bb:17 + bb], scalar2=self.gvec[:, 19:20], op0=ALU.mult, op1=ALU.mult),
                          [B1, self.Bvecs], [BW])
            S.barrier()
            with ExitStack() as e2:
                w = e2.enter_context(self.sbuf("s5w_sb2", [128, S5X], F32)); self.Bs5w = Buf()
                S.dma("sp", w[:], self.dram["s5w"][:, l, :], reads=[self.dbuf["s5w"]], writes=[self.Bs5w])
                aP = w[:, oAP:oAP + 32].rearrange("p (g r) -> p g r", r=2)
                lP = w[:, oLP:oLP + 16]
                bP = w[:, oBP:oBP + 1024].rearrange("p (g r c) -> p g r c", g=16, r=2)
                cP = w[:, oCP:oCP + 1024].rearrange("p (g r c) -> p g r c", g=16, r=2)
                lre, lim, fre, fim, B2 = self.s5_lambda(e2, "p", aP[:, :, 0], aP[:, :, 1], lP, [128, 16])
                tq = e2.enter_context(self.sbuf("ptq", [128, 16], F32))
                Pp = e2.enter_context(self.sbuf("pPw", [128, 9, 2, 16], F32))
                V(lambda: nc.vector.memset(Pp[:, 0, 0, :], 1.0), [], [B2])
                V(lambda: nc.vector.memset(Pp[:, 0, 1, :], 0.0), [], [B2])
                for m in range(1, 9):
                    self.cmul(Pp[:, m, 0, :], Pp[:, m, 1, :], Pp[:, m - 1, 0, :], Pp[:, m - 1, 1, :], lre[:], lim[:], tq[:], [B2], [B2])
                lkr = e2.enter_context(self.sbuf("lkr", [128, 8, 16], F32)); lki = e2.enter_context(self.sbuf("lki", [128, 8, 16], F32))
                V(lambda: nc.vector.tensor_copy(out=lkr[:, 0, :], in_=Pp[:, 8, 0, :]), [B2], [B2])
                V(lambda: nc.vector.tensor_copy(out=lki[:, 0, :], in_=Pp[:, 8, 1, :]), [B2], [B2])
                for lev in range(1, 8):
                    self.cmul(lkr[:, lev, :], lki[:, lev, :], lkr[:, lev - 1, :], lki[:, lev - 1, :], lkr[:, lev - 1, :], lki[:, lev - 1, :], tq[:], [B2], [B2])
                V(lambda: nc.vector.tensor_copy(out=LK[:, :, :, 0], in_=lkr[:].rearrange("p l g -> p g l")), [B2], [BW])
                V(lambda: nc.vector.tensor_copy(out=LK[:, :, :, 1], in_=lki[:].rearrange("p l g -> p g l")), [B2], [BW])
                V(lambda: nc.vector.tensor_scalar(out=LK[:, :, :, 2], in0=lki[:].rearrange("p l g -> p g l"), scalar1=-1.0, scalar2=None, op0=ALU.mult), [B2], [BW])
                Bpr = e2.enter_context(self.sbuf("pBre", [128, 16, 64], F32)); Bpi = e2.enter_context(self.sbuf("pBim", [128, 16, 64], F32))
                V(lambda: nc.vector.memset(Bpr[:], 0.0), [], [B2])
                V(lambda: nc.vector.memset(Bpi[:], 0.0), [], [B2])
                t3 = e2.enter_context(self.sbuf("pt3", [128, 16, 32], F32))
                bc = lambda t: t[:].unsqueeze(2).to_broadcast([128, 16, 32])
                self.cmul(Bpr[:, :, 32:64], Bpi[:, :, 32:64], bc(fre), bc(fim), bP[:, :, 0, :], bP[:, :, 1, :], t3[:], [B2, self.Bs5w], [B2])
                CLr = e2.enter_context(self.sbuf("CLr", [128, 16, 9, 32], F32)); CLn = e2.enter_context(self.sbuf("CLn", [128, 16, 9, 32], F32))
                for m in range(9):
                    pr = Pp[:, m, 0, :].unsqueeze(2).to_broadcast([128, 16, 32])
                    pi = Pp[:, m, 1, :].unsqueeze(2).to_broadcast([128, 16, 32])
                    V(lambda: nc.vector.tensor_tensor(out=CLr[:, :, m, :], in0=cP[:, :, 0, :], in1=pr, op=ALU.mult), [B2, self.Bs5w], [B2])
                    V(lambda: nc.vector.tensor_tensor(out=t3[:], in0=cP[:, :, 1, :], in1=pi, op=ALU.mult), [B2, self.Bs5w], [B2])
                    V(lambda: nc.vector.tensor_tensor(out=CLr[:, :, m, :], in0=CLr[:, :, m, :], in1=t3[:], op=ALU.subtract), [B2], [B2])
                    V(lambda: nc.vector.tensor_tensor(out=CLn[:, :, m, :], in0=cP[:, :, 0, :], in1=pi, op=ALU.mult), [B2, self.Bs5w], [B2])
                    V(lambda: nc.vector.tensor_tensor(out=t3[:], in0=cP[:, :, 1, :], in1=pr, op=ALU.mult), [B2, self.Bs5w], [B2])
                    V(lambda: nc.vector.scalar_tensor_tensor(out=CLn[:, :, m, :], in0=CLn[:, :, m, :], scalar=-1.0, in1=t3[:], op0=ALU.mult, op1=ALU.subtract), [B2], [B2])
                V(lambda: nc.vector.tensor_copy(out=M2re[:], in_=CLr[:, :, 1:9, :]), [B2], [BW])
                V(lambda: nc.vector.tensor_copy(out=M2im[:], in_=CLn[:, :, 1:9, :]), [B2], [BW])
                V(lambda: nc.vector.memset(M2hr[:], 0.0), [], [BW])
                V(lambda: nc.vector.memset(M2hi[:], 0.0), [], [BW])
                for gt in range(4):
                    V(lambda: nc.vector.tensor_copy(out=M2hr[:, gt, :, 32:64], in_=CLr[:, 4 * gt + 3, 1:9, :]), [B2], [BW])
                    V(lambda: nc.vector.tensor_copy(out=M2hi[:, gt, :, 32:64], in_=CLn[:, 4 * gt + 3, 1:9, :]), [B2], [BW])
                KbdF = e2.enter_context(self.sbuf("KbdF", [128, 4, 8, 128], F32))
                V(lambda: nc.vector.memset(KbdF[:], 0.0), [], [B2])
                for pair in range(16):
                    gt, gp = pair // 4, pair % 4
                    pt, Bp = self.psum()
                    rs = slice(32 * gp, 32 * gp + 32) if gp < 3 else slice(64, 128)
                    cs_ = slice(32, 64) if gp < 3 else slice(0, 64)
                    P(lambda: nc.tensor.matmul(pt[rs, 0:256], lhsT=Bpr[:, pair, cs_], rhs=CLr[:, pair, 0:8, :], start=True, stop=False), [B2], [Bp])
                    P(lambda: nc.tensor.matmul(pt[rs, 0:256], lhsT=Bpi[:, pair, cs_], rhs=CLn[:, pair, 0:8, :], start=False, stop=True), [B2], [Bp])
                    V(lambda: nc.vector.tensor_copy(out=KbdF[rs, gt, :, 32 * gp:32 * gp + 32], in_=pt[rs, 0:256].rearrange("p (m c) -> p m c", c=32)), [Bp, B2], [B2])
                for gt in range(4):
                    V(lambda: nc.vector.scalar_tensor_tensor(out=KbdF[:, gt, 0, :], in0=self.ident_f[:], scalar=self.vcol(l, "s5_d", gt, 1), in1=KbdF[:, gt, 0, :],
                                                             op0=ALU.mult, op1=ALU.add), [B2, Bc, self.Bvecs], [B2])
                V(lambda: nc.vector.tensor_copy(out=Kbd[:], in_=KbdF[:]), [B2], [BW])
            S.barrier()
            if "s5pre" in self.debug:
                self.dump("dbg_M1re", M1re[:], [BW]); self.dump("dbg_M1im", M1im[:], [BW]); self.dump("dbg_M2re", M2re[:], [BW])
                self.dump("dbg_M2im", M2im[:], [BW]); self.dump("dbg_Kbd", Kbd[:], [BW]); self.dump("dbg_LK", LK[:], [BW])
            for sq in range(NSEQ):
                with ExitStack() as e3:
                    self.s5_seq(e3, l, sq, (M1re, M1im, M1hr, M1hi), (M2re, M2im, M2hr, M2hi), Kbd, LK, wglu, BW, Bwg)
                    S.barrier()

    def s5_seq(self, es, l, sq, M1s, M2s, Kbd, LK, wglu, BW, Bwg):
        M1re, M1im, M1hr, M1hi = M1s
        M2re, M2im, M2hr, M2hi = M2s
        nc, S = self.nc, self.S
        V, A, P = self.V, self.A, self.P
        pT, BpT = self.dram["pT"], self.dbuf["pT"]
        ym, Bym = self.dram["ymix"], self.dbuf["ymix"]
        t0 = sq * SEQ
        L = SEQ
        NC8 = L // 8
        us = self.tiles(es, "s5u", [128, L], BF16, 4)
        for gt in range(4):
            S.dma("sp", us[gt][0][:], pT[S50 + 128 * gt:S50 + 128 * gt + 128, t0:t0 + L], reads=[BpT], writes=[us[gt][1]])
        S16r = es.enter_context(self.sbuf("s5Sr", [128, 16, NC8 + 1], BF16)); S16i = es.enter_context(self.sbuf("s5Si", [128, 16, NC8 + 1], BF16))
        BS16 = [Buf() for _ in range(16)]
        Bz = Buf()
        V(lambda: nc.vector.memset(S16r[:, :, 0:1], 0.0), [], [Bz])
        V(lambda: nc.vector.memset(S16i[:, :, 0:1], 0.0), [], [Bz])
        for b_ in BS16:
            b_.lw = Bz.lw
        sc = [[[self.tiles(es, "s5sc", [128, 2 * NC8], F32, 2) for _ri in range(2)] for _pp in range(1)] for _slot in range(2)]
        for slot in range(2):
            for ri in range(2):
                for pp in range(2):
                    tt_, Bt = sc[slot][0][ri][pp]
                    V(lambda: nc.vector.memset(tt_[:, 0:NC8], 0.0), [], [Bt])
        for p0 in range(0, 16, 2):
            prs = [p0, p0 + 1]
            for si, pair in enumerate(prs):
                gt, gp = pair // 4, pair % 4
                rs = slice(32 * gp, 32 * gp + 32) if gp < 3 else slice(64, 128)
                u, Bu = us[gt]
                u3 = u[:].rearrange("p (c j) -> p j c", j=8)
                for ri, M1 in enumerate((M1re, M1im) if gp < 3 else (M1hr, M1hi)):
                    pt, Bp = self.psum()
                    for j in range(8):
                        P(lambda: nc.tensor.matmul(pt[:, 0:NC8], lhsT=M1[rs, gt, j, :], rhs=u3[rs, j, :], start=(j == 0), stop=(j == 7)), [BW, Bu], [Bp])
                    tt_, Bt = sc[si][0][ri][0]
                    A(lambda: nc.scalar.copy(out=tt_[:, NC8:2 * NC8], in_=pt[:, 0:NC8]), [Bp], [Bt])
            for lev in range(8):
                sh = 1 << lev
                src, dst = lev % 2, (lev + 1) % 2
                for step in range(2):
                    for si, pair in enumerate(prs):
                        (re_s, Brs), (im_s, Bis) = sc[si][0][0][src], sc[si][0][1][src]
                        (re_d, Brd), (im_d, Bid) = sc[si][0][0][dst], sc[si][0][1][dst]
                        lr, li, nli = LK[:, pair, lev, 0:1], LK[:, pair, lev, 1:2], LK[:, pair, lev, 2:3]
                        cur = slice(NC8, 2 * NC8); shf = slice(NC8 - sh, 2 * NC8 - sh)
                        if step == 0:
                            V(lambda: nc.vector.scalar_tensor_tensor(out=re_d[:, cur], in0=re_s[:, shf], scalar=lr, in1=re_s[:, cur], op0=ALU.mult, op1=ALU.add), [Brs, BW], [Brd])
                            V(lambda: nc.vector.scalar_tensor_tensor(out=im_d[:, cur], in0=im_s[:, shf], scalar=lr, in1=im_s[:, cur], op0=ALU.mult, op1=ALU.add), [Bis, BW], [Bid])
                        else:
                            V(lambda: nc.vector.scalar_tensor_tensor(out=re_d[:, cur], in0=im_s[:, shf], scalar=nli, in1=re_d[:, cur], op0=ALU.mult, op1=ALU.add), [Bis, Brd, BW], [Brd])
                            V(lambda: nc.vector.scalar_tensor_tensor(out=im_d[:, cur], in0=re_s[:, shf], scalar=li, in1=im_d[:, cur], op0=ALU.mult, op1=ALU.add), [Brs, Bid, BW], [Bid])
            for si, pair in enumerate(prs):
                (re_f, Brf), (im_f, Bif) = sc[si][0][0][0], sc[si][0][1][0]
                A(lambda: nc.scalar.copy(out=S16r[:, pair, 1:NC8 + 1], in_=re_f[:, NC8:2 * NC8]), [Brf], [BS16[pair]])
                A(lambda: nc.scalar.copy(out=S16i[:, pair, 1:NC8 + 1], in_=im_f[:, NC8:2 * NC8]), [Bif], [BS16[pair]])
        ys = self.tiles(es, "s5y", [128, L], F32, 2)
        yg = self.tiles(es, "s5yg", [128, L], BF16, 4)
        ta = self.tiles(es, "s5ta", [128, 512], F32, 2)
        for gt in range(4):
            y, By = ys[gt % 2]
            u, Bu = us[gt]
            u3 = u[:].rearrange("p (c j) -> p j c", j=8)
            y3 = y[:].rearrange("p (c j) -> p j c", j=8)
            for j in range(8):
                pt, Bp = self.psum()
                for i in range(j + 1):
                    P(lambda: nc.tensor.matmul(pt[:, 0:NC8], lhsT=Kbd[:, gt, j - i, :], rhs=u3[:, i, :], start=(i == 0), stop=False), [BW, Bu], [Bp])
                for gp in range(4):
                    pair = 4 * gt + gp
                    if gp < 3:
                        rs = slice(32 * gp, 32 * gp + 32)
                        l_re, l_im = M2re[:, pair, j, :], M2im[:, pair, j, :]
                    else:
                        rs = slice(64, 128)
                        l_re, l_im = M2hr[:, gt, j, :], M2hi[:, gt, j, :]
                    P(lambda: nc.tensor.matmul(pt[rs, 0:NC8], lhsT=l_re, rhs=S16r[:, pair, 0:NC8], start=False, stop=False), [BW, BS16[pair]], [Bp])
                    P(lambda: nc.tensor.matmul(pt[rs, 0:NC8], lhsT=l_im, rhs=S16i[:, pair, 0:NC8], start=False, stop=(gp == 3)), [BW, BS16[pair]], [Bp])
                A(lambda: nc.scalar.copy(out=y3[:, j, :], in_=pt[:, 0:NC8]), [Bp], [By])
            g16, Bg = yg[gt]
            for tb in range(L // 512):
                cs = slice(tb * 512, tb * 512 + 512)
                t_, Bt = ta[tb % 2]
                A(lambda: nc.scalar.activation(out=t_[:], in_=y[:, cs], func=AF.Square), [By], [Bt])
                V(lambda: nc.vector.tensor_scalar(out=t_[:], in0=t_[:], scalar1=0.044715, scalar2=1.0, op0=ALU.mult, op1=ALU.add), [Bt], [Bt])
                V(lambda: nc.vector.tensor_tensor(out=t_[:], in0=t_[:], in1=y[:, cs], op=ALU.mult), [Bt, By], [Bt])
                A(lambda: nc.scalar.activation(out=t_[:], in_=t_[:], func=AF.Sigmoid, scale=1.5957691216057308), [Bt], [Bt])
                V(lambda: nc.vector.tensor_tensor(out=g16[:, cs], in0=t_[:], in1=y[:, cs], op=ALU.mult), [Bt, By], [Bg])
        yo = self.tiles(es, "s5yo", [128, 512], BF16, 2)
        sg = self.tiles(es, "s5sg", [128, 512], F32, 2)
        cnt = 0
        for m in range(4):
            for tb in range(L // 512):
                cs = slice(tb * 512, tb * 512 + 512)
                pt, Bp = self.psum()
                for kt in range(4):
                    P(lambda: nc.tensor.matmul(pt[:], lhsT=wglu[:, kt, m * 128:(m + 1) * 128], rhs=yg[kt][0][:, cs], start=(kt == 0), stop=(kt == 3)), [Bwg, yg[kt][1]], [Bp])
                s_, Bs = sg[cnt % 2]; o_, Bo = yo[cnt % 2]; cnt += 1
                A(lambda: nc.scalar.activation(out=s_[:], in_=pt[:], func=AF.Sigmoid, bias=self.vcol(l, "s5_b_glu", m, 1)), [Bp, self.Bvecs], [Bs])
                V(lambda: nc.vector.tensor_tensor(out=o_[:], in0=s_[:], in1=yg[m][0][:, cs], op=ALU.mult), [Bs, yg[m][1]], [Bo])
                S.dma("sp", ym[1, 128 * m:128 * m + 128, t0 + tb * 512:t0 + tb * 512 + 512], o_[:], reads=[Bo], writes=[Bym])

    def stage_rwkv(self, l, sq):
        nc, S = self.nc, self.S
        V, A, P = self.V, self.A, self.P
        pT, BpT = self.dram["pT"], self.dbuf["pT"]
        ym, Bym = self.dram["ymix"], self.dbuf["ymix"]
        Bc = self.Bconst
        Bv = self.Bvecs
        t0 = sq * SEQ
        L = SEQ
        NCH = L // 64
        ALPHA = float(np.exp(-0.5))
        with ExitStack() as es:
            WU = es.enter_context(self.sbuf("rwWU", [128, 512], BF16)); BWU = Buf()
            GU = es.enter_context(self.sbuf("rwGU", [128, 512], BF16)); BGU = Buf()
            S.dma("pool", WU[0:64, :], self.dram["rw_w_up"][l], reads=[self.dbuf["rw_w_up"]], writes=[BWU])
            S.dma("pool", WU[64:128, :], self.dram["rw_a_up"][l], reads=[self.dbuf["rw_a_up"]], writes=[BWU])
            S.dma("pool", GU[:], self.dram["rw_g_up"][l], reads=[self.dbuf["rw_g_up"]], writes=[BGU])
            TW = es.enter_context(self.sbuf("rwTW", [128, L], BF16)); BTW = Buf()
            SG = es.enter_context(self.sbuf("rwSG", [128, L], BF16)); BSG = Buf()
            bdn = ["Rt", "Kt", "Bt", "At", "Vb"]
            BD = {n: es.enter_context(self.sbuf("rw" + n, [128, NCH, 128], BF16)) for n in bdn}
            BBD = {n: Buf(n) for n in bdn}
            for n in bdn:
                V(lambda: nc.vector.memset(BD[n][:], 0.0), [], [BBD[n]])
            Dt = es.enter_context(self.sbuf("rwD", [128, NCH], F32)); BD_ = Buf()
            g16 = es.enter_context(self.sbuf("rwg16", [128, L], BF16)); Bg16 = Buf()
            bon = es.enter_context(self.sbuf("rwbon", [128, L], BF16)); Bbon = Buf()
            Yfm = es.enter_context(self.sbuf("rwY", [128, L], F32)); BY = Buf()
            Sall = es.enter_context(self.sbuf("rwSall", [128, NCH + 1, 128], BF16)); BSall = [Buf() for _ in range(NCH + 1)]
            with ExitStack() as e2:
                xin = e2.enter_context(self.sbuf("rwxin", [128, L + 1], BF16)); Bxin = Buf()
                tmp = e2.enter_context(self.sbuf("rwtmp", [128, L], F32)); Btmp = Buf()
                for (r0, mucol, which) in [(1536, 12, 0), (1664, 13, 1)]:
                    V(lambda: nc.vector.memset(xin[:, 0:1], 0.0), [], [Bxin])
                    S.dma("sp", xin[:, 1:L + 1], pT[RW0 + r0:RW0 + r0 + 128, t0:t0 + L], reads=[BpT], writes=[Bxin])
                    V(lambda: nc.vector.tensor_tensor(out=tmp[:], in0=xin[:, 0:L], in1=xin[:, 1:L + 1], op=ALU.subtract), [Bxin], [Btmp])
                    V(lambda: nc.vector.scalar_tensor_tensor(out=tmp[:], in0=tmp[:], scalar=self.vcol(l, "rw_mu", mucol, 1), in1=xin[:, 1:L + 1],
                                                             op0=ALU.mult, op1=ALU.add), [Btmp, Bxin, Bv], [Btmp])
                    if which == 0:
                        A(lambda: nc.scalar.activation(out=TW[0:64, :], in_=tmp[0:64, :], func=AF.Tanh), [Btmp], [BTW])
                        V(lambda: nc.vector.tensor_copy(out=TW[64:128, :], in_=tmp[64:128, :]), [Btmp], [BTW])
                    else:
                        A(lambda: nc.scalar.activation(out=SG[:], in_=tmp[:], func=AF.Sigmoid), [Btmp], [BSG])
                S.barrier()
            for hp in range(4):
                V(lambda: nc.vector.memset(Sall[:, 0, :], 0.0), [], [BSall[0]])
                with ExitStack() as e2:
                    self.rwkv_prologue(e2, l, sq, hp, WU, BWU, GU, BGU, TW, BTW, SG, BSG, BD, BBD, Dt, BD_, g16, Bg16, bon, Bbon)
                    S.barrier()
                with ExitStack() as e2:
                    self.rwkv_chunks(e2, BD, BBD, Dt, BD_, Yfm, BY, Sall, BSall)
                    S.barrier()
                with ExitStack() as e2:
                    self.rwkv_epilogue(e2, l, sq, hp, Yfm, BY, g16, Bg16, bon, Bbon)
                    S.barrier()

    def rwkv_prologue(self, es, l, sq, hp, WU, BWU, GU, BGU, TW, BTW, SG, BSG, BD, BBD, Dt, BDt, g16, Bg16, bon, Bbon):
        nc, S = self.nc, self.S
        V, A, P = self.V, self.A, self.P
        pT, BpT = self.dram["pT"], self.dbuf["pT"]
        Bc, Bv = self.Bconst, self.Bvecs
        t0 = sq * SEQ
        L = SEQ
        NCH = L // 64
        ALPHA = float(np.exp(-0.5))
        cs_hp = slice(128 * hp, 128 * hp + 128)
        def f32t(n):
            return es.enter_context(self.sbuf("rwp" + n, [128, L + 1], F32)), Buf(n)
        xin = [(es.enter_context(self.sbuf("rwpx%d" % i, [128, L + 1], BF16)), Buf()) for i in range(3)]
        (X1, B1), (X2, B2), (X3, B3) = [(es.enter_context(self.sbuf("rwpb%d" % i, [128, L], BF16 if i != 1 else F32)), Buf()) for i in range(3)]
        (X4, B4), (X5, B5), (X6, B6), (X7, B7), (X9, B9), (X10, B10) = [f32t(n) for n in ["4", "5", "6", "7", "9", "10"]]
        for i, (dst, Bd, r0) in enumerate([(X1, B1, 0), (X2, B2, 512), (X3, B3, 1024)]):
            xt, Bx = xin[i]
            V(lambda: nc.vector.memset(xt[:, 0:1], 0.0), [], [Bx])
            S.dma("sp", xt[:, 1:L + 1], pT[RW0 + r0 + 128 * hp:RW0 + r0 + 128 * hp + 128, t0:t0 + L], reads=[BpT], writes=[Bx])
            V(lambda: nc.vector.tensor_tensor(out=X7[:, 0:L], in0=xt[:, 0:L], in1=xt[:, 1:L + 1], op=ALU.subtract), [Bx], [B7])
            V(lambda: nc.vector.scalar_tensor_tensor(out=dst[:], in0=X7[:, 0:L], scalar=self.vcol(l, "rw_mu", 4 * i + hp, 1), in1=xt[:, 1:L + 1],
                                                     op0=ALU.mult, op1=ALU.add), [B7, Bx, Bv], [Bd])
        for tb in range(L // 512):
            cs = slice(tb * 512, tb * 512 + 512)
            pt, Bp = self.psum()
            P(lambda: nc.tensor.matmul(pt[:], lhsT=WU[0:64, cs_hp], rhs=TW[0:64, cs], start=True, stop=True), [BWU, BTW], [Bp])
            A(lambda: nc.scalar.activation(out=X4[:, cs], in_=pt[:], func=AF.Sigmoid, bias=self.vcol(l, "rw_w0", hp, 1)), [Bp, Bv], [B4])
            pt, Bp = self.psum()
            P(lambda: nc.tensor.matmul(pt[:], lhsT=WU[64:128, cs_hp], rhs=TW[64:128, cs], start=True, stop=True), [BWU, BTW], [Bp])
            A(lambda: nc.scalar.activation(out=X5[:, cs], in_=pt[:], func=AF.Sigmoid, bias=self.vcol(l, "rw_a0", hp, 1)), [Bp, Bv], [B5])
            pt, Bp = self.psum()
            P(lambda: nc.tensor.matmul(pt[:], lhsT=GU[:, cs_hp], rhs=SG[:, cs], start=True, stop=True), [BGU, BSG], [Bp])
            V(lambda: nc.vector.tensor_copy(out=g16[:, cs], in_=pt[:]), [Bp], [Bg16])
        V(lambda: nc.vector.memset(X9[:, 0:1], 0.0), [], [B9])
        V(lambda: nc.vector.tensor_tensor_scan(out=X9[:, 1:L + 1], data0=self.cst[:, 0:1].to_broadcast([128, L]), data1=X4[:, 0:L],
                                               initial=0.0, op0=ALU.mult, op1=ALU.add), [B4, Bc], [B9])
        V(lambda: nc.vector.tensor_scalar(out=X6[:, 0:L], in0=X2[:], scalar1=self.vcol(l, "rw_k_k", hp, 1), scalar2=None, op0=ALU.mult), [B2, Bv], [B6])
        A(lambda: nc.scalar.activation(out=X7[:, 0:L], in_=X6[:, 0:L], func=AF.Square), [B6], [B7])
        for tb in range(L // 512):
            cs = slice(tb * 512, tb * 512 + 512)
            pt, Bp = self.psum()
            P(lambda: nc.tensor.matmul(pt[:], lhsT=self.ones_bd[:], rhs=X7[:, cs], start=True, stop=True), [Bc, B7], [Bp])
            A(lambda: nc.scalar.activation(out=X4[:, cs], in_=pt[:], func=AF.Sqrt, bias=self.cst[:, 2:3]), [Bp, Bc, B9], [B4])
        V(lambda: nc.vector.reciprocal(out=X4[:, 0:L], in_=X4[:, 0:L]), [B4], [B4])
        V(lambda: nc.vector.tensor_tensor(out=X6[:, 0:L], in0=X6[:, 0:L], in1=X4[:, 0:L], op=ALU.mult), [B6, B4], [B6])
        V(lambda: nc.vector.tensor_scalar(out=X7[:, 0:L], in0=X5[:, 0:L], scalar1=-1.0, scalar2=self.vcol(l, "rw_k_a", hp, 1), op0=ALU.add, op1=ALU.mult), [B5, Bv], [B7])
        V(lambda: nc.vector.scalar_tensor_tensor(out=X2[:], in0=X7[:, 0:L], scalar=1.0, in1=X2[:], op0=ALU.add, op1=ALU.mult), [B7, B2], [B2])
        V(lambda: nc.vector.tensor_tensor(out=X5[:, 0:L], in0=X6[:, 0:L], in1=X5[:, 0:L], op=ALU.mult), [B6, B5], [B5])
        V(lambda: nc.vector.tensor_tensor(out=X7[:, 0:L], in0=X1[:], in1=X2[:], op=ALU.mult), [B1, B2], [B7])
        V(lambda: nc.vector.tensor_scalar(out=X7[:, 0:L], in0=X7[:, 0:L], scalar1=self.vcol(l, "rw_r_k", hp, 1), scalar2=None, op0=ALU.mult), [B7, Bv], [B7])
        for tb in range(L // 512):
            cs = slice(tb * 512, tb * 512 + 512)
            pt, Bp = self.psum()
            P(lambda: nc.tensor.matmul(pt[:], lhsT=self.ones_bd[:], rhs=X7[:, cs], start=True, stop=True), [Bc, B7], [Bp])
            V(lambda: nc.vector.tensor_tensor(out=bon[:, cs], in0=pt[:], in1=X3[:, cs], op=ALU.mult), [Bp, B3], [Bbon])
        c3 = lambda ap: ap.rearrange("p (c t) -> p c t", t=64)
        base = c3(X9[:, 0:L])[:, :, 0:1].to_broadcast([128, NCH, 64])
        V(lambda: nc.vector.tensor_tensor(out=c3(X7[:, 0:L]), in0=c3(X9[:, 1:L + 1]), in1=base, op=ALU.subtract), [B9, Bbon], [B7])
        A(lambda: nc.scalar.activation(out=X4[:, 0:L], in_=X7[:, 0:L], func=AF.Exp, scale=-ALPHA), [B7], [B4])
        A(lambda: nc.scalar.activation(out=X10[:, 0:L], in_=X7[:, 0:L], func=AF.Exp, scale=ALPHA), [B7], [B10])
        V(lambda: nc.vector.tensor_tensor(out=c3(X7[:, 0:L]), in0=c3(X9[:, 0:L]), in1=base, op=ALU.subtract), [B9, B4, B10], [B7])
        A(lambda: nc.scalar.activation(out=X7[:, 0:L], in_=X7[:, 0:L], func=AF.Exp, scale=-ALPHA), [B7], [B7])
        V(lambda: nc.vector.tensor_copy(out=Dt[:], in_=c3(X4[:, 0:L])[:, :, 63]), [B4], [BDt])
        for hf in range(2):
            ps_ = slice(64 * hf, 64 * hf + 64)
            fs_ = slice(64 * hf, 64 * hf + 64)
            V(lambda: nc.vector.tensor_tensor(out=BD["Rt"][ps_, :, fs_], in0=c3(X1[ps_, :]), in1=c3(X4[ps_, 0:L]), op=ALU.mult), [B1, B4], [BBD["Rt"]])
            V(lambda: nc.vector.tensor_tensor(out=BD["Kt"][ps_, :, fs_], in0=c3(X2[ps_, :]), in1=c3(X10[ps_, 0:L]), op=ALU.mult), [B2, B10], [BBD["Kt"]])
            V(lambda: nc.vector.tensor_tensor(out=BD["Bt"][ps_, :, fs_], in0=c3(X5[ps_, 0:L]), in1=c3(X10[ps_, 0:L]), op=ALU.mult), [B5, B10], [BBD["Bt"]])
            V(lambda: nc.vector.scalar_tensor_tensor(out=BD["At"][ps_, :, fs_], in0=c3(X6[ps_, 0:L]), scalar=-1.0, in1=c3(X7[ps_, 0:L]), op0=ALU.mult, op1=ALU.mult),
              [B6, B7], [BBD["At"]])
            A(lambda: nc.scalar.copy(out=BD["Vb"][ps_, :, fs_], in_=c3(X3[ps_, :])), [B3], [BBD["Vb"]])

    def rwkv_chunks(self, es, BD, BBD, Dt, BDt, Yfm, BY, Sall, BSall):
        nc, S = self.nc, self.S
        V, A, P = self.V, self.A, self.P
        Bc = self.Bconst
        NCH = SEQ // 64
        W = 4
        NU = NCH // 2
        Rt, Kt, Bt, At, Vb = [BD[n] for n in ["Rt", "Kt", "Bt", "At", "Vb"]]
        BRt, BKt, BBt, BAt, BVb = [BBD[n] for n in ["Rt", "Kt", "Bt", "At", "Vb"]]
        NR = 2 * W
        TM3 = self.tiles(es, "rcTM", [128, 2, 3, 128], BF16, NR)
        ZA = self.tiles(es, "rcZA", [128, 2, 256], BF16, NR)
        ZB = self.tiles(es, "rcZB", [128, 2, 256], BF16, NR)
        SC1 = self.tiles(es, "rcS1", [128, 2, 2, 128], BF16, NR)
        SC2 = self.tiles(es, "rcS2", [128, 2, 2, 128], BF16, NR)
        SC3 = self.tiles(es, "rcS3", [128, 2, 128], BF16, NR)
        PPA = self.tiles(es, "rcPA", [128, 2, 2, 128], BF16, NR)
        PPB = self.tiles(es, "rcPB", [128, 2, 2, 128], BF16, NR)
        IGQ = self.tiles(es, "rcIG", [128, 2, 2, 128], BF16, NR)
        HD = self.tiles(es, "rcHD", [128, 2, 128], F32, NR)
        m01 = self.mbd[:, 0:2, :].unsqueeze(1).to_broadcast([128, 2, 2, 128])
        m12 = self.mbd[:, 1:3, :].unsqueeze(1).to_broadcast([128, 2, 2, 128])
        m2 = self.mbd[:, 2:3, :].to_broadcast([128, 2, 128])
        identb = self.ident_f[:].unsqueeze(1).to_broadcast([128, 2, 128])

        pending = []
        post = []

        def drain(n):
            for _ in range(n):
                if pending:
                    pending.pop(0)()

        ngroups = NU // W
        for gi in range(ngroups):
            units = list(range(gi * W, gi * W + W))
            sl = {u: (u % NR) for u in units}
            for u in units:
                i = sl[u]; c0 = 2 * u
                pt, Bp = self.psum()
                pb = pt[:].bitcast(BF16).rearrange("p (k s t) -> p k s t", k=2, s=4)
                for k in range(2):
                    for si, (src, Bs) in enumerate([(Bt, BBt), (Kt, BKt), (Vb, BVb), (At, BAt)]):
                        P(lambda: nc.tensor.transpose(out=pb[:, k, si, :], in_=src[:, c0 + k, :], identity=self.ident_b[:]), [Bs, Bc], [Bp])
                A(lambda: nc.scalar.copy(out=TM3[i][0][:], in_=pb[:, :, 0:3, :]), [Bp], [TM3[i][1]])
                A(lambda: nc.scalar.copy(out=ZA[i][0][:, :, 0:128], in_=pb[:, :, 3, :]), [Bp], [ZA[i][1]])
            drain(1)
            for u in units:
                i = sl[u]; c0 = 2 * u
                pt, Bp = self.psum(); p4 = pt[:].rearrange("p (k s t) -> p k s t", k=2, s=2)
                for k in range(2):
                    c = c0 + k
                    P(lambda: nc.tensor.matmul(p4[:, k, 0, :], lhsT=At[:, c, :], rhs=Bt[:, c, :], start=True, stop=True), [BAt, BBt], [Bp])
                    P(lambda: nc.tensor.matmul(p4[:, k, 1, :], lhsT=Bt[:, c, :], rhs=At[:, c, :], start=True, stop=True), [BAt, BBt], [Bp])
                V(lambda: nc.vector.tensor_tensor(out=SC1[i][0][:], in0=p4, in1=m01, op=ALU.mult), [Bp, Bc], [SC1[i][1]])
                pt, Bp = self.psum(); p4 = pt[:].rearrange("p (k s t) -> p k s t", k=2, s=2)
                for k in range(2):
                    c = c0 + k
                    P(lambda: nc.tensor.matmul(p4[:, k, 0, :], lhsT=Kt[:, c, :], rhs=At[:, c, :], start=True, stop=True), [BAt, BKt], [Bp])
                    P(lambda: nc.tensor.matmul(p4[:, k, 1, :], lhsT=Bt[:, c, :], rhs=Rt[:, c, :], start=True, stop=True), [BRt, BBt], [Bp])
                V(lambda: nc.vector.tensor_tensor(out=SC2[i][0][:], in0=p4, in1=m12, op=ALU.mult), [Bp, Bc], [SC2[i][1]])
                pt, Bp = self.psum(); p3 = pt[:, 0:256].rearrange("p (k t) -> p k t", k=2)
                for k in range(2):
                    c = c0 + k
                    P(lambda: nc.tensor.matmul(p3[:, k, :], lhsT=Kt[:, c, :], rhs=Rt[:, c, :], start=True, stop=True), [BRt, BKt], [Bp])
                V(lambda: nc.vector.tensor_tensor(out=SC3[i][0][:], in0=p3, in1=m2, op=ALU.mult), [Bp, Bc], [SC3[i][1]])
            drain(1)
            for u in units:
                i = sl[u]
                pt, Bp = self.psum(); p3 = pt[:, 0:256].rearrange("p (k t) -> p k t", k=2)
                for k in range(2):
                    P(lambda: nc.tensor.matmul(p3[:, k, :], lhsT=SC2[i][0][:, k, 0, :], rhs=TM3[i][0][:, k, 2, :], start=True, stop=True), [SC2[i][1], TM3[i][1]], [Bp])
                A(lambda: nc.scalar.copy(out=ZA[i][0][:, :, 128:256], in_=p3), [Bp], [ZA[i][1]])
            drain(1)
            for lev in range(6):
                for u in units:
                    i = sl[u]
                    PPs = SC1[i] if lev == 0 else (PPA[i] if lev % 2 == 1 else PPB[i])
                    PPd = PPA[i] if lev % 2 == 0 else PPB[i]
                    Zs = ZA[i] if lev % 2 == 0 else ZB[i]
                    Zd = ZB[i] if lev % 2 == 0 else ZA[i]
                    pt, Bp = self.psum(); pz = pt[:].rearrange("p (k t) -> p k t", k=2)
                    for k in range(2):
                        P(lambda: nc.tensor.matmul(pz[:, k, :], lhsT=PPs[0][:, k, 1, :], rhs=Zs[0][:, k, :], start=True, stop=True), [PPs[1], Zs[1]], [Bp])
                    V(lambda: nc.vector.tensor_tensor(out=Zd[0][:], in0=pz, in1=Zs[0][:], op=ALU.add), [Bp, Zs[1]], [Zd[1]])
                    if lev < 5:
                        pt, Bp = self.psum(); p4 = pt[:].rearrange("p (k s t) -> p k s t", k=2, s=2)
                        for k in range(2):
                            P(lambda: nc.tensor.matmul(p4[:, k, 0, :], lhsT=PPs[0][:, k, 1, :], rhs=PPs[0][:, k, 0, :], start=True, stop=True), [PPs[1]], [Bp])
                            P(lambda: nc.tensor.matmul(p4[:, k, 1, :], lhsT=PPs[0][:, k, 0, :], rhs=PPs[0][:, k, 1, :], start=True, stop=True), [PPs[1]], [Bp])
                        A(lambda: nc.scalar.copy(out=PPd[0][:], in_=p4), [Bp], [PPd[1]])
                drain(1)
            for u in units:
                i = sl[u]; c0 = 2 * u
                pt, Bp = self.psum(); p4 = pt[:].rearrange("p (k s t) -> p k s t", k=2, s=2)
                for k in range(2):
                    P(lambda: nc.tensor.matmul(p4[:, k, 0, :], lhsT=ZA[i][0][:, k, 0:128], rhs=TM3[i][0][:, k, 0, :], start=True, stop=True), [ZA[i][1], TM3[i][1]], [Bp])
                    P(lambda: nc.tensor.matmul(p4[:, k, 1, :], lhsT=ZA[i][0][:, k, 0:128], rhs=SC2[i][0][:, k, 1, :], start=True, stop=True), [ZA[i][1], SC2[i][1]], [Bp])
                V(lambda: nc.vector.tensor_tensor(out=IGQ[i][0][:, :, 0, :], in0=p4[:, :, 0, :], in1=identb, op=ALU.add), [Bp, Bc], [IGQ[i][1]])
                V(lambda: nc.vector.tensor_tensor(out=IGQ[i][0][:, :, 1, :], in0=p4[:, :, 1, :], in1=Rt[:, c0:c0 + 2, :], op=ALU.add), [Bp, BRt], [IGQ[i][1]])
            drain(1)
            for u in units:
                i = sl[u]; c0 = 2 * u
                pt, Bp = self.psum(); p3 = pt[:, 0:256].rearrange("p (k t) -> p k t", k=2)
                for k in range(2):
                    P(lambda: nc.tensor.matmul(p3[:, k, :], lhsT=TM3[i][0][:, k, 0, :], rhs=ZA[i][0][:, k, 128:256], start=True, stop=False), [ZA[i][1], TM3[i][1]], [Bp])
                    P(lambda: nc.tensor.matmul(p3[:, k, :], lhsT=TM3[i][0][:, k, 1, :], rhs=TM3[i][0][:, k, 2, :], start=False, stop=True), [TM3[i][1]], [Bp])
                for k in range(2):
                    A(lambda: nc.scalar.activation(out=HD[i][0][:, k, :], in_=p3[:, k, :], func=AF.Copy, scale=Dt[:, c0 + k:c0 + k + 1]), [Bp, BDt], [HD[i][1]])
                pt, Bp = self.psum(); p3 = pt[:, 0:256].rearrange("p (k t) -> p k t", k=2)
                for k in range(2):
                    P(lambda: nc.tensor.matmul(p3[:, k, :], lhsT=ZA[i][0][:, k, 128:256], rhs=SC2[i][0][:, k, 1, :], start=True, stop=False), [ZA[i][1], SC2[i][1]], [Bp])
                    P(lambda: nc.tensor.matmul(p3[:, k, :], lhsT=TM3[i][0][:, k, 2, :], rhs=SC3[i][0][:, k, :], start=False, stop=True), [TM3[i][1], SC3[i][1]], [Bp])
                for hf in range(2):
                    hs = slice(64 * hf, 64 * hf + 64)
                    A(lambda: nc.scalar.copy(out=Yfm[hs, 128 * u:128 * u + 128].rearrange("p (k t) -> p k t", k=2), in_=p3[hs, :, hs]), [Bp], [BY])
            drain(len(pending))
            for f in post:
                f()
            post = []
            for u in units:
                i = sl[u]; c0 = 2 * u
                for k in range(2):
                    def link(i=i, c=c0 + k, k=k):
                        pt, Bp = self.psum()
                        P(lambda: nc.tensor.matmul(pt[:, 0:128], lhsT=IGQ[i][0][:, k, 0, :], rhs=Sall[:, c, :], start=True, stop=True), [IGQ[i][1], BSall[c]], [Bp])
                        V(lambda: nc.vector.scalar_tensor_tensor(out=Sall[:, c + 1, :], in0=pt[:, 0:128], scalar=Dt[:, c:c + 1], in1=HD[i][0][:, k, :],
                                                                 op0=ALU.mult, op1=ALU.add), [Bp, BDt, HD[i][1]], [BSall[c + 1]])
                    pending.append(link)
                def ph8(i=i, u=u, c0=c0):
                    pt, Bp = self.psum(); p3 = pt[:, 0:256].rearrange("p (k t) -> p k t", k=2)
                    for k in range(2):
                        P(lambda: nc.tensor.matmul(p3[:, k, :], lhsT=Sall[:, c0 + k, :], rhs=IGQ[i][0][:, k, 1, :], start=True, stop=True), [BSall[c0 + k], IGQ[i][1]], [Bp])
                    for hf in range(2):
                        hs = slice(64 * hf, 64 * hf + 64)
                        yv = Yfm[hs, 128 * u:128 * u + 128].rearrange("p (k t) -> p k t", k=2)
                        V(lambda: nc.vector.tensor_tensor(out=yv, in0=p3[hs, :, hs], in1=yv, op=ALU.add), [Bp, BY], [BY])
                post.append(ph8)
        drain(len(pending))
        for f in post:
            f()

    def rwkv_epilogue(self, es, l, sq, hp, Yfm, BY, g16, Bg16, bon, Bbon):
        nc, S = self.nc, self.S
        V, A, P = self.V, self.A, self.P
        ym, Bym = self.dram["ymix"], self.dbuf["ymix"]
        Bc, Bv = self.Bconst, self.Bvecs
        t0 = sq * SEQ
        L = SEQ
        ta = self.tiles(es, "reA", [128, 512], F32, 2)
        tb_ = self.tiles(es, "reB", [128, 512], F32, 2)
        yo = self.tiles(es, "reO", [128, 512], BF16, 2)
        for tb in range(L // 512):
            cs = slice(tb * 512, tb * 512 + 512)
            a_, Ba = ta[tb % 2]; b_, Bb = tb_[tb % 2]; o_, Bo = yo[tb % 2]
            pt, Bp = self.psum()
            P(lambda: nc.tensor.matmul(pt[:], lhsT=self.ones_bd[:], rhs=Yfm[:, cs], start=True, stop=True), [Bc, BY], [Bp])
            V(lambda: nc.vector.scalar_tensor_tensor(out=a_[:], in0=pt[:], scalar=-1.0 / 64, in1=Yfm[:, cs], op0=ALU.mult, op1=ALU.add), [Bp, BY], [Ba])
            A(lambda: nc.scalar.activation(out=b_[:], in_=a_[:], func=AF.Square), [Ba], [Bb])
            pt, Bp = self.psum()
            P(lambda: nc.tensor.matmul(pt[:], lhsT=self.ones_bd[:], rhs=b_[:], start=True, stop=True), [Bc, Bb], [Bp])
            A(lambda: nc.scalar.activation(out=b_[:], in_=pt[:], func=AF.Sqrt, scale=1.0 / 64, bias=self.cst[:, 3:4]), [Bp, Bc], [Bb])
            V(lambda: nc.vector.reciprocal(out=b_[:], in_=b_[:]), [Bb], [Bb])
            V(lambda: nc.vector.tensor_tensor(out=a_[:], in0=a_[:], in1=b_[:], op=ALU.mult), [Ba, Bb], [Ba])
            V(lambda: nc.vector.tensor_scalar(out=a_[:], in0=a_[:], scalar1=self.vcol(l, "rw_ln_w", hp, 1), scalar2=self.vcol(l, "rw_ln_b", hp, 1), op0=ALU.mult, op1=ALU.add),
              [Ba, Bv], [Ba])
            V(lambda: nc.vector.tensor_tensor(out=a_[:], in0=a_[:], in1=bon[:, cs], op=ALU.add), [Ba, Bbon], [Ba])
            V(lambda: nc.vector.tensor_tensor(out=o_[:], in0=a_[:], in1=g16[:, cs], op=ALU.mult), [Ba, Bg16], [Bo])
            S.dma("sp", ym[0, 128 * hp:128 * hp + 128, t0 + tb * 512:t0 + tb * 512 + 512], o_[:], reads=[Bo], writes=[Bym])

    def load_w(self, wt, Bw_list, src3, Bsrc, kc_n, step=4):
        for k0 in range(0, kc_n, step):
            k1 = min(kc_n, k0 + step)
            self.S.dma("pool", wt[:, k0:k1, :], src3[:, k0:k1, :], reads=[Bsrc], writes=[Bw_list[k0 // step]])

    def outproj_res(self, actf, Bact, kc_n, w, Bw_of, xt, Bx, ntt, dst_ap, Bdst, tok0, kstep=4):
        nc, S = self.nc, self.S
        for j in range(ntt):
            for fh in range(2):
                pt, Bp = self.psum()
                for kc in range(kc_n):
                    self.P(lambda: nc.tensor.matmul(pt[:], lhsT=actf(kc, j), rhs=w[:, kc, fh * 512:(fh + 1) * 512], start=(kc == 0), stop=(kc == kc_n - 1)),
                           [Bact, Bw_of(kc)], [Bp])
                self.V(lambda: nc.vector.tensor_tensor(out=xt[:, j, fh * 512:(fh + 1) * 512], in0=pt[:], in1=xt[:, j, fh * 512:(fh + 1) * 512], op=ALU.add),
                       [Bp, Bx[j]], [Bx[j]])
            S.dma("sp", dst_ap[tok0 + j * 128:tok0 + (j + 1) * 128, :], xt[:, j, :], reads=[Bx[j]], writes=[Bdst])

    def xsrc(self, l):
        return (self.dram["x"], self.dbuf["x"]) if l == 0 else (self.dram["xres"], self.dbuf["xres"])

    def stage_merge(self, l):
        nc, S = self.nc, self.S
        V, A, P = self.V, self.A, self.P
        pT, BpT = self.dram["pT"], self.dbuf["pT"]
        ym, Bym = self.dram["ymix"], self.dbuf["ymix"]
        xs_ap, Bxs = self.xsrc(l)
        xd_ap, Bxd = self.dram["xres"], self.dbuf["xres"]
        L = SEQ
        with ExitStack() as es:
            wb = es.enter_context(self.sbuf("mgwb", [128, 12, D], BF16)); Bwb = [Buf() for _ in range(3)]
            wo = es.enter_context(self.sbuf("mgwo", [128, 8, D], BF16)); Bwo = [Buf() for _ in range(2)]
            for i in range(3):
                S.dma("pool", wb[:, 4 * i:4 * i + 4, :], self.dram["w_branch"][l, i].rearrange("(k p) c -> p k c", p=128), reads=[self.dbuf["w_branch"]], writes=[Bwb[i]])
            self.load_w(wo, Bwo, self.dram["w_out"][l].rearrange("(k p) c -> p k c", p=128), self.dbuf["w_out"], 8)
            yt = es.enter_context(self.sbuf("mgy", [128, 12, L], BF16)); Byt = [Buf() for _ in range(12)]
            mg = es.enter_context(self.sbuf("mgm", [128, 8, L], BF16)); Bmg = Buf()
            gts = self.tiles(es, "mgg", [128, 3, 512], BF16, 2)
            acc = self.tiles(es, "mga", [128, 512], F32, 2)
            tmp = self.tiles(es, "mgt", [128, 512], F32, 2)
            xts = [(es.enter_context(self.sbuf("mgx%d" % i, [128, 4, D], F32)), [Buf() for _ in range(4)]) for i in range(2)]
            for sq in range(NSEQ):
                t0 = sq * L
                for i in range(3):
                    for kt in range(4):
                        S.dma("sp", yt[:, 4 * i + kt, :], ym[i, 128 * kt:128 * kt + 128, t0:t0 + L], reads=[Bym], writes=[Byt[4 * i + kt]])
                cnt = 0
                for ft in range(8):
                    for tb in range(L // 512):
                        cs = slice(tb * 512, tb * 512 + 512)
                        g_, Bg = gts[cnt % 2]; a_, Ba = acc[cnt % 2]; t_, Bt = tmp[cnt % 2]; cnt += 1
                        S.dma("sp", g_[:], pT[GT0 + 128 * ft:GT0 + 3072:1024, t0 + tb * 512:t0 + tb * 512 + 512].rearrange("(i p) t -> p i t", p=128) if False else
                              bass.AP(tensor=pT.tensor, offset=pT[GT0 + 128 * ft, t0 + tb * 512].offset, ap=[[T, 128], [1024 * T, 3], [1, 512]]),
                              reads=[BpT], writes=[Bg])
                        for i in range(3):
                            pt, Bp = self.psum()
                            for kt in range(4):
                                P(lambda: nc.tensor.matmul(pt[:], lhsT=wb[:, 4 * i + kt, ft * 128:(ft + 1) * 128], rhs=yt[:, 4 * i + kt, cs], start=(kt == 0), stop=(kt == 3)),
                                  [Bwb[i], Byt[4 * i + kt]], [Bp])
                            if i == 0:
                                V(lambda: nc.vector.tensor_tensor(out=a_[:], in0=pt[:], in1=g_[:, 0, :], op=ALU.mult), [Bp, Bg], [Ba])
                            else:
                                V(lambda: nc.vector.tensor_tensor(out=t_[:], in0=pt[:], in1=g_[:, i, :], op=ALU.mult), [Bp, Bg], [Bt])
                                if i == 1:
                                    V(lambda: nc.vector.tensor_tensor(out=a_[:], in0=a_[:], in1=t_[:], op=ALU.add), [Ba, Bt], [Ba])
                                else:
                                    V(lambda: nc.vector.tensor_tensor(out=mg[:, ft, cs], in0=a_[:], in1=t_[:], op=ALU.add), [Ba, Bt], [Bmg])
                for blk in range(L // 512):
                    xt, Bx = xts[blk % 2]
                    tok0 = t0 + blk * 512
                    for j in range(4):
                        S.dma("sp", xt[:, j, :], xs_ap[tok0 + j * 128:tok0 + (j + 1) * 128, :], reads=[Bxs], writes=[Bx[j]])
                    self.outproj_res(lambda kc, j: mg[:, kc, blk * 512 + j * 128:blk * 512 + (j + 1) * 128], Bmg, 8, wo, lambda kc: Bwo[kc // 4], xt, Bx, 4, xd_ap, Bxd, tok0)
                S.barrier()

    def stage_memn(self):
        nc, S = self.nc, self.S
        self.memn = nc.alloc_sbuf_tensor("memn", [128, 8, NSEQ * NMEM], BF16); self.Bmemn = Buf("memn")
        with ExitStack() as es:
            self.rmsnorm_fm(es, self.dram["mem"], self.dbuf["mem"], NSEQ * NMEM, self.gvec[:, 0:8], self.memn, self.Bmemn, self.eps6[:, 0:1])
            S.barrier()

    def stage_xattn(self, l):
        nc, S = self.nc, self.S
        V, A, P = self.V, self.A, self.P
        Bc = self.Bconst
        xs_ap, Bxs = self.dram["xres"], self.dbuf["xres"]
        NM = NSEQ * NMEM
        with ExitStack() as es:
            kT = es.enter_context(self.sbuf("xakT", [128, 8, NM], BF16)); BkT = Buf()
            vtm = es.enter_context(self.sbuf("xavtm", [128, NM // 128, D], BF16)); Bvtm = Buf()
            wq = es.enter_context(self.sbuf("xawq", [128, 8, D], BF16)); Bwq = [Buf() for _ in range(2)]
            wo = es.enter_context(self.sbuf("xawo", [128, 8, D], BF16)); Bwo = [Buf() for _ in range(2)]
            ones_b = es.enter_context(self.sbuf("xaones", [128, 128], BF16)); Bob = Buf()
            V(lambda: nc.vector.memset(ones_b[:], 1.0), [], [Bob])
            with ExitStack() as e2:
                wts = [(e2.enter_context(self.sbuf("xawkv%d" % i, [128, 8, 512], BF16)), [Buf(), Buf()]) for i in range(2)]
                wkv = self.dram["xa_wkv"][l]
                for g in range(4):
                    wt, Bw = wts[g % 2]
                    self.load_w(wt, Bw, wkv[:, g * 512:(g + 1) * 512].rearrange("(k p) c -> p k c", p=128), self.dbuf["xa_wkv"], 8)
                    if g < 2:
                        for m in range(4):
                            pt, Bp = self.psum()
                            for kc in range(8):
                                P(lambda: nc.tensor.matmul(pt[:, 0:NM], lhsT=wt[:, kc, m * 128:(m + 1) * 128], rhs=self.memn[:, kc, :], start=(kc == 0), stop=(kc == 7)),
                                  [Bw[kc // 4], self.Bmemn], [Bp])
                            A(lambda: nc.scalar.copy(out=kT[:, g * 4 + m, :], in_=pt[:, 0:NM]), [Bp], [BkT])
                    else:
                        for mt in range(NM // 128):
                            pt, Bp = self.psum()
                            for kc in range(8):
                                P(lambda: nc.tensor.matmul(pt[:], lhsT=self.memn[:, kc, mt * 128:(mt + 1) * 128], rhs=wt[:, kc, :], start=(kc == 0), stop=(kc == 7)),
                                  [Bw[kc // 4], self.Bmemn], [Bp])
                            A(lambda: nc.scalar.copy(out=vtm[:, mt, (g - 2) * 512:(g - 1) * 512], in_=pt[:]), [Bp], [Bvtm])
                S.barrier()
            self.load_w(wq, Bwq, self.dram["xa_wq"][l].rearrange("(k p) c -> p k c", p=128), self.dbuf["xa_wq"], 8)
            self.load_w(wo, Bwo, self.dram["xa_wo"][l].rearrange("(k p) c -> p k c", p=128), self.dbuf["xa_wo"], 8)
            xts = [(es.enter_context(self.sbuf("xax%d" % i, [128, 4, D], F32)), [Buf() for _ in range(4)]) for i in range(2)]
            hTs = self.tiles(es, "xah", [128, 8, 512], BF16, 2)
            qTs = self.tiles(es, "xaq", [128, 2, 512], BF16, 2)
            Es = self.tiles(es, "xaE", [128, 2, 512], BF16, 2)
            rd = self.tiles(es, "xard", [128, 512], F32, 2)
            oTs = self.tiles(es, "xao", [128, 8, 512], BF16, 2)
            xss = self.tiles(es, "xaxs", [128, D], BF16, 2)
            junk = es.enter_context(self.sbuf("xajunk", [128, D], BF16)); Bjunk = Buf()
            st = self.tiles(es, "xast", [128, 4], F32, 2)
            cnt = 0
            hc = 0
            for blk in range(T // 512):
                sq = (blk * 512) // SEQ
                tok0 = blk * 512
                xt, Bx = xts[blk % 2]
                hT, BhT = hTs[blk % 2]
                oT, BoT = oTs[blk % 2]
                for j in range(4):
                    S.dma("sp", xt[:, j, :], xs_ap[tok0 + j * 128:tok0 + (j + 1) * 128, :], reads=[Bxs], writes=[Bx[j]])
                for j in range(4):
                    s_, Bs = st[cnt % 2]; xs, Bxs_ = xss[cnt % 2]; cnt += 1
                    A(lambda: nc.scalar.activation(out=junk[:], in_=xt[:, j, :], func=AF.Square, accum_out=s_[:, 0:1]), [Bx[j]], [Bjunk, Bs])
                    A(lambda: nc.scalar.activation(out=s_[:, 1:2], in_=s_[:, 0:1], func=AF.Sqrt, scale=1.0 / D, bias=self.eps6[:, 0:1]), [Bs, Bc], [Bs])
                    V(lambda: nc.vector.reciprocal(out=s_[:, 2:3], in_=s_[:, 1:2]), [Bs], [Bs])
                    V(lambda: nc.vector.tensor_scalar(out=xs[:], in0=xt[:, j, :], scalar1=s_[:, 2:3], scalar2=None, op0=ALU.mult), [Bx[j], Bs], [Bxs_])
                    pt, Bp = self.psum()
                    pb = pt[:].bitcast(BF16)
                    for kc in range(8):
                        P(lambda: nc.tensor.transpose(out=pb[:, kc * 128:(kc + 1) * 128], in_=xs[:, kc * 128:(kc + 1) * 128], identity=self.ident_b[:]), [Bxs_, Bc], [Bp])
                    V(lambda: nc.vector.tensor_tensor(out=hT[:, :, j * 128:(j + 1) * 128], in0=pb.rearrange("p (k t) -> p k t", k=8),
                                                      in1=self.vcol(l, "norm_xattn", 0, 8).unsqueeze(2).to_broadcast([128, 8, 128]), op=ALU.mult), [Bp, self.Bvecs], [BhT])
                for h in range(4):
                    qT, BqT = qTs[hc % 2]; E, BE = Es[hc % 2]; r_, Br = rd[hc % 2]; hc += 1
                    for dt in range(2):
                        pt, Bp = self.psum()
                        c0 = h * 256 + dt * 128
                        for kc in range(8):
                            P(lambda: nc.tensor.matmul(pt[:], lhsT=wq[:, kc, c0:c0 + 128], rhs=hT[:, kc, :], start=(kc == 0), stop=(kc == 7)), [Bwq[kc // 4], BhT], [Bp])
                        A(lambda: nc.scalar.copy(out=qT[:, dt, :], in_=pt[:]), [Bp], [BqT])
                    for mt in range(2):
                        ms = slice(sq * NMEM + mt * 128, sq * NMEM + (mt + 1) * 128)
                        pt, Bp = self.psum()
                        for dt in range(2):
                            P(lambda: nc.tensor.matmul(pt[:], lhsT=kT[:, h * 2 + dt, ms], rhs=qT[:, dt, :], start=(dt == 0), stop=(dt == 1)), [BkT, BqT], [Bp])
                        A(lambda: nc.scalar.activation(out=E[:, mt, :], in_=pt[:], func=AF.Exp, scale=1.0 / 16), [Bp], [BE])
                    pt, Bp = self.psum()
                    for mt in range(2):
                        P(lambda: nc.tensor.matmul(pt[:], lhsT=ones_b[:], rhs=E[:, mt, :], start=(mt == 0), stop=(mt == 1)), [Bob, BE], [Bp])
                    V(lambda: nc.vector.reciprocal(out=r_[:], in_=pt[:]), [Bp], [Br])
                    for dt in range(2):
                        pt, Bp = self.psum()
                        c0 = h * 256 + dt * 128
                        for mt in range(2):
                            P(lambda: nc.tensor.matmul(pt[:], lhsT=vtm[:, sq * 2 + mt, c0:c0 + 128], rhs=E[:, mt, :], start=(mt == 0), stop=(mt == 1)), [Bvtm, BE], [Bp])
                        V(lambda: nc.vector.tensor_tensor(out=oT[:, h * 2 + dt, :], in0=pt[:], in1=r_[:], op=ALU.mult), [Bp, Br], [BoT])
                self.outproj_res(lambda kc, j: oT[:, kc, j * 128:(j + 1) * 128], BoT, 8, wo, lambda kc: Bwo[kc // 4], xt, Bx, 4, xs_ap, Bxs, tok0)
            S.barrier()

    def stage_ffn(self, l):
        nc, S = self.nc, self.S
        V, A, P = self.V, self.A, self.P
        Bc, Bv = self.Bconst, self.Bvecs
        xs_ap, Bxs = self.dram["xres"], self.dbuf["xres"]
        aT, BaT = self.dram["actT"], self.dbuf["actT"]
        L = SEQ
        NP_ = DFF // 128
        with ExitStack() as es:
            hT = es.enter_context(self.sbuf("ffh", [128, 8, T], BF16)); BhT = Buf("ffh")
            with ExitStack() as e2:
                self.rmsnorm_fm(e2, xs_ap, Bxs, T, self.vcol(l, "norm_ffn", 0, 8), hT, BhT, self.eps6[:, 0:1])
                S.barrier()
            with ExitStack() as e2:
                wts = [(e2.enter_context(self.sbuf("ffw%d" % i, [128, 8, 256], BF16)), [Buf(), Buf()]) for i in range(2)]
                ug = self.tiles(e2, "ffug", [128, L + 2], F32, 2)
                uv = self.tiles(e2, "ffuv", [128, L + 2], F32, 2)
                cg = self.tiles(e2, "ffcg", [128, L], F32, 2)
                cv = self.tiles(e2, "ffcv", [128, L], F32, 2)
                sg = self.tiles(e2, "ffsg", [128, L], F32, 2)
                ao = self.tiles(e2, "ffao", [128, L], BF16, 2)
                for t_, B_ in ug + uv:
                    V(lambda: nc.vector.memset(t_[:, 0:2], 0.0), [], [B_])
                wup = self.dram["ffn_w_up"][l]
                cnt = 0
                for i in range(NP_):
                    wt, Bw = wts[i % 2]
                    S.dma("pool", wt[:, :, 0:128], wup[:, 128 * i:128 * i + 128].rearrange("(k p) c -> p k c", p=128), reads=[self.dbuf["ffn_w_up"]], writes=[Bw[0]])
                    S.dma("pool", wt[:, :, 128:256], wup[:, DFF + 128 * i:DFF + 128 * i + 128].rearrange("(k p) c -> p k c", p=128), reads=[self.dbuf["ffn_w_up"]], writes=[Bw[1]])
                    for sq in range(NSEQ):
                        g_, Bg = ug[cnt % 2]; v_, Bvv = uv[cnt % 2]; cg_, Bcg = cg[cnt % 2]; cv_, Bcv = cv[cnt % 2]; s_, Bs = sg[cnt % 2]; a_, Ba = ao[cnt % 2]
                        cnt += 1
                        for tb in range(L // 512):
                            ts = slice(sq * L + tb * 512, sq * L + tb * 512 + 512)
                            for half, (dst, Bd) in enumerate([(g_, Bg), (v_, Bvv)]):
                                pt, Bp = self.psum()
                                for kc in range(8):
                                    P(lambda: nc.tensor.matmul(pt[:], lhsT=wt[:, kc, half * 128:(half + 1) * 128], rhs=hT[:, kc, ts], start=(kc == 0), stop=(kc == 7)),
                                      [Bw[half], BhT], [Bp])
                                A(lambda: nc.scalar.copy(out=dst[:, 2 + tb * 512:2 + tb * 512 + 512], in_=pt[:]), [Bp], [Bd])
                        for half, (src, Bsrc_, dst, Bd) in enumerate([(g_, Bg, cg_, Bcg), (v_, Bvv, cv_, Bcv)]):
                            col = i + half * NP_
                            A(lambda: nc.scalar.activation(out=dst[:], in_=src[:, 2:L + 2], func=AF.Identity, scale=self.vcol(l, "ffn_conv2", col, 1), bias=self.vcol(l, "ffn_conv_b", col, 1)),
                              [Bsrc_, Bv], [Bd])
                            V(lambda: nc.vector.scalar_tensor_tensor(out=dst[:], in0=src[:, 1:L + 1], scalar=self.vcol(l, "ffn_conv1", col, 1), in1=dst[:], op0=ALU.mult, op1=ALU.add),
                              [Bsrc_, Bd, Bv], [Bd])
                            V(lambda: nc.vector.scalar_tensor_tensor(out=dst[:], in0=src[:, 0:L], scalar=self.vcol(l, "ffn_conv0", col, 1), in1=dst[:], op0=ALU.mult, op1=ALU.add),
                              [Bsrc_, Bd, Bv], [Bd])
                        A(lambda: nc.scalar.activation(out=s_[:], in_=cg_[:], func=AF.Sigmoid), [Bcg], [Bs])
                        self.G(lambda: nc.gpsimd.tensor_tensor(out=s_[:], in0=s_[:], in1=cg_[:], op=ALU.mult), [Bs, Bcg], [Bs])
                        V(lambda: nc.vector.tensor_tensor(out=a_[:], in0=s_[:], in1=cv_[:], op=ALU.mult), [Bs, Bcv], [Ba])
                        S.dma("sp", aT[128 * i:128 * i + 128, sq * L:(sq + 1) * L], a_[:], reads=[Ba], writes=[BaT])
                S.barrier()
        with ExitStack() as es:
            wd = es.enter_context(self.sbuf("ffwd", [128, NP_, D], BF16)); Bwd = [Buf() for _ in range((NP_ + 3) // 4)]
            self.load_w(wd, Bwd, self.dram["ffn_w_down"][l].rearrange("(k p) c -> p k c", p=128), self.dbuf["ffn_w_down"], NP_)
            xts = [(es.enter_context(self.sbuf("ffx%d" % i, [128, 4, D], F32)), [Buf() for _ in range(4)]) for i in range(2)]
            ats = self.tiles(es, "ffat", [128, NP_, 512], BF16, 2)
            for blk in range(T // 512):
                tok0 = blk * 512
                xt, Bx = xts[blk % 2]
                at, Bat = ats[blk % 2]
                for j in range(4):
                    S.dma("sp", xt[:, j, :], xs_ap[tok0 + j * 128:tok0 + (j + 1) * 128, :], reads=[Bxs], writes=[Bx[j]])
                S.dma("sp", at[:], aT[:, tok0:tok0 + 512].rearrange("(k p) t -> p k t", p=128), reads=[BaT], writes=[Bat])
                self.outproj_res(lambda kc, j: at[:, kc, j * 128:(j + 1) * 128], Bat, NP_, wd, lambda kc: Bwd[kc // 4], xt, Bx, 4, xs_ap, Bxs, tok0)
            S.barrier()

    def stage_final(self):
        nc, S = self.nc, self.S
        V, A, P = self.V, self.A, self.P
        Bc = self.Bconst
        xs_ap, Bxs = self.dram["xres"], self.dbuf["xres"]
        with ExitStack() as es:
            gbc = es.enter_context(self.sbuf("fngb", [128, D], F32)); Bg = Buf()
            S.dma("sp", gbc[:], self.dram["norm_final_bc"], reads=[self.dbuf["norm_final_bc"]], writes=[Bg])
            xts = self.tiles(es, "fnx", [128, D], F32, 3)
            junk = es.enter_context(self.sbuf("fnjunk", [128, D], BF16)); Bjunk = Buf()
            st = self.tiles(es, "fnst", [128, 4], F32, 2)
            for tt in range(T // 128):
                xt, Bx = xts[tt % 3]
                s_, Bs = st[tt % 2]
                S.dma("sp", xt[:], xs_ap[tt * 128:(tt + 1) * 128, :], reads=[Bxs], writes=[Bx])
                A(lambda: nc.scalar.activation(out=junk[:], in_=xt[:], func=AF.Square, accum_out=s_[:, 0:1]), [Bx], [Bjunk, Bs])
                A(lambda: nc.scalar.activation(out=s_[:, 1:2], in_=s_[:, 0:1], func=AF.Sqrt, scale=1.0 / D, bias=self.eps6[:, 0:1]), [Bs, Bc], [Bs])
                V(lambda: nc.vector.reciprocal(out=s_[:, 2:3], in_=s_[:, 1:2]), [Bs], [Bs])
                V(lambda: nc.vector.scalar_tensor_tensor(out=xt[:], in0=xt[:], scalar=s_[:, 2:3], in1=gbc[:], op0=ALU.mult, op1=ALU.mult), [Bx, Bs, Bg], [Bx])
                S.dma("sp", self.out[tt * 128:(tt + 1) * 128, :], xt[:], reads=[Bx], writes=[self.Bout])
            S.barrier()

    def finish(self):
        S = self.S
        S.barrier()
        print("ops", S.n_ops, "waits", S.n_waits, "sems", S.nsem + S.n_dma_sems)


def build(debug=None, n_layers=DEPTH, final=True):
    k = K(debug, n_layers)
    k.stage_memn()
    for l in range(n_layers):
        k.stage_mixproj(l)
        for sq in range(NSEQ):
            k.stage_rwkv(l, sq)
        k.stage_s5(l)
        for sq in range(NSEQ):
            k.stage_gla(l, sq)
        k.stage_merge(l)
        k.stage_xattn(l)
        k.stage_ffn(l)
    if final:
        k.stage_final()
    k.finish()
    return k


def make_in_maps(inp):
    vecs, gvec = pack_vecs(inp)
    s5w = pack_s5(inp)
    maps = []
    for c in range(NCORES):
        m = {
            "x": np.ascontiguousarray(inp["x"][NSEQ * c:NSEQ * (c + 1)].reshape(T, D)),
            "mem": np.ascontiguousarray(inp["mem"][NSEQ * c:NSEQ * (c + 1)].reshape(NSEQ * NMEM, D)),
            "vecs": vecs, "gvec": gvec,
            "w_in": inp["w_in"],
            "gla_gk_up": inp["gla_gk_up"],
            "s5w": s5w, "s5_w_glu": inp["s5_w_glu"],
            "w_branch": inp["w_branch"], "w_out": inp["w_out"], "xa_wq": inp["xa_wq"], "xa_wkv": inp["xa_wkv"], "xa_wo": inp["xa_wo"],
            "ffn_w_up": inp["ffn_w_up"], "ffn_w_down": inp["ffn_w_down"],
            "norm_final_bc": np.ascontiguousarray(np.broadcast_to(np.asarray(inp["norm_final"], np.float32)[None, :], (128, D))),
            "rw_w_up": inp["rw_w_up"], "rw_a_up": inp["rw_a_up"], "rw_g_up": inp["rw_g_up"],
        }
        maps.append(m)
    return maps


def kernel(**inp):
    inp = {k: np.asarray(v) for k, v in inp.items()}
    k = build()
    res = run_bass_kernel_spmd(k.nc, make_in_maps(inp), core_ids=list(range(NCORES)))
    out = np.concatenate([r["out"].reshape(NSEQ, SEQ, D) for r in res.results], axis=0)
    return out.astype(np.float32)
```

```python
import numpy as np
from contextlib import ExitStack
import concourse.bass as bass
import concourse.mybir as mybir
from concourse.bass_utils import run_bass_kernel_spmd

F32 = mybir.dt.float32
BF16 = mybir.dt.bfloat16
ALU = mybir.AluOpType
AF = mybir.ActivationFunctionType
AX = mybir.AxisListType

NCORES = 8
DEPTH = 4
D = 1024
SEQ = 2048
NSEQ = 2
T = NSEQ * SEQ
NMEM = 256
N_IN = 6928
RW0, S50, GL0, GT0 = 0, 1792, 2304, 3856
DFF = 2816
EPOCH = 30000


class Buf:
    __slots__ = ("name", "lw", "rd")

    def __init__(self, name=""):
        self.name = name
        self.lw = None
        self.rd = {}


class Sched:
    def __init__(self, nc):
        self.nc = nc
        self.E = {"pe": nc.tensor, "dve": nc.vector, "act": nc.scalar, "pool": nc.gpsimd, "sp": nc.sync}
        self.sems = {}
        self.cur = {}
        self.nsem = 0
        for e in self.E:
            self._new_epoch(e)
        self.seen = {e: {} for e in self.E}
        self.dma_pools = {}
        self.dma_rr = {}
        nsd = 0
        for q, n in (("sp", 36), ("pool", 16), ("act", 8)):
            pl = []
            for i in range(n):
                k = "d%s%d" % (q, i)
                self.sems[k] = nc.alloc_semaphore("sd%d" % nsd)
                nsd += 1
                pl.append([k, 0])
            self.dma_pools[q] = pl
            self.dma_rr[q] = 0
        self.n_dma_sems = nsd
        self.n_ops = 0
        self.n_waits = 0

    def _new_epoch(self, e):
        k = "%s_%d" % (e, self.nsem)
        self.nsem += 1
        self.sems[k] = self.nc.alloc_semaphore("s" + k)
        self.cur[e] = [k, 0]

    def _wait(self, eng, deps):
        best = {}
        for d in deps:
            if d is None:
                continue
            k, v, de = d
            if de == eng and eng == "pe":
                continue
            if best.get(k, 0) < v:
                best[k] = v
        for k, v in best.items():
            if self.seen[eng].get(k, 0) >= v:
                continue
            self.E[eng].wait_ge(self.sems[k], v)
            self.seen[eng][k] = v
            self.n_waits += 1

    @staticmethod
    def _deps(reads, writes):
        deps = []
        for b in reads:
            deps.append(b.lw)
        for b in writes:
            deps.append(b.lw)
            deps.extend(b.rd.values())
        return deps

    def op(self, eng, fn, reads=(), writes=()):
        self._wait(eng, self._deps(reads, writes))
        ins = fn()
        c = self.cur[eng]
        c[1] += 1
        ins.then_inc(self.sems[c[0]], 1)
        t = (c[0], c[1], eng)
        for b in reads:
            b.rd[eng] = t
        for b in writes:
            b.lw = t
            b.rd = {}
        if c[1] >= EPOCH:
            self._new_epoch(eng)
        self.n_ops += 1
        return t

    def dma(self, q, out, in_, reads=(), writes=(), **kw):
        pl = self.dma_pools[q]
        slot = self.dma_rr[q]
        self.dma_rr[q] = (slot + 1) % len(pl)
        s = pl[slot]
        deps = self._deps(reads, writes)
        if s[1] > 0:
            deps.append((s[0], s[1], "dma"))
        self._wait(q, deps)
        s[1] += 16
        self.E[q].dma_start(out=out, in_=in_, **kw).then_inc(self.sems[s[0]], 16)
        t = (s[0], s[1], "dma")
        for b in reads:
            b.rd["dma_%s%d" % (q, slot)] = t
        for b in writes:
            b.lw = t
            b.rd = {}
        self.n_ops += 1
        return t

    def barrier(self):
        deps = []
        for e, c in self.cur.items():
            if c[1] > 0:
                deps.append((c[0], c[1], "x"))
        for pl in self.dma_pools.values():
            for k, v in pl:
                if v > 0:
                    deps.append((k, v, "x"))
        for e in self.E:
            self._wait(e, deps)


VEC_SPECS = [
    ("norm_mix", 1024), ("norm_xattn", 1024), ("norm_ffn", 1024),
    ("rw_mu", 1792), ("rw_w0", 512), ("rw_a0", 512), ("rw_k_k", 512), ("rw_k_a", 512), ("rw_r_k", 512),
    ("rw_ln_w", 512), ("rw_ln_b", 512),
    ("s5_b_glu", 512), ("s5_d", 512),
    ("gla_gk_b", 256), ("gla_norm", 128),
    ("ffn_conv0", 5632), ("ffn_conv1", 5632), ("ffn_conv2", 5632), ("ffn_conv_b", 5632),
]
VEC_OFF = {}
_o = 0
for _n, _l in VEC_SPECS:
    VEC_OFF[_n] = _o
    _o += _l // 128
NVEC = _o
GVEC_OFF = {"norm_mem": 0, "norm_final": 8}
NGVEC = 24


def pack_vecs(inp):
    v = np.zeros((DEPTH, 128, NVEC), np.float32)
    for l in range(DEPTH):
        for n, ln in VEC_SPECS:
            if n.startswith("ffn_conv") and n != "ffn_conv_b":
                a = inp["ffn_conv"][l, int(n[-1])]
            else:
                a = inp[n][l]
            a = np.asarray(a, np.float32).reshape(-1)
            v[l, :, VEC_OFF[n]:VEC_OFF[n] + ln // 128] = a.reshape(ln // 128, 128).T
    g = np.zeros((128, NGVEC), np.float32)
    g[:, 0:8] = np.asarray(inp["norm_mem"], np.float32).reshape(8, 128).T
    g[:, 8:16] = np.asarray(inp["norm_final"], np.float32).reshape(8, 128).T
    pp = np.arange(128)
    g[:, 16] = ((pp // 16) % 2 == 0)
    g[:, 17] = ((pp // 16) % 2 == 1)
    g[:, 18] = np.pi / 2
    g[:, 19] = (pp >= 96)
    return np.ascontiguousarray(v.transpose(1, 0, 2)), g


S5X = 3124
oAC, oBC, oLC, oAP, oLP, oBP, oCP = 0, 512, 1024, 1028, 1060, 1076, 2100


def pack_s5(inp):
    out = np.zeros((128, DEPTH, S5X), np.float32)
    for l in range(DEPTH):
        a = np.stack([inp["s5_a_re"][l], inp["s5_a_im"][l]]).astype(np.float32)
        b = np.stack([inp["s5_b_re"][l], inp["s5_b_im"][l]]).astype(np.float32)
        c = np.stack([inp["s5_c_re"][l], inp["s5_c_im"][l]]).astype(np.float32)
        ls = np.asarray(inp["s5_log_step"][l], np.float32)
        aC = np.broadcast_to(a.reshape(2, 4, 8, 64).transpose(2, 1, 0, 3)[:, None], (8, 16, 4, 2, 64)).reshape(128, 512)
        bC = b.reshape(2, 4, 8, 64, 16).transpose(2, 4, 1, 0, 3).reshape(128, 512)
        lC = np.broadcast_to(ls.reshape(4, 8).T[:, None, :], (8, 16, 4)).reshape(128, 4)
        aP = a.reshape(2, 16, 2, 64).transpose(2, 3, 1, 0).reshape(128, 32)
        lP = np.broadcast_to(ls.reshape(16, 2).T[:, None, :], (2, 64, 16)).reshape(128, 16)
        bP = np.zeros((2, 64, 16, 2, 2, 16), np.float32)
        cP = np.zeros((2, 64, 16, 2, 2, 16), np.float32)
        b5 = b.reshape(2, 16, 2, 64, 16)
        c5 = c.reshape(2, 16, 2, 16, 64)
        for bb in range(2):
            bP[bb, :, :, :, bb, :] = b5[:, :, bb].transpose(2, 1, 0, 3)
            cP[bb, :, :, :, bb, :] = c5[:, :, bb].transpose(3, 1, 0, 2)
        out[:, l, oAC:oAC + 512] = aC
        out[:, l, oBC:oBC + 512] = bC
        out[:, l, oLC:oLC + 4] = lC
        out[:, l, oAP:oAP + 32] = aP
        out[:, l, oLP:oLP + 16] = lP
        out[:, l, oBP:oBP + 1024] = bP.reshape(128, 1024)
        out[:, l, oCP:oCP + 1024] = cP.reshape(128, 1024)
    return out


class K:
    def __init__(self, debug=None, n_layers=DEPTH):
        self.debug = debug or {}
        self.n_layers = n_layers
        nc = self.nc = bass.Bass("TRN2", target_bir_lowering=False)
        self.S = Sched(nc)
        self.uid = 0
        self.dram = {}
        self.dbuf = {}
        di = self.dram_in
        di("x", [T, D]); di("mem", [NSEQ * NMEM, D]); di("vecs", [128, DEPTH, NVEC]); di("gvec", [128, NGVEC])
        di("w_in", [DEPTH, D, N_IN])
        di("gla_gk_up", [DEPTH, 16, 256])
        di("s5w", [128, DEPTH, S5X]); di("s5_w_glu", [DEPTH, 512, 512])
        di("w_branch", [DEPTH, 3, 512, D]); di("w_out", [DEPTH, D, D])
        di("xa_wq", [DEPTH, D, D]); di("xa_wkv", [DEPTH, D, 2 * D]); di("xa_wo", [DEPTH, D, D])
        di("ffn_w_up", [DEPTH, D, 2 * DFF]); di("ffn_w_down", [DEPTH, DFF, D]); di("norm_final_bc", [128, D])
        di("rw_w_up", [DEPTH, 64, 512]); di("rw_a_up", [DEPTH, 64, 512]); di("rw_g_up", [DEPTH, 128, 512])
        self.out = nc.dram_tensor("out", [T, D], F32, kind="ExternalOutput").ap()
        self.Bout = Buf("out")
        self.scr("xres", [T, D], F32)
        self.scr("pT", [N_IN, T], BF16)
        self.scr("ymix", [3, 512, T], BF16)
        self.scr("actT", [DFF, T], BF16)
        self.ps = []
        for i in range(8):
            t = nc.alloc_psum_tensor("ps%d" % i, [128, 512], F32)
            self.ps.append((t, Buf("ps%d" % i)))
        self.ps_rr = 0
        self.ev_rr = 0
        self.consts()

    def dram_in(self, name, shape, dt=F32):
        self.dram[name] = self.nc.dram_tensor(name, shape, dt, kind="ExternalInput").ap()
        self.dbuf[name] = Buf(name)

    def scr(self, name, shape, dt):
        kind = "ExternalOutput" if name in self.debug else "Internal"
        self.dram[name] = self.nc.dram_tensor(name, shape, dt, kind=kind).ap()
        self.dbuf[name] = Buf(name)

    def psum(self):
        t, b = self.ps[self.ps_rr]
        self.ps_rr = (self.ps_rr + 1) % 8
        return t, b

    def ev_eng(self):
        self.ev_rr ^= 1
        return "act" if self.ev_rr else "dve"

    def consts(self):
        nc, S = self.nc, self.S
        self.vecs = nc.alloc_sbuf_tensor("vecs_sb", [128, DEPTH, NVEC], F32)
        self.Bvecs = Buf("vecs")
        if "delay" in self.debug:
            dj = nc.alloc_sbuf_tensor("dly", [128, 512], F32)
            Bd = Buf()
            for i in range(60):
                S.op("pool", lambda: nc.gpsimd.memset(dj[:], 0.0), writes=[Bd])
            S.barrier()
        S.dma("sp", self.vecs[:], self.dram["vecs"], writes=[self.Bvecs])
        self.gvec = nc.alloc_sbuf_tensor("gvec_sb", [128, NGVEC], F32)
        S.dma("sp", self.gvec[:], self.dram["gvec"], writes=[self.Bvecs])
        self.ident_f = nc.alloc_sbuf_tensor("ident_f", [128, 128], F32)
        self.ident_b = nc.alloc_sbuf_tensor("ident_b", [128, 128], BF16)
        self.Bconst = Buf("const")
        self.eps6 = nc.alloc_sbuf_tensor("eps6", [128, 1], F32)
        S.op("pool", lambda: nc.gpsimd.memset(self.ident_f[:], 0.0), writes=[self.Bconst])
        S.op("pool", lambda: nc.gpsimd.affine_select(out=self.ident_f[:], in_=self.ident_f[:], pattern=[[-1, 128]],
                                                     compare_op=ALU.not_equal, fill=1.0, base=0, channel_multiplier=1),
             reads=[self.Bconst], writes=[self.Bconst])
        S.op("dve", lambda: nc.vector.tensor_copy(out=self.ident_b[:], in_=self.ident_f[:]), reads=[self.Bconst], writes=[self.Bconst])
        S.op("dve", lambda: nc.vector.memset(self.eps6[:], 1e-6), writes=[self.Bconst])
        self.cst = nc.alloc_sbuf_tensor("cst", [128, 8], F32)
        for i, v in enumerate([1.0, 1e-5, 1e-12, 64e-5, 0.0]):
            S.op("dve", lambda: nc.vector.memset(self.cst[:, i:i + 1], v), writes=[self.Bconst])
        self.ones_f = nc.alloc_sbuf_tensor("ones_f", [128, 128], F32)
        S.op("dve", lambda: nc.vector.memset(self.ones_f[:], 1.0), writes=[self.Bconst])
        self.ones_bd = nc.alloc_sbuf_tensor("ones_bd", [128, 128], F32)
        S.op("dve", lambda: nc.vector.memset(self.ones_bd[:], 0.0), writes=[self.Bconst])
        S.op("dve", lambda: nc.vector.memset(self.ones_bd[0:64, 0:64], 1.0), writes=[self.Bconst])
        S.op("dve", lambda: nc.vector.memset(self.ones_bd[64:128, 64:128], 1.0), writes=[self.Bconst])
        self.mbd = nc.alloc_sbuf_tensor("mbd", [128, 3, 128], F32)
        S.op("pool", lambda: nc.gpsimd.memset(self.mbd[:], 1.0), writes=[self.Bconst])
        for i, (cm, st, op) in enumerate([(1, -1, ALU.is_gt), (-1, 1, ALU.is_gt), (-1, 1, ALU.is_ge)]):
            S.op("pool", lambda: nc.gpsimd.affine_select(out=self.mbd[:, i, :], in_=self.mbd[:, i, :], pattern=[[st, 128]],
                                                         compare_op=op, fill=0.0, base=0, channel_multiplier=cm),
                 reads=[self.Bconst], writes=[self.Bconst])
        S.op("pool", lambda: nc.gpsimd.memset(self.mbd[0:64, :, 64:128], 0.0), reads=[self.Bconst], writes=[self.Bconst])
        S.op("pool", lambda: nc.gpsimd.memset(self.mbd[64:128, :, 0:64], 0.0), reads=[self.Bconst], writes=[self.Bconst])
        self.mask128 = nc.alloc_sbuf_tensor("mask128", [128, 128], F32)
        S.op("pool", lambda: nc.gpsimd.memset(self.mask128[:], 1.0), writes=[self.Bconst])
        S.op("pool", lambda: nc.gpsimd.affine_select(out=self.mask128[:], in_=self.mask128[:], pattern=[[1, 128]],
                                                     compare_op=ALU.is_ge, fill=0.0, base=0, channel_multiplier=-1),
             reads=[self.Bconst], writes=[self.Bconst])

    def sbuf(self, name, shape, dt):
        self.uid += 1
        return self.nc.sbuf_tensor("%s_u%d" % (name, self.uid), shape, dt)

    def tiles(self, es, name, shape, dt, n):
        return [(es.enter_context(self.sbuf("%s%d" % (name, i), shape, dt)), Buf(name)) for i in range(n)]

    def V(self, fn, r=(), w=()):
        return self.S.op("dve", fn, r, w)

    def A(self, fn, r=(), w=()):
        return self.S.op("act", fn, r, w)

    def P(self, fn, r=(), w=()):
        return self.S.op("pe", fn, r, w)

    def G(self, fn, r=(), w=()):
        return self.S.op("pool", fn, r, w)

    def vcol(self, l, name, j=0, n=1):
        o = VEC_OFF[name] + j
        return self.vecs[:, l, o:o + n]

    def rmsnorm_fm(self, es, src_ap, Bsrc, ntok, gain_ap, hT, BhT, eps_ap):
        nc, S = self.nc, self.S
        TB = 4
        xts = [(es.enter_context(self.sbuf("nx%d" % i, [128, TB, D], F32)), [Buf() for _ in range(TB)]) for i in range(2)]
        xss = [(es.enter_context(self.sbuf("nxs%d" % i, [128, D], BF16)), Buf()) for i in range(4)]
        junk = es.enter_context(self.sbuf("njunk", [128, D], BF16)); Bjunk = Buf()
        st = [(es.enter_context(self.sbuf("nst%d" % i, [128, 4], F32)), Buf()) for i in range(4)]
        nblk = ntok // (128 * TB)
        cnt = 0
        for blk in range(nblk):
            xt, Bxs_ = xts[blk % 2]
            for j in range(TB):
                r0 = (blk * TB + j) * 128
                S.dma("sp", xt[:, j, :], src_ap[r0:r0 + 128, :], reads=[Bsrc], writes=[Bxs_[j]])
            for j in range(TB):
                Bx = Bxs_[j]
                s_, Bs = st[cnt % 4]
                xs, Bxs = xss[cnt % 4]
                cnt += 1
                S.op("act", lambda: nc.scalar.activation(out=junk[:], in_=xt[:, j, :], func=AF.Square, accum_out=s_[:, 0:1]),
                     reads=[Bx], writes=[Bjunk, Bs])
                S.op("act", lambda: nc.scalar.activation(out=s_[:, 1:2], in_=s_[:, 0:1], func=AF.Sqrt, scale=1.0 / D, bias=eps_ap),
                     reads=[Bs, self.Bconst], writes=[Bs])
                S.op("dve", lambda: nc.vector.reciprocal(out=s_[:, 2:3], in_=s_[:, 1:2]), reads=[Bs], writes=[Bs])
                S.op("dve", lambda: nc.vector.tensor_scalar(out=xs[:], in0=xt[:, j, :], scalar1=s_[:, 2:3], scalar2=None, op0=ALU.mult),
                     reads=[Bx, Bs], writes=[Bxs])
                pt, Bp = self.psum()
                pb = pt[:].bitcast(BF16)
                for kc in range(8):
                    S.op("pe", lambda: nc.tensor.transpose(out=pb[:, kc * 128:(kc + 1) * 128], in_=xs[:, kc * 128:(kc + 1) * 128], identity=self.ident_b[:]),
                         reads=[Bxs, self.Bconst], writes=[Bp])
                t0 = (blk * TB + j) * 128
                S.op("dve", lambda: nc.vector.tensor_tensor(out=hT[:, :, t0:t0 + 128], in0=pb.rearrange("p (k t) -> p k t", k=8),
                                                            in1=gain_ap.unsqueeze(2).to_broadcast([128, 8, 128]), op=ALU.mult),
                     reads=[Bp, self.Bvecs], writes=[BhT])

    def proj_fm(self, es, w_ap, Bw, segs, hT, BhT, ntok, dst_ap, Bdst, kc_n=8):
        nc, S = self.nc, self.S
        wts = [(es.enter_context(self.sbuf("pw%d" % i, [128, kc_n, 512], BF16)), [Buf() for _ in range((kc_n + 3) // 4)]) for i in range(2)]
        ots = [(es.enter_context(self.sbuf("po%d" % i, [128, 512], BF16)), Buf()) for i in range(4)]
        wi = 0
        oi = 0
        for (c0, ncol, func, r0) in segs:
            for g0 in range(0, ncol, 512):
                gn = min(512, ncol - g0)
                wt, Bwt = wts[wi % 2]; wi += 1
                src = w_ap[:, c0 + g0:c0 + g0 + gn].rearrange("(k p) c -> p k c", p=128)
                Bwt_l = Bwt
                for k0 in range(0, kc_n, 4):
                    S.dma("pool", wt[:, k0:k0 + 4, 0:gn], src[:, k0:k0 + 4, :], reads=[Bw], writes=[Bwt_l[k0 // 4]])
                for m0 in range(0, gn, 128):
                    mn = min(128, gn - m0)
                    for tb in range(ntok // 512):
                        pt, Bp = self.psum()
                        for kc in range(kc_n):
                            S.op("pe", lambda: nc.tensor.matmul(pt[0:mn, :], lhsT=wt[:, kc, m0:m0 + mn], rhs=hT[:, kc, tb * 512:(tb + 1) * 512],
                                                                start=(kc == 0), stop=(kc == kc_n - 1)),
                                 reads=[Bwt[kc // 4], BhT], writes=[Bp])
                        ot, Bo = ots[oi % 4]; oi += 1
                        if func is not None:
                            S.op("act", lambda: nc.scalar.activation(out=ot[0:mn, :], in_=pt[0:mn, :], func=func), reads=[Bp], writes=[Bo])
                        else:
                            e = self.ev_eng()
                            if e == "act":
                                S.op("act", lambda: nc.scalar.copy(out=ot[0:mn, :], in_=pt[0:mn, :]), reads=[Bp], writes=[Bo])
                            else:
                                S.op("dve", lambda: nc.vector.tensor_copy(out=ot[0:mn, :], in_=pt[0:mn, :]), reads=[Bp], writes=[Bo])
                        rr = r0 + g0 + m0
                        S.dma("sp", dst_ap[rr:rr + mn, tb * 512:(tb + 1) * 512], ot[0:mn, :], reads=[Bo], writes=[Bdst])

    def stage_mixproj(self, l):
        nc, S = self.nc, self.S
        with ExitStack() as es:
            hT = es.enter_context(self.sbuf("hT", [128, 8, T], BF16)); BhT = Buf("hT")
            xsrc, Bx = (self.dram["x"], self.dbuf["x"]) if l == 0 else (self.dram["xres"], self.dbuf["xres"])
            with ExitStack() as es2:
                self.rmsnorm_fm(es2, xsrc, Bx, T, self.vcol(l, "norm_mix", 0, 8), hT, BhT, self.eps6[:, 0:1])
                S.barrier()
            if "hT" in self.debug:
                self.dump("hT_dbg", hT[:], [BhT])
            segs = [(RW0, 1792, None, RW0), (S50, 512, None, S50), (GL0, 1552, None, GL0), (GT0, 3072, AF.Sigmoid, GT0)]
            if "segs" in self.debug:
                segs = self.debug["segs"]
            with ExitStack() as es2:
                self.proj_fm(es2, self.dram["w_in"][l], self.dbuf["w_in"], segs, hT, BhT, T, self.dram["pT"], self.dbuf["pT"])
                S.barrier()
        S.barrier()

    def dump(self, name, ap, bufs, dt=None):
        d = self.nc.dram_tensor(name, list(ap.shape), dt or ap.dtype, kind="ExternalOutput").ap()
        self.S.dma("sp", d, ap, reads=bufs, writes=[Buf()])

    def stage_gla(self, l, sq):
        nc, S = self.nc, self.S
        V, A, P = self.V, self.A, self.P
        pT, BpT = self.dram["pT"], self.dbuf["pT"]
        ym, Bym = self.dram["ymix"], self.dbuf["ymix"]
        t0 = sq * SEQ
        L = SEQ
        NCH = L // 128
        qr, kr, vr, gr, gor = GL0, GL0 + 256, GL0 + 512, GL0 + 1024, GL0 + 1040
        Bc = self.Bconst
        with ExitStack() as es:
            gkup = es.enter_context(self.sbuf("gkup", [16, 256], BF16)); Bgk = Buf()
            S.dma("pool", gkup[:], self.dram["gla_gk_up"][l], reads=[self.dbuf["gla_gk_up"]], writes=[Bgk])
            gkd = es.enter_context(self.sbuf("gkd", [16, L], BF16)); Bgkd = Buf()
            S.dma("sp", gkd[:], pT[gr:gr + 16, t0:t0 + L], reads=[BpT], writes=[Bgkd])
            negb = es.enter_context(self.sbuf("negb", [128, 2], F32)); Bnb = Buf()
            V(lambda: nc.vector.tensor_scalar(out=negb[:], in0=self.vcol(l, "gla_gk_b", 0, 2), scalar1=-1.0, scalar2=None, op0=ALU.mult),
              [self.Bvecs], [Bnb])
            qk = self.tiles(es, "gqk", [128, L], BF16, 2)
            qd = es.enter_context(self.sbuf("gqd", [128, L], BF16)); Bqd = Buf()
            ki = es.enter_context(self.sbuf("gki", [128, L], BF16)); Bki = Buf()
            csz = es.enter_context(self.sbuf("gcsz", [128, L + 1], F32)); Bcs = Buf()
            lt = es.enter_context(self.sbuf("glt", [128, L], F32)); Blt = Buf()
            csl = es.enter_context(self.sbuf("gcsl", [128, L], F32)); Bcsl = Buf()
            Eq = es.enter_context(self.sbuf("gEq", [128, L], F32)); BEq = Buf()
            Ek = es.enter_context(self.sbuf("gEk", [128, L], F32)); BEk = Buf()
            vts = self.tiles(es, "gv", [128, L], BF16, 2)
            ofm = self.tiles(es, "gofm", [128, L], F32, 2)
            S32 = self.tiles(es, "gS32", [128, 128], F32, 2)
            S16 = self.tiles(es, "gS16", [128, 128], BF16, 2)
            vtm = self.tiles(es, "gvtm", [128, 128], BF16, 4)
            ktm = self.tiles(es, "gktm", [128, 128], BF16, 2)
            att = self.tiles(es, "gatt", [128, 128], BF16, 4)
            gos = self.tiles(es, "ggo", [128, 512], BF16, 2)
            tmpa = self.tiles(es, "gta", [128, 512], F32, 2)
            tmpb = self.tiles(es, "gtb", [128, 512], F32, 2)
            tmpc = self.tiles(es, "gtc", [128, 512], F32, 2)
            yo = self.tiles(es, "gyo", [128, 512], BF16, 2)
            V(lambda: nc.vector.memset(csz[:, 0:1], 0.0), [], [Bcs])
            for j in range(2):
                qt, Bq = qk[0]; kt, Bk = qk[1]
                S.dma("sp", qt[:], pT[qr + 128 * j:qr + 128 * j + 128, t0:t0 + L], reads=[BpT], writes=[Bq])
                S.dma("sp", kt[:], pT[kr + 128 * j:kr + 128 * j + 128, t0:t0 + L], reads=[BpT], writes=[Bk])
                for tb in range(L // 512):
                    pt, Bp = self.psum()
                    P(lambda: nc.tensor.matmul(pt[:], lhsT=gkup[0:16, 128 * j:128 * j + 128], rhs=gkd[0:16, tb * 512:(tb + 1) * 512], start=True, stop=True),
                      [Bgk, Bgkd], [Bp])
                    A(lambda: nc.scalar.activation(out=lt[:, tb * 512:(tb + 1) * 512], in_=pt[:], func=AF.Exp, scale=-1.0, bias=negb[:, j:j + 1]),
                      [Bp, Bnb], [Blt])
                A(lambda: nc.scalar.activation(out=lt[:], in_=lt[:], func=AF.Ln, scale=1.0, bias=self.cst[:, 0:1]), [Blt, Bc], [Blt])
                V(lambda: nc.vector.tensor_tensor_scan(out=csz[:, 1:L + 1], data0=self.cst[:, 0:1].to_broadcast([128, L]), data1=lt[:],
                                                       initial=0.0, op0=ALU.mult, op1=ALU.add), [Blt, Bc], [Bcs])
                V(lambda: nc.vector.tensor_tensor(out=csl[:].rearrange("p (c t) -> p c t", t=128),
                                                  in0=csz[:, 1:L + 1].rearrange("p (c t) -> p c t", t=128),
                                                  in1=csz[:, 0:L].rearrange("p (c t) -> p c t", t=128)[:, :, 0:1].to_broadcast([128, NCH, 128]),
                                                  op=ALU.subtract), [Bcs], [Bcsl])
                A(lambda: nc.scalar.activation(out=Eq[:], in_=csl[:], func=AF.Exp, scale=-1.0 / 16), [Bcsl], [BEq])
                A(lambda: nc.scalar.activation(out=Ek[:], in_=csl[:], func=AF.Exp, scale=1.0 / 16), [Bcsl], [BEk])
                V(lambda: nc.vector.scalar_tensor_tensor(out=qd[:], in0=qt[:], scalar=0.125, in1=Eq[:], op0=ALU.mult, op1=ALU.mult), [Bq, BEq], [Bqd])
                V(lambda: nc.vector.tensor_tensor(out=ki[:], in0=kt[:], in1=Ek[:], op=ALU.mult), [Bk, BEk], [Bki])
                Eq3 = Eq[:].rearrange("p (c t) -> p c t", t=128)
                heads = [2 * j, 2 * j + 1]
                for hh in range(2):
                    h = heads[hh]
                    vt, Bv = vts[hh]
                    S.dma("sp", vt[:], pT[vr + 128 * h:vr + 128 * h + 128, t0:t0 + L], reads=[BpT], writes=[Bv])
                    V(lambda: nc.vector.memset(S32[hh][0][:], 0.0), [], [S32[hh][1]])
                    V(lambda: nc.vector.memset(S16[hh][0][:], 0.0), [], [S16[hh][1]])
                cnt = 0
                for ci in range(NCH):
                    ts = slice(ci * 128, ci * 128 + 128)
                    ktmt, Bktm = ktm[ci % 2]
                    pt, Bp = self.psum()
                    pb = pt[:].bitcast(BF16)
                    P(lambda: nc.tensor.transpose(out=pb[:, 0:128], in_=ki[:, ts], identity=self.ident_b[:]), [Bki, Bc], [Bp])
                    A(lambda: nc.scalar.copy(out=ktmt[:], in_=pb[:, 0:128]), [Bp], [Bktm])
                    for hh in range(2):
                        h = heads[hh]
                        hp = slice(64 * hh, 64 * hh + 64)
                        vt, Bv = vts[hh]
                        vtmt, Bvtm = vtm[cnt % 4]
                        attt, Batt = att[cnt % 4]
                        cnt += 1
                        pt, Bp = self.psum()
                        pb = pt[:].bitcast(BF16)
                        P(lambda: nc.tensor.transpose(out=pb[:, 0:128], in_=vt[:, ts], identity=self.ident_b[:]), [Bv, Bc], [Bp])
                        A(lambda: nc.scalar.copy(out=vtmt[:], in_=pb[:, 0:128]), [Bp], [Bvtm])
                        pt2, Bp2 = self.psum()
                        P(lambda: nc.tensor.matmul(pt2[:, 0:128], lhsT=ki[hp, ts], rhs=qd[hp, ts], start=True, stop=True), [Bki, Bqd], [Bp2])
                        V(lambda: nc.vector.tensor_tensor(out=attt[:], in0=pt2[:, 0:128], in1=self.mask128[:], op=ALU.mult), [Bp2, Bc], [Batt])
                        pt3, Bp3 = self.psum()
                        P(lambda: nc.tensor.matmul(pt3[:, 0:128], lhsT=vtmt[:], rhs=attt[:], start=True, stop=False), [Bvtm, Batt], [Bp3])
                        P(lambda: nc.tensor.matmul(pt3[:, 0:128], lhsT=S16[hh][0][:], rhs=qd[:, ts], start=False, stop=True), [S16[hh][1], Bqd], [Bp3])
                        A(lambda: nc.scalar.copy(out=ofm[hh][0][:, ts], in_=pt3[:, 0:128]), [Bp3], [ofm[hh][1]])
                        if ci < NCH - 1:
                            pt4, Bp4 = self.psum()
                            P(lambda: nc.tensor.matmul(pt4[:, 0:128], lhsT=ktmt[:], rhs=vtmt[:], start=True, stop=True), [Bktm, Bvtm], [Bp4])
                            s32, Bs32 = S32[hh]
                            V(lambda: nc.vector.tensor_tensor(out=s32[hp, :], in0=s32[hp, :], in1=pt4[hp, 0:128], op=ALU.add), [Bs32, Bp4], [Bs32])
                            V(lambda: nc.vector.tensor_scalar(out=s32[hp, :], in0=s32[hp, :], scalar1=Eq3[hp, ci, 127:128], scalar2=None, op0=ALU.mult),
                              [Bs32, BEq], [Bs32])
                            V(lambda: nc.vector.tensor_copy(out=S16[hh][0][hp, :], in_=s32[hp, :]), [Bs32], [S16[hh][1]])
                for hh in range(2):
                    h = heads[hh]
                    o, Bo = ofm[hh]
                    for tb in range(L // 512):
                        cs = slice(tb * 512, tb * 512 + 512)
                        got, Bgo = gos[tb % 2]
                        S.dma("sp", got[:], pT[gor + 128 * h:gor + 128 * h + 128, t0 + tb * 512:t0 + tb * 512 + 512], reads=[BpT], writes=[Bgo])
                        ta, Bta = tmpa[tb % 2]; tb_, Btb = tmpb[tb % 2]; tc, Btc = tmpc[tb % 2]
                        A(lambda: nc.scalar.activation(out=ta[:], in_=o[:, cs], func=AF.Square), [Bo], [Bta])
                        pt, Bp = self.psum()
                        P(lambda: nc.tensor.matmul(pt[:], lhsT=self.ones_f[:], rhs=ta[:], start=True, stop=True), [Bc, Bta], [Bp])
                        A(lambda: nc.scalar.activation(out=tb_[:], in_=pt[:], func=AF.Sqrt, scale=1.0 / 128, bias=self.cst[:, 1:2]), [Bp, Bc], [Btb])
                        V(lambda: nc.vector.reciprocal(out=tb_[:], in_=tb_[:]), [Btb], [Btb])
                        V(lambda: nc.vector.scalar_tensor_tensor(out=tb_[:], in0=o[:, cs], scalar=self.vcol(l, "gla_norm", 0, 1), in1=tb_[:],
                                                                 op0=ALU.mult, op1=ALU.mult), [Bo, Btb, self.Bvecs], [Btb])
                        A(lambda: nc.scalar.activation(out=tc[:], in_=got[:], func=AF.Sigmoid), [Bgo], [Btc])
                        V(lambda: nc.vector.tensor_tensor(out=tc[:], in0=tc[:], in1=got[:], op=ALU.mult), [Btc, Bgo], [Btc])
                        yt, By = yo[tb % 2]
                        V(lambda: nc.vector.tensor_tensor(out=yt[:], in0=tb_[:], in1=tc[:], op=ALU.mult), [Btb, Btc], [By])
                        S.dma("sp", ym[2, 128 * h:128 * h + 128, t0 + tb * 512:t0 + tb * 512 + 512], yt[:], reads=[By], writes=[Bym])
            S.barrier()

    def s5_lambda(self, es, tag, are, aim, ls_bc, shp):
        nc = self.nc
        V, A = self.V, self.A
        F = int(np.prod(shp[1:]))
        def new(n):
            return es.enter_context(self.sbuf(tag + n, [128, F], F32))
        def vw(t):
            return t[:] if len(shp) == 2 else (t[:].rearrange("p (a b) -> p a b", b=shp[2]) if len(shp) == 3 else t[:])
        B = Buf()
        Bin = self.Bs5w
        dt, mag, ang, c, sn, t1, t2, lre, lim, fre, fim = [new(n) for n in ["dt", "mag", "ang", "c", "s", "t1", "t2", "lre", "lim", "fre", "fim"]]
        A(lambda: nc.scalar.activation(out=vw(dt), in_=ls_bc, func=AF.Exp), [Bin], [B])
        V(lambda: nc.vector.tensor_tensor(out=vw(mag), in0=vw(dt), in1=are, op=ALU.mult), [B, Bin], [B])
        A(lambda: nc.scalar.activation(out=mag[:], in_=mag[:], func=AF.Exp), [B], [B])
        V(lambda: nc.vector.tensor_tensor(out=vw(ang), in0=vw(dt), in1=aim, op=ALU.mult), [B, Bin], [B])
        A(lambda: nc.scalar.activation(out=sn[:], in_=ang[:], func=AF.Sin, scale=1.0 / 16), [B], [B])
        A(lambda: nc.scalar.activation(out=c[:], in_=ang[:], func=AF.Sin, scale=-1.0 / 16, bias=self.gvec[:, 18:19]), [B, self.Bvecs], [B])
        for _ in range(4):
            V(lambda: nc.vector.tensor_tensor(out=t1[:], in0=c[:], in1=sn[:], op=ALU.mult), [B], [B])
            V(lambda: nc.vector.tensor_tensor(out=c[:], in0=c[:], in1=c[:], op=ALU.mult), [B], [B])
            V(lambda: nc.vector.tensor_tensor(out=t2[:], in0=sn[:], in1=sn[:], op=ALU.mult), [B], [B])
            V(lambda: nc.vector.tensor_tensor(out=c[:], in0=c[:], in1=t2[:], op=ALU.subtract), [B], [B])
            V(lambda: nc.vector.tensor_scalar(out=sn[:], in0=t1[:], scalar1=2.0, scalar2=None, op0=ALU.mult), [B], [B])
        V(lambda: nc.vector.tensor_tensor(out=lre[:], in0=mag[:], in1=c[:], op=ALU.mult), [B], [B])
        V(lambda: nc.vector.tensor_tensor(out=lim[:], in0=mag[:], in1=sn[:], op=ALU.mult), [B], [B])
        V(lambda: nc.vector.tensor_tensor(out=vw(t1), in0=are, in1=are, op=ALU.mult), [Bin, B], [B])
        V(lambda: nc.vector.tensor_tensor(out=vw(t2), in0=aim, in1=aim, op=ALU.mult), [Bin, B], [B])
        V(lambda: nc.vector.tensor_tensor(out=t1[:], in0=t1[:], in1=t2[:], op=ALU.add), [B], [B])
        V(lambda: nc.vector.reciprocal(out=t1[:], in_=t1[:]), [B], [B])
        V(lambda: nc.vector.tensor_scalar(out=c[:], in0=lre[:], scalar1=-1.0, scalar2=None, op0=ALU.add), [B], [B])
        V(lambda: nc.vector.tensor_tensor(out=vw(fre), in0=vw(c), in1=are, op=ALU.mult), [B, Bin], [B])
        V(lambda: nc.vector.tensor_tensor(out=vw(t2), in0=vw(lim), in1=aim, op=ALU.mult), [B, Bin], [B])
        V(lambda: nc.vector.tensor_tensor(out=fre[:], in0=fre[:], in1=t2[:], op=ALU.add), [B], [B])
        V(lambda: nc.vector.tensor_tensor(out=fre[:], in0=fre[:], in1=t1[:], op=ALU.mult), [B], [B])
        V(lambda: nc.vector.tensor_tensor(out=vw(fim), in0=vw(lim), in1=are, op=ALU.mult), [B, Bin], [B])
        V(lambda: nc.vector.tensor_tensor(out=vw(t2), in0=vw(c), in1=aim, op=ALU.mult), [B, Bin], [B])
        V(lambda: nc.vector.tensor_tensor(out=fim[:], in0=fim[:], in1=t2[:], op=ALU.subtract), [B], [B])
        V(lambda: nc.vector.tensor_tensor(out=fim[:], in0=fim[:], in1=t1[:], op=ALU.mult), [B], [B])
        return lre, lim, fre, fim, B

    def cmul(self, ore, oim, are, aim, bre, bim, t1, r, w):
        nc, V = self.nc, self.V
        V(lambda: nc.vector.tensor_tensor(out=ore, in0=are, in1=bre, op=ALU.mult), r, w)
        V(lambda: nc.vector.tensor_tensor(out=t1, in0=aim, in1=bim, op=ALU.mult), r, w)
        V(lambda: nc.vector.tensor_tensor(out=ore, in0=ore, in1=t1, op=ALU.subtract), r, w)
        V(lambda: nc.vector.tensor_tensor(out=oim, in0=are, in1=bim, op=ALU.mult), r, w)
        V(lambda: nc.vector.tensor_tensor(out=t1, in0=aim, in1=bre, op=ALU.mult), r, w)
        V(lambda: nc.vector.tensor_tensor(out=oim, in0=oim, in1=t1, op=ALU.add), r, w)

    def stage_s5(self, l):
        nc, S = self.nc, self.S
        V, A, P = self.V, self.A, self.P
        pT, BpT = self.dram["pT"], self.dbuf["pT"]
        ym, Bym = self.dram["ymix"], self.dbuf["ymix"]
        Bc = self.Bconst
        with ExitStack() as es:
            M1re = es.enter_context(self.sbuf("M1re", [128, 4, 8, 128], BF16))
            M1im = es.enter_context(self.sbuf("M1im", [128, 4, 8, 128], BF16))
            M1hr = es.enter_context(self.sbuf("M1hr", [128, 4, 8, 128], BF16))
            M1hi = es.enter_context(self.sbuf("M1hi", [128, 4, 8, 128], BF16))
            M2hr = es.enter_context(self.sbuf("M2hr", [128, 4, 8, 64], BF16))
            M2hi = es.enter_context(self.sbuf("M2hi", [128, 4, 8, 64], BF16))
            M2re = es.enter_context(self.sbuf("M2re", [128, 16, 8, 32], BF16))
            M2im = es.enter_context(self.sbuf("M2im", [128, 16, 8, 32], BF16))
            Kbd = es.enter_context(self.sbuf("Kbd", [128, 4, 8, 128], BF16))
            LK = es.enter_context(self.sbuf("LK", [128, 16, 8, 3], F32))
            wglu = es.enter_context(self.sbuf("wglu", [128, 4, 512], BF16))
            BW = Buf("s5w")
            Bwg = Buf()
            S.dma("pool", wglu[:], self.dram["s5_w_glu"][l].rearrange("(k p) c -> p k c", p=128), reads=[self.dbuf["s5_w_glu"]], writes=[Bwg])
            with ExitStack() as e2:
                w = e2.enter_context(self.sbuf("s5w_sb", [128, S5X], F32)); self.Bs5w = Buf()
                S.dma("sp", w[:], self.dram["s5w"][:, l, :], reads=[self.dbuf["s5w"]], writes=[self.Bs5w])
                aC = w[:, oAC:oAC + 512].rearrange("p (g r n) -> p g r n", g=4, r=2)
                bC = w[:, oBC:oBC + 512].rearrange("p (g r n) -> p g r n", g=4, r=2)
                lC = w[:, oLC:oLC + 4].unsqueeze(2).to_broadcast([128, 4, 64])
                lre, lim, fre, fim, B1 = self.s5_lambda(e2, "c", aC[:, :, 0, :], aC[:, :, 1, :], lC, [128, 4, 64])
                v3 = lambda t: t[:].rearrange("p (a b) -> p a b", b=64)
                Bre = e2.enter_context(self.sbuf("cBre", [128, 256], F32)); Bim = e2.enter_context(self.sbuf("cBim", [128, 256], F32))
                tq = e2.enter_context(self.sbuf("ctq", [128, 256], F32))
                self.cmul(v3(Bre), v3(Bim), v3(fre), v3(fim), bC[:, :, 0, :], bC[:, :, 1, :], v3(tq), [B1, self.Bs5w], [B1])
                Pw = e2.enter_context(self.sbuf("cPw", [128, 8, 2, 256], F32))
                V(lambda: nc.vector.memset(Pw[:, 0, 0, :], 1.0), [], [B1])
                V(lambda: nc.vector.memset(Pw[:, 0, 1, :], 0.0), [], [B1])
                for m in range(1, 8):
                    self.cmul(Pw[:, m, 0, :], Pw[:, m, 1, :], Pw[:, m - 1, 0, :], Pw[:, m - 1, 1, :], lre[:], lim[:], tq[:], [B1], [B1])
                vre = e2.enter_context(self.sbuf("cvre", [128, 256], F32)); vim = e2.enter_context(self.sbuf("cvim", [128, 256], F32))
                for j in range(8):
                    m = 7 - j
                    self.cmul(vre[:], vim[:], Pw[:, m, 0, :], Pw[:, m, 1, :], Bre[:], Bim[:], tq[:], [B1], [B1])
                    for bb in range(2):
                        V(lambda: nc.vector.tensor_scalar(out=M1re[:, :, j, bb * 64:(bb + 1) * 64], in0=v3(vre), scalar1=self.gvec[:, 16 + bb:17 + bb], scalar2=None, op0=ALU.mult),
                          [B1, self.Bvecs], [BW])
                        V(lambda: nc.vector.tensor_scalar(out=M1im[:, :, j, bb * 64:(bb + 1) * 64], in0=v3(vim), scalar1=self.gvec[:, 16 + bb:17 + bb], scalar2=None, op0=ALU.mult),
                          [B1, self.Bvecs], [BW])
                        V(lambda: nc.vector.tensor_scalar(out=M1hr[:, :, j, bb * 64:(bb + 1) * 64], in0=v3(vre), scalar1=self.gvec[:, 16 + bb:17 + bb], scalar2=self.gvec[:, 19:20], op0=ALU.mult, op1=ALU.mult),
                          [B1, self.Bvecs], [BW])
                        V(lambda: nc.vector.tensor_scalar(out=M1hi[:, :, j, bb * 64:(bb + 1) * 64], in0=v3(vim), scalar1=self.gvec[:, 16 + bb:17 + bb], scalar2=self.gvec[:, 19:20], op0=ALU.mult, op1=ALU.mult),
                          [B1, self.Bvecs], [BW])
            S.barrier()
            with ExitStack() as e2:
                w = e2.enter_context(self.sbuf("s5w_sb2", [128, S5X], F32)); self.Bs5w = Buf()
                S.dma("sp", w[:], self.dram["s5w"][:, l, :], reads=[self.dbuf["s5w"]], writes=[self.Bs5w])
                aP = w[:, oAP:oAP + 32].rearrange("p (g r) -> p g r", r=2)
                lP = w[:, oLP:oLP + 16]
                bP = w[:, oBP:oBP + 1024].rearrange("p (g r c) -> p g r c", g=16, r=2)
                cP = w[:, oCP:oCP + 1024].rearrange("p (g r c) -> p g r c", g=16, r=2)
                lre, lim, fre, fim, B2 = self.s5_lambda(e2, "p", aP[:, :, 0], aP[:, :, 1], lP, [128, 16])
                tq = e2.enter_context(self.sbuf("ptq", [128, 16], F32))
                Pp = e2.enter_context(self.sbuf("pPw", [128, 9, 2, 16], F32))
                V(lambda: nc.vector.memset(Pp[:, 0, 0, :], 1.0), [], [B2])
                V(lambda: nc.vector.memset(Pp[:, 0, 1, :], 0.0), [], [B2])
                for m in range(1, 9):
                    self.cmul(Pp[:, m, 0, :], Pp[:, m, 1, :], Pp[:, m - 1, 0, :], Pp[:, m - 1, 1, :], lre[:], lim[:], tq[:], [B2], [B2])
                lkr = e2.enter_context(self.sbuf("lkr", [128, 8, 16], F32)); lki = e2.enter_context(self.sbuf("lki", [128, 8, 16], F32))
                V(lambda: nc.vector.tensor_copy(out=lkr[:, 0, :], in_=Pp[:, 8, 0, :]), [B2], [B2])
                V(lambda: nc.vector.tensor_copy(out=lki[:, 0, :], in_=Pp[:, 8, 1, :]), [B2], [B2])
                for lev in range(1, 8):
                    self.cmul(lkr[:, lev, :], lki[:, lev, :], lkr[:, lev - 1, :], lki[:, lev - 1, :], lkr[:, lev - 1, :], lki[:, lev - 1, :], tq[:], [B2], [B2])
                V(lambda: nc.vector.tensor_copy(out=LK[:, :, :, 0], in_=lkr[:].rearrange("p l g -> p g l")), [B2], [BW])
                V(lambda: nc.vector.tensor_copy(out=LK[:, :, :, 1], in_=lki[:].rearrange("p l g -> p g l")), [B2], [BW])
                V(lambda: nc.vector.tensor_scalar(out=LK[:, :, :, 2], in0=lki[:].rearrange("p l g -> p g l"), scalar1=-1.0, scalar2=None, op0=ALU.mult), [B2], [BW])
                Bpr = e2.enter_context(self.sbuf("pBre", [128, 16, 64], F32)); Bpi = e2.enter_context(self.sbuf("pBim", [128, 16, 64], F32))
                V(lambda: nc.vector.memset(Bpr[:], 0.0), [], [B2])
                V(lambda: nc.vector.memset(Bpi[:], 0.0), [], [B2])
                t3 = e2.enter_context(self.sbuf("pt3", [128, 16, 32], F32))
                bc = lambda t: t[:].unsqueeze(2).to_broadcast([128, 16, 32])
                self.cmul(Bpr[:, :, 32:64], Bpi[:, :, 32:64], bc(fre), bc(fim), bP[:, :, 0, :], bP[:, :, 1, :], t3[:], [B2, self.Bs5w], [B2])
                CLr = e2.enter_context(self.sbuf("CLr", [128, 16, 9, 32], F32)); CLn = e2.enter_context(self.sbuf("CLn", [128, 16, 9, 32], F32))
                for m in range(9):
                    pr = Pp[:, m, 0, :].unsqueeze(2).to_broadcast([128, 16, 32])
                    pi = Pp[:, m, 1, :].unsqueeze(2).to_broadcast([128, 16, 32])
                    V(lambda: nc.vector.tensor_tensor(out=CLr[:, :, m, :], in0=cP[:, :, 0, :], in1=pr, op=ALU.mult), [B2, self.Bs5w], [B2])
                    V(lambda: nc.vector.tensor_tensor(out=t3[:], in0=cP[:, :, 1, :], in1=pi, op=ALU.mult), [B2, self.Bs5w], [B2])
                    V(lambda: nc.vector.tensor_tensor(out=CLr[:, :, m, :], in0=CLr[:, :, m, :], in1=t3[:], op=ALU.subtract), [B2], [B2])
                    V(lambda: nc.vector.tensor_tensor(out=CLn[:, :, m, :], in0=cP[:, :, 0, :], in1=pi, op=ALU.mult), [B2, self.Bs5w], [B2])
                    V(lambda: nc.vector.tensor_tensor(out=t3[:], in0=cP[:, :, 1, :], in1=pr, op=ALU.mult), [B2, self.Bs5w], [B2])
                    V(lambda: nc.vector.scalar_tensor_tensor(out=CLn[:, :, m, :], in0=CLn[:, :, m, :], scalar=-1.0, in1=t3[:], op0=ALU.mult, op1=ALU.subtract), [B2], [B2])
                V(lambda: nc.vector.tensor_copy(out=M2re[:], in_=CLr[:, :, 1:9, :]), [B2], [BW])
                V(lambda: nc.vector.tensor_copy(out=M2im[:], in_=CLn[:, :, 1:9, :]), [B2], [BW])
                V(lambda: nc.vector.memset(M2hr[:], 0.0), [], [BW])
                V(lambda: nc.vector.memset(M2hi[:], 0.0), [], [BW])
                for gt in range(4):
                    V(lambda: nc.vector.tensor_copy(out=M2hr[:, gt, :, 32:64], in_=CLr[:, 4 * gt + 3, 1:9, :]), [B2], [BW])
                    V(lambda: nc.vector.tensor_copy(out=M2hi[:, gt, :, 32:64], in_=CLn[:, 4 * gt + 3, 1:9, :]), [B2], [BW])
                KbdF = e2.enter_context(self.sbuf("KbdF", [128, 4, 8, 128], F32))
                V(lambda: nc.vector.memset(KbdF[:], 0.0), [], [B2])
                for pair in range(16):
                    gt, gp = pair // 4, pair % 4
                    pt, Bp = self.psum()
                    rs = slice(32 * gp, 32 * gp + 32) if gp < 3 else slice(64, 128)
                    cs_ = slice(32, 64) if gp < 3 else slice(0, 64)
                    P(lambda: nc.tensor.matmul(pt[rs, 0:256], lhsT=Bpr[:, pair, cs_], rhs=CLr[:, pair, 0:8, :], start=True, stop=False), [B2], [Bp])
                    P(lambda: nc.tensor.matmul(pt[rs, 0:256], lhsT=Bpi[:, pair, cs_], rhs=CLn[:, pair, 0:8, :], start=False, stop=True), [B2], [Bp])
                    V(lambda: nc.vector.tensor_copy(out=KbdF[rs, gt, :, 32 * gp:32 * gp + 32], in_=pt[rs, 0:256].rearrange("p (m c) -> p m c", c=32)), [Bp, B2], [B2])
                for gt in range(4):
                    V(lambda: nc.vector.scalar_tensor_tensor(out=KbdF[:, gt, 0, :], in0=self.ident_f[:], scalar=self.vcol(l, "s5_d", gt, 1), in1=KbdF[:, gt, 0, :],
                                                             op0=ALU.mult, op1=ALU.add), [B2, Bc, self.Bvecs], [B2])
                V(lambda: nc.vector.tensor_copy(out=Kbd[:], in_=KbdF[:]), [B2], [BW])
            S.barrier()
            if "s5pre" in self.debug:
                self.dump("dbg_M1re", M1re[:], [BW]); self.dump("dbg_M1im", M1im[:], [BW]); self.dump("dbg_M2re", M2re[:], [BW])
                self.dump("dbg_M2im", M2im[:], [BW]); self.dump("dbg_Kbd", Kbd[:], [BW]); self.dump("dbg_LK", LK[:], [BW])
            for sq in range(NSEQ):
                with ExitStack() as e3:
                    self.s5_seq(e3, l, sq, (M1re, M1im, M1hr, M1hi), (M2re, M2im, M2hr, M2hi), Kbd, LK, wglu, BW, Bwg)
                    S.barrier()

    def s5_seq(self, es, l, sq, M1s, M2s, Kbd, LK, wglu, BW, Bwg):
        M1re, M1im, M1hr, M1hi = M1s
        M2re, M2im, M2hr, M2hi = M2s
        nc, S = self.nc, self.S
        V, A, P = self.V, self.A, self.P
        pT, BpT = self.dram["pT"], self.dbuf["pT"]
        ym, Bym = self.dram["ymix"], self.dbuf["ymix"]
        t0 = sq * SEQ
        L = SEQ
        NC8 = L // 8
        us = self.tiles(es, "s5u", [128, L], BF16, 4)
        for gt in range(4):
            S.dma("sp", us[gt][0][:], pT[S50 + 128 * gt:S50 + 128 * gt + 128, t0:t0 + L], reads=[BpT], writes=[us[gt][1]])
        S16r = es.enter_context(self.sbuf("s5Sr", [128, 16, NC8 + 1], BF16)); S16i = es.enter_context(self.sbuf("s5Si", [128, 16, NC8 + 1], BF16))
        BS16 = [Buf() for _ in range(16)]
        Bz = Buf()
        V(lambda: nc.vector.memset(S16r[:, :, 0:1], 0.0), [], [Bz])
        V(lambda: nc.vector.memset(S16i[:, :, 0:1], 0.0), [], [Bz])
        for b_ in BS16:
            b_.lw = Bz.lw
        sc = [[[self.tiles(es, "s5sc", [128, 2 * NC8], F32, 2) for _ri in range(2)] for _pp in range(1)] for _slot in range(2)]
        for slot in range(2):
            for ri in range(2):
                for pp in range(2):
                    tt_, Bt = sc[slot][0][ri][pp]
                    V(lambda: nc.vector.memset(tt_[:, 0:NC8], 0.0), [], [Bt])
        for p0 in range(0, 16, 2):
            prs = [p0, p0 + 1]
            for si, pair in enumerate(prs):
                gt, gp = pair // 4, pair % 4
                rs = slice(32 * gp, 32 * gp + 32) if gp < 3 else slice(64, 128)
                u, Bu = us[gt]
                u3 = u[:].rearrange("p (c j) -> p j c", j=8)
                for ri, M1 in enumerate((M1re, M1im) if gp < 3 else (M1hr, M1hi)):
                    pt, Bp = self.psum()
                    for j in range(8):
                        P(lambda: nc.tensor.matmul(pt[:, 0:NC8], lhsT=M1[rs, gt, j, :], rhs=u3[rs, j, :], start=(j == 0), stop=(j == 7)), [BW, Bu], [Bp])
                    tt_, Bt = sc[si][0][ri][0]
                    A(lambda: nc.scalar.copy(out=tt_[:, NC8:2 * NC8], in_=pt[:, 0:NC8]), [Bp], [Bt])
            for lev in range(8):
                sh = 1 << lev
                src, dst = lev % 2, (lev + 1) % 2
                for step in range(2):
                    for si, pair in enumerate(prs):
                        (re_s, Brs), (im_s, Bis) = sc[si][0][0][src], sc[si][0][1][src]
                        (re_d, Brd), (im_d, Bid) = sc[si][0][0][dst], sc[si][0][1][dst]
                        lr, li, nli = LK[:, pair, lev, 0:1], LK[:, pair, lev, 1:2], LK[:, pair, lev, 2:3]
                        cur = slice(NC8, 2 * NC8); shf = slice(NC8 - sh, 2 * NC8 - sh)
                        if step == 0:
                            V(lambda: nc.vector.scalar_tensor_tensor(out=re_d[:, cur], in0=re_s[:, shf], scalar=lr, in1=re_s[:, cur], op0=ALU.mult, op1=ALU.add), [Brs, BW], [Brd])
                            V(lambda: nc.vector.scalar_tensor_tensor(out=im_d[:, cur], in0=im_s[:, shf], scalar=lr, in1=im_s[:, cur], op0=ALU.mult, op1=ALU.add), [Bis, BW], [Bid])
                        else:
                            V(lambda: nc.vector.scalar_tensor_tensor(out=re_d[:, cur], in0=im_s[:, shf], scalar=nli, in1=re_d[:, cur], op0=ALU.mult, op1=ALU.add), [Bis, Brd, BW], [Brd])
                            V(lambda: nc.vector.scalar_tensor_tensor(out=im_d[:, cur], in0=re_s[:, shf], scalar=li, in1=im_d[:, cur], op0=ALU.mult, op1=ALU.add), [Brs, Bid, BW], [Bid])
            for si, pair in enumerate(prs):
                (re_f, Brf), (im_f, Bif) = sc[si][0][0][0], sc[si][0][1][0]
                A(lambda: nc.scalar.copy(out=S16r[:, pair, 1:NC8 + 1], in_=re_f[:, NC8:2 * NC8]), [Brf], [BS16[pair]])
                A(lambda: nc.scalar.copy(out=S16i[:, pair, 1:NC8 + 1], in_=im_f[:, NC8:2 * NC8]), [Bif], [BS16[pair]])
        ys = self.tiles(es, "s5y", [128, L], F32, 2)
        yg = self.tiles(es, "s5yg", [128, L], BF16, 4)
        ta = self.tiles(es, "s5ta", [128, 512], F32, 2)
        for gt in range(4):
            y, By = ys[gt % 2]
            u, Bu = us[gt]
            u3 = u[:].rearrange("p (c j) -> p j c", j=8)
            y3 = y[:].rearrange("p (c j) -> p j c", j=8)
            for j in range(8):
                pt, Bp = self.psum()
                for i in range(j + 1):
                    P(lambda: nc.tensor.matmul(pt[:, 0:NC8], lhsT=Kbd[:, gt, j - i, :], rhs=u3[:, i, :], start=(i == 0), stop=False), [BW, Bu], [Bp])
                for gp in range(4):
                    pair = 4 * gt + gp
                    if gp < 3:
                        rs = slice(32 * gp, 32 * gp + 32)
                        l_re, l_im = M2re[:, pair, j, :], M2im[:, pair, j, :]
                    else:
                        rs = slice(64, 128)
                        l_re, l_im = M2hr[:, gt, j, :], M2hi[:, gt, j, :]
                    P(lambda: nc.tensor.matmul(pt[rs, 0:NC8], lhsT=l_re, rhs=S16r[:, pair, 0:NC8], start=False, stop=False), [BW, BS16[pair]], [Bp])
                    P(lambda: nc.tensor.matmul(pt[rs, 0:NC8], lhsT=l_im, rhs=S16i[:, pair, 0:NC8], start=False, stop=(gp == 3)), [BW, BS16[pair]], [Bp])
                A(lambda: nc.scalar.copy(out=y3[:, j, :], in_=pt[:, 0:NC8]), [Bp], [By])
            g16, Bg = yg[gt]
            for tb in range(L // 512):
                cs = slice(tb * 512, tb * 512 + 512)
                t_, Bt = ta[tb % 2]
                A(lambda: nc.scalar.activation(out=t_[:], in_=y[:, cs], func=AF.Square), [By], [Bt])
                V(lambda: nc.vector.tensor_scalar(out=t_[:], in0=t_[:], scalar1=0.044715, scalar2=1.0, op0=ALU.mult, op1=ALU.add), [Bt], [Bt])
                V(lambda: nc.vector.tensor_tensor(out=t_[:], in0=t_[:], in1=y[:, cs], op=ALU.mult), [Bt, By], [Bt])
                A(lambda: nc.scalar.activation(out=t_[:], in_=t_[:], func=AF.Sigmoid, scale=1.5957691216057308), [Bt], [Bt])
                V(lambda: nc.vector.tensor_tensor(out=g16[:, cs], in0=t_[:], in1=y[:, cs], op=ALU.mult), [Bt, By], [Bg])
        yo = self.tiles(es, "s5yo", [128, 512], BF16, 2)
        sg = self.tiles(es, "s5sg", [128, 512], F32, 2)
        cnt = 0
        for m in range(4):
            for tb in range(L // 512):
                cs = slice(tb * 512, tb * 512 + 512)
                pt, Bp = self.psum()
                for kt in range(4):
                    P(lambda: nc.tensor.matmul(pt[:], lhsT=wglu[:, kt, m * 128:(m + 1) * 128], rhs=yg[kt][0][:, cs], start=(kt == 0), stop=(kt == 3)), [Bwg, yg[kt][1]], [Bp])
                s_, Bs = sg[cnt % 2]; o_, Bo = yo[cnt % 2]; cnt += 1
                A(lambda: nc.scalar.activation(out=s_[:], in_=pt[:], func=AF.Sigmoid, bias=self.vcol(l, "s5_b_glu", m, 1)), [Bp, self.Bvecs], [Bs])
                V(lambda: nc.vector.tensor_tensor(out=o_[:], in0=s_[:], in1=yg[m][0][:, cs], op=ALU.mult), [Bs, yg[m][1]], [Bo])
                S.dma("sp", ym[1, 128 * m:128 * m + 128, t0 + tb * 512:t0 + tb * 512 + 512], o_[:], reads=[Bo], writes=[Bym])

    def stage_rwkv(self, l, sq):
        nc, S = self.nc, self.S
        V, A, P = self.V, self.A, self.P
        pT, BpT = self.dram["pT"], self.dbuf["pT"]
        ym, Bym = self.dram["ymix"], self.dbuf["ymix"]
        Bc = self.Bconst
        Bv = self.Bvecs
        t0 = sq * SEQ
        L = SEQ
        NCH = L // 64
        ALPHA = float(np.exp(-0.5))
        with ExitStack() as es:
            WU = es.enter_context(self.sbuf("rwWU", [128, 512], BF16)); BWU = Buf()
            GU = es.enter_context(self.sbuf("rwGU", [128, 512], BF16)); BGU = Buf()
            S.dma("pool", WU[0:64, :], self.dram["rw_w_up"][l], reads=[self.dbuf["rw_w_up"]], writes=[BWU])
            S.dma("pool", WU[64:128, :], self.dram["rw_a_up"][l], reads=[self.dbuf["rw_a_up"]], writes=[BWU])
            S.dma("pool", GU[:], self.dram["rw_g_up"][l], reads=[self.dbuf["rw_g_up"]], writes=[BGU])
            TW = es.enter_context(self.sbuf("rwTW", [128, L], BF16)); BTW = Buf()
            SG = es.enter_context(self.sbuf("rwSG", [128, L], BF16)); BSG = Buf()
            bdn = ["Rt", "Kt", "Bt", "At", "Vb"]
            BD = {n: es.enter_context(self.sbuf("rw" + n, [128, NCH, 128], BF16)) for n in bdn}
            BBD = {n: Buf(n) for n in bdn}
            for n in bdn:
                V(lambda: nc.vector.memset(BD[n][:], 0.0), [], [BBD[n]])
            Dt = es.enter_context(self.sbuf("rwD", [128, NCH], F32)); BD_ = Buf()
            g16 = es.enter_context(self.sbuf("rwg16", [128, L], BF16)); Bg16 = Buf()
            bon = es.enter_context(self.sbuf("rwbon", [128, L], BF16)); Bbon = Buf()
            Yfm = es.enter_context(self.sbuf("rwY", [128, L], F32)); BY = Buf()
            Sall = es.enter_context(self.sbuf("rwSall", [128, NCH + 1, 128], BF16)); BSall = [Buf() for _ in range(NCH + 1)]
            with ExitStack() as e2:
                xin = e2.enter_context(self.sbuf("rwxin", [128, L + 1], BF16)); Bxin = Buf()
                tmp = e2.enter_context(self.sbuf("rwtmp", [128, L], F32)); Btmp = Buf()
                for (r0, mucol, which) in [(1536, 12, 0), (1664, 13, 1)]:
                    V(lambda: nc.vector.memset(xin[:, 0:1], 0.0), [], [Bxin])
                    S.dma("sp", xin[:, 1:L + 1], pT[RW0 + r0:RW0 + r0 + 128, t0:t0 + L], reads=[BpT], writes=[Bxin])
                    V(lambda: nc.vector.tensor_tensor(out=tmp[:], in0=xin[:, 0:L], in1=xin[:, 1:L + 1], op=ALU.subtract), [Bxin], [Btmp])
                    V(lambda: nc.vector.scalar_tensor_tensor(out=tmp[:], in0=tmp[:], scalar=self.vcol(l, "rw_mu", mucol, 1), in1=xin[:, 1:L + 1],
                                                             op0=ALU.mult, op1=ALU.add), [Btmp, Bxin, Bv], [Btmp])
                    if which == 0:
                        A(lambda: nc.scalar.activation(out=TW[0:64, :], in_=tmp[0:64, :], func=AF.Tanh), [Btmp], [BTW])
                        V(lambda: nc.vector.tensor_copy(out=TW[64:128, :], in_=tmp[64:128, :]), [Btmp], [BTW])
                    else:
                        A(lambda: nc.scalar.activation(out=SG[:], in_=tmp[:], func=AF.Sigmoid), [Btmp], [BSG])
                S.barrier()
            for hp in range(4):
                V(lambda: nc.vector.memset(Sall[:, 0, :], 0.0), [], [BSall[0]])
                with ExitStack() as e2:
                    self.rwkv_prologue(e2, l, sq, hp, WU, BWU, GU, BGU, TW, BTW, SG, BSG, BD, BBD, Dt, BD_, g16, Bg16, bon, Bbon)
                    S.barrier()
                with ExitStack() as e2:
                    self.rwkv_chunks(e2, BD, BBD, Dt, BD_, Yfm, BY, Sall, BSall)
                    S.barrier()
                with ExitStack() as e2:
                    self.rwkv_epilogue(e2, l, sq, hp, Yfm, BY, g16, Bg16, bon, Bbon)
                    S.barrier()

    def rwkv_prologue(self, es, l, sq, hp, WU, BWU, GU, BGU, TW, BTW, SG, BSG, BD, BBD, Dt, BDt, g16, Bg16, bon, Bbon):
        nc, S = self.nc, self.S
        V, A, P = self.V, self.A, self.P
        pT, BpT = self.dram["pT"], self.dbuf["pT"]
        Bc, Bv = self.Bconst, self.Bvecs
        t0 = sq * SEQ
        L = SEQ
        NCH = L // 64
        ALPHA = float(np.exp(-0.5))
        cs_hp = slice(128 * hp, 128 * hp + 128)
        def f32t(n):
            return es.enter_context(self.sbuf("rwp" + n, [128, L + 1], F32)), Buf(n)
        xin = [(es.enter_context(self.sbuf("rwpx%d" % i, [128, L + 1], BF16)), Buf()) for i in range(3)]
        (X1, B1), (X2, B2), (X3, B3) = [(es.enter_context(self.sbuf("rwpb%d" % i, [128, L], BF16 if i != 1 else F32)), Buf()) for i in range(3)]
        (X4, B4), (X5, B5), (X6, B6), (X7, B7), (X9, B9), (X10, B10) = [f32t(n) for n in ["4", "5", "6", "7", "9", "10"]]
        for i, (dst, Bd, r0) in enumerate([(X1, B1, 0), (X2, B2, 512), (X3, B3, 1024)]):
            xt, Bx = xin[i]
            V(lambda: nc.vector.memset(xt[:, 0:1], 0.0), [], [Bx])
            S.dma("sp", xt[:, 1:L + 1], pT[RW0 + r0 + 128 * hp:RW0 + r0 + 128 * hp + 128, t0:t0 + L], reads=[BpT], writes=[Bx])
            V(lambda: nc.vector.tensor_tensor(out=X7[:, 0:L], in0=xt[:, 0:L], in1=xt[:, 1:L + 1], op=ALU.subtract), [Bx], [B7])
            V(lambda: nc.vector.scalar_tensor_tensor(out=dst[:], in0=X7[:, 0:L], scalar=self.vcol(l, "rw_mu", 4 * i + hp, 1), in1=xt[:, 1:L + 1],
                                                     op0=ALU.mult, op1=ALU.add), [B7, Bx, Bv], [Bd])
        for tb in range(L // 512):
            cs = slice(tb * 512, tb * 512 + 512)
            pt, Bp = self.psum()
            P(lambda: nc.tensor.matmul(pt[:], lhsT=WU[0:64, cs_hp], rhs=TW[0:64, cs], start=True, stop=True), [BWU, BTW], [Bp])
            A(lambda: nc.scalar.activation(out=X4[:, cs], in_=pt[:], func=AF.Sigmoid, bias=self.vcol(l, "rw_w0", hp, 1)), [Bp, Bv], [B4])
            pt, Bp = self.psum()
            P(lambda: nc.tensor.matmul(pt[:], lhsT=WU[64:128, cs_hp], rhs=TW[64:128, cs], start=True, stop=True), [BWU, BTW], [Bp])
            A(lambda: nc.scalar.activation(out=X5[:, cs], in_=pt[:], func=AF.Sigmoid, bias=self.vcol(l, "rw_a0", hp, 1)), [Bp, Bv], [B5])
            pt, Bp = self.psum()
            P(lambda: nc.tensor.matmul(pt[:], lhsT=GU[:, cs_hp], rhs=SG[:, cs], start=True, stop=True), [BGU, BSG], [Bp])
            V(lambda: nc.vector.tensor_copy(out=g16[:, cs], in_=pt[:]), [Bp], [Bg16])
        V(lambda: nc.vector.memset(X9[:, 0:1], 0.0), [], [B9])
        V(lambda: nc.vector.tensor_tensor_scan(out=X9[:, 1:L + 1], data0=self.cst[:, 0:1].to_broadcast([128, L]), data1=X4[:, 0:L],
                                               initial=0.0, op0=ALU.mult, op1=ALU.add), [B4, Bc], [B9])
        V(lambda: nc.vector.tensor_scalar(out=X6[:, 0:L], in0=X2[:], scalar1=self.vcol(l, "rw_k_k", hp, 1), scalar2=None, op0=ALU.mult), [B2, Bv], [B6])
        A(lambda: nc.scalar.activation(out=X7[:, 0:L], in_=X6[:, 0:L], func=AF.Square), [B6], [B7])
        for tb in range(L // 512):
            cs = slice(tb * 512, tb * 512 + 512)
            pt, Bp = self.psum()
            P(lambda: nc.tensor.matmul(pt[:], lhsT=self.ones_bd[:], rhs=X7[:, cs], start=True, stop=True), [Bc, B7], [Bp])
            A(lambda: nc.scalar.activation(out=X4[:, cs], in_=pt[:], func=AF.Sqrt, bias=self.cst[:, 2:3]), [Bp, Bc, B9], [B4])
        V(lambda: nc.vector.reciprocal(out=X4[:, 0:L], in_=X4[:, 0:L]), [B4], [B4])
        V(lambda: nc.vector.tensor_tensor(out=X6[:, 0:L], in0=X6[:, 0:L], in1=X4[:, 0:L], op=ALU.mult), [B6, B4], [B6])
        V(lambda: nc.vector.tensor_scalar(out=X7[:, 0:L], in0=X5[:, 0:L], scalar1=-1.0, scalar2=self.vcol(l, "rw_k_a", hp, 1), op0=ALU.add, op1=ALU.mult), [B5, Bv], [B7])
        V(lambda: nc.vector.scalar_tensor_tensor(out=X2[:], in0=X7[:, 0:L], scalar=1.0, in1=X2[:], op0=ALU.add, op1=ALU.mult), [B7, B2], [B2])
        V(lambda: nc.vector.tensor_tensor(out=X5[:, 0:L], in0=X6[:, 0:L], in1=X5[:, 0:L], op=ALU.mult), [B6, B5], [B5])
        V(lambda: nc.vector.tensor_tensor(out=X7[:, 0:L], in0=X1[:], in1=X2[:], op=ALU.mult), [B1, B2], [B7])
        V(lambda: nc.vector.tensor_scalar(out=X7[:, 0:L], in0=X7[:, 0:L], scalar1=self.vcol(l, "rw_r_k", hp, 1), scalar2=None, op0=ALU.mult), [B7, Bv], [B7])
        for tb in range(L // 512):
            cs = slice(tb * 512, tb * 512 + 512)
            pt, Bp = self.psum()
            P(lambda: nc.tensor.matmul(pt[:], lhsT=self.ones_bd[:], rhs=X7[:, cs], start=True, stop=True), [Bc, B7], [Bp])
            V(lambda: nc.vector.tensor_tensor(out=bon[:, cs], in0=pt[:], in1=X3[:, cs], op=ALU.mult), [Bp, B3], [Bbon])
        c3 = lambda ap: ap.rearrange("p (c t) -> p c t", t=64)
        base = c3(X9[:, 0:L])[:, :, 0:1].to_broadcast([128, NCH, 64])
        V(lambda: nc.vector.tensor_tensor(out=c3(X7[:, 0:L]), in0=c3(X9[:, 1:L + 1]), in1=base, op=ALU.subtract), [B9, Bbon], [B7])
        A(lambda: nc.scalar.activation(out=X4[:, 0:L], in_=X7[:, 0:L], func=AF.Exp, scale=-ALPHA), [B7], [B4])
        A(lambda: nc.scalar.activation(out=X10[:, 0:L], in_=X7[:, 0:L], func=AF.Exp, scale=ALPHA), [B7], [B10])
        V(lambda: nc.vector.tensor_tensor(out=c3(X7[:, 0:L]), in0=c3(X9[:, 0:L]), in1=base, op=ALU.subtract), [B9, B4, B10], [B7])
        A(lambda: nc.scalar.activation(out=X7[:, 0:L], in_=X7[:, 0:L], func=AF.Exp, scale=-ALPHA), [B7], [B7])
        V(lambda: nc.vector.tensor_copy(out=Dt[:], in_=c3(X4[:, 0:L])[:, :, 63]), [B4], [BDt])
        for hf in range(2):
            ps_ = slice(64 * hf, 64 * hf + 64)
            fs_ = slice(64 * hf, 64 * hf + 64)
            V(lambda: nc.vector.tensor_tensor(out=BD["Rt"][ps_, :, fs_], in0=c3(X1[ps_, :]), in1=c3(X4[ps_, 0:L]), op=ALU.mult), [B1, B4], [BBD["Rt"]])
            V(lambda: nc.vector.tensor_tensor(out=BD["Kt"][ps_, :, fs_], in0=c3(X2[ps_, :]), in1=c3(X10[ps_, 0:L]), op=ALU.mult), [B2, B10], [BBD["Kt"]])
            V(lambda: nc.vector.tensor_tensor(out=BD["Bt"][ps_, :, fs_], in0=c3(X5[ps_, 0:L]), in1=c3(X10[ps_, 0:L]), op=ALU.mult), [B5, B10], [BBD["Bt"]])
            V(lambda: nc.vector.scalar_tensor_tensor(out=BD["At"][ps_, :, fs_], in0=c3(X6[ps_, 0:L]), scalar=-1.0, in1=c3(X7[ps_, 0:L]), op0=ALU.mult, op1=ALU.mult),
              [B6, B7], [BBD["At"]])
            A(lambda: nc.scalar.copy(out=BD["Vb"][ps_, :, fs_], in_=c3(X3[ps_, :])), [B3], [BBD["Vb"]])

    def rwkv_chunks(self, es, BD, BBD, Dt, BDt, Yfm, BY, Sall, BSall):
        nc, S = self.nc, self.S
        V, A, P = self.V, self.A, self.P
        Bc = self.Bconst
        NCH = SEQ // 64
        W = 4
        NU = NCH // 2
        Rt, Kt, Bt, At, Vb = [BD[n] for n in ["Rt", "Kt", "Bt", "At", "Vb"]]
        BRt, BKt, BBt, BAt, BVb = [BBD[n] for n in ["Rt", "Kt", "Bt", "At", "Vb"]]
        NR = 2 * W
        TM3 = self.tiles(es, "rcTM", [128, 2, 3, 128], BF16, NR)
        ZA = self.tiles(es, "rcZA", [128, 2, 256], BF16, NR)
        ZB = self.tiles(es, "rcZB", [128, 2, 256], BF16, NR)
        SC1 = self.tiles(es, "rcS1", [128, 2, 2, 128], BF16, NR)
        SC2 = self.tiles(es, "rcS2", [128, 2, 2, 128], BF16, NR)
        SC3 = self.tiles(es, "rcS3", [128, 2, 128], BF16, NR)
        PPA = self.tiles(es, "rcPA", [128, 2, 2, 128], BF16, NR)
        PPB = self.tiles(es, "rcPB", [128, 2, 2, 128], BF16, NR)
        IGQ = self.tiles(es, "rcIG", [128, 2, 2, 128], BF16, NR)
        HD = self.tiles(es, "rcHD", [128, 2, 128], F32, NR)
        m01 = self.mbd[:, 0:2, :].unsqueeze(1).to_broadcast([128, 2, 2, 128])
        m12 = self.mbd[:, 1:3, :].unsqueeze(1).to_broadcast([128, 2, 2, 128])
        m2 = self.mbd[:, 2:3, :].to_broadcast([128, 2, 128])
        identb = self.ident_f[:].unsqueeze(1).to_broadcast([128, 2, 128])

        pending = []
        post = []

        def drain(n):
            for _ in range(n):
                if pending:
                    pending.pop(0)()

        ngroups = NU // W
        for gi in range(ngroups):
            units = list(range(gi * W, gi * W + W))
            sl = {u: (u % NR) for u in units}
            for u in units:
                i = sl[u]; c0 = 2 * u
                pt, Bp = self.psum()
                pb = pt[:].bitcast(BF16).rearrange("p (k s t) -> p k s t", k=2, s=4)
                for k in range(2):
                    for si, (src, Bs) in enumerate([(Bt, BBt), (Kt, BKt), (Vb, BVb), (At, BAt)]):
                        P(lambda: nc.tensor.transpose(out=pb[:, k, si, :], in_=src[:, c0 + k, :], identity=self.ident_b[:]), [Bs, Bc], [Bp])
                A(lambda: nc.scalar.copy(out=TM3[i][0][:], in_=pb[:, :, 0:3, :]), [Bp], [TM3[i][1]])
                A(lambda: nc.scalar.copy(out=ZA[i][0][:, :, 0:128], in_=pb[:, :, 3, :]), [Bp], [ZA[i][1]])
            drain(1)
            for u in units:
                i = sl[u]; c0 = 2 * u
                pt, Bp = self.psum(); p4 = pt[:].rearrange("p (k s t) -> p k s t", k=2, s=2)
                for k in range(2):
                    c = c0 + k
                    P(lambda: nc.tensor.matmul(p4[:, k, 0, :], lhsT=At[:, c, :], rhs=Bt[:, c, :], start=True, stop=True), [BAt, BBt], [Bp])
                    P(lambda: nc.tensor.matmul(p4[:, k, 1, :], lhsT=Bt[:, c, :], rhs=At[:, c, :], start=True, stop=True), [BAt, BBt], [Bp])
                V(lambda: nc.vector.tensor_tensor(out=SC1[i][0][:], in0=p4, in1=m01, op=ALU.mult), [Bp, Bc], [SC1[i][1]])
                pt, Bp = self.psum(); p4 = pt[:].rearrange("p (k s t) -> p k s t", k=2, s=2)
                for k in range(2):
                    c = c0 + k
                    P(lambda: nc.tensor.matmul(p4[:, k, 0, :], lhsT=Kt[:, c, :], rhs=At[:, c, :], start=True, stop=True), [BAt, BKt], [Bp])
                    P(lambda: nc.tensor.matmul(p4[:, k, 1, :], lhsT=Bt[:, c, :], rhs=Rt[:, c, :], start=True, stop=True), [BRt, BBt], [Bp])
                V(lambda: nc.vector.tensor_tensor(out=SC2[i][0][:], in0=p4, in1=m12, op=ALU.mult), [Bp, Bc], [SC2[i][1]])
                pt, Bp = self.psum(); p3 = pt[:, 0:256].rearrange("p (k t) -> p k t", k=2)
                for k in range(2):
                    c = c0 + k
                    P(lambda: nc.tensor.matmul(p3[:, k, :], lhsT=Kt[:, c, :], rhs=Rt[:, c, :], start=True, stop=True), [BRt, BKt], [Bp])
                V(lambda: nc.vector.tensor_tensor(out=SC3[i][0][:], in0=p3, in1=m2, op=ALU.mult), [Bp, Bc], [SC3[i][1]])
            drain(1)
            for u in units:
                i = sl[u]
                pt, Bp = self.psum(); p3 = pt[:, 0:256].rearrange("p (k t) -> p k t", k=2)
                for k in range(2):
                    P(lambda: nc.tensor.matmul(p3[:, k, :], lhsT=SC2[i][0][:, k, 0, :], rhs=TM3[i][0][:, k, 2, :], start=True, stop=True), [SC2[i][1], TM3[i][1]], [Bp])
                A(lambda: nc.scalar.copy(out=ZA[i][0][:, :, 128:256], in_=p3), [Bp], [ZA[i][1]])
            drain(1)
            for lev in range(6):
                for u in units:
                    i = sl[u]
                    PPs = SC1[i] if lev == 0 else (PPA[i] if lev % 2 == 1 else PPB[i])
                    PPd = PPA[i] if lev % 2 == 0 else PPB[i]
                    Zs = ZA[i] if lev % 2 == 0 else ZB[i]
                    Zd = ZB[i] if lev % 2 == 0 else ZA[i]
                    pt, Bp = self.psum(); pz = pt[:].rearrange("p (k t) -> p k t", k=2)
                    if lev % 2 == 1:
                        for k in range(2):
                            P(lambda: nc.tensor.matmul(pz[:, k, :], lhsT=self.ident_b[:], rhs=Zs[0][:, k, :], start=True, stop=False), [Bc, Zs[1]], [Bp])
                            P(lambda: nc.tensor.matmul(pz[:, k, :], lhsT=PPs[0][:, k, 1, :], rhs=Zs[0][:, k, :], start=False, stop=True), [PPs[1], Zs[1]], [Bp])
                        A(lambda: nc.scalar.copy(out=Zd[0][:], in_=pz), [Bp], [Zd[1]])
                    else:
                        for k in range(2):
                            P(lambda: nc.tensor.matmul(pz[:, k, :], lhsT=PPs[0][:, k, 1, :], rhs=Zs[0][:, k, :], start=True, stop=True), [PPs[1], Zs[1]], [Bp])
                        V(lambda: nc.vector.tensor_tensor(out=Zd[0][:], in0=pz, in1=Zs[0][:], op=ALU.add), [Bp, Zs[1]], [Zd[1]])
                    if lev < 5:
                        pt, Bp = self.psum(); p4 = pt[:].rearrange("p (k s t) -> p k s t", k=2, s=2)
                        for k in range(2):
                            P(lambda: nc.tensor.matmul(p4[:, k, 0, :], lhsT=PPs[0][:, k, 1, :], rhs=PPs[0][:, k, 0, :], start=True, stop=True), [PPs[1]], [Bp])
                            P(lambda: nc.tensor.matmul(p4[:, k, 1, :], lhsT=PPs[0][:, k, 0, :], rhs=PPs[0][:, k, 1, :], start=True, stop=True), [PPs[1]], [Bp])
                        A(lambda: nc.scalar.copy(out=PPd[0][:], in_=p4), [Bp], [PPd[1]])
                drain(1)
            for u in units:
                i = sl[u]; c0 = 2 * u
                pt, Bp = self.psum(); p4 = pt[:].rearrange("p (k s t) -> p k s t", k=2, s=2)
                for k in range(2):
                    P(lambda: nc.tensor.matmul(p4[:, k, 0, :], lhsT=ZA[i][0][:, k, 0:128], rhs=TM3[i][0][:, k, 0, :], start=True, stop=True), [ZA[i][1], TM3[i][1]], [Bp])
                    P(lambda: nc.tensor.matmul(p4[:, k, 1, :], lhsT=ZA[i][0][:, k, 0:128], rhs=SC2[i][0][:, k, 1, :], start=True, stop=True), [ZA[i][1], SC2[i][1]], [Bp])
                V(lambda: nc.vector.tensor_tensor(out=IGQ[i][0][:, :, 0, :], in0=p4[:, :, 0, :], in1=identb, op=ALU.add), [Bp, Bc], [IGQ[i][1]])
                V(lambda: nc.vector.tensor_tensor(out=IGQ[i][0][:, :, 1, :], in0=p4[:, :, 1, :], in1=Rt[:, c0:c0 + 2, :], op=ALU.add), [Bp, BRt], [IGQ[i][1]])
            drain(1)
            for u in units:
                i = sl[u]; c0 = 2 * u
                pt, Bp = self.psum(); p3 = pt[:, 0:256].rearrange("p (k t) -> p k t", k=2)
                for k in range(2):
                    P(lambda: nc.tensor.matmul(p3[:, k, :], lhsT=TM3[i][0][:, k, 0, :], rhs=ZA[i][0][:, k, 128:256], start=True, stop=False), [ZA[i][1], TM3[i][1]], [Bp])
                    P(lambda: nc.tensor.matmul(p3[:, k, :], lhsT=TM3[i][0][:, k, 1, :], rhs=TM3[i][0][:, k, 2, :], start=False, stop=True), [TM3[i][1]], [Bp])
                for k in range(2):
                    A(lambda: nc.scalar.activation(out=HD[i][0][:, k, :], in_=p3[:, k, :], func=AF.Copy, scale=Dt[:, c0 + k:c0 + k + 1]), [Bp, BDt], [HD[i][1]])
                pt, Bp = self.psum(); p3 = pt[:, 0:256].rearrange("p (k t) -> p k t", k=2)
                for k in range(2):
                    P(lambda: nc.tensor.matmul(p3[:, k, :], lhsT=ZA[i][0][:, k, 128:256], rhs=SC2[i][0][:, k, 1, :], start=True, stop=False), [ZA[i][1], SC2[i][1]], [Bp])
                    P(lambda: nc.tensor.matmul(p3[:, k, :], lhsT=TM3[i][0][:, k, 2, :], rhs=SC3[i][0][:, k, :], start=False, stop=True), [TM3[i][1], SC3[i][1]], [Bp])
                for hf in range(2):
                    hs = slice(64 * hf, 64 * hf + 64)
                    A(lambda: nc.scalar.copy(out=Yfm[hs, 128 * u:128 * u + 128].rearrange("p (k t) -> p k t", k=2), in_=p3[hs, :, hs]), [Bp], [BY])
            drain(len(pending))
            for f in post:
                f()
            post = []
            for u in units:
                i = sl[u]; c0 = 2 * u
                for k in range(2):
                    def link(i=i, c=c0 + k, k=k):
                        pt, Bp = self.psum()
                        P(lambda: nc.tensor.matmul(pt[:, 0:128], lhsT=IGQ[i][0][:, k, 0, :], rhs=Sall[:, c, :], start=True, stop=True), [IGQ[i][1], BSall[c]], [Bp])
                        V(lambda: nc.vector.scalar_tensor_tensor(out=Sall[:, c + 1, :], in0=pt[:, 0:128], scalar=Dt[:, c:c + 1], in1=HD[i][0][:, k, :],
                                                                 op0=ALU.mult, op1=ALU.add), [Bp, BDt, HD[i][1]], [BSall[c + 1]])
                    pending.append(link)
                def ph8(i=i, u=u, c0=c0):
                    pt, Bp = self.psum(); p3 = pt[:, 0:256].rearrange("p (k t) -> p k t", k=2)
                    for k in range(2):
                        P(lambda: nc.tensor.matmul(p3[:, k, :], lhsT=Sall[:, c0 + k, :], rhs=IGQ[i][0][:, k, 1, :], start=True, stop=True), [BSall[c0 + k], IGQ[i][1]], [Bp])
                    for hf in range(2):
                        hs = slice(64 * hf, 64 * hf + 64)
                        yv = Yfm[hs, 128 * u:128 * u + 128].rearrange("p (k t) -> p k t", k=2)
                        V(lambda: nc.vector.tensor_tensor(out=yv, in0=p3[hs, :, hs], in1=yv, op=ALU.add), [Bp, BY], [BY])
                post.append(ph8)
        drain(len(pending))
        for f in post:
            f()

    def rwkv_epilogue(self, es, l, sq, hp, Yfm, BY, g16, Bg16, bon, Bbon):
        nc, S = self.nc, self.S
        V, A, P = self.V, self.A, self.P
        ym, Bym = self.dram["ymix"], self.dbuf["ymix"]
        Bc, Bv = self.Bconst, self.Bvecs
        t0 = sq * SEQ
        L = SEQ
        ta = self.tiles(es, "reA", [128, 512], F32, 2)
        tb_ = self.tiles(es, "reB", [128, 512], F32, 2)
        yo = self.tiles(es, "reO", [128, 512], BF16, 2)
        for tb in range(L // 512):
            cs = slice(tb * 512, tb * 512 + 512)
            a_, Ba = ta[tb % 2]; b_, Bb = tb_[tb % 2]; o_, Bo = yo[tb % 2]
            pt, Bp = self.psum()
            P(lambda: nc.tensor.matmul(pt[:], lhsT=self.ones_bd[:], rhs=Yfm[:, cs], start=True, stop=True), [Bc, BY], [Bp])
            V(lambda: nc.vector.scalar_tensor_tensor(out=a_[:], in0=pt[:], scalar=-1.0 / 64, in1=Yfm[:, cs], op0=ALU.mult, op1=ALU.add), [Bp, BY], [Ba])
            A(lambda: nc.scalar.activation(out=b_[:], in_=a_[:], func=AF.Square), [Ba], [Bb])
            pt, Bp = self.psum()
            P(lambda: nc.tensor.matmul(pt[:], lhsT=self.ones_bd[:], rhs=b_[:], start=True, stop=True), [Bc, Bb], [Bp])
            A(lambda: nc.scalar.activation(out=b_[:], in_=pt[:], func=AF.Sqrt, scale=1.0 / 64, bias=self.cst[:, 3:4]), [Bp, Bc], [Bb])
            V(lambda: nc.vector.reciprocal(out=b_[:], in_=b_[:]), [Bb], [Bb])
            V(lambda: nc.vector.tensor_tensor(out=a_[:], in0=a_[:], in1=b_[:], op=ALU.mult), [Ba, Bb], [Ba])
            V(lambda: nc.vector.tensor_scalar(out=a_[:], in0=a_[:], scalar1=self.vcol(l, "rw_ln_w", hp, 1), scalar2=self.vcol(l, "rw_ln_b", hp, 1), op0=ALU.mult, op1=ALU.add),
              [Ba, Bv], [Ba])
            V(lambda: nc.vector.tensor_tensor(out=a_[:], in0=a_[:], in1=bon[:, cs], op=ALU.add), [Ba, Bbon], [Ba])
            V(lambda: nc.vector.tensor_tensor(out=o_[:], in0=a_[:], in1=g16[:, cs], op=ALU.mult), [Ba, Bg16], [Bo])
            S.dma("sp", ym[0, 128 * hp:128 * hp + 128, t0 + tb * 512:t0 + tb * 512 + 512], o_[:], reads=[Bo], writes=[Bym])

    def load_w(self, wt, Bw_list, src3, Bsrc, kc_n, step=4):
        for k0 in range(0, kc_n, step):
            k1 = min(kc_n, k0 + step)
            self.S.dma("pool", wt[:, k0:k1, :], src3[:, k0:k1, :], reads=[Bsrc], writes=[Bw_list[k0 // step]])

    def outproj_res(self, actf, Bact, kc_n, w, Bw_of, xt, Bx, ntt, dst_ap, Bdst, tok0, kstep=4):
        nc, S = self.nc, self.S
        for j in range(ntt):
            for fh in range(2):
                pt, Bp = self.psum()
                for kc in range(kc_n):
                    ba = Bact(kc) if callable(Bact) else Bact
                    self.P(lambda: nc.tensor.matmul(pt[:], lhsT=actf(kc, j), rhs=w[:, kc, fh * 512:(fh + 1) * 512], start=(kc == 0), stop=(kc == kc_n - 1)),
                           [ba, Bw_of(kc)], [Bp])
                self.V(lambda: nc.vector.tensor_tensor(out=xt[:, j, fh * 512:(fh + 1) * 512], in0=pt[:], in1=xt[:, j, fh * 512:(fh + 1) * 512], op=ALU.add),
                       [Bp, Bx[j]], [Bx[j]])
            S.dma("sp", dst_ap[tok0 + j * 128:tok0 + (j + 1) * 128, :], xt[:, j, :], reads=[Bx[j]], writes=[Bdst])

    def xsrc(self, l):
        return (self.dram["x"], self.dbuf["x"]) if l == 0 else (self.dram["xres"], self.dbuf["xres"])

    def stage_merge(self, l):
        nc, S = self.nc, self.S
        V, A, P = self.V, self.A, self.P
        pT, BpT = self.dram["pT"], self.dbuf["pT"]
        ym, Bym = self.dram["ymix"], self.dbuf["ymix"]
        xs_ap, Bxs = self.xsrc(l)
        xd_ap, Bxd = self.dram["xres"], self.dbuf["xres"]
        L = SEQ
        with ExitStack() as es:
            wb = es.enter_context(self.sbuf("mgwb", [128, 12, D], BF16)); Bwb = [Buf() for _ in range(3)]
            wo = es.enter_context(self.sbuf("mgwo", [128, 8, D], BF16)); Bwo = [Buf() for _ in range(2)]
            for i in range(3):
                S.dma("pool", wb[:, 4 * i:4 * i + 4, :], self.dram["w_branch"][l, i].rearrange("(k p) c -> p k c", p=128), reads=[self.dbuf["w_branch"]], writes=[Bwb[i]])
            self.load_w(wo, Bwo, self.dram["w_out"][l].rearrange("(k p) c -> p k c", p=128), self.dbuf["w_out"], 8)
            yt = es.enter_context(self.sbuf("mgy", [128, 12, L], BF16)); Byt = [Buf() for _ in range(12)]
            mg = es.enter_context(self.sbuf("mgm", [128, 8, L], BF16)); Bmg = Buf()
            gts = self.tiles(es, "mgg", [128, 3, 512], BF16, 2)
            acc = self.tiles(es, "mga", [128, 512], F32, 2)
            tmp = self.tiles(es, "mgt", [128, 512], F32, 2)
            xts = [(es.enter_context(self.sbuf("mgx%d" % i, [128, 4, D], F32)), [Buf() for _ in range(4)]) for i in range(2)]
            for sq in range(NSEQ):
                t0 = sq * L
                for i in range(3):
                    for kt in range(4):
                        S.dma("sp", yt[:, 4 * i + kt, :], ym[i, 128 * kt:128 * kt + 128, t0:t0 + L], reads=[Bym], writes=[Byt[4 * i + kt]])
                cnt = 0
                for ft in range(8):
                    for tb in range(L // 512):
                        cs = slice(tb * 512, tb * 512 + 512)
                        g_, Bg = gts[cnt % 2]; a_, Ba = acc[cnt % 2]; t_, Bt = tmp[cnt % 2]; cnt += 1
                        S.dma("sp", g_[:], pT[GT0 + 128 * ft:GT0 + 3072:1024, t0 + tb * 512:t0 + tb * 512 + 512].rearrange("(i p) t -> p i t", p=128) if False else
                              bass.AP(tensor=pT.tensor, offset=pT[GT0 + 128 * ft, t0 + tb * 512].offset, ap=[[T, 128], [1024 * T, 3], [1, 512]]),
                              reads=[BpT], writes=[Bg])
                        for i in range(3):
                            pt, Bp = self.psum()
                            for kt in range(4):
                                P(lambda: nc.tensor.matmul(pt[:], lhsT=wb[:, 4 * i + kt, ft * 128:(ft + 1) * 128], rhs=yt[:, 4 * i + kt, cs], start=(kt == 0), stop=(kt == 3)),
                                  [Bwb[i], Byt[4 * i + kt]], [Bp])
                            if i == 0:
                                V(lambda: nc.vector.tensor_tensor(out=a_[:], in0=pt[:], in1=g_[:, 0, :], op=ALU.mult), [Bp, Bg], [Ba])
                            else:
                                V(lambda: nc.vector.tensor_tensor(out=t_[:], in0=pt[:], in1=g_[:, i, :], op=ALU.mult), [Bp, Bg], [Bt])
                                if i == 1:
                                    V(lambda: nc.vector.tensor_tensor(out=a_[:], in0=a_[:], in1=t_[:], op=ALU.add), [Ba, Bt], [Ba])
                                else:
                                    V(lambda: nc.vector.tensor_tensor(out=mg[:, ft, cs], in0=a_[:], in1=t_[:], op=ALU.add), [Ba, Bt], [Bmg])
                def ldx(blk):
                    xt, Bx = xts[blk % 2]
                    tok0 = t0 + blk * 512
                    for j in range(4):
                        S.dma("sp", xt[:, j, :], xs_ap[tok0 + j * 128:tok0 + (j + 1) * 128, :], reads=[Bxs], writes=[Bx[j]])
                ldx(0)
                for blk in range(L // 512):
                    xt, Bx = xts[blk % 2]
                    tok0 = t0 + blk * 512
                    if blk + 1 < L // 512:
                        ldx(blk + 1)
                    self.outproj_res(lambda kc, j: mg[:, kc, blk * 512 + j * 128:blk * 512 + (j + 1) * 128], Bmg, 8, wo, lambda kc: Bwo[kc // 4], xt, Bx, 4, xd_ap, Bxd, tok0)
                S.barrier()

    def stage_memn(self):
        nc, S = self.nc, self.S
        self.memn = nc.alloc_sbuf_tensor("memn", [128, 8, NSEQ * NMEM], BF16); self.Bmemn = Buf("memn")
        with ExitStack() as es:
            self.rmsnorm_fm(es, self.dram["mem"], self.dbuf["mem"], NSEQ * NMEM, self.gvec[:, 0:8], self.memn, self.Bmemn, self.eps6[:, 0:1])
            S.barrier()

    def stage_xattn(self, l):
        nc, S = self.nc, self.S
        V, A, P = self.V, self.A, self.P
        Bc = self.Bconst
        xs_ap, Bxs = self.dram["xres"], self.dbuf["xres"]
        NM = NSEQ * NMEM
        with ExitStack() as es:
            kT = es.enter_context(self.sbuf("xakT", [128, 8, NM], BF16)); BkT = Buf()
            vtm = es.enter_context(self.sbuf("xavtm", [128, NM // 128, D], BF16)); Bvtm = Buf()
            wq = es.enter_context(self.sbuf("xawq", [128, 8, D], BF16)); Bwq = [Buf() for _ in range(2)]
            wo = es.enter_context(self.sbuf("xawo", [128, 8, D], BF16)); Bwo = [Buf() for _ in range(2)]
            ones_b = es.enter_context(self.sbuf("xaones", [128, 128], BF16)); Bob = Buf()
            V(lambda: nc.vector.memset(ones_b[:], 1.0), [], [Bob])
            with ExitStack() as e2:
                wts = [(e2.enter_context(self.sbuf("xawkv%d" % i, [128, 8, 512], BF16)), [Buf(), Buf()]) for i in range(2)]
                wkv = self.dram["xa_wkv"][l]
                for g in range(4):
                    wt, Bw = wts[g % 2]
                    self.load_w(wt, Bw, wkv[:, g * 512:(g + 1) * 512].rearrange("(k p) c -> p k c", p=128), self.dbuf["xa_wkv"], 8)
                    if g < 2:
                        for m in range(4):
                            pt, Bp = self.psum()
                            for kc in range(8):
                                P(lambda: nc.tensor.matmul(pt[:, 0:NM], lhsT=wt[:, kc, m * 128:(m + 1) * 128], rhs=self.memn[:, kc, :], start=(kc == 0), stop=(kc == 7)),
                                  [Bw[kc // 4], self.Bmemn], [Bp])
                            A(lambda: nc.scalar.copy(out=kT[:, g * 4 + m, :], in_=pt[:, 0:NM]), [Bp], [BkT])
                    else:
                        for mt in range(NM // 128):
                            pt, Bp = self.psum()
                            for kc in range(8):
                                P(lambda: nc.tensor.matmul(pt[:], lhsT=self.memn[:, kc, mt * 128:(mt + 1) * 128], rhs=wt[:, kc, :], start=(kc == 0), stop=(kc == 7)),
                                  [Bw[kc // 4], self.Bmemn], [Bp])
                            A(lambda: nc.scalar.copy(out=vtm[:, mt, (g - 2) * 512:(g - 1) * 512], in_=pt[:]), [Bp], [Bvtm])
                S.barrier()
            self.load_w(wq, Bwq, self.dram["xa_wq"][l].rearrange("(k p) c -> p k c", p=128), self.dbuf["xa_wq"], 8)
            self.load_w(wo, Bwo, self.dram["xa_wo"][l].rearrange("(k p) c -> p k c", p=128), self.dbuf["xa_wo"], 8)
            xts = [(es.enter_context(self.sbuf("xax%d" % i, [128, 4, D], F32)), [Buf() for _ in range(4)]) for i in range(2)]
            hTs = self.tiles(es, "xah", [128, 8, 512], BF16, 2)
            qTs = self.tiles(es, "xaq", [128, 2, 512], BF16, 2)
            Es = self.tiles(es, "xaE", [128, 2, 512], BF16, 2)
            rd = self.tiles(es, "xard", [128, 512], F32, 2)
            oTs = self.tiles(es, "xao", [128, 8, 512], BF16, 2)
            xss = self.tiles(es, "xaxs", [128, D], BF16, 2)
            junk = es.enter_context(self.sbuf("xajunk", [128, D], BF16)); Bjunk = Buf()
            st = self.tiles(es, "xast", [128, 4], F32, 2)
            cnt = 0
            hc = 0
            for blk in range(T // 512):
                sq = (blk * 512) // SEQ
                tok0 = blk * 512
                xt, Bx = xts[blk % 2]
                hT, BhT = hTs[blk % 2]
                oT, BoT = oTs[blk % 2]
                if blk == 0:
                    for j in range(4):
                        S.dma("sp", xt[:, j, :], xs_ap[tok0 + j * 128:tok0 + (j + 1) * 128, :], reads=[Bxs], writes=[Bx[j]])
                if blk + 1 < T // 512:
                    xtn, Bxn = xts[(blk + 1) % 2]
                    for j in range(4):
                        S.dma("sp", xtn[:, j, :], xs_ap[tok0 + 512 + j * 128:tok0 + 512 + (j + 1) * 128, :], reads=[Bxs], writes=[Bxn[j]])
                for j in range(4):
                    s_, Bs = st[cnt % 2]; xs, Bxs_ = xss[cnt % 2]; cnt += 1
                    A(lambda: nc.scalar.activation(out=junk[:], in_=xt[:, j, :], func=AF.Square, accum_out=s_[:, 0:1]), [Bx[j]], [Bjunk, Bs])
                    A(lambda: nc.scalar.activation(out=s_[:, 1:2], in_=s_[:, 0:1], func=AF.Sqrt, scale=1.0 / D, bias=self.eps6[:, 0:1]), [Bs, Bc], [Bs])
                    V(lambda: nc.vector.reciprocal(out=s_[:, 2:3], in_=s_[:, 1:2]), [Bs], [Bs])
                    V(lambda: nc.vector.tensor_scalar(out=xs[:], in0=xt[:, j, :], scalar1=s_[:, 2:3], scalar2=None, op0=ALU.mult), [Bx[j], Bs], [Bxs_])
                    pt, Bp = self.psum()
                    pb = pt[:].bitcast(BF16)
                    for kc in range(8):
                        P(lambda: nc.tensor.transpose(out=pb[:, kc * 128:(kc + 1) * 128], in_=xs[:, kc * 128:(kc + 1) * 128], identity=self.ident_b[:]), [Bxs_, Bc], [Bp])
                    V(lambda: nc.vector.tensor_tensor(out=hT[:, :, j * 128:(j + 1) * 128], in0=pb.rearrange("p (k t) -> p k t", k=8),
                                                      in1=self.vcol(l, "norm_xattn", 0, 8).unsqueeze(2).to_broadcast([128, 8, 128]), op=ALU.mult), [Bp, self.Bvecs], [BhT])
                for h in range(4):
                    qT, BqT = qTs[hc % 2]; E, BE = Es[hc % 2]; r_, Br = rd[hc % 2]; hc += 1
                    for dt in range(2):
                        pt, Bp = self.psum()
                        c0 = h * 256 + dt * 128
                        for kc in range(8):
                            P(lambda: nc.tensor.matmul(pt[:], lhsT=wq[:, kc, c0:c0 + 128], rhs=hT[:, kc, :], start=(kc == 0), stop=(kc == 7)), [Bwq[kc // 4], BhT], [Bp])
                        A(lambda: nc.scalar.copy(out=qT[:, dt, :], in_=pt[:]), [Bp], [BqT])
                    for mt in range(2):
                        ms = slice(sq * NMEM + mt * 128, sq * NMEM + (mt + 1) * 128)
                        pt, Bp = self.psum()
                        for dt in range(2):
                            P(lambda: nc.tensor.matmul(pt[:], lhsT=kT[:, h * 2 + dt, ms], rhs=qT[:, dt, :], start=(dt == 0), stop=(dt == 1)), [BkT, BqT], [Bp])
                        A(lambda: nc.scalar.activation(out=E[:, mt, :], in_=pt[:], func=AF.Exp, scale=1.0 / 16), [Bp], [BE])
                    pt, Bp = self.psum()
                    for mt in range(2):
                        P(lambda: nc.tensor.matmul(pt[:], lhsT=ones_b[:], rhs=E[:, mt, :], start=(mt == 0), stop=(mt == 1)), [Bob, BE], [Bp])
                    V(lambda: nc.vector.reciprocal(out=r_[:], in_=pt[:]), [Bp], [Br])
                    for dt in range(2):
                        pt, Bp = self.psum()
                        c0 = h * 256 + dt * 128
                        for mt in range(2):
                            P(lambda: nc.tensor.matmul(pt[:], lhsT=vtm[:, sq * 2 + mt, c0:c0 + 128], rhs=E[:, mt, :], start=(mt == 0), stop=(mt == 1)), [Bvtm, BE], [Bp])
                        V(lambda: nc.vector.tensor_tensor(out=oT[:, h * 2 + dt, :], in0=pt[:], in1=r_[:], op=ALU.mult), [Bp, Br], [BoT])
                self.outproj_res(lambda kc, j: oT[:, kc, j * 128:(j + 1) * 128], BoT, 8, wo, lambda kc: Bwo[kc // 4], xt, Bx, 4, xs_ap, Bxs, tok0)
            S.barrier()

    def stage_ffn(self, l):
        nc, S = self.nc, self.S
        V, A, P = self.V, self.A, self.P
        Bc, Bv = self.Bconst, self.Bvecs
        xs_ap, Bxs = self.dram["xres"], self.dbuf["xres"]
        aT, BaT = self.dram["actT"], self.dbuf["actT"]
        L = SEQ
        NP_ = DFF // 128
        with ExitStack() as es:
            hT = es.enter_context(self.sbuf("ffh", [128, 8, T], BF16)); BhT = Buf("ffh")
            with ExitStack() as e2:
                self.rmsnorm_fm(e2, xs_ap, Bxs, T, self.vcol(l, "norm_ffn", 0, 8), hT, BhT, self.eps6[:, 0:1])
                S.barrier()
            with ExitStack() as e2:
                wts = [(e2.enter_context(self.sbuf("ffw%d" % i, [128, 8, 256], BF16)), [Buf(), Buf()]) for i in range(2)]
                ug = self.tiles(e2, "ffug", [128, L + 2], BF16, 3)
                uv = self.tiles(e2, "ffuv", [128, L + 2], BF16, 3)
                dgs = self.tiles(e2, "ffdg", [128, 2, 3, 128], BF16, 2)
                sg = self.tiles(e2, "ffsg", [128, 512], F32, 4)
                ao = self.tiles(e2, "ffao", [128, L], BF16, 2)
                for t_, B_ in ug + uv:
                    V(lambda: nc.vector.memset(t_[:, 0:2], 0.0), [], [B_])
                wup = self.dram["ffn_w_up"][l]
                cnt = 0
                sc_ = 0
                for i in range(NP_):
                    wt, Bw = wts[i % 2]
                    dg, Bdg = dgs[i % 2]
                    S.dma("pool", wt[:, :, 0:128], wup[:, 128 * i:128 * i + 128].rearrange("(k p) c -> p k c", p=128), reads=[self.dbuf["ffn_w_up"]], writes=[Bw[0]])
                    S.dma("pool", wt[:, :, 128:256], wup[:, DFF + 128 * i:DFF + 128 * i + 128].rearrange("(k p) c -> p k c", p=128), reads=[self.dbuf["ffn_w_up"]], writes=[Bw[1]])
                    for half in range(2):
                        col = i + half * NP_
                        for j in range(3):
                            V(lambda: nc.vector.tensor_scalar(out=dg[:, half, j, :], in0=self.ident_f[:], scalar1=self.vcol(l, "ffn_conv%d" % j, col, 1), scalar2=None, op0=ALU.mult),
                              [Bc, Bv], [Bdg])
                    for sq in range(NSEQ):
                        g_, Bg = ug[cnt % 3]; v_, Bvv = uv[cnt % 3]; a_, Ba = ao[cnt % 2]
                        cnt += 1
                        for tb in range(L // 512):
                            ts = slice(sq * L + tb * 512, sq * L + tb * 512 + 512)
                            for half, (dst, Bd) in enumerate([(g_, Bg), (v_, Bvv)]):
                                pt, Bp = self.psum()
                                for kc in range(8):
                                    P(lambda: nc.tensor.matmul(pt[:], lhsT=wt[:, kc, half * 128:(half + 1) * 128], rhs=hT[:, kc, ts], start=(kc == 0), stop=(kc == 7)),
                                      [Bw[half], BhT], [Bp])
                                A(lambda: nc.scalar.copy(out=dst[:, 2 + tb * 512:2 + tb * 512 + 512], in_=pt[:]), [Bp], [Bd])
                        for tb in range(L // 512):
                            s_, Bs = sg[sc_ % 4]; sc_ += 1
                            ptg, Bpg = self.psum()
                            for j in range(3):
                                P(lambda: nc.tensor.matmul(ptg[:], lhsT=dg[:, 0, j, :], rhs=g_[:, tb * 512 + j:tb * 512 + j + 512], start=(j == 0), stop=(j == 2)), [Bdg, Bg], [Bpg])
                            ptv, Bpv = self.psum()
                            for j in range(3):
                                P(lambda: nc.tensor.matmul(ptv[:], lhsT=dg[:, 1, j, :], rhs=v_[:, tb * 512 + j:tb * 512 + j + 512], start=(j == 0), stop=(j == 2)), [Bdg, Bvv], [Bpv])
                            A(lambda: nc.scalar.activation(out=s_[:], in_=ptg[:], func=AF.Sigmoid, bias=self.vcol(l, "ffn_conv_b", i, 1)), [Bpg, Bv], [Bs])
                            V(lambda: nc.vector.scalar_tensor_tensor(out=s_[:], in0=ptg[:], scalar=self.vcol(l, "ffn_conv_b", i, 1), in1=s_[:], op0=ALU.add, op1=ALU.mult),
                              [Bpg, Bs, Bv], [Bs])
                            V(lambda: nc.vector.scalar_tensor_tensor(out=a_[:, tb * 512:tb * 512 + 512], in0=ptv[:], scalar=self.vcol(l, "ffn_conv_b", i + NP_, 1), in1=s_[:],
                                                                     op0=ALU.add, op1=ALU.mult), [Bpv, Bs, Bv], [Ba])
                        S.dma("sp", aT[128 * i:128 * i + 128, sq * L:(sq + 1) * L], a_[:], reads=[Ba], writes=[BaT])
                S.barrier()
        with ExitStack() as es:
            wd = es.enter_context(self.sbuf("ffwd", [128, NP_, D], BF16)); Bwd = [Buf() for _ in range((NP_ + 3) // 4)]
            self.load_w(wd, Bwd, self.dram["ffn_w_down"][l].rearrange("(k p) c -> p k c", p=128), self.dbuf["ffn_w_down"], NP_)
            xts = [(es.enter_context(self.sbuf("ffx%d" % i, [128, 4, D], F32)), [Buf() for _ in range(4)]) for i in range(2)]
            ats = [(es.enter_context(self.sbuf("ffat%d" % i, [128, NP_, 512], BF16)), [Buf(), Buf()]) for i in range(2)]
            def ldb(blk):
                tok0 = blk * 512
                xt, Bx = xts[blk % 2]
                at, Bat = ats[blk % 2]
                S.dma("sp", at[:, 0:11, :], aT[0:11 * 128, tok0:tok0 + 512].rearrange("(k p) t -> p k t", p=128), reads=[BaT], writes=[Bat[0]])
                S.dma("sp", at[:, 11:22, :], aT[11 * 128:22 * 128, tok0:tok0 + 512].rearrange("(k p) t -> p k t", p=128), reads=[BaT], writes=[Bat[1]])
                for j in range(4):
                    S.dma("sp", xt[:, j, :], xs_ap[tok0 + j * 128:tok0 + (j + 1) * 128, :], reads=[Bxs], writes=[Bx[j]])
            ldb(0)
            for blk in range(T // 512):
                tok0 = blk * 512
                xt, Bx = xts[blk % 2]
                at, Bat = ats[blk % 2]
                if blk + 1 < T // 512:
                    ldb(blk + 1)
                self.outproj_res(lambda kc, j: at[:, kc, j * 128:(j + 1) * 128], (lambda kc: Bat[0 if kc < 11 else 1]), NP_, wd, lambda kc: Bwd[kc // 4], xt, Bx, 4, xs_ap, Bxs, tok0)
            S.barrier()

    def stage_final(self):
        nc, S = self.nc, self.S
        V, A, P = self.V, self.A, self.P
        Bc = self.Bconst
        xs_ap, Bxs = self.dram["xres"], self.dbuf["xres"]
        with ExitStack() as es:
            gbc = es.enter_context(self.sbuf("fngb", [128, D], F32)); Bg = Buf()
            S.dma("sp", gbc[:], self.dram["norm_final_bc"], reads=[self.dbuf["norm_final_bc"]], writes=[Bg])
            xts = self.tiles(es, "fnx", [128, D], F32, 3)
            junk = es.enter_context(self.sbuf("fnjunk", [128, D], BF16)); Bjunk = Buf()
            st = self.tiles(es, "fnst", [128, 4], F32, 2)
            for tt in range(T // 128):
                xt, Bx = xts[tt % 3]
                s_, Bs = st[tt % 2]
                S.dma("sp", xt[:], xs_ap[tt * 128:(tt + 1) * 128, :], reads=[Bxs], writes=[Bx])
                A(lambda: nc.scalar.activation(out=junk[:], in_=xt[:], func=AF.Square, accum_out=s_[:, 0:1]), [Bx], [Bjunk, Bs])
                A(lambda: nc.scalar.activation(out=s_[:, 1:2], in_=s_[:, 0:1], func=AF.Sqrt, scale=1.0 / D, bias=self.eps6[:, 0:1]), [Bs, Bc], [Bs])
                V(lambda: nc.vector.reciprocal(out=s_[:, 2:3], in_=s_[:, 1:2]), [Bs], [Bs])
                V(lambda: nc.vector.scalar_tensor_tensor(out=xt[:], in0=xt[:], scalar=s_[:, 2:3], in1=gbc[:], op0=ALU.mult, op1=ALU.mult), [Bx, Bs, Bg], [Bx])
                S.dma("sp", self.out[tt * 128:(tt + 1) * 128, :], xt[:], reads=[Bx], writes=[self.Bout])
            S.barrier()

    def finish(self):
        S = self.S
        S.barrier()
        print("ops", S.n_ops, "waits", S.n_waits, "sems", S.nsem + S.n_dma_sems)


def build(debug=None, n_layers=DEPTH, final=True):
    k = K(debug, n_layers)
    k.stage_memn()
    for l in range(n_layers):
        k.stage_mixproj(l)
        for sq in range(NSEQ):
            k.stage_rwkv(l, sq)
        k.stage_s5(l)
        for sq in range(NSEQ):
            k.stage_gla(l, sq)
        k.stage_merge(l)
        k.stage_xattn(l)
        k.stage_ffn(l)
    if final:
        k.stage_final()
    k.finish()
    return k


def make_in_maps(inp):
    vecs, gvec = pack_vecs(inp)
    s5w = pack_s5(inp)
    maps = []
    for c in range(NCORES):
        m = {
            "x": np.ascontiguousarray(inp["x"][NSEQ * c:NSEQ * (c + 1)].reshape(T, D)),
            "mem": np.ascontiguousarray(inp["mem"][NSEQ * c:NSEQ * (c + 1)].reshape(NSEQ * NMEM, D)),
            "vecs": vecs, "gvec": gvec,
            "w_in": inp["w_in"],
            "gla_gk_up": inp["gla_gk_up"],
            "s5w": s5w, "s5_w_glu": inp["s5_w_glu"],
            "w_branch": inp["w_branch"], "w_out": inp["w_out"], "xa_wq": inp["xa_wq"], "xa_wkv": inp["xa_wkv"], "xa_wo": inp["xa_wo"],
            "ffn_w_up": inp["ffn_w_up"], "ffn_w_down": inp["ffn_w_down"],
            "norm_final_bc": np.ascontiguousarray(np.broadcast_to(np.asarray(inp["norm_final"], np.float32)[None, :], (128, D))),
            "rw_w_up": inp["rw_w_up"], "rw_a_up": inp["rw_a_up"], "rw_g_up": inp["rw_g_up"],
        }
        maps.append(m)
    return maps


def kernel(**inp):
    inp = {k: np.asarray(v) for k, v in inp.items()}
    k = build()
    res = run_bass_kernel_spmd(k.nc, make_in_maps(inp), core_ids=list(range(NCORES)))
    out = np.concatenate([r["out"].reshape(NSEQ, SEQ, D) for r in res.results], axis=0)
    return out.astype(np.float32)
```

```python
import numpy as np
from contextlib import ExitStack
import concourse.bass as bass
import concourse.mybir as mybir
from concourse.bass_utils import run_bass_kernel_spmd

F32 = mybir.dt.float32
BF16 = mybir.dt.bfloat16
ALU = mybir.AluOpType
AF = mybir.ActivationFunctionType
AX = mybir.AxisListType

NCORES = 8
DEPTH = 4
D = 1024
SEQ = 2048
NSEQ = 2
T = NSEQ * SEQ
NMEM = 256
N_IN = 6928
RW0, S50, GL0, GT0 = 0, 1792, 2304, 3856
DFF = 2816
EPOCH = 30000


class Buf:
    __slots__ = ("name", "lw", "rd")

    def __init__(self, name=""):
        self.name = name
        self.lw = None
        self.rd = {}


class Sched:
    def __init__(self, nc):
        self.nc = nc
        self.E = {"pe": nc.tensor, "dve": nc.vector, "act": nc.scalar, "pool": nc.gpsimd, "sp": nc.sync}
        self.sems = {}
        self.cur = {}
        self.nsem = 0
        for e in self.E:
            self._new_epoch(e)
        self.seen = {e: {} for e in self.E}
        self.dma_pools = {}
        self.dma_rr = {}
        nsd = 0
        for q, n in (("sp", 36), ("pool", 16), ("act", 8)):
            pl = []
            for i in range(n):
                k = "d%s%d" % (q, i)
                self.sems[k] = nc.alloc_semaphore("sd%d" % nsd)
                nsd += 1
                pl.append([k, 0])
            self.dma_pools[q] = pl
            self.dma_rr[q] = 0
        self.n_dma_sems = nsd
        self.n_ops = 0
        self.n_waits = 0

    def _new_epoch(self, e):
        k = "%s_%d" % (e, self.nsem)
        self.nsem += 1
        self.sems[k] = self.nc.alloc_semaphore("s" + k)
        self.cur[e] = [k, 0]

    def _wait(self, eng, deps):
        best = {}
        for d in deps:
            if d is None:
                continue
            k, v, de = d
            if de == eng and eng == "pe":
                continue
            if best.get(k, 0) < v:
                best[k] = v
        for k, v in best.items():
            if self.seen[eng].get(k, 0) >= v:
                continue
            self.E[eng].wait_ge(self.sems[k], v)
            self.seen[eng][k] = v
            self.n_waits += 1

    @staticmethod
    def _deps(reads, writes):
        deps = []
        for b in reads:
            deps.append(b.lw)
        for b in writes:
            deps.append(b.lw)
            deps.extend(b.rd.values())
        return deps

    def op(self, eng, fn, reads=(), writes=()):
        self._wait(eng, self._deps(reads, writes))
        ins = fn()
        c = self.cur[eng]
        c[1] += 1
        ins.then_inc(self.sems[c[0]], 1)
        t = (c[0], c[1], eng)
        for b in reads:
            b.rd[eng] = t
        for b in writes:
            b.lw = t
            b.rd = {}
        if c[1] >= EPOCH:
            self._new_epoch(eng)
        self.n_ops += 1
        return t

    def dma(self, q, out, in_, reads=(), writes=(), **kw):
        pl = self.dma_pools[q]
        slot = self.dma_rr[q]
        self.dma_rr[q] = (slot + 1) % len(pl)
        s = pl[slot]
        deps = self._deps(reads, writes)
        if s[1] > 0:
            deps.append((s[0], s[1], "dma"))
        self._wait(q, deps)
        s[1] += 16
        self.E[q].dma_start(out=out, in_=in_, **kw).then_inc(self.sems[s[0]], 16)
        t = (s[0], s[1], "dma")
        for b in reads:
            b.rd["dma_%s%d" % (q, slot)] = t
        for b in writes:
            b.lw = t
            b.rd = {}
        self.n_ops += 1
        return t

    def barrier(self):
        deps = []
        for e, c in self.cur.items():
            if c[1] > 0:
                deps.append((c[0], c[1], "x"))
        for pl in self.dma_pools.values():
            for k, v in pl:
                if v > 0:
                    deps.append((k, v, "x"))
        for e in self.E:
            self._wait(e, deps)


VEC_SPECS = [
    ("norm_mix", 1024), ("norm_xattn", 1024), ("norm_ffn", 1024),
    ("rw_mu", 1792), ("rw_w0", 512), ("rw_a0", 512), ("rw_k_k", 512), ("rw_k_a", 512), ("rw_r_k", 512),
    ("rw_ln_w", 512), ("rw_ln_b", 512),
    ("s5_b_glu", 512), ("s5_d", 512),
    ("gla_gk_b", 256), ("gla_norm", 128),
    ("ffn_conv0", 5632), ("ffn_conv1", 5632), ("ffn_conv2", 5632), ("ffn_conv_b", 5632),
]
VEC_OFF = {}
_o = 0
for _n, _l in VEC_SPECS:
    VEC_OFF[_n] = _o
    _o += _l // 128
NVEC = _o
GVEC_OFF = {"norm_mem": 0, "norm_final": 8}
NGVEC = 24


def pack_vecs(inp):
    v = np.zeros((DEPTH, 128, NVEC), np.float32)
    for l in range(DEPTH):
        for n, ln in VEC_SPECS:
            if n.startswith("ffn_conv") and n != "ffn_conv_b":
                a = inp["ffn_conv"][l, int(n[-1])]
            else:
                a = inp[n][l]
            a = np.asarray(a, np.float32).reshape(-1)
            v[l, :, VEC_OFF[n]:VEC_OFF[n] + ln // 128] = a.reshape(ln // 128, 128).T
    g = np.zeros((128, NGVEC), np.float32)
    g[:, 0:8] = np.asarray(inp["norm_mem"], np.float32).reshape(8, 128).T
    g[:, 8:16] = np.asarray(inp["norm_final"], np.float32).reshape(8, 128).T
    pp = np.arange(128)
    g[:, 16] = ((pp // 16) % 2 == 0)
    g[:, 17] = ((pp // 16) % 2 == 1)
    g[:, 18] = np.pi / 2
    g[:, 19] = (pp >= 96)
    return np.ascontiguousarray(v.transpose(1, 0, 2)), g


S5X = 3124
oAC, oBC, oLC, oAP, oLP, oBP, oCP = 0, 512, 1024, 1028, 1060, 1076, 2100


def pack_s5(inp):
    out = np.zeros((128, DEPTH, S5X), np.float32)
    for l in range(DEPTH):
        a = np.stack([inp["s5_a_re"][l], inp["s5_a_im"][l]]).astype(np.float32)
        b = np.stack([inp["s5_b_re"][l], inp["s5_b_im"][l]]).astype(np.float32)
        c = np.stack([inp["s5_c_re"][l], inp["s5_c_im"][l]]).astype(np.float32)
        ls = np.asarray(inp["s5_log_step"][l], np.float32)
        aC = np.broadcast_to(a.reshape(2, 4, 8, 64).transpose(2, 1, 0, 3)[:, None], (8, 16, 4, 2, 64)).reshape(128, 512)
        bC = b.reshape(2, 4, 8, 64, 16).transpose(2, 4, 1, 0, 3).reshape(128, 512)
        lC = np.broadcast_to(ls.reshape(4, 8).T[:, None, :], (8, 16, 4)).reshape(128, 4)
        aP = a.reshape(2, 16, 2, 64).transpose(2, 3, 1, 0).reshape(128, 32)
        lP = np.broadcast_to(ls.reshape(16, 2).T[:, None, :], (2, 64, 16)).reshape(128, 16)
        bP = np.zeros((2, 64, 16, 2, 2, 16), np.float32)
        cP = np.zeros((2, 64, 16, 2, 2, 16), np.float32)
        b5 = b.reshape(2, 16, 2, 64, 16)
        c5 = c.reshape(2, 16, 2, 16, 64)
        for bb in range(2):
            bP[bb, :, :, :, bb, :] = b5[:, :, bb].transpose(2, 1, 0, 3)
            cP[bb, :, :, :, bb, :] = c5[:, :, bb].transpose(3, 1, 0, 2)
        out[:, l, oAC:oAC + 512] = aC
        out[:, l, oBC:oBC + 512] = bC
        out[:, l, oLC:oLC + 4] = lC
        out[:, l, oAP:oAP + 32] = aP
        out[:, l, oLP:oLP + 16] = lP
        out[:, l, oBP:oBP + 1024] = bP.reshape(128, 1024)
        out[:, l, oCP:oCP + 1024] = cP.reshape(128, 1024)
    return out


class K:
    def __init__(self, debug=None, n_layers=DEPTH):
        self.debug = debug or {}
        self.n_layers = n_layers
        nc = self.nc = bass.Bass("TRN2", target_bir_lowering=False)
        self.S = Sched(nc)
        self.uid = 0
        self.dram = {}
        self.dbuf = {}
        di = self.dram_in
        di("x", [T, D]); di("mem", [NSEQ * NMEM, D]); di("vecs", [128, DEPTH, NVEC]); di("gvec", [128, NGVEC])
        di("w_in", [DEPTH, D, N_IN])
        di("gla_gk_up", [DEPTH, 16, 256])
        di("s5w", [128, DEPTH, S5X]); di("s5_w_glu", [DEPTH, 512, 512])
        di("w_branch", [DEPTH, 3, 512, D]); di("w_out", [DEPTH, D, D])
        di("xa_wq", [DEPTH, D, D]); di("xa_wkv", [DEPTH, D, 2 * D]); di("xa_wo", [DEPTH, D, D])
        di("ffn_w_up", [DEPTH, D, 2 * DFF]); di("ffn_w_down", [DEPTH, DFF, D]); di("norm_final_bc", [128, D])
        di("rw_w_up", [DEPTH, 64, 512]); di("rw_a_up", [DEPTH, 64, 512]); di("rw_g_up", [DEPTH, 128, 512])
        self.out = nc.dram_tensor("out", [T, D], F32, kind="ExternalOutput").ap()
        self.Bout = Buf("out")
        self.scr("xres", [T, D], F32)
        self.scr("pT", [N_IN, T], BF16)
        self.scr("ymix", [3, 512, T], BF16)
        self.scr("actT", [DFF, T], BF16)
        self.ps = []
        for i in range(8):
            t = nc.alloc_psum_tensor("ps%d" % i, [128, 512], F32)
            self.ps.append((t, Buf("ps%d" % i)))
        self.ps_rr = 0
        self.ev_rr = 0
        self.consts()

    def dram_in(self, name, shape, dt=F32):
        self.dram[name] = self.nc.dram_tensor(name, shape, dt, kind="ExternalInput").ap()
        self.dbuf[name] = Buf(name)

    def scr(self, name, shape, dt):
        kind = "ExternalOutput" if name in self.debug else "Internal"
        self.dram[name] = self.nc.dram_tensor(name, shape, dt, kind=kind).ap()
        self.dbuf[name] = Buf(name)

    def psum(self):
        t, b = self.ps[self.ps_rr]
        self.ps_rr = (self.ps_rr + 1) % 8
        return t, b

    def ev_eng(self):
        self.ev_rr ^= 1
        return "act" if self.ev_rr else "dve"

    def consts(self):
        nc, S = self.nc, self.S
        self.vecs = nc.alloc_sbuf_tensor("vecs_sb", [128, DEPTH, NVEC], F32)
        self.Bvecs = Buf("vecs")
        if "delay" in self.debug:
            dj = nc.alloc_sbuf_tensor("dly", [128, 512], F32)
            Bd = Buf()
            for i in range(60):
                S.op("pool", lambda: nc.gpsimd.memset(dj[:], 0.0), writes=[Bd])
            S.barrier()
        S.dma("sp", self.vecs[:], self.dram["vecs"], writes=[self.Bvecs])
        self.gvec = nc.alloc_sbuf_tensor("gvec_sb", [128, NGVEC], F32)
        S.dma("sp", self.gvec[:], self.dram["gvec"], writes=[self.Bvecs])
        self.ident_f = nc.alloc_sbuf_tensor("ident_f", [128, 128], F32)
        self.ident_b = nc.alloc_sbuf_tensor("ident_b", [128, 128], BF16)
        self.Bconst = Buf("const")
        self.eps6 = nc.alloc_sbuf_tensor("eps6", [128, 1], F32)
        S.op("pool", lambda: nc.gpsimd.memset(self.ident_f[:], 0.0), writes=[self.Bconst])
        S.op("pool", lambda: nc.gpsimd.affine_select(out=self.ident_f[:], in_=self.ident_f[:], pattern=[[-1, 128]],
                                                     compare_op=ALU.not_equal, fill=1.0, base=0, channel_multiplier=1),
             reads=[self.Bconst], writes=[self.Bconst])
        S.op("dve", lambda: nc.vector.tensor_copy(out=self.ident_b[:], in_=self.ident_f[:]), reads=[self.Bconst], writes=[self.Bconst])
        S.op("dve", lambda: nc.vector.memset(self.eps6[:], 1e-6), writes=[self.Bconst])
        self.cst = nc.alloc_sbuf_tensor("cst", [128, 8], F32)
        for i, v in enumerate([1.0, 1e-5, 1e-12, 64e-5, 0.0]):
            S.op("dve", lambda: nc.vector.memset(self.cst[:, i:i + 1], v), writes=[self.Bconst])
        self.ones_f = nc.alloc_sbuf_tensor("ones_f", [128, 128], F32)
        S.op("dve", lambda: nc.vector.memset(self.ones_f[:], 1.0), writes=[self.Bconst])
        self.ones_bd = nc.alloc_sbuf_tensor("ones_bd", [128, 128], F32)
        S.op("dve", lambda: nc.vector.memset(self.ones_bd[:], 0.0), writes=[self.Bconst])
        S.op("dve", lambda: nc.vector.memset(self.ones_bd[0:64, 0:64], 1.0), writes=[self.Bconst])
        S.op("dve", lambda: nc.vector.memset(self.ones_bd[64:128, 64:128], 1.0), writes=[self.Bconst])
        self.mbd = nc.alloc_sbuf_tensor("mbd", [128, 3, 128], F32)
        S.op("pool", lambda: nc.gpsimd.memset(self.mbd[:], 1.0), writes=[self.Bconst])
        for i, (cm, st, op) in enumerate([(1, -1, ALU.is_gt), (-1, 1, ALU.is_gt), (-1, 1, ALU.is_ge)]):
            S.op("pool", lambda: nc.gpsimd.affine_select(out=self.mbd[:, i, :], in_=self.mbd[:, i, :], pattern=[[st, 128]],
                                                         compare_op=op, fill=0.0, base=0, channel_multiplier=cm),
                 reads=[self.Bconst], writes=[self.Bconst])
        S.op("pool", lambda: nc.gpsimd.memset(self.mbd[0:64, :, 64:128], 0.0), reads=[self.Bconst], writes=[self.Bconst])
        S.op("pool", lambda: nc.gpsimd.memset(self.mbd[64:128, :, 0:64], 0.0), reads=[self.Bconst], writes=[self.Bconst])
        self.mask128 = nc.alloc_sbuf_tensor("mask128", [128, 128], F32)
        S.op("pool", lambda: nc.gpsimd.memset(self.mask128[:], 1.0), writes=[self.Bconst])
        S.op("pool", lambda: nc.gpsimd.affine_select(out=self.mask128[:], in_=self.mask128[:], pattern=[[1, 128]],
                                                     compare_op=ALU.is_ge, fill=0.0, base=0, channel_multiplier=-1),
             reads=[self.Bconst], writes=[self.Bconst])

    def sbuf(self, name, shape, dt):
        self.uid += 1
        return self.nc.sbuf_tensor("%s_u%d" % (name, self.uid), shape, dt)

    def tiles(self, es, name, shape, dt, n):
        return [(es.enter_context(self.sbuf("%s%d" % (name, i), shape, dt)), Buf(name)) for i in range(n)]

    def V(self, fn, r=(), w=()):
        return self.S.op("dve", fn, r, w)

    def A(self, fn, r=(), w=()):
        return self.S.op("act", fn, r, w)

    def P(self, fn, r=(), w=()):
        return self.S.op("pe", fn, r, w)

    def G(self, fn, r=(), w=()):
        return self.S.op("pool", fn, r, w)

    def vcol(self, l, name, j=0, n=1):
        o = VEC_OFF[name] + j
        return self.vecs[:, l, o:o + n]

    def rmsnorm_fm(self, es, src_ap, Bsrc, ntok, gain_ap, hT, BhT, eps_ap):
        nc, S = self.nc, self.S
        TB = 4
        xts = [(es.enter_context(self.sbuf("nx%d" % i, [128, TB, D], F32)), [Buf() for _ in range(TB)]) for i in range(2)]
        xss = [(es.enter_context(self.sbuf("nxs%d" % i, [128, D], BF16)), Buf()) for i in range(4)]
        junk = es.enter_context(self.sbuf("njunk", [128, D], BF16)); Bjunk = Buf()
        st = [(es.enter_context(self.sbuf("nst%d" % i, [128, 4], F32)), Buf()) for i in range(4)]
        nblk = ntok // (128 * TB)
        cnt = 0
        for blk in range(nblk):
            xt, Bxs_ = xts[blk % 2]
            for j in range(TB):
                r0 = (blk * TB + j) * 128
                S.dma("sp", xt[:, j, :], src_ap[r0:r0 + 128, :], reads=[Bsrc], writes=[Bxs_[j]])
            for j in range(TB):
                Bx = Bxs_[j]
                s_, Bs = st[cnt % 4]
                xs, Bxs = xss[cnt % 4]
                cnt += 1
                S.op("act", lambda: nc.scalar.activation(out=junk[:], in_=xt[:, j, :], func=AF.Square, accum_out=s_[:, 0:1]),
                     reads=[Bx], writes=[Bjunk, Bs])
                S.op("act", lambda: nc.scalar.activation(out=s_[:, 1:2], in_=s_[:, 0:1], func=AF.Sqrt, scale=1.0 / D, bias=eps_ap),
                     reads=[Bs, self.Bconst], writes=[Bs])
                S.op("dve", lambda: nc.vector.reciprocal(out=s_[:, 2:3], in_=s_[:, 1:2]), reads=[Bs], writes=[Bs])
                S.op("dve", lambda: nc.vector.tensor_scalar(out=xs[:], in0=xt[:, j, :], scalar1=s_[:, 2:3], scalar2=None, op0=ALU.mult),
                     reads=[Bx, Bs], writes=[Bxs])
                pt, Bp = self.psum()
                pb = pt[:].bitcast(BF16)
                for kc in range(8):
                    S.op("pe", lambda: nc.tensor.transpose(out=pb[:, kc * 128:(kc + 1) * 128], in_=xs[:, kc * 128:(kc + 1) * 128], identity=self.ident_b[:]),
                         reads=[Bxs, self.Bconst], writes=[Bp])
                t0 = (blk * TB + j) * 128
                S.op("dve", lambda: nc.vector.tensor_tensor(out=hT[:, :, t0:t0 + 128], in0=pb.rearrange("p (k t) -> p k t", k=8),
                                                            in1=gain_ap.unsqueeze(2).to_broadcast([128, 8, 128]), op=ALU.mult),
                     reads=[Bp, self.Bvecs], writes=[BhT])

    def proj_fm(self, es, w_ap, Bw, segs, hT, BhT, ntok, dst_ap, Bdst, kc_n=8):
        nc, S = self.nc, self.S
        wts = [(es.enter_context(self.sbuf("pw%d" % i, [128, kc_n, 512], BF16)), [Buf() for _ in range((kc_n + 3) // 4)]) for i in range(2)]
        ots = [(es.enter_context(self.sbuf("po%d" % i, [128, 512], BF16)), Buf()) for i in range(4)]
        wi = 0
        oi = 0
        for (c0, ncol, func, r0) in segs:
            for g0 in range(0, ncol, 512):
                gn = min(512, ncol - g0)
                wt, Bwt = wts[wi % 2]; wi += 1
                src = w_ap[:, c0 + g0:c0 + g0 + gn].rearrange("(k p) c -> p k c", p=128)
                Bwt_l = Bwt
                for k0 in range(0, kc_n, 4):
                    S.dma("pool", wt[:, k0:k0 + 4, 0:gn], src[:, k0:k0 + 4, :], reads=[Bw], writes=[Bwt_l[k0 // 4]])
                for m0 in range(0, gn, 128):
                    mn = min(128, gn - m0)
                    for tb in range(ntok // 512):
                        pt, Bp = self.psum()
                        for kc in range(kc_n):
                            S.op("pe", lambda: nc.tensor.matmul(pt[0:mn, :], lhsT=wt[:, kc, m0:m0 + mn], rhs=hT[:, kc, tb * 512:(tb + 1) * 512],
                                                                start=(kc == 0), stop=(kc == kc_n - 1)),
                                 reads=[Bwt[kc // 4], BhT], writes=[Bp])
                        ot, Bo = ots[oi % 4]; oi += 1
                        if func is not None:
                            S.op("act", lambda: nc.scalar.activation(out=ot[0:mn, :], in_=pt[0:mn, :], func=func), reads=[Bp], writes=[Bo])
                        else:
                            e = self.ev_eng()
                            if e == "act":
                                S.op("act", lambda: nc.scalar.copy(out=ot[0:mn, :], in_=pt[0:mn, :]), reads=[Bp], writes=[Bo])
                            else:
                                S.op("dve", lambda: nc.vector.tensor_copy(out=ot[0:mn, :], in_=pt[0:mn, :]), reads=[Bp], writes=[Bo])
                        rr = r0 + g0 + m0
                        S.dma("sp", dst_ap[rr:rr + mn, tb * 512:(tb + 1) * 512], ot[0:mn, :], reads=[Bo], writes=[Bdst])

    def stage_mixproj(self, l):
        nc, S = self.nc, self.S
        with ExitStack() as es:
            hT = es.enter_context(self.sbuf("hT", [128, 8, T], BF16)); BhT = Buf("hT")
            xsrc, Bx = (self.dram["x"], self.dbuf["x"]) if l == 0 else (self.dram["xres"], self.dbuf["xres"])
            with ExitStack() as es2:
                self.rmsnorm_fm(es2, xsrc, Bx, T, self.vcol(l, "norm_mix", 0, 8), hT, BhT, self.eps6[:, 0:1])
                S.barrier()
            if "hT" in self.debug:
                self.dump("hT_dbg", hT[:], [BhT])
            segs = [(RW0, 1792, None, RW0), (S50, 512, None, S50), (GL0, 1552, None, GL0), (GT0, 3072, AF.Sigmoid, GT0)]
            if "segs" in self.debug:
                segs = self.debug["segs"]
            with ExitStack() as es2:
                self.proj_fm(es2, self.dram["w_in"][l], self.dbuf["w_in"], segs, hT, BhT, T, self.dram["pT"], self.dbuf["pT"])
                S.barrier()
        S.barrier()

    def dump(self, name, ap, bufs, dt=None):
        d = self.nc.dram_tensor(name, list(ap.shape), dt or ap.dtype, kind="ExternalOutput").ap()
        self.S.dma("sp", d, ap, reads=bufs, writes=[Buf()])

    def stage_gla(self, l, sq):
        nc, S = self.nc, self.S
        V, A, P = self.V, self.A, self.P
        pT, BpT = self.dram["pT"], self.dbuf["pT"]
        ym, Bym = self.dram["ymix"], self.dbuf["ymix"]
        t0 = sq * SEQ
        L = SEQ
        NCH = L // 128
        qr, kr, vr, gr, gor = GL0, GL0 + 256, GL0 + 512, GL0 + 1024, GL0 + 1040
        Bc = self.Bconst
        with ExitStack() as es:
            gkup = es.enter_context(self.sbuf("gkup", [16, 256], BF16)); Bgk = Buf()
            S.dma("pool", gkup[:], self.dram["gla_gk_up"][l], reads=[self.dbuf["gla_gk_up"]], writes=[Bgk])
            gkd = es.enter_context(self.sbuf("gkd", [16, L], BF16)); Bgkd = Buf()
            S.dma("sp", gkd[:], pT[gr:gr + 16, t0:t0 + L], reads=[BpT], writes=[Bgkd])
            negb = es.enter_context(self.sbuf("negb", [128, 2], F32)); Bnb = Buf()
            V(lambda: nc.vector.tensor_scalar(out=negb[:], in0=self.vcol(l, "gla_gk_b", 0, 2), scalar1=-1.0, scalar2=None, op0=ALU.mult),
              [self.Bvecs], [Bnb])
            qk = self.tiles(es, "gqk", [128, L], BF16, 2)
            qd = es.enter_context(self.sbuf("gqd", [128, L], BF16)); Bqd = Buf()
            ki = es.enter_context(self.sbuf("gki", [128, L], BF16)); Bki = Buf()
            csz = es.enter_context(self.sbuf("gcsz", [128, L + 1], F32)); Bcs = Buf()
            lt = es.enter_context(self.sbuf("glt", [128, L], F32)); Blt = Buf()
            csl = es.enter_context(self.sbuf("gcsl", [128, L], F32)); Bcsl = Buf()
            Eq = es.enter_context(self.sbuf("gEq", [128, L], F32)); BEq = Buf()
            Ek = es.enter_context(self.sbuf("gEk", [128, L], F32)); BEk = Buf()
            vts = self.tiles(es, "gv", [128, L], BF16, 2)
            ofm = self.tiles(es, "gofm", [128, L], F32, 2)
            S32 = self.tiles(es, "gS32", [128, 128], F32, 2)
            S16 = self.tiles(es, "gS16", [128, 128], BF16, 2)
            vtm = self.tiles(es, "gvtm", [128, 128], BF16, 4)
            ktm = self.tiles(es, "gktm", [128, 128], BF16, 2)
            att = self.tiles(es, "gatt", [128, 128], BF16, 4)
            gos = self.tiles(es, "ggo", [128, 512], BF16, 4)
            tmpa = self.tiles(es, "gta", [128, 512], F32, 4)
            tmpb = self.tiles(es, "gtb", [128, 512], F32, 4)
            tmpc = self.tiles(es, "gtc", [128, 512], F32, 4)
            yo = self.tiles(es, "gyo", [128, 512], BF16, 4)
            V(lambda: nc.vector.memset(csz[:, 0:1], 0.0), [], [Bcs])
            for j in range(2):
                qt, Bq = qk[0]; kt, Bk = qk[1]
                S.dma("sp", qt[:], pT[qr + 128 * j:qr + 128 * j + 128, t0:t0 + L], reads=[BpT], writes=[Bq])
                S.dma("sp", kt[:], pT[kr + 128 * j:kr + 128 * j + 128, t0:t0 + L], reads=[BpT], writes=[Bk])
                for tb in range(L // 512):
                    pt, Bp = self.psum()
                    P(lambda: nc.tensor.matmul(pt[:], lhsT=gkup[0:16, 128 * j:128 * j + 128], rhs=gkd[0:16, tb * 512:(tb + 1) * 512], start=True, stop=True),
                      [Bgk, Bgkd], [Bp])
                    A(lambda: nc.scalar.activation(out=lt[:, tb * 512:(tb + 1) * 512], in_=pt[:], func=AF.Exp, scale=-1.0, bias=negb[:, j:j + 1]),
                      [Bp, Bnb], [Blt])
                A(lambda: nc.scalar.activation(out=lt[:], in_=lt[:], func=AF.Ln, scale=1.0, bias=self.cst[:, 0:1]), [Blt, Bc], [Blt])
                V(lambda: nc.vector.tensor_tensor_scan(out=csz[:, 1:L + 1], data0=self.cst[:, 0:1].to_broadcast([128, L]), data1=lt[:],
                                                       initial=0.0, op0=ALU.mult, op1=ALU.add), [Blt, Bc], [Bcs])
                V(lambda: nc.vector.tensor_tensor(out=csl[:].rearrange("p (c t) -> p c t", t=128),
                                                  in0=csz[:, 1:L + 1].rearrange("p (c t) -> p c t", t=128),
                                                  in1=csz[:, 0:L].rearrange("p (c t) -> p c t", t=128)[:, :, 0:1].to_broadcast([128, NCH, 128]),
                                                  op=ALU.subtract), [Bcs], [Bcsl])
                A(lambda: nc.scalar.activation(out=Eq[:], in_=csl[:], func=AF.Exp, scale=-1.0 / 16), [Bcsl], [BEq])
                A(lambda: nc.scalar.activation(out=Ek[:], in_=csl[:], func=AF.Exp, scale=1.0 / 16), [Bcsl], [BEk])
                V(lambda: nc.vector.scalar_tensor_tensor(out=qd[:], in0=qt[:], scalar=0.125, in1=Eq[:], op0=ALU.mult, op1=ALU.mult), [Bq, BEq], [Bqd])
                V(lambda: nc.vector.tensor_tensor(out=ki[:], in0=kt[:], in1=Ek[:], op=ALU.mult), [Bk, BEk], [Bki])
                Eq3 = Eq[:].rearrange("p (c t) -> p c t", t=128)
                heads = [2 * j, 2 * j + 1]
                for hh in range(2):
                    h = heads[hh]
                    vt, Bv = vts[hh]
                    S.dma("sp", vt[:], pT[vr + 128 * h:vr + 128 * h + 128, t0:t0 + L], reads=[BpT], writes=[Bv])
                    V(lambda: nc.vector.memset(S32[hh][0][:], 0.0), [], [S32[hh][1]])
                    V(lambda: nc.vector.memset(S16[hh][0][:], 0.0), [], [S16[hh][1]])
                cnt = 0
                for ci in range(NCH):
                    ts = slice(ci * 128, ci * 128 + 128)
                    ktmt, Bktm = ktm[ci % 2]
                    pt, Bp = self.psum()
                    pb = pt[:].bitcast(BF16)
                    P(lambda: nc.tensor.transpose(out=pb[:, 0:128], in_=ki[:, ts], identity=self.ident_b[:]), [Bki, Bc], [Bp])
                    A(lambda: nc.scalar.copy(out=ktmt[:], in_=pb[:, 0:128]), [Bp], [Bktm])
                    for hh in range(2):
                        h = heads[hh]
                        hp = slice(64 * hh, 64 * hh + 64)
                        vt, Bv = vts[hh]
                        vtmt, Bvtm = vtm[cnt % 4]
                        attt, Batt = att[cnt % 4]
                        cnt += 1
                        pt, Bp = self.psum()
                        pb = pt[:].bitcast(BF16)
                        P(lambda: nc.tensor.transpose(out=pb[:, 0:128], in_=vt[:, ts], identity=self.ident_b[:]), [Bv, Bc], [Bp])
                        A(lambda: nc.scalar.copy(out=vtmt[:], in_=pb[:, 0:128]), [Bp], [Bvtm])
                        pt2, Bp2 = self.psum()
                        P(lambda: nc.tensor.matmul(pt2[:, 0:128], lhsT=ki[hp, ts], rhs=qd[hp, ts], start=True, stop=True), [Bki, Bqd], [Bp2])
                        V(lambda: nc.vector.tensor_tensor(out=attt[:], in0=pt2[:, 0:128], in1=self.mask128[:], op=ALU.mult), [Bp2, Bc], [Batt])
                        pt3, Bp3 = self.psum()
                        P(lambda: nc.tensor.matmul(pt3[:, 0:128], lhsT=vtmt[:], rhs=attt[:], start=True, stop=False), [Bvtm, Batt], [Bp3])
                        P(lambda: nc.tensor.matmul(pt3[:, 0:128], lhsT=S16[hh][0][:], rhs=qd[:, ts], start=False, stop=True), [S16[hh][1], Bqd], [Bp3])
                        A(lambda: nc.scalar.copy(out=ofm[hh][0][:, ts], in_=pt3[:, 0:128]), [Bp3], [ofm[hh][1]])
                        if ci < NCH - 1:
                            pt4, Bp4 = self.psum()
                            P(lambda: nc.tensor.matmul(pt4[:, 0:128], lhsT=ktmt[:], rhs=vtmt[:], start=True, stop=True), [Bktm, Bvtm], [Bp4])
                            s32, Bs32 = S32[hh]
                            V(lambda: nc.vector.tensor_tensor(out=s32[hp, :], in0=s32[hp, :], in1=pt4[hp, 0:128], op=ALU.add), [Bs32, Bp4], [Bs32])
                            V(lambda: nc.vector.tensor_scalar(out=s32[hp, :], in0=s32[hp, :], scalar1=Eq3[hp, ci, 127:128], scalar2=None, op0=ALU.mult),
                              [Bs32, BEq], [Bs32])
                            V(lambda: nc.vector.tensor_copy(out=S16[hh][0][hp, :], in_=s32[hp, :]), [Bs32], [S16[hh][1]])
                NB = L // 512
                for hh in range(2):
                    h = heads[hh]
                    o, Bo = ofm[hh]
                    CS = [slice(tb * 512, tb * 512 + 512) for tb in range(NB)]
                    for tb in range(NB):
                        got, Bgo = gos[tb]
                        S.dma("sp", got[:], pT[gor + 128 * h:gor + 128 * h + 128, t0 + tb * 512:t0 + tb * 512 + 512], reads=[BpT], writes=[Bgo])
                    for tb in range(NB):
                        ta, Bta = tmpa[tb]
                        A(lambda: nc.scalar.activation(out=ta[:], in_=o[:, CS[tb]], func=AF.Square), [Bo], [Bta])
                    pts = {}
                    for tb in range(NB):
                        ta, Bta = tmpa[tb]
                        pts[tb] = self.psum()
                        pt, Bp = pts[tb]
                        P(lambda: nc.tensor.matmul(pt[:], lhsT=self.ones_f[:], rhs=ta[:], start=True, stop=True), [Bc, Bta], [Bp])
                    for tb in range(NB):
                        pt, Bp = pts[tb]; tb_, Btb = tmpb[tb]
                        A(lambda: nc.scalar.activation(out=tb_[:], in_=pt[:], func=AF.Sqrt, scale=1.0 / 128, bias=self.cst[:, 1:2]), [Bp, Bc], [Btb])
                    for tb in range(NB):
                        got, Bgo = gos[tb]; tc, Btc = tmpc[tb]
                        A(lambda: nc.scalar.activation(out=tc[:], in_=got[:], func=AF.Sigmoid), [Bgo], [Btc])
                    for tb in range(NB):
                        tb_, Btb = tmpb[tb]
                        V(lambda: nc.vector.reciprocal(out=tb_[:], in_=tb_[:]), [Btb], [Btb])
                    for tb in range(NB):
                        tb_, Btb = tmpb[tb]
                        V(lambda: nc.vector.scalar_tensor_tensor(out=tb_[:], in0=o[:, CS[tb]], scalar=self.vcol(l, "gla_norm", 0, 1), in1=tb_[:],
                                                                 op0=ALU.mult, op1=ALU.mult), [Bo, Btb, self.Bvecs], [Btb])
                    for tb in range(NB):
                        got, Bgo = gos[tb]; tc, Btc = tmpc[tb]
                        V(lambda: nc.vector.tensor_tensor(out=tc[:], in0=tc[:], in1=got[:], op=ALU.mult), [Btc, Bgo], [Btc])
                    for tb in range(NB):
                        tb_, Btb = tmpb[tb]; tc, Btc = tmpc[tb]; yt, By = yo[tb]
                        V(lambda: nc.vector.tensor_tensor(out=yt[:], in0=tb_[:], in1=tc[:], op=ALU.mult), [Btb, Btc], [By])
                        S.dma("sp", ym[2, 128 * h:128 * h + 128, t0 + tb * 512:t0 + tb * 512 + 512], yt[:], reads=[By], writes=[Bym])
            S.barrier()

    def s5_lambda(self, es, tag, are, aim, ls_bc, shp):
        nc = self.nc
        V, A = self.V, self.A
        F = int(np.prod(shp[1:]))
        def new(n):
            return es.enter_context(self.sbuf(tag + n, [128, F], F32))
        def vw(t):
            return t[:] if len(shp) == 2 else (t[:].rearrange("p (a b) -> p a b", b=shp[2]) if len(shp) == 3 else t[:])
        B = Buf()
        Bin = self.Bs5w
        dt, mag, ang, c, sn, t1, t2, lre, lim, fre, fim = [new(n) for n in ["dt", "mag", "ang", "c", "s", "t1", "t2", "lre", "lim", "fre", "fim"]]
        A(lambda: nc.scalar.activation(out=vw(dt), in_=ls_bc, func=AF.Exp), [Bin], [B])
        V(lambda: nc.vector.tensor_tensor(out=vw(mag), in0=vw(dt), in1=are, op=ALU.mult), [B, Bin], [B])
        A(lambda: nc.scalar.activation(out=mag[:], in_=mag[:], func=AF.Exp), [B], [B])
        V(lambda: nc.vector.tensor_tensor(out=vw(ang), in0=vw(dt), in1=aim, op=ALU.mult), [B, Bin], [B])
        A(lambda: nc.scalar.activation(out=sn[:], in_=ang[:], func=AF.Sin, scale=1.0 / 16), [B], [B])
        A(lambda: nc.scalar.activation(out=c[:], in_=ang[:], func=AF.Sin, scale=-1.0 / 16, bias=self.gvec[:, 18:19]), [B, self.Bvecs], [B])
        for _ in range(4):
            V(lambda: nc.vector.tensor_tensor(out=t1[:], in0=c[:], in1=sn[:], op=ALU.mult), [B], [B])
            V(lambda: nc.vector.tensor_tensor(out=c[:], in0=c[:], in1=c[:], op=ALU.mult), [B], [B])
            V(lambda: nc.vector.tensor_tensor(out=t2[:], in0=sn[:], in1=sn[:], op=ALU.mult), [B], [B])
            V(lambda: nc.vector.tensor_tensor(out=c[:], in0=c[:], in1=t2[:], op=ALU.subtract), [B], [B])
            V(lambda: nc.vector.tensor_scalar(out=sn[:], in0=t1[:], scalar1=2.0, scalar2=None, op0=ALU.mult), [B], [B])
        V(lambda: nc.vector.tensor_tensor(out=lre[:], in0=mag[:], in1=c[:], op=ALU.mult), [B], [B])
        V(lambda: nc.vector.tensor_tensor(out=lim[:], in0=mag[:], in1=sn[:], op=ALU.mult), [B], [B])
        V(lambda: nc.vector.tensor_tensor(out=vw(t1), in0=are, in1=are, op=ALU.mult), [Bin, B], [B])
        V(lambda: nc.vector.tensor_tensor(out=vw(t2), in0=aim, in1=aim, op=ALU.mult), [Bin, B], [B])
        V(lambda: nc.vector.tensor_tensor(out=t1[:], in0=t1[:], in1=t2[:], op=ALU.add), [B], [B])
        V(lambda: nc.vector.reciprocal(out=t1[:], in_=t1[:]), [B], [B])
        V(lambda: nc.vector.tensor_scalar(out=c[:], in0=lre[:], scalar1=-1.0, scalar2=None, op0=ALU.add), [B], [B])
        V(lambda: nc.vector.tensor_tensor(out=vw(fre), in0=vw(c), in1=are, op=ALU.mult), [B, Bin], [B])
        V(lambda: nc.vector.tensor_tensor(out=vw(t2), in0=vw(lim), in1=aim, op=ALU.mult), [B, Bin], [B])
        V(lambda: nc.vector.tensor_tensor(out=fre[:], in0=fre[:], in1=t2[:], op=ALU.add), [B], [B])
        V(lambda: nc.vector.tensor_tensor(out=fre[:], in0=fre[:], in1=t1[:], op=ALU.mult), [B], [B])
        V(lambda: nc.vector.tensor_tensor(out=vw(fim), in0=vw(lim), in1=are, op=ALU.mult), [B, Bin], [B])
        V(lambda: nc.vector.tensor_tensor(out=vw(t2), in0=vw(c), in1=aim, op=ALU.mult), [B, Bin], [B])
        V(lambda: nc.vector.tensor_tensor(out=fim[:], in0=fim[:], in1=t2[:], op=ALU.subtract), [B], [B])
        V(lambda: nc.vector.tensor_tensor(out=fim[:], in0=fim[:], in1=t1[:], op=ALU.mult), [B], [B])
        return lre, lim, fre, fim, B

    def cmul(self, ore, oim, are, aim, bre, bim, t1, r, w):
        nc, V = self.nc, self.V
        V(lambda: nc.vector.tensor_tensor(out=ore, in0=are, in1=bre, op=ALU.mult), r, w)
        V(lambda: nc.vector.tensor_tensor(out=t1, in0=aim, in1=bim, op=ALU.mult), r, w)
        V(lambda: nc.vector.tensor_tensor(out=ore, in0=ore, in1=t1, op=ALU.subtract), r, w)
        V(lambda: nc.vector.tensor_tensor(out=oim, in0=are, in1=bim, op=ALU.mult), r, w)
        V(lambda: nc.vector.tensor_tensor(out=t1, in0=aim, in1=bre, op=ALU.mult), r, w)
        V(lambda: nc.vector.tensor_tensor(out=oim, in0=oim, in1=t1, op=ALU.add), r, w)

    def stage_s5(self, l):
        nc, S = self.nc, self.S
        V, A, P = self.V, self.A, self.P
        pT, BpT = self.dram["pT"], self.dbuf["pT"]
        ym, Bym = self.dram["ymix"], self.dbuf["ymix"]
        Bc = self.Bconst
        with ExitStack() as es:
            M1re = es.enter_context(self.sbuf("M1re", [128, 4, 8, 128], BF16))
            M1im = es.enter_context(self.sbuf("M1im", [128, 4, 8, 128], BF16))
            M1hr = es.enter_context(self.sbuf("M1hr", [128, 4, 8, 128], BF16))
            M1hi = es.enter_context(self.sbuf("M1hi", [128, 4, 8, 128], BF16))
            M2hr = es.enter_context(self.sbuf("M2hr", [128, 4, 8, 64], BF16))
            M2hi = es.enter_context(self.sbuf("M2hi", [128, 4, 8, 64], BF16))
            M2re = es.enter_context(self.sbuf("M2re", [128, 16, 8, 32], BF16))
            M2im = es.enter_context(self.sbuf("M2im", [128, 16, 8, 32], BF16))
            Kbd = es.enter_context(self.sbuf("Kbd", [128, 4, 8, 128], BF16))
            LK = es.enter_context(self.sbuf("LK", [128, 16, 8, 3], F32))
            wglu = es.enter_context(self.sbuf("wglu", [128, 4, 512], BF16))
            BW = Buf("s5w")
            Bwg = Buf()
            S.dma("pool", wglu[:], self.dram["s5_w_glu"][l].rearrange("(k p) c -> p k c", p=128), reads=[self.dbuf["s5_w_glu"]], writes=[Bwg])
            with ExitStack() as e2:
                w = e2.enter_context(self.sbuf("s5w_sb", [128, S5X], F32)); self.Bs5w = Buf()
                S.dma("sp", w[:], self.dram["s5w"][:, l, :], reads=[self.dbuf["s5w"]], writes=[self.Bs5w])
                aC = w[:, oAC:oAC + 512].rearrange("p (g r n) -> p g r n", g=4, r=2)
                bC = w[:, oBC:oBC + 512].rearrange("p (g r n) -> p g r n", g=4, r=2)
                lC = w[:, oLC:oLC + 4].unsqueeze(2).to_broadcast([128, 4, 64])
                lre, lim, fre, fim, B1 = self.s5_lambda(e2, "c", aC[:, :, 0, :], aC[:, :, 1, :], lC, [128, 4, 64])
                v3 = lambda t: t[:].rearrange("p (a b) -> p a b", b=64)
                Bre = e2.enter_context(self.sbuf("cBre", [128, 256], F32)); Bim = e2.enter_context(self.sbuf("cBim", [128, 256], F32))
                tq = e2.enter_context(self.sbuf("ctq", [128, 256], F32))
                self.cmul(v3(Bre), v3(Bim), v3(fre), v3(fim), bC[:, :, 0, :], bC[:, :, 1, :], v3(tq), [B1, self.Bs5w], [B1])
                Pw = e2.enter_context(self.sbuf("cPw", [128, 8, 2, 256], F32))
                V(lambda: nc.vector.memset(Pw[:, 0, 0, :], 1.0), [], [B1])
                V(lambda: nc.vector.memset(Pw[:, 0, 1, :], 0.0), [], [B1])
                for m in range(1, 8):
                    self.cmul(Pw[:, m, 0, :], Pw[:, m, 1, :], Pw[:, m - 1, 0, :], Pw[:, m - 1, 1, :], lre[:], lim[:], tq[:], [B1], [B1])
                vre = e2.enter_context(self.sbuf("cvre", [128, 256], F32)); vim = e2.enter_context(self.sbuf("cvim", [128, 256], F32))
                for j in range(8):
                    m = 7 - j
                    self.cmul(vre[:], vim[:], Pw[:, m, 0, :], Pw[:, m, 1, :], Bre[:], Bim[:], tq[:], [B1], [B1])
                    for bb in range(2):
                        V(lambda: nc.vector.tensor_scalar(out=M1re[:, :, j, bb * 64:(bb + 1) * 64], in0=v3(vre), scalar1=self.gvec[:, 16 + bb:17 + bb], scalar2=None, op0=ALU.mult),
                          [B1, self.Bvecs], [BW])
                        V(lambda: nc.vector.tensor_scalar(out=M1im[:, :, j, bb * 64:(bb + 1) * 64], in0=v3(vim), scalar1=self.gvec[:, 16 + bb:17 + bb], scalar2=None, op0=ALU.mult),
                          [B1, self.Bvecs], [BW])
                        V(lambda: nc.vector.tensor_scalar(out=M1hr[:, :, j, bb * 64:(bb + 1) * 64], in0=v3(vre), scalar1=self.gvec[:, 16 + bb:17 + bb], scalar2=self.gvec[:, 19:20], op0=ALU.mult, op1=ALU.mult),
                          [B1, self.Bvecs], [BW])
                        V(lambda: nc.vector.tensor_scalar(out=M1hi[:, :, j, bb * 64:(bb + 1) * 64], in0=v3(vim), scalar1=self.gvec[:, 16 + bb:17 + bb], scalar2=self.gvec[:, 19:20], op0=ALU.mult, op1=ALU.mult),
                          [B1, self.Bvecs], [BW])
            S.barrier()
            with ExitStack() as e2:
                w = e2.enter_context(self.sbuf("s5w_sb2", [128, S5X], F32)); self.Bs5w = Buf()
                S.dma("sp", w[:], self.dram["s5w"][:, l, :], reads=[self.dbuf["s5w"]], writes=[self.Bs5w])
                aP = w[:, oAP:oAP + 32].rearrange("p (g r) -> p g r", r=2)
                lP = w[:, oLP:oLP + 16]
                bP = w[:, oBP:oBP + 1024].rearrange("p (g r c) -> p g r c", g=16, r=2)
                cP = w[:, oCP:oCP + 1024].rearrange("p (g r c) -> p g r c", g=16, r=2)
                lre, lim, fre, fim, B2 = self.s5_lambda(e2, "p", aP[:, :, 0], aP[:, :, 1], lP, [128, 16])
                tq = e2.enter_context(self.sbuf("ptq", [128, 16], F32))
                Pp = e2.enter_context(self.sbuf("pPw", [128, 9, 2, 16], F32))
                V(lambda: nc.vector.memset(Pp[:, 0, 0, :], 1.0), [], [B2])
                V(lambda: nc.vector.memset(Pp[:, 0, 1, :], 0.0), [], [B2])
                for m in range(1, 9):
                    self.cmul(Pp[:, m, 0, :], Pp[:, m, 1, :], Pp[:, m - 1, 0, :], Pp[:, m - 1, 1, :], lre[:], lim[:], tq[:], [B2], [B2])
                lkr = e2.enter_context(self.sbuf("lkr", [128, 8, 16], F32)); lki = e2.enter_context(self.sbuf("lki", [128, 8, 16], F32))
                V(lambda: nc.vector.tensor_copy(out=lkr[:, 0, :], in_=Pp[:, 8, 0, :]), [B2], [B2])
                V(lambda: nc.vector.tensor_copy(out=lki[:, 0, :], in_=Pp[:, 8, 1, :]), [B2], [B2])
                for lev in range(1, 8):
                    self.cmul(lkr[:, lev, :], lki[:, lev, :], lkr[:, lev - 1, :], lki[:, lev - 1, :], lkr[:, lev - 1, :], lki[:, lev - 1, :], tq[:], [B2], [B2])
                V(lambda: nc.vector.tensor_copy(out=LK[:, :, :, 0], in_=lkr[:].rearrange("p l g -> p g l")), [B2], [BW])
                V(lambda: nc.vector.tensor_copy(out=LK[:, :, :, 1], in_=lki[:].rearrange("p l g -> p g l")), [B2], [BW])
                V(lambda: nc.vector.tensor_scalar(out=LK[:, :, :, 2], in0=lki[:].rearrange("p l g -> p g l"), scalar1=-1.0, scalar2=None, op0=ALU.mult), [B2], [BW])
                Bpr = e2.enter_context(self.sbuf("pBre", [128, 16, 64], F32)); Bpi = e2.enter_context(self.sbuf("pBim", [128, 16, 64], F32))
                V(lambda: nc.vector.memset(Bpr[:], 0.0), [], [B2])
                V(lambda: nc.vector.memset(Bpi[:], 0.0), [], [B2])
                t3 = e2.enter_context(self.sbuf("pt3", [128, 16, 32], F32))
                bc = lambda t: t[:].unsqueeze(2).to_broadcast([128, 16, 32])
                self.cmul(Bpr[:, :, 32:64], Bpi[:, :, 32:64], bc(fre), bc(fim), bP[:, :, 0, :], bP[:, :, 1, :], t3[:], [B2, self.Bs5w], [B2])
                CLr = e2.enter_context(self.sbuf("CLr", [128, 16, 9, 32], F32)); CLn = e2.enter_context(self.sbuf("CLn", [128, 16, 9, 32], F32))
                for m in range(9):
                    pr = Pp[:, m, 0, :].unsqueeze(2).to_broadcast([128, 16, 32])
                    pi = Pp[:, m, 1, :].unsqueeze(2).to_broadcast([128, 16, 32])
                    V(lambda: nc.vector.tensor_tensor(out=CLr[:, :, m, :], in0=cP[:, :, 0, :], in1=pr, op=ALU.mult), [B2, self.Bs5w], [B2])
                    V(lambda: nc.vector.tensor_tensor(out=t3[:], in0=cP[:, :, 1, :], in1=pi, op=ALU.mult), [B2, self.Bs5w], [B2])
                    V(lambda: nc.vector.tensor_tensor(out=CLr[:, :, m, :], in0=CLr[:, :, m, :], in1=t3[:], op=ALU.subtract), [B2], [B2])
                    V(lambda: nc.vector.tensor_tensor(out=CLn[:, :, m, :], in0=cP[:, :, 0, :], in1=pi, op=ALU.mult), [B2, self.Bs5w], [B2])
                    V(lambda: nc.vector.tensor_tensor(out=t3[:], in0=cP[:, :, 1, :], in1=pr, op=ALU.mult), [B2, self.Bs5w], [B2])
                    V(lambda: nc.vector.scalar_tensor_tensor(out=CLn[:, :, m, :], in0=CLn[:, :, m, :], scalar=-1.0, in1=t3[:], op0=ALU.mult, op1=ALU.subtract), [B2], [B2])
                V(lambda: nc.vector.tensor_copy(out=M2re[:], in_=CLr[:, :, 1:9, :]), [B2], [BW])
                V(lambda: nc.vector.tensor_copy(out=M2im[:], in_=CLn[:, :, 1:9, :]), [B2], [BW])
                V(lambda: nc.vector.memset(M2hr[:], 0.0), [], [BW])
                V(lambda: nc.vector.memset(M2hi[:], 0.0), [], [BW])
                for gt in range(4):
                    V(lambda: nc.vector.tensor_copy(out=M2hr[:, gt, :, 32:64], in_=CLr[:, 4 * gt + 3, 1:9, :]), [B2], [BW])
                    V(lambda: nc.vector.tensor_copy(out=M2hi[:, gt, :, 32:64], in_=CLn[:, 4 * gt + 3, 1:9, :]), [B2], [BW])
                KbdF = e2.enter_context(self.sbuf("KbdF", [128, 4, 8, 128], F32))
                V(lambda: nc.vector.memset(KbdF[:], 0.0), [], [B2])
                for pair in range(16):
                    gt, gp = pair // 4, pair % 4
                    pt, Bp = self.psum()
                    rs = slice(32 * gp, 32 * gp + 32) if gp < 3 else slice(64, 128)
                    cs_ = slice(32, 64) if gp < 3 else slice(0, 64)
                    P(lambda: nc.tensor.matmul(pt[rs, 0:256], lhsT=Bpr[:, pair, cs_], rhs=CLr[:, pair, 0:8, :], start=True, stop=False), [B2], [Bp])
                    P(lambda: nc.tensor.matmul(pt[rs, 0:256], lhsT=Bpi[:, pair, cs_], rhs=CLn[:, pair, 0:8, :], start=False, stop=True), [B2], [Bp])
                    V(lambda: nc.vector.tensor_copy(out=KbdF[rs, gt, :, 32 * gp:32 * gp + 32], in_=pt[rs, 0:256].rearrange("p (m c) -> p m c", c=32)), [Bp, B2], [B2])
                for gt in range(4):
                    V(lambda: nc.vector.scalar_tensor_tensor(out=KbdF[:, gt, 0, :], in0=self.ident_f[:], scalar=self.vcol(l, "s5_d", gt, 1), in1=KbdF[:, gt, 0, :],
                                                             op0=ALU.mult, op1=ALU.add), [B2, Bc, self.Bvecs], [B2])
                V(lambda: nc.vector.tensor_copy(out=Kbd[:], in_=KbdF[:]), [B2], [BW])
            S.barrier()
            if "s5pre" in self.debug:
                self.dump("dbg_M1re", M1re[:], [BW]); self.dump("dbg_M1im", M1im[:], [BW]); self.dump("dbg_M2re", M2re[:], [BW])
                self.dump("dbg_M2im", M2im[:], [BW]); self.dump("dbg_Kbd", Kbd[:], [BW]); self.dump("dbg_LK", LK[:], [BW])
            for sq in range(NSEQ):
                with ExitStack() as e3:
                    self.s5_seq(e3, l, sq, (M1re, M1im, M1hr, M1hi), (M2re, M2im, M2hr, M2hi), Kbd, LK, wglu, BW, Bwg)
                    S.barrier()

    def s5_seq(self, es, l, sq, M1s, M2s, Kbd, LK, wglu, BW, Bwg):
        M1re, M1im, M1hr, M1hi = M1s
        M2re, M2im, M2hr, M2hi = M2s
        nc, S = self.nc, self.S
        V, A, P = self.V, self.A, self.P
        pT, BpT = self.dram["pT"], self.dbuf["pT"]
        ym, Bym = self.dram["ymix"], self.dbuf["ymix"]
        t0 = sq * SEQ
        L = SEQ
        NC8 = L // 8
        us = self.tiles(es, "s5u", [128, L], BF16, 4)
        for gt in range(4):
            S.dma("sp", us[gt][0][:], pT[S50 + 128 * gt:S50 + 128 * gt + 128, t0:t0 + L], reads=[BpT], writes=[us[gt][1]])
        S16r = es.enter_context(self.sbuf("s5Sr", [128, 16, NC8 + 1], BF16)); S16i = es.enter_context(self.sbuf("s5Si", [128, 16, NC8 + 1], BF16))
        BS16 = [Buf() for _ in range(16)]
        Bz = Buf()
        V(lambda: nc.vector.memset(S16r[:, :, 0:1], 0.0), [], [Bz])
        V(lambda: nc.vector.memset(S16i[:, :, 0:1], 0.0), [], [Bz])
        for b_ in BS16:
            b_.lw = Bz.lw
        sc = [[[self.tiles(es, "s5sc", [128, 2 * NC8], F32, 2) for _ri in range(2)] for _pp in range(1)] for _slot in range(2)]
        for slot in range(2):
            for ri in range(2):
                for pp in range(2):
                    tt_, Bt = sc[slot][0][ri][pp]
                    V(lambda: nc.vector.memset(tt_[:, 0:NC8], 0.0), [], [Bt])
        for p0 in range(0, 16, 2):
            prs = [p0, p0 + 1]
            for si, pair in enumerate(prs):
                gt, gp = pair // 4, pair % 4
                rs = slice(32 * gp, 32 * gp + 32) if gp < 3 else slice(64, 128)
                u, Bu = us[gt]
                u3 = u[:].rearrange("p (c j) -> p j c", j=8)
                for ri, M1 in enumerate((M1re, M1im) if gp < 3 else (M1hr, M1hi)):
                    pt, Bp = self.psum()
                    for j in range(8):
                        P(lambda: nc.tensor.matmul(pt[:, 0:NC8], lhsT=M1[rs, gt, j, :], rhs=u3[rs, j, :], start=(j == 0), stop=(j == 7)), [BW, Bu], [Bp])
                    tt_, Bt = sc[si][0][ri][0]
                    A(lambda: nc.scalar.copy(out=tt_[:, NC8:2 * NC8], in_=pt[:, 0:NC8]), [Bp], [Bt])
            for lev in range(8):
                sh = 1 << lev
                src, dst = lev % 2, (lev + 1) % 2
                for step in range(2):
                    for si, pair in enumerate(prs):
                        (re_s, Brs), (im_s, Bis) = sc[si][0][0][src], sc[si][0][1][src]
                        (re_d, Brd), (im_d, Bid) = sc[si][0][0][dst], sc[si][0][1][dst]
                        lr, li, nli = LK[:, pair, lev, 0:1], LK[:, pair, lev, 1:2], LK[:, pair, lev, 2:3]
                        cur = slice(NC8, 2 * NC8); shf = slice(NC8 - sh, 2 * NC8 - sh)
                        if step == 0:
                            V(lambda: nc.vector.scalar_tensor_tensor(out=re_d[:, cur], in0=re_s[:, shf], scalar=lr, in1=re_s[:, cur], op0=ALU.mult, op1=ALU.add), [Brs, BW], [Brd])
                            V(lambda: nc.vector.scalar_tensor_tensor(out=im_d[:, cur], in0=im_s[:, shf], scalar=lr, in1=im_s[:, cur], op0=ALU.mult, op1=ALU.add), [Bis, BW], [Bid])
                        else:
                            V(lambda: nc.vector.scalar_tensor_tensor(out=re_d[:, cur], in0=im_s[:, shf], scalar=nli, in1=re_d[:, cur], op0=ALU.mult, op1=ALU.add), [Bis, Brd, BW], [Brd])
                            V(lambda: nc.vector.scalar_tensor_tensor(out=im_d[:, cur], in0=re_s[:, shf], scalar=li, in1=im_d[:, cur], op0=ALU.mult, op1=ALU.add), [Brs, Bid, BW], [Bid])
            for si, pair in enumerate(prs):
                (re_f, Brf), (im_f, Bif) = sc[si][0][0][0], sc[si][0][1][0]
                A(lambda: nc.scalar.copy(out=S16r[:, pair, 1:NC8 + 1], in_=re_f[:, NC8:2 * NC8]), [Brf], [BS16[pair]])
                A(lambda: nc.scalar.copy(out=S16i[:, pair, 1:NC8 + 1], in_=im_f[:, NC8:2 * NC8]), [Bif], [BS16[pair]])
        ys = self.tiles(es, "s5y", [128, L], F32, 2)
        yg = self.tiles(es, "s5yg", [128, L], BF16, 4)
        ta = self.tiles(es, "s5ta", [128, 512], F32, 2)
        for gt in range(4):
            y, By = ys[gt % 2]
            u, Bu = us[gt]
            u3 = u[:].rearrange("p (c j) -> p j c", j=8)
            y3 = y[:].rearrange("p (c j) -> p j c", j=8)
            for j in range(8):
                pt, Bp = self.psum()
                for i in range(j + 1):
                    P(lambda: nc.tensor.matmul(pt[:, 0:NC8], lhsT=Kbd[:, gt, j - i, :], rhs=u3[:, i, :], start=(i == 0), stop=False), [BW, Bu], [Bp])
                for gp in range(4):
                    pair = 4 * gt + gp
                    if gp < 3:
                        rs = slice(32 * gp, 32 * gp + 32)
                        l_re, l_im = M2re[:, pair, j, :], M2im[:, pair, j, :]
                    else:
                        rs = slice(64, 128)
                        l_re, l_im = M2hr[:, gt, j, :], M2hi[:, gt, j, :]
                    P(lambda: nc.tensor.matmul(pt[rs, 0:NC8], lhsT=l_re, rhs=S16r[:, pair, 0:NC8], start=False, stop=False), [BW, BS16[pair]], [Bp])
                    P(lambda: nc.tensor.matmul(pt[rs, 0:NC8], lhsT=l_im, rhs=S16i[:, pair, 0:NC8], start=False, stop=(gp == 3)), [BW, BS16[pair]], [Bp])
                A(lambda: nc.scalar.copy(out=y3[:, j, :], in_=pt[:, 0:NC8]), [Bp], [By])
            g16, Bg = yg[gt]
            for tb in range(L // 512):
                cs = slice(tb * 512, tb * 512 + 512)
                t_, Bt = ta[tb % 2]
                A(lambda: nc.scalar.activation(out=t_[:], in_=y[:, cs], func=AF.Square), [By], [Bt])
                V(lambda: nc.vector.tensor_scalar(out=t_[:], in0=t_[:], scalar1=0.044715, scalar2=1.0, op0=ALU.mult, op1=ALU.add), [Bt], [Bt])
                V(lambda: nc.vector.tensor_tensor(out=t_[:], in0=t_[:], in1=y[:, cs], op=ALU.mult), [Bt, By], [Bt])
                A(lambda: nc.scalar.activation(out=t_[:], in_=t_[:], func=AF.Sigmoid, scale=1.5957691216057308), [Bt], [Bt])
                V(lambda: nc.vector.tensor_tensor(out=g16[:, cs], in0=t_[:], in1=y[:, cs], op=ALU.mult), [Bt, By], [Bg])
        yo = self.tiles(es, "s5yo", [128, 512], BF16, 2)
        sg = self.tiles(es, "s5sg", [128, 512], F32, 2)
        cnt = 0
        for m in range(4):
            for tb in range(L // 512):
                cs = slice(tb * 512, tb * 512 + 512)
                pt, Bp = self.psum()
                for kt in range(4):
                    P(lambda: nc.tensor.matmul(pt[:], lhsT=wglu[:, kt, m * 128:(m + 1) * 128], rhs=yg[kt][0][:, cs], start=(kt == 0), stop=(kt == 3)), [Bwg, yg[kt][1]], [Bp])
                s_, Bs = sg[cnt % 2]; o_, Bo = yo[cnt % 2]; cnt += 1
                A(lambda: nc.scalar.activation(out=s_[:], in_=pt[:], func=AF.Sigmoid, bias=self.vcol(l, "s5_b_glu", m, 1)), [Bp, self.Bvecs], [Bs])
                V(lambda: nc.vector.tensor_tensor(out=o_[:], in0=s_[:], in1=yg[m][0][:, cs], op=ALU.mult), [Bs, yg[m][1]], [Bo])
                S.dma("sp", ym[1, 128 * m:128 * m + 128, t0 + tb * 512:t0 + tb * 512 + 512], o_[:], reads=[Bo], writes=[Bym])

    def stage_rwkv(self, l, sq):
        nc, S = self.nc, self.S
        V, A, P = self.V, self.A, self.P
        pT, BpT = self.dram["pT"], self.dbuf["pT"]
        ym, Bym = self.dram["ymix"], self.dbuf["ymix"]
        Bc = self.Bconst
        Bv = self.Bvecs
        t0 = sq * SEQ
        L = SEQ
        NCH = L // 64
        ALPHA = float(np.exp(-0.5))
        with ExitStack() as es:
            WU = es.enter_context(self.sbuf("rwWU", [128, 512], BF16)); BWU = Buf()
            GU = es.enter_context(self.sbuf("rwGU", [128, 512], BF16)); BGU = Buf()
            S.dma("pool", WU[0:64, :], self.dram["rw_w_up"][l], reads=[self.dbuf["rw_w_up"]], writes=[BWU])
            S.dma("pool", WU[64:128, :], self.dram["rw_a_up"][l], reads=[self.dbuf["rw_a_up"]], writes=[BWU])
            S.dma("pool", GU[:], self.dram["rw_g_up"][l], reads=[self.dbuf["rw_g_up"]], writes=[BGU])
            TW = es.enter_context(self.sbuf("rwTW", [128, L], BF16)); BTW = Buf()
            SG = es.enter_context(self.sbuf("rwSG", [128, L], BF16)); BSG = Buf()
            bdn = ["Rt", "Kt", "Bt", "At", "Vb"]
            BD = {n: es.enter_context(self.sbuf("rw" + n, [128, NCH, 128], BF16)) for n in bdn}
            BBD = {n: Buf(n) for n in bdn}
            for n in bdn:
                V(lambda: nc.vector.memset(BD[n][:], 0.0), [], [BBD[n]])
            Dt = es.enter_context(self.sbuf("rwD", [128, NCH], F32)); BD_ = Buf()
            g16 = es.enter_context(self.sbuf("rwg16", [128, L], BF16)); Bg16 = Buf()
            bon = es.enter_context(self.sbuf("rwbon", [128, L], BF16)); Bbon = Buf()
            Yfm = es.enter_context(self.sbuf("rwY", [128, L], F32)); BY = Buf()
            Sall = es.enter_context(self.sbuf("rwSall", [128, NCH + 1, 128], BF16)); BSall = [Buf() for _ in range(NCH + 1)]
            with ExitStack() as e2:
                xin = e2.enter_context(self.sbuf("rwxin", [128, L + 1], BF16)); Bxin = Buf()
                tmp = e2.enter_context(self.sbuf("rwtmp", [128, L], F32)); Btmp = Buf()
                for (r0, mucol, which) in [(1536, 12, 0), (1664, 13, 1)]:
                    V(lambda: nc.vector.memset(xin[:, 0:1], 0.0), [], [Bxin])
                    S.dma("sp", xin[:, 1:L + 1], pT[RW0 + r0:RW0 + r0 + 128, t0:t0 + L], reads=[BpT], writes=[Bxin])
                    V(lambda: nc.vector.tensor_tensor(out=tmp[:], in0=xin[:, 0:L], in1=xin[:, 1:L + 1], op=ALU.subtract), [Bxin], [Btmp])
                    V(lambda: nc.vector.scalar_tensor_tensor(out=tmp[:], in0=tmp[:], scalar=self.vcol(l, "rw_mu", mucol, 1), in1=xin[:, 1:L + 1],
                                                             op0=ALU.mult, op1=ALU.add), [Btmp, Bxin, Bv], [Btmp])
                    if which == 0:
                        A(lambda: nc.scalar.activation(out=TW[0:64, :], in_=tmp[0:64, :], func=AF.Tanh), [Btmp], [BTW])
                        V(lambda: nc.vector.tensor_copy(out=TW[64:128, :], in_=tmp[64:128, :]), [Btmp], [BTW])
                    else:
                        A(lambda: nc.scalar.activation(out=SG[:], in_=tmp[:], func=AF.Sigmoid), [Btmp], [BSG])
                S.barrier()
            for hp in range(4):
                V(lambda: nc.vector.memset(Sall[:, 0, :], 0.0), [], [BSall[0]])
                with ExitStack() as e2:
                    self.rwkv_prologue(e2, l, sq, hp, WU, BWU, GU, BGU, TW, BTW, SG, BSG, BD, BBD, Dt, BD_, g16, Bg16, bon, Bbon)
                    S.barrier()
                with ExitStack() as e2:
                    self.rwkv_chunks(e2, BD, BBD, Dt, BD_, Yfm, BY, Sall, BSall)
                    S.barrier()
                with ExitStack() as e2:
                    self.rwkv_epilogue(e2, l, sq, hp, Yfm, BY, g16, Bg16, bon, Bbon)
                    S.barrier()

    def rwkv_prologue(self, es, l, sq, hp, WU, BWU, GU, BGU, TW, BTW, SG, BSG, BD, BBD, Dt, BDt, g16, Bg16, bon, Bbon):
        nc, S = self.nc, self.S
        V, A, P = self.V, self.A, self.P
        pT, BpT = self.dram["pT"], self.dbuf["pT"]
        Bc, Bv = self.Bconst, self.Bvecs
        t0 = sq * SEQ
        L = SEQ
        NCH = L // 64
        ALPHA = float(np.exp(-0.5))
        cs_hp = slice(128 * hp, 128 * hp + 128)
        def f32t(n):
            return es.enter_context(self.sbuf("rwp" + n, [128, L + 1], F32)), Buf(n)
        xin = [(es.enter_context(self.sbuf("rwpx%d" % i, [128, L + 1], BF16)), Buf()) for i in range(3)]
        (X1, B1), (X2, B2), (X3, B3) = [(es.enter_context(self.sbuf("rwpb%d" % i, [128, L], BF16 if i != 1 else F32)), Buf()) for i in range(3)]
        (X4, B4), (X5, B5), (X6, B6), (X7, B7), (X9, B9), (X10, B10) = [f32t(n) for n in ["4", "5", "6", "7", "9", "10"]]
        for i, (dst, Bd, r0) in enumerate([(X1, B1, 0), (X2, B2, 512), (X3, B3, 1024)]):
            xt, Bx = xin[i]
            V(lambda: nc.vector.memset(xt[:, 0:1], 0.0), [], [Bx])
            S.dma("sp", xt[:, 1:L + 1], pT[RW0 + r0 + 128 * hp:RW0 + r0 + 128 * hp + 128, t0:t0 + L], reads=[BpT], writes=[Bx])
            V(lambda: nc.vector.tensor_tensor(out=X7[:, 0:L], in0=xt[:, 0:L], in1=xt[:, 1:L + 1], op=ALU.subtract), [Bx], [B7])
            V(lambda: nc.vector.scalar_tensor_tensor(out=dst[:], in0=X7[:, 0:L], scalar=self.vcol(l, "rw_mu", 4 * i + hp, 1), in1=xt[:, 1:L + 1],
                                                     op0=ALU.mult, op1=ALU.add), [B7, Bx, Bv], [Bd])
        for tb in range(L // 512):
            cs = slice(tb * 512, tb * 512 + 512)
            pt, Bp = self.psum()
            P(lambda: nc.tensor.matmul(pt[:], lhsT=WU[0:64, cs_hp], rhs=TW[0:64, cs], start=True, stop=True), [BWU, BTW], [Bp])
            A(lambda: nc.scalar.activation(out=X4[:, cs], in_=pt[:], func=AF.Sigmoid, bias=self.vcol(l, "rw_w0", hp, 1)), [Bp, Bv], [B4])
            pt, Bp = self.psum()
            P(lambda: nc.tensor.matmul(pt[:], lhsT=WU[64:128, cs_hp], rhs=TW[64:128, cs], start=True, stop=True), [BWU, BTW], [Bp])
            A(lambda: nc.scalar.activation(out=X5[:, cs], in_=pt[:], func=AF.Sigmoid, bias=self.vcol(l, "rw_a0", hp, 1)), [Bp, Bv], [B5])
            pt, Bp = self.psum()
            P(lambda: nc.tensor.matmul(pt[:], lhsT=GU[:, cs_hp], rhs=SG[:, cs], start=True, stop=True), [BGU, BSG], [Bp])
            V(lambda: nc.vector.tensor_copy(out=g16[:, cs], in_=pt[:]), [Bp], [Bg16])
        V(lambda: nc.vector.memset(X9[:, 0:1], 0.0), [], [B9])
        V(lambda: nc.vector.tensor_tensor_scan(out=X9[:, 1:L + 1], data0=self.cst[:, 0:1].to_broadcast([128, L]), data1=X4[:, 0:L],
                                               initial=0.0, op0=ALU.mult, op1=ALU.add), [B4, Bc], [B9])
        V(lambda: nc.vector.tensor_scalar(out=X6[:, 0:L], in0=X2[:], scalar1=self.vcol(l, "rw_k_k", hp, 1), scalar2=None, op0=ALU.mult), [B2, Bv], [B6])
        A(lambda: nc.scalar.activation(out=X7[:, 0:L], in_=X6[:, 0:L], func=AF.Square), [B6], [B7])
        for tb in range(L // 512):
            cs = slice(tb * 512, tb * 512 + 512)
            pt, Bp = self.psum()
            P(lambda: nc.tensor.matmul(pt[:], lhsT=self.ones_bd[:], rhs=X7[:, cs], start=True, stop=True), [Bc, B7], [Bp])
            A(lambda: nc.scalar.activation(out=X4[:, cs], in_=pt[:], func=AF.Sqrt, bias=self.cst[:, 2:3]), [Bp, Bc, B9], [B4])
        V(lambda: nc.vector.reciprocal(out=X4[:, 0:L], in_=X4[:, 0:L]), [B4], [B4])
        V(lambda: nc.vector.tensor_tensor(out=X6[:, 0:L], in0=X6[:, 0:L], in1=X4[:, 0:L], op=ALU.mult), [B6, B4], [B6])
        V(lambda: nc.vector.tensor_scalar(out=X7[:, 0:L], in0=X5[:, 0:L], scalar1=-1.0, scalar2=self.vcol(l, "rw_k_a", hp, 1), op0=ALU.add, op1=ALU.mult), [B5, Bv], [B7])
        V(lambda: nc.vector.scalar_tensor_tensor(out=X2[:], in0=X7[:, 0:L], scalar=1.0, in1=X2[:], op0=ALU.add, op1=ALU.mult), [B7, B2], [B2])
        V(lambda: nc.vector.tensor_tensor(out=X5[:, 0:L], in0=X6[:, 0:L], in1=X5[:, 0:L], op=ALU.mult), [B6, B5], [B5])
        V(lambda: nc.vector.tensor_tensor(out=X7[:, 0:L], in0=X1[:], in1=X2[:], op=ALU.mult), [B1, B2], [B7])
        V(lambda: nc.vector.tensor_scalar(out=X7[:, 0:L], in0=X7[:, 0:L], scalar1=self.vcol(l, "rw_r_k", hp, 1), scalar2=None, op0=ALU.mult), [B7, Bv], [B7])
        for tb in range(L // 512):
            cs = slice(tb * 512, tb * 512 + 512)
            pt, Bp = self.psum()
            P(lambda: nc.tensor.matmul(pt[:], lhsT=self.ones_bd[:], rhs=X7[:, cs], start=True, stop=True), [Bc, B7], [Bp])
            V(lambda: nc.vector.tensor_tensor(out=bon[:, cs], in0=pt[:], in1=X3[:, cs], op=ALU.mult), [Bp, B3], [Bbon])
        c3 = lambda ap: ap.rearrange("p (c t) -> p c t", t=64)
        base = c3(X9[:, 0:L])[:, :, 0:1].to_broadcast([128, NCH, 64])
        V(lambda: nc.vector.tensor_tensor(out=c3(X7[:, 0:L]), in0=c3(X9[:, 1:L + 1]), in1=base, op=ALU.subtract), [B9, Bbon], [B7])
        A(lambda: nc.scalar.activation(out=X4[:, 0:L], in_=X7[:, 0:L], func=AF.Exp, scale=-ALPHA), [B7], [B4])
        A(lambda: nc.scalar.activation(out=X10[:, 0:L], in_=X7[:, 0:L], func=AF.Exp, scale=ALPHA), [B7], [B10])
        V(lambda: nc.vector.tensor_tensor(out=c3(X7[:, 0:L]), in0=c3(X9[:, 0:L]), in1=base, op=ALU.subtract), [B9, B4, B10], [B7])
        A(lambda: nc.scalar.activation(out=X7[:, 0:L], in_=X7[:, 0:L], func=AF.Exp, scale=-ALPHA), [B7], [B7])
        V(lambda: nc.vector.tensor_copy(out=Dt[:], in_=c3(X4[:, 0:L])[:, :, 63]), [B4], [BDt])
        for hf in range(2):
            ps_ = slice(64 * hf, 64 * hf + 64)
            fs_ = slice(64 * hf, 64 * hf + 64)
            V(lambda: nc.vector.tensor_tensor(out=BD["Rt"][ps_, :, fs_], in0=c3(X1[ps_, :]), in1=c3(X4[ps_, 0:L]), op=ALU.mult), [B1, B4], [BBD["Rt"]])
            V(lambda: nc.vector.tensor_tensor(out=BD["Kt"][ps_, :, fs_], in0=c3(X2[ps_, :]), in1=c3(X10[ps_, 0:L]), op=ALU.mult), [B2, B10], [BBD["Kt"]])
            V(lambda: nc.vector.tensor_tensor(out=BD["Bt"][ps_, :, fs_], in0=c3(X5[ps_, 0:L]), in1=c3(X10[ps_, 0:L]), op=ALU.mult), [B5, B10], [BBD["Bt"]])
            V(lambda: nc.vector.scalar_tensor_tensor(out=BD["At"][ps_, :, fs_], in0=c3(X6[ps_, 0:L]), scalar=-1.0, in1=c3(X7[ps_, 0:L]), op0=ALU.mult, op1=ALU.mult),
              [B6, B7], [BBD["At"]])
            A(lambda: nc.scalar.copy(out=BD["Vb"][ps_, :, fs_], in_=c3(X3[ps_, :])), [B3], [BBD["Vb"]])

    def rwkv_chunks(self, es, BD, BBD, Dt, BDt, Yfm, BY, Sall, BSall):
        nc, S = self.nc, self.S
        V, A, P = self.V, self.A, self.P
        Bc = self.Bconst
        NCH = SEQ // 64
        W = 4
        NU = NCH // 2
        Rt, Kt, Bt, At, Vb = [BD[n] for n in ["Rt", "Kt", "Bt", "At", "Vb"]]
        BRt, BKt, BBt, BAt, BVb = [BBD[n] for n in ["Rt", "Kt", "Bt", "At", "Vb"]]
        NR = 2 * W
        TM3 = self.tiles(es, "rcTM", [128, 2, 3, 128], BF16, NR)
        ZA = self.tiles(es, "rcZA", [128, 2, 256], BF16, NR)
        ZB = self.tiles(es, "rcZB", [128, 2, 256], BF16, NR)
        SC1 = self.tiles(es, "rcS1", [128, 2, 2, 128], BF16, NR)
        SC2 = self.tiles(es, "rcS2", [128, 2, 2, 128], BF16, NR)
        SC3 = self.tiles(es, "rcS3", [128, 2, 128], BF16, NR)
        PPA = self.tiles(es, "rcPA", [128, 2, 2, 128], BF16, NR)
        PPB = self.tiles(es, "rcPB", [128, 2, 2, 128], BF16, NR)
        IGQ = self.tiles(es, "rcIG", [128, 2, 2, 128], BF16, NR)
        HD = self.tiles(es, "rcHD", [128, 2, 128], F32, NR)
        m01 = self.mbd[:, 0:2, :].unsqueeze(1).to_broadcast([128, 2, 2, 128])
        m12 = self.mbd[:, 1:3, :].unsqueeze(1).to_broadcast([128, 2, 2, 128])
        m2 = self.mbd[:, 2:3, :].to_broadcast([128, 2, 128])
        identb = self.ident_f[:].unsqueeze(1).to_broadcast([128, 2, 128])

        pending = []
        post = []

        def drain(n):
            for _ in range(n):
                if pending:
                    pending.pop(0)()

        ngroups = NU // W
        for gi in range(ngroups):
            units = list(range(gi * W, gi * W + W))
            sl = {u: (u % NR) for u in units}
            for u in units:
                i = sl[u]; c0 = 2 * u
                pt, Bp = self.psum()
                pb = pt[:].bitcast(BF16).rearrange("p (k s t) -> p k s t", k=2, s=4)
                for k in range(2):
                    for si, (src, Bs) in enumerate([(Bt, BBt), (Kt, BKt), (Vb, BVb), (At, BAt)]):
                        P(lambda: nc.tensor.transpose(out=pb[:, k, si, :], in_=src[:, c0 + k, :], identity=self.ident_b[:]), [Bs, Bc], [Bp])
                A(lambda: nc.scalar.copy(out=TM3[i][0][:], in_=pb[:, :, 0:3, :]), [Bp], [TM3[i][1]])
                A(lambda: nc.scalar.copy(out=ZA[i][0][:, :, 0:128], in_=pb[:, :, 3, :]), [Bp], [ZA[i][1]])
            drain(1)
            for u in units:
                i = sl[u]; c0 = 2 * u
                pt, Bp = self.psum(); p4 = pt[:].rearrange("p (k s t) -> p k s t", k=2, s=2)
                for k in range(2):
                    c = c0 + k
                    P(lambda: nc.tensor.matmul(p4[:, k, 0, :], lhsT=At[:, c, :], rhs=Bt[:, c, :], start=True, stop=True), [BAt, BBt], [Bp])
                    P(lambda: nc.tensor.matmul(p4[:, k, 1, :], lhsT=Bt[:, c, :], rhs=At[:, c, :], start=True, stop=True), [BAt, BBt], [Bp])
                V(lambda: nc.vector.tensor_tensor(out=SC1[i][0][:], in0=p4, in1=m01, op=ALU.mult), [Bp, Bc], [SC1[i][1]])
                pt, Bp = self.psum(); p4 = pt[:].rearrange("p (k s t) -> p k s t", k=2, s=2)
                for k in range(2):
                    c = c0 + k
                    P(lambda: nc.tensor.matmul(p4[:, k, 0, :], lhsT=Kt[:, c, :], rhs=At[:, c, :], start=True, stop=True), [BAt, BKt], [Bp])
                    P(lambda: nc.tensor.matmul(p4[:, k, 1, :], lhsT=Bt[:, c, :], rhs=Rt[:, c, :], start=True, stop=True), [BRt, BBt], [Bp])
                V(lambda: nc.vector.tensor_tensor(out=SC2[i][0][:], in0=p4, in1=m12, op=ALU.mult), [Bp, Bc], [SC2[i][1]])
                pt, Bp = self.psum(); p3 = pt[:, 0:256].rearrange("p (k t) -> p k t", k=2)
                for k in range(2):
                    c = c0 + k
                    P(lambda: nc.tensor.matmul(p3[:, k, :], lhsT=Kt[:, c, :], rhs=Rt[:, c, :], start=True, stop=True), [BRt, BKt], [Bp])
                V(lambda: nc.vector.tensor_tensor(out=SC3[i][0][:], in0=p3, in1=m2, op=ALU.mult), [Bp, Bc], [SC3[i][1]])
            drain(1)
            for u in units:
                i = sl[u]
                pt, Bp = self.psum(); p3 = pt[:, 0:256].rearrange("p (k t) -> p k t", k=2)
                for k in range(2):
                    P(lambda: nc.tensor.matmul(p3[:, k, :], lhsT=SC2[i][0][:, k, 0, :], rhs=TM3[i][0][:, k, 2, :], start=True, stop=True), [SC2[i][1], TM3[i][1]], [Bp])
                A(lambda: nc.scalar.copy(out=ZA[i][0][:, :, 128:256], in_=p3), [Bp], [ZA[i][1]])
            drain(1)
            for lev in range(6):
                for u in units:
                    i = sl[u]
                    PPs = SC1[i] if lev == 0 else (PPA[i] if lev % 2 == 1 else PPB[i])
                    PPd = PPA[i] if lev % 2 == 0 else PPB[i]
                    Zs = ZA[i] if lev % 2 == 0 else ZB[i]
                    Zd = ZB[i] if lev % 2 == 0 else ZA[i]
                    pt, Bp = self.psum(); pz = pt[:].rearrange("p (k t) -> p k t", k=2)
                    if lev % 2 == 1:
                        for k in range(2):
                            P(lambda: nc.tensor.matmul(pz[:, k, :], lhsT=self.ident_b[:], rhs=Zs[0][:, k, :], start=True, stop=False), [Bc, Zs[1]], [Bp])
                            P(lambda: nc.tensor.matmul(pz[:, k, :], lhsT=PPs[0][:, k, 1, :], rhs=Zs[0][:, k, :], start=False, stop=True), [PPs[1], Zs[1]], [Bp])
                        A(lambda: nc.scalar.copy(out=Zd[0][:], in_=pz), [Bp], [Zd[1]])
                    else:
                        for k in range(2):
                            P(lambda: nc.tensor.matmul(pz[:, k, :], lhsT=PPs[0][:, k, 1, :], rhs=Zs[0][:, k, :], start=True, stop=True), [PPs[1], Zs[1]], [Bp])
                        V(lambda: nc.vector.tensor_tensor(out=Zd[0][:], in0=pz, in1=Zs[0][:], op=ALU.add), [Bp, Zs[1]], [Zd[1]])
                    if lev < 5:
                        pt, Bp = self.psum(); p4 = pt[:].rearrange("p (k s t) -> p k s t", k=2, s=2)
                        for k in range(2):
                            P(lambda: nc.tensor.matmul(p4[:, k, 0, :], lhsT=PPs[0][:, k, 1, :], rhs=PPs[0][:, k, 0, :], start=True, stop=True), [PPs[1]], [Bp])
                            P(lambda: nc.tensor.matmul(p4[:, k, 1, :], lhsT=PPs[0][:, k, 0, :], rhs=PPs[0][:, k, 1, :], start=True, stop=True), [PPs[1]], [Bp])
                        A(lambda: nc.scalar.copy(out=PPd[0][:], in_=p4), [Bp], [PPd[1]])
                drain(1)
            for u in units:
                i = sl[u]; c0 = 2 * u
                pt, Bp = self.psum(); p4 = pt[:].rearrange("p (k s t) -> p k s t", k=2, s=2)
                for k in range(2):
                    P(lambda: nc.tensor.matmul(p4[:, k, 0, :], lhsT=ZA[i][0][:, k, 0:128], rhs=TM3[i][0][:, k, 0, :], start=True, stop=True), [ZA[i][1], TM3[i][1]], [Bp])
                    P(lambda: nc.tensor.matmul(p4[:, k, 1, :], lhsT=ZA[i][0][:, k, 0:128], rhs=SC2[i][0][:, k, 1, :], start=True, stop=True), [ZA[i][1], SC2[i][1]], [Bp])
                V(lambda: nc.vector.tensor_tensor(out=IGQ[i][0][:, :, 0, :], in0=p4[:, :, 0, :], in1=identb, op=ALU.add), [Bp, Bc], [IGQ[i][1]])
                V(lambda: nc.vector.tensor_tensor(out=IGQ[i][0][:, :, 1, :], in0=p4[:, :, 1, :], in1=Rt[:, c0:c0 + 2, :], op=ALU.add), [Bp, BRt], [IGQ[i][1]])
            drain(1)
            for u in units:
                i = sl[u]; c0 = 2 * u
                pt, Bp = self.psum(); p3 = pt[:, 0:256].rearrange("p (k t) -> p k t", k=2)
                for k in range(2):
                    P(lambda: nc.tensor.matmul(p3[:, k, :], lhsT=TM3[i][0][:, k, 0, :], rhs=ZA[i][0][:, k, 128:256], start=True, stop=False), [ZA[i][1], TM3[i][1]], [Bp])
                    P(lambda: nc.tensor.matmul(p3[:, k, :], lhsT=TM3[i][0][:, k, 1, :], rhs=TM3[i][0][:, k, 2, :], start=False, stop=True), [TM3[i][1]], [Bp])
                for k in range(2):
                    A(lambda: nc.scalar.activation(out=HD[i][0][:, k, :], in_=p3[:, k, :], func=AF.Copy, scale=Dt[:, c0 + k:c0 + k + 1]), [Bp, BDt], [HD[i][1]])
                pt, Bp = self.psum(); p3 = pt[:, 0:256].rearrange("p (k t) -> p k t", k=2)
                for k in range(2):
                    P(lambda: nc.tensor.matmul(p3[:, k, :], lhsT=ZA[i][0][:, k, 128:256], rhs=SC2[i][0][:, k, 1, :], start=True, stop=False), [ZA[i][1], SC2[i][1]], [Bp])
                    P(lambda: nc.tensor.matmul(p3[:, k, :], lhsT=TM3[i][0][:, k, 2, :], rhs=SC3[i][0][:, k, :], start=False, stop=True), [TM3[i][1], SC3[i][1]], [Bp])
                for hf in range(2):
                    hs = slice(64 * hf, 64 * hf + 64)
                    A(lambda: nc.scalar.copy(out=Yfm[hs, 128 * u:128 * u + 128].rearrange("p (k t) -> p k t", k=2), in_=p3[hs, :, hs]), [Bp], [BY])
            drain(len(pending))
            for f in post:
                f()
            post = []
            for u in units:
                i = sl[u]; c0 = 2 * u
                for k in range(2):
                    def link(i=i, c=c0 + k, k=k):
                        pt, Bp = self.psum()
                        P(lambda: nc.tensor.matmul(pt[:, 0:128], lhsT=IGQ[i][0][:, k, 0, :], rhs=Sall[:, c, :], start=True, stop=True), [IGQ[i][1], BSall[c]], [Bp])
                        V(lambda: nc.vector.scalar_tensor_tensor(out=Sall[:, c + 1, :], in0=pt[:, 0:128], scalar=Dt[:, c:c + 1], in1=HD[i][0][:, k, :],
                                                                 op0=ALU.mult, op1=ALU.add), [Bp, BDt, HD[i][1]], [BSall[c + 1]])
                    pending.append(link)
                def ph8(i=i, u=u, c0=c0):
                    pt, Bp = self.psum(); p3 = pt[:, 0:256].rearrange("p (k t) -> p k t", k=2)
                    for k in range(2):
                        P(lambda: nc.tensor.matmul(p3[:, k, :], lhsT=Sall[:, c0 + k, :], rhs=IGQ[i][0][:, k, 1, :], start=True, stop=True), [BSall[c0 + k], IGQ[i][1]], [Bp])
                    for hf in range(2):
                        hs = slice(64 * hf, 64 * hf + 64)
                        yv = Yfm[hs, 128 * u:128 * u + 128].rearrange("p (k t) -> p k t", k=2)
                        V(lambda: nc.vector.tensor_tensor(out=yv, in0=p3[hs, :, hs], in1=yv, op=ALU.add), [Bp, BY], [BY])
                post.append(ph8)
        drain(len(pending))
        for f in post:
            f()

    def rwkv_epilogue(self, es, l, sq, hp, Yfm, BY, g16, Bg16, bon, Bbon):
        nc, S = self.nc, self.S
        V, A, P = self.V, self.A, self.P
        ym, Bym = self.dram["ymix"], self.dbuf["ymix"]
        Bc, Bv = self.Bconst, self.Bvecs
        t0 = sq * SEQ
        L = SEQ
        NB = L // 512
        ta = self.tiles(es, "reA", [128, 512], F32, NB)
        tb_ = self.tiles(es, "reB", [128, 512], F32, NB)
        yo = self.tiles(es, "reO", [128, 512], BF16, NB)
        CS = [slice(tb * 512, tb * 512 + 512) for tb in range(NB)]
        pts = {}
        for tb in range(NB):
            pts[tb] = self.psum()
            pt, Bp = pts[tb]
            P(lambda: nc.tensor.matmul(pt[:], lhsT=self.ones_bd[:], rhs=Yfm[:, CS[tb]], start=True, stop=True), [Bc, BY], [Bp])
        for tb in range(NB):
            pt, Bp = pts[tb]; a_, Ba = ta[tb]
            V(lambda: nc.vector.scalar_tensor_tensor(out=a_[:], in0=pt[:], scalar=-1.0 / 64, in1=Yfm[:, CS[tb]], op0=ALU.mult, op1=ALU.add), [Bp, BY], [Ba])
        for tb in range(NB):
            a_, Ba = ta[tb]; b_, Bb = tb_[tb]
            A(lambda: nc.scalar.activation(out=b_[:], in_=a_[:], func=AF.Square), [Ba], [Bb])
        for tb in range(NB):
            b_, Bb = tb_[tb]
            pts[tb] = self.psum()
            pt, Bp = pts[tb]
            P(lambda: nc.tensor.matmul(pt[:], lhsT=self.ones_bd[:], rhs=b_[:], start=True, stop=True), [Bc, Bb], [Bp])
        for tb in range(NB):
            pt, Bp = pts[tb]; b_, Bb = tb_[tb]
            A(lambda: nc.scalar.activation(out=b_[:], in_=pt[:], func=AF.Sqrt, scale=1.0 / 64, bias=self.cst[:, 3:4]), [Bp, Bc], [Bb])
        for tb in range(NB):
            b_, Bb = tb_[tb]
            V(lambda: nc.vector.reciprocal(out=b_[:], in_=b_[:]), [Bb], [Bb])
        for tb in range(NB):
            a_, Ba = ta[tb]; b_, Bb = tb_[tb]
            V(lambda: nc.vector.tensor_tensor(out=a_[:], in0=a_[:], in1=b_[:], op=ALU.mult), [Ba, Bb], [Ba])
        for tb in range(NB):
            a_, Ba = ta[tb]
            V(lambda: nc.vector.tensor_scalar(out=a_[:], in0=a_[:], scalar1=self.vcol(l, "rw_ln_w", hp, 1), scalar2=self.vcol(l, "rw_ln_b", hp, 1), op0=ALU.mult, op1=ALU.add),
              [Ba, Bv], [Ba])
        for tb in range(NB):
            a_, Ba = ta[tb]
            V(lambda: nc.vector.tensor_tensor(out=a_[:], in0=a_[:], in1=bon[:, CS[tb]], op=ALU.add), [Ba, Bbon], [Ba])
        for tb in range(NB):
            a_, Ba = ta[tb]; o_, Bo = yo[tb]
            V(lambda: nc.vector.tensor_tensor(out=o_[:], in0=a_[:], in1=g16[:, CS[tb]], op=ALU.mult), [Ba, Bg16], [Bo])
            S.dma("sp", ym[0, 128 * hp:128 * hp + 128, t0 + tb * 512:t0 + tb * 512 + 512], o_[:], reads=[Bo], writes=[Bym])

    def load_w(self, wt, Bw_list, src3, Bsrc, kc_n, step=4):
        for k0 in range(0, kc_n, step):
            k1 = min(kc_n, k0 + step)
            self.S.dma("pool", wt[:, k0:k1, :], src3[:, k0:k1, :], reads=[Bsrc], writes=[Bw_list[k0 // step]])

    def outproj_res(self, actf, Bact, kc_n, w, Bw_of, xt, Bx, ntt, dst_ap, Bdst, tok0, kstep=4):
        nc, S = self.nc, self.S
        for j in range(ntt):
            for fh in range(2):
                pt, Bp = self.psum()
                for kc in range(kc_n):
                    ba = Bact(kc) if callable(Bact) else Bact
                    self.P(lambda: nc.tensor.matmul(pt[:], lhsT=actf(kc, j), rhs=w[:, kc, fh * 512:(fh + 1) * 512], start=(kc == 0), stop=(kc == kc_n - 1)),
                           [ba, Bw_of(kc)], [Bp])
                self.V(lambda: nc.vector.tensor_tensor(out=xt[:, j, fh * 512:(fh + 1) * 512], in0=pt[:], in1=xt[:, j, fh * 512:(fh + 1) * 512], op=ALU.add),
                       [Bp, Bx[j]], [Bx[j]])
            S.dma("sp", dst_ap[tok0 + j * 128:tok0 + (j + 1) * 128, :], xt[:, j, :], reads=[Bx[j]], writes=[Bdst])

    def xsrc(self, l):
        return (self.dram["x"], self.dbuf["x"]) if l == 0 else (self.dram["xres"], self.dbuf["xres"])

    def stage_merge(self, l):
        nc, S = self.nc, self.S
        V, A, P = self.V, self.A, self.P
        pT, BpT = self.dram["pT"], self.dbuf["pT"]
        ym, Bym = self.dram["ymix"], self.dbuf["ymix"]
        xs_ap, Bxs = self.xsrc(l)
        xd_ap, Bxd = self.dram["xres"], self.dbuf["xres"]
        L = SEQ
        with ExitStack() as es:
            wb = es.enter_context(self.sbuf("mgwb", [128, 12, D], BF16)); Bwb = [Buf() for _ in range(3)]
            wo = es.enter_context(self.sbuf("mgwo", [128, 8, D], BF16)); Bwo = [Buf() for _ in range(2)]
            for i in range(3):
                S.dma("pool", wb[:, 4 * i:4 * i + 4, :], self.dram["w_branch"][l, i].rearrange("(k p) c -> p k c", p=128), reads=[self.dbuf["w_branch"]], writes=[Bwb[i]])
            self.load_w(wo, Bwo, self.dram["w_out"][l].rearrange("(k p) c -> p k c", p=128), self.dbuf["w_out"], 8)
            yt = es.enter_context(self.sbuf("mgy", [128, 12, L], BF16)); Byt = [Buf() for _ in range(12)]
            mg = es.enter_context(self.sbuf("mgm", [128, 8, L], BF16)); Bmg = Buf()
            gts = self.tiles(es, "mgg", [128, 3, 512], BF16, 2)
            acc = self.tiles(es, "mga", [128, 512], F32, 2)
            tmp = self.tiles(es, "mgt", [128, 512], F32, 2)
            xts = [(es.enter_context(self.sbuf("mgx%d" % i, [128, 4, D], F32)), [Buf() for _ in range(4)]) for i in range(2)]
            for sq in range(NSEQ):
                t0 = sq * L
                for i in range(3):
                    for kt in range(4):
                        S.dma("sp", yt[:, 4 * i + kt, :], ym[i, 128 * kt:128 * kt + 128, t0:t0 + L], reads=[Bym], writes=[Byt[4 * i + kt]])
                cnt = 0
                for ft in range(8):
                    for tb in range(L // 512):
                        cs = slice(tb * 512, tb * 512 + 512)
                        g_, Bg = gts[cnt % 2]; a_, Ba = acc[cnt % 2]; t_, Bt = tmp[cnt % 2]; cnt += 1
                        S.dma("sp", g_[:], pT[GT0 + 128 * ft:GT0 + 3072:1024, t0 + tb * 512:t0 + tb * 512 + 512].rearrange("(i p) t -> p i t", p=128) if False else
                              bass.AP(tensor=pT.tensor, offset=pT[GT0 + 128 * ft, t0 + tb * 512].offset, ap=[[T, 128], [1024 * T, 3], [1, 512]]),
                              reads=[BpT], writes=[Bg])
                        for i in range(3):
                            pt, Bp = self.psum()
                            for kt in range(4):
                                P(lambda: nc.tensor.matmul(pt[:], lhsT=wb[:, 4 * i + kt, ft * 128:(ft + 1) * 128], rhs=yt[:, 4 * i + kt, cs], start=(kt == 0), stop=(kt == 3)),
                                  [Bwb[i], Byt[4 * i + kt]], [Bp])
                            if i == 0:
                                V(lambda: nc.vector.tensor_tensor(out=a_[:], in0=pt[:], in1=g_[:, 0, :], op=ALU.mult), [Bp, Bg], [Ba])
                            else:
                                V(lambda: nc.vector.tensor_tensor(out=t_[:], in0=pt[:], in1=g_[:, i, :], op=ALU.mult), [Bp, Bg], [Bt])
                                if i == 1:
                                    V(lambda: nc.vector.tensor_tensor(out=a_[:], in0=a_[:], in1=t_[:], op=ALU.add), [Ba, Bt], [Ba])
                                else:
                                    V(lambda: nc.vector.tensor_tensor(out=mg[:, ft, cs], in0=a_[:], in1=t_[:], op=ALU.add), [Ba, Bt], [Bmg])
                def ldx(blk):
                    xt, Bx = xts[blk % 2]
                    tok0 = t0 + blk * 512
                    for j in range(4):
                        S.dma("sp", xt[:, j, :], xs_ap[tok0 + j * 128:tok0 + (j + 1) * 128, :], reads=[Bxs], writes=[Bx[j]])
                ldx(0)
                for blk in range(L // 512):
                    xt, Bx = xts[blk % 2]
                    tok0 = t0 + blk * 512
                    if blk + 1 < L // 512:
                        ldx(blk + 1)
                    self.outproj_res(lambda kc, j: mg[:, kc, blk * 512 + j * 128:blk * 512 + (j + 1) * 128], Bmg, 8, wo, lambda kc: Bwo[kc // 4], xt, Bx, 4, xd_ap, Bxd, tok0)
                S.barrier()

    def stage_memn(self):
        nc, S = self.nc, self.S
        self.memn = nc.alloc_sbuf_tensor("memn", [128, 8, NSEQ * NMEM], BF16); self.Bmemn = Buf("memn")
        with ExitStack() as es:
            self.rmsnorm_fm(es, self.dram["mem"], self.dbuf["mem"], NSEQ * NMEM, self.gvec[:, 0:8], self.memn, self.Bmemn, self.eps6[:, 0:1])
            S.barrier()

    def stage_xattn(self, l):
        nc, S = self.nc, self.S
        V, A, P = self.V, self.A, self.P
        Bc = self.Bconst
        xs_ap, Bxs = self.dram["xres"], self.dbuf["xres"]
        NM = NSEQ * NMEM
        with ExitStack() as es:
            kT = es.enter_context(self.sbuf("xakT", [128, 8, NM], BF16)); BkT = Buf()
            vtm = es.enter_context(self.sbuf("xavtm", [128, NM // 128, D], BF16)); Bvtm = Buf()
            wq = es.enter_context(self.sbuf("xawq", [128, 8, D], BF16)); Bwq = [Buf() for _ in range(2)]
            wo = es.enter_context(self.sbuf("xawo", [128, 8, D], BF16)); Bwo = [Buf() for _ in range(2)]
            ones_b = es.enter_context(self.sbuf("xaones", [128, 128], BF16)); Bob = Buf()
            V(lambda: nc.vector.memset(ones_b[:], 1.0), [], [Bob])
            with ExitStack() as e2:
                wts = [(e2.enter_context(self.sbuf("xawkv%d" % i, [128, 8, 512], BF16)), [Buf(), Buf()]) for i in range(2)]
                wkv = self.dram["xa_wkv"][l]
                for g in range(4):
                    wt, Bw = wts[g % 2]
                    self.load_w(wt, Bw, wkv[:, g * 512:(g + 1) * 512].rearrange("(k p) c -> p k c", p=128), self.dbuf["xa_wkv"], 8)
                    if g < 2:
                        for m in range(4):
                            pt, Bp = self.psum()
                            for kc in range(8):
                                P(lambda: nc.tensor.matmul(pt[:, 0:NM], lhsT=wt[:, kc, m * 128:(m + 1) * 128], rhs=self.memn[:, kc, :], start=(kc == 0), stop=(kc == 7)),
                                  [Bw[kc // 4], self.Bmemn], [Bp])
                            A(lambda: nc.scalar.copy(out=kT[:, g * 4 + m, :], in_=pt[:, 0:NM]), [Bp], [BkT])
                    else:
                        for mt in range(NM // 128):
                            pt, Bp = self.psum()
                            for kc in range(8):
                                P(lambda: nc.tensor.matmul(pt[:], lhsT=self.memn[:, kc, mt * 128:(mt + 1) * 128], rhs=wt[:, kc, :], start=(kc == 0), stop=(kc == 7)),
                                  [Bw[kc // 4], self.Bmemn], [Bp])
                            A(lambda: nc.scalar.copy(out=vtm[:, mt, (g - 2) * 512:(g - 1) * 512], in_=pt[:]), [Bp], [Bvtm])
                S.barrier()
            self.load_w(wq, Bwq, self.dram["xa_wq"][l].rearrange("(k p) c -> p k c", p=128), self.dbuf["xa_wq"], 8)
            self.load_w(wo, Bwo, self.dram["xa_wo"][l].rearrange("(k p) c -> p k c", p=128), self.dbuf["xa_wo"], 8)
            xts = [(es.enter_context(self.sbuf("xax%d" % i, [128, 4, D], F32)), [Buf() for _ in range(4)]) for i in range(2)]
            hTs = self.tiles(es, "xah", [128, 8, 512], BF16, 2)
            qTs = self.tiles(es, "xaq", [128, 2, 512], BF16, 2)
            Es = self.tiles(es, "xaE", [128, 2, 512], BF16, 2)
            rd = self.tiles(es, "xard", [128, 512], F32, 2)
            oTs = self.tiles(es, "xao", [128, 8, 512], BF16, 2)
            xss = self.tiles(es, "xaxs", [128, D], BF16, 2)
            junk = es.enter_context(self.sbuf("xajunk", [128, D], BF16)); Bjunk = Buf()
            st = self.tiles(es, "xast", [128, 4], F32, 2)
            cnt = 0
            hc = 0
            for blk in range(T // 512):
                sq = (blk * 512) // SEQ
                tok0 = blk * 512
                xt, Bx = xts[blk % 2]
                hT, BhT = hTs[blk % 2]
                oT, BoT = oTs[blk % 2]
                if blk == 0:
                    for j in range(4):
                        S.dma("sp", xt[:, j, :], xs_ap[tok0 + j * 128:tok0 + (j + 1) * 128, :], reads=[Bxs], writes=[Bx[j]])
                if blk + 1 < T // 512:
                    xtn, Bxn = xts[(blk + 1) % 2]
                    for j in range(4):
                        S.dma("sp", xtn[:, j, :], xs_ap[tok0 + 512 + j * 128:tok0 + 512 + (j + 1) * 128, :], reads=[Bxs], writes=[Bxn[j]])
                for j in range(4):
                    s_, Bs = st[cnt % 2]; xs, Bxs_ = xss[cnt % 2]; cnt += 1
                    A(lambda: nc.scalar.activation(out=junk[:], in_=xt[:, j, :], func=AF.Square, accum_out=s_[:, 0:1]), [Bx[j]], [Bjunk, Bs])
                    A(lambda: nc.scalar.activation(out=s_[:, 1:2], in_=s_[:, 0:1], func=AF.Sqrt, scale=1.0 / D, bias=self.eps6[:, 0:1]), [Bs, Bc], [Bs])
                    V(lambda: nc.vector.reciprocal(out=s_[:, 2:3], in_=s_[:, 1:2]), [Bs], [Bs])
                    V(lambda: nc.vector.tensor_scalar(out=xs[:], in0=xt[:, j, :], scalar1=s_[:, 2:3], scalar2=None, op0=ALU.mult), [Bx[j], Bs], [Bxs_])
                    pt, Bp = self.psum()
                    pb = pt[:].bitcast(BF16)
                    for kc in range(8):
                        P(lambda: nc.tensor.transpose(out=pb[:, kc * 128:(kc + 1) * 128], in_=xs[:, kc * 128:(kc + 1) * 128], identity=self.ident_b[:]), [Bxs_, Bc], [Bp])
                    V(lambda: nc.vector.tensor_tensor(out=hT[:, :, j * 128:(j + 1) * 128], in0=pb.rearrange("p (k t) -> p k t", k=8),
                                                      in1=self.vcol(l, "norm_xattn", 0, 8).unsqueeze(2).to_broadcast([128, 8, 128]), op=ALU.mult), [Bp, self.Bvecs], [BhT])
                for h in range(4):
                    qT, BqT = qTs[hc % 2]; E, BE = Es[hc % 2]; r_, Br = rd[hc % 2]; hc += 1
                    for dt in range(2):
                        pt, Bp = self.psum()
                        c0 = h * 256 + dt * 128
                        for kc in range(8):
                            P(lambda: nc.tensor.matmul(pt[:], lhsT=wq[:, kc, c0:c0 + 128], rhs=hT[:, kc, :], start=(kc == 0), stop=(kc == 7)), [Bwq[kc // 4], BhT], [Bp])
                        A(lambda: nc.scalar.copy(out=qT[:, dt, :], in_=pt[:]), [Bp], [BqT])
                    for mt in range(2):
                        ms = slice(sq * NMEM + mt * 128, sq * NMEM + (mt + 1) * 128)
                        pt, Bp = self.psum()
                        for dt in range(2):
                            P(lambda: nc.tensor.matmul(pt[:], lhsT=kT[:, h * 2 + dt, ms], rhs=qT[:, dt, :], start=(dt == 0), stop=(dt == 1)), [BkT, BqT], [Bp])
                        A(lambda: nc.scalar.activation(out=E[:, mt, :], in_=pt[:], func=AF.Exp, scale=1.0 / 16), [Bp], [BE])
                    pt, Bp = self.psum()
                    for mt in range(2):
                        P(lambda: nc.tensor.matmul(pt[:], lhsT=ones_b[:], rhs=E[:, mt, :], start=(mt == 0), stop=(mt == 1)), [Bob, BE], [Bp])
                    V(lambda: nc.vector.reciprocal(out=r_[:], in_=pt[:]), [Bp], [Br])
                    for dt in range(2):
                        pt, Bp = self.psum()
                        c0 = h * 256 + dt * 128
                        for mt in range(2):
                            P(lambda: nc.tensor.matmul(pt[:], lhsT=vtm[:, sq * 2 + mt, c0:c0 + 128], rhs=E[:, mt, :], start=(mt == 0), stop=(mt == 1)), [Bvtm, BE], [Bp])
                        V(lambda: nc.vector.tensor_tensor(out=oT[:, h * 2 + dt, :], in0=pt[:], in1=r_[:], op=ALU.mult), [Bp, Br], [BoT])
                self.outproj_res(lambda kc, j: oT[:, kc, j * 128:(j + 1) * 128], BoT, 8, wo, lambda kc: Bwo[kc // 4], xt, Bx, 4, xs_ap, Bxs, tok0)
            S.barrier()

    def stage_ffn(self, l):
        nc, S = self.nc, self.S
        V, A, P = self.V, self.A, self.P
        Bc, Bv = self.Bconst, self.Bvecs
        xs_ap, Bxs = self.dram["xres"], self.dbuf["xres"]
        aT, BaT = self.dram["actT"], self.dbuf["actT"]
        L = SEQ
        NP_ = DFF // 128
        with ExitStack() as es:
            hT = es.enter_context(self.sbuf("ffh", [128, 8, T], BF16)); BhT = Buf("ffh")
            with ExitStack() as e2:
                self.rmsnorm_fm(e2, xs_ap, Bxs, T, self.vcol(l, "norm_ffn", 0, 8), hT, BhT, self.eps6[:, 0:1])
                S.barrier()
            with ExitStack() as e2:
                wts = [(e2.enter_context(self.sbuf("ffw%d" % i, [128, 8, 256], BF16)), [Buf(), Buf()]) for i in range(2)]
                ug = self.tiles(e2, "ffug", [128, L + 2], BF16, 3)
                uv = self.tiles(e2, "ffuv", [128, L + 2], BF16, 3)
                dgs = self.tiles(e2, "ffdg", [128, 2, 3, 128], BF16, 2)
                sg = self.tiles(e2, "ffsg", [128, 512], F32, 4)
                ao = self.tiles(e2, "ffao", [128, L], BF16, 2)
                for t_, B_ in ug + uv:
                    V(lambda: nc.vector.memset(t_[:, 0:2], 0.0), [], [B_])
                wup = self.dram["ffn_w_up"][l]
                cnt = 0
                sc_ = 0
                for i in range(NP_):
                    wt, Bw = wts[i % 2]
                    dg, Bdg = dgs[i % 2]
                    S.dma("pool", wt[:, :, 0:128], wup[:, 128 * i:128 * i + 128].rearrange("(k p) c -> p k c", p=128), reads=[self.dbuf["ffn_w_up"]], writes=[Bw[0]])
                    S.dma("pool", wt[:, :, 128:256], wup[:, DFF + 128 * i:DFF + 128 * i + 128].rearrange("(k p) c -> p k c", p=128), reads=[self.dbuf["ffn_w_up"]], writes=[Bw[1]])
                    for half in range(2):
                        col = i + half * NP_
                        for j in range(3):
                            V(lambda: nc.vector.tensor_scalar(out=dg[:, half, j, :], in0=self.ident_f[:], scalar1=self.vcol(l, "ffn_conv%d" % j, col, 1), scalar2=None, op0=ALU.mult),
                              [Bc, Bv], [Bdg])
                    for sq in range(NSEQ):
                        g_, Bg = ug[cnt % 3]; v_, Bvv = uv[cnt % 3]; a_, Ba = ao[cnt % 2]
                        cnt += 1
                        for tb in range(L // 512):
                            ts = slice(sq * L + tb * 512, sq * L + tb * 512 + 512)
                            for half, (dst, Bd) in enumerate([(g_, Bg), (v_, Bvv)]):
                                pt, Bp = self.psum()
                                for kc in range(8):
                                    P(lambda: nc.tensor.matmul(pt[:], lhsT=wt[:, kc, half * 128:(half + 1) * 128], rhs=hT[:, kc, ts], start=(kc == 0), stop=(kc == 7)),
                                      [Bw[half], BhT], [Bp])
                                A(lambda: nc.scalar.copy(out=dst[:, 2 + tb * 512:2 + tb * 512 + 512], in_=pt[:]), [Bp], [Bd])
                        for tb in range(L // 512):
                            s_, Bs = sg[sc_ % 4]; sc_ += 1
                            ptg, Bpg = self.psum()
                            for j in range(3):
                                P(lambda: nc.tensor.matmul(ptg[:], lhsT=dg[:, 0, j, :], rhs=g_[:, tb * 512 + j:tb * 512 + j + 512], start=(j == 0), stop=(j == 2)), [Bdg, Bg], [Bpg])
                            ptv, Bpv = self.psum()
                            for j in range(3):
                                P(lambda: nc.tensor.matmul(ptv[:], lhsT=dg[:, 1, j, :], rhs=v_[:, tb * 512 + j:tb * 512 + j + 512], start=(j == 0), stop=(j == 2)), [Bdg, Bvv], [Bpv])
                            A(lambda: nc.scalar.activation(out=s_[:], in_=ptg[:], func=AF.Sigmoid, bias=self.vcol(l, "ffn_conv_b", i, 1)), [Bpg, Bv], [Bs])
                            V(lambda: nc.vector.scalar_tensor_tensor(out=s_[:], in0=ptg[:], scalar=self.vcol(l, "ffn_conv_b", i, 1), in1=s_[:], op0=ALU.add, op1=ALU.mult),
                              [Bpg, Bs, Bv], [Bs])
                            V(lambda: nc.vector.scalar_tensor_tensor(out=a_[:, tb * 512:tb * 512 + 512], in0=ptv[:], scalar=self.vcol(l, "ffn_conv_b", i + NP_, 1), in1=s_[:],
                                                                     op0=ALU.add, op1=ALU.mult), [Bpv, Bs, Bv], [Ba])
                        S.dma("sp", aT[128 * i:128 * i + 128, sq * L:(sq + 1) * L], a_[:], reads=[Ba], writes=[BaT])
                S.barrier()
        with ExitStack() as es:
            wd = es.enter_context(self.sbuf("ffwd", [128, NP_, D], BF16)); Bwd = [Buf() for _ in range((NP_ + 3) // 4)]
            self.load_w(wd, Bwd, self.dram["ffn_w_down"][l].rearrange("(k p) c -> p k c", p=128), self.dbuf["ffn_w_down"], NP_)
            xts = [(es.enter_context(self.sbuf("ffx%d" % i, [128, 4, D], F32)), [Buf() for _ in range(4)]) for i in range(2)]
            ats = [(es.enter_context(self.sbuf("ffat%d" % i, [128, NP_, 512], BF16)), [Buf(), Buf()]) for i in range(2)]
            def ldb(blk):
                tok0 = blk * 512
                xt, Bx = xts[blk % 2]
                at, Bat = ats[blk % 2]
                S.dma("sp", at[:, 0:11, :], aT[0:11 * 128, tok0:tok0 + 512].rearrange("(k p) t -> p k t", p=128), reads=[BaT], writes=[Bat[0]])
                S.dma("sp", at[:, 11:22, :], aT[11 * 128:22 * 128, tok0:tok0 + 512].rearrange("(k p) t -> p k t", p=128), reads=[BaT], writes=[Bat[1]])
                for j in range(4):
                    S.dma("sp", xt[:, j, :], xs_ap[tok0 + j * 128:tok0 + (j + 1) * 128, :], reads=[Bxs], writes=[Bx[j]])
            ldb(0)
            for blk in range(T // 512):
                tok0 = blk * 512
                xt, Bx = xts[blk % 2]
                at, Bat = ats[blk % 2]
                if blk + 1 < T // 512:
                    ldb(blk + 1)
                self.outproj_res(lambda kc, j: at[:, kc, j * 128:(j + 1) * 128], (lambda kc: Bat[0 if kc < 11 else 1]), NP_, wd, lambda kc: Bwd[kc // 4], xt, Bx, 4, xs_ap, Bxs, tok0)
            S.barrier()

    def stage_final(self):
        nc, S = self.nc, self.S
        V, A, P = self.V, self.A, self.P
        Bc = self.Bconst
        xs_ap, Bxs = self.dram["xres"], self.dbuf["xres"]
        with ExitStack() as es:
            gbc = es.enter_context(self.sbuf("fngb", [128, D], F32)); Bg = Buf()
            S.dma("sp", gbc[:], self.dram["norm_final_bc"], reads=[self.dbuf["norm_final_bc"]], writes=[Bg])
            xts = self.tiles(es, "fnx", [128, D], F32, 3)
            junk = es.enter_context(self.sbuf("fnjunk", [128, D], BF16)); Bjunk = Buf()
            st = self.tiles(es, "fnst", [128, 4], F32, 2)
            for tt in range(T // 128):
                xt, Bx = xts[tt % 3]
                s_, Bs = st[tt % 2]
                S.dma("sp", xt[:], xs_ap[tt * 128:(tt + 1) * 128, :], reads=[Bxs], writes=[Bx])
                A(lambda: nc.scalar.activation(out=junk[:], in_=xt[:], func=AF.Square, accum_out=s_[:, 0:1]), [Bx], [Bjunk, Bs])
                A(lambda: nc.scalar.activation(out=s_[:, 1:2], in_=s_[:, 0:1], func=AF.Sqrt, scale=1.0 / D, bias=self.eps6[:, 0:1]), [Bs, Bc], [Bs])
                V(lambda: nc.vector.reciprocal(out=s_[:, 2:3], in_=s_[:, 1:2]), [Bs], [Bs])
                V(lambda: nc.vector.scalar_tensor_tensor(out=xt[:], in0=xt[:], scalar=s_[:, 2:3], in1=gbc[:], op0=ALU.mult, op1=ALU.mult), [Bx, Bs, Bg], [Bx])
                S.dma("sp", self.out[tt * 128:(tt + 1) * 128, :], xt[:], reads=[Bx], writes=[self.Bout])
            S.barrier()

    def finish(self):
        S = self.S
        S.barrier()
        print("ops", S.n_ops, "waits", S.n_waits, "sems", S.nsem + S.n_dma_sems)


def build(debug=None, n_layers=DEPTH, final=True):
    k = K(debug, n_layers)
    k.stage_memn()
    for l in range(n_layers):
        k.stage_mixproj(l)
        for sq in range(NSEQ):
            k.stage_rwkv(l, sq)
        k.stage_s5(l)
        for sq in range(NSEQ):
            k.stage_gla(l, sq)
        k.stage_merge(l)
        k.stage_xattn(l)
        k.stage_ffn(l)
    if final:
        k.stage_final()
    k.finish()
    return k


def make_in_maps(inp):
    vecs, gvec = pack_vecs(inp)
    s5w = pack_s5(inp)
    maps = []
    for c in range(NCORES):
        m = {
            "x": np.ascontiguousarray(inp["x"][NSEQ * c:NSEQ * (c + 1)].reshape(T, D)),
            "mem": np.ascontiguousarray(inp["mem"][NSEQ * c:NSEQ * (c + 1)].reshape(NSEQ * NMEM, D)),
            "vecs": vecs, "gvec": gvec,
            "w_in": inp["w_in"],
            "gla_gk_up": inp["gla_gk_up"],
            "s5w": s5w, "s5_w_glu": inp["s5_w_glu"],
            "w_branch": inp["w_branch"], "w_out": inp["w_out"], "xa_wq": inp["xa_wq"], "xa_wkv": inp["xa_wkv"], "xa_wo": inp["xa_wo"],
            "ffn_w_up": inp["ffn_w_up"], "ffn_w_down": inp["ffn_w_down"],
            "norm_final_bc": np.ascontiguousarray(np.broadcast_to(np.asarray(inp["norm_final"], np.float32)[None, :], (128, D))),
            "rw_w_up": inp["rw_w_up"], "rw_a_up": inp["rw_a_up"], "rw_g_up": inp["rw_g_up"],
        }
        maps.append(m)
    return maps


def kernel(**inp):
    inp = {k: np.asarray(v) for k, v in inp.items()}
    k = build()
    res = run_bass_kernel_spmd(k.nc, make_in_maps(inp), core_ids=list(range(NCORES)))
    out = np.concatenate([r["out"].reshape(NSEQ, SEQ, D) for r in res.results], axis=0)
    return out.astype(np.float32)
```

```python
import numpy as np
from contextlib import ExitStack
import concourse.bass as bass
import concourse.mybir as mybir
from concourse.bass_utils import run_bass_kernel_spmd

F32 = mybir.dt.float32
BF16 = mybir.dt.bfloat16
ALU = mybir.AluOpType
AF = mybir.ActivationFunctionType
AX = mybir.AxisListType

NCORES = 8
DEPTH = 4
D = 1024
SEQ = 2048
NSEQ = 2
T = NSEQ * SEQ
NMEM = 256
N_IN = 6928
RW0, S50, GL0, GT0 = 0, 1792, 2304, 3856
DFF = 2816
EPOCH = 30000


class Buf:
    __slots__ = ("name", "lw", "rd")

    def __init__(self, name=""):
        self.name = name
        self.lw = None
        self.rd = {}


class Sched:
    def __init__(self, nc):
        self.nc = nc
        self.E = {"pe": nc.tensor, "dve": nc.vector, "act": nc.scalar, "pool": nc.gpsimd, "sp": nc.sync}
        self.sems = {}
        self.cur = {}
        self.nsem = 0
        for e in self.E:
            self._new_epoch(e)
        self.seen = {e: {} for e in self.E}
        self.dma_pools = {}
        self.dma_rr = {}
        nsd = 0
        for q, n in (("sp", 36), ("pool", 16), ("act", 8)):
            pl = []
            for i in range(n):
                k = "d%s%d" % (q, i)
                self.sems[k] = nc.alloc_semaphore("sd%d" % nsd)
                nsd += 1
                pl.append([k, 0])
            self.dma_pools[q] = pl
            self.dma_rr[q] = 0
        self.n_dma_sems = nsd
        self.n_ops = 0
        self.n_waits = 0

    def _new_epoch(self, e):
        k = "%s_%d" % (e, self.nsem)
        self.nsem += 1
        self.sems[k] = self.nc.alloc_semaphore("s" + k)
        self.cur[e] = [k, 0]

    def _wait(self, eng, deps):
        best = {}
        for d in deps:
            if d is None:
                continue
            k, v, de = d
            if de == eng and eng == "pe":
                continue
            if best.get(k, 0) < v:
                best[k] = v
        for k, v in best.items():
            if self.seen[eng].get(k, 0) >= v:
                continue
            self.E[eng].wait_ge(self.sems[k], v)
            self.seen[eng][k] = v
            self.n_waits += 1

    @staticmethod
    def _deps(reads, writes):
        deps = []
        for b in reads:
            deps.append(b.lw)
        for b in writes:
            deps.append(b.lw)
            deps.extend(b.rd.values())
        return deps

    def op(self, eng, fn, reads=(), writes=()):
        self._wait(eng, self._deps(reads, writes))
        ins = fn()
        c = self.cur[eng]
        c[1] += 1
        ins.then_inc(self.sems[c[0]], 1)
        t = (c[0], c[1], eng)
        for b in reads:
            b.rd[eng] = t
        for b in writes:
            b.lw = t
            b.rd = {}
        if c[1] >= EPOCH:
            self._new_epoch(eng)
        self.n_ops += 1
        return t

    def dma(self, q, out, in_, reads=(), writes=(), **kw):
        pl = self.dma_pools[q]
        slot = self.dma_rr[q]
        self.dma_rr[q] = (slot + 1) % len(pl)
        s = pl[slot]
        deps = self._deps(reads, writes)
        if s[1] > 0:
            deps.append((s[0], s[1], "dma"))
        self._wait(q, deps)
        s[1] += 16
        self.E[q].dma_start(out=out, in_=in_, **kw).then_inc(self.sems[s[0]], 16)
        t = (s[0], s[1], "dma")
        for b in reads:
            b.rd["dma_%s%d" % (q, slot)] = t
        for b in writes:
            b.lw = t
            b.rd = {}
        self.n_ops += 1
        return t

    def barrier(self):
        deps = []
        for e, c in self.cur.items():
            if c[1] > 0:
                deps.append((c[0], c[1], "x"))
        for pl in self.dma_pools.values():
            for k, v in pl:
                if v > 0:
                    deps.append((k, v, "x"))
        for e in self.E:
            self._wait(e, deps)


VEC_SPECS = [
    ("norm_mix", 1024), ("norm_xattn", 1024), ("norm_ffn", 1024),
    ("rw_mu", 1792), ("rw_w0", 512), ("rw_a0", 512), ("rw_k_k", 512), ("rw_k_a", 512), ("rw_r_k", 512),
    ("rw_ln_w", 512), ("rw_ln_b", 512),
    ("s5_b_glu", 512), ("s5_d", 512),
    ("gla_gk_b", 256), ("gla_norm", 128),
    ("ffn_conv0", 5632), ("ffn_conv1", 5632), ("ffn_conv2", 5632), ("ffn_conv_b", 5632),
]
VEC_OFF = {}
_o = 0
for _n, _l in VEC_SPECS:
    VEC_OFF[_n] = _o
    _o += _l // 128
NVEC = _o
GVEC_OFF = {"norm_mem": 0, "norm_final": 8}
NGVEC = 24


def pack_vecs(inp):
    v = np.zeros((DEPTH, 128, NVEC), np.float32)
    for l in range(DEPTH):
        for n, ln in VEC_SPECS:
            if n.startswith("ffn_conv") and n != "ffn_conv_b":
                a = inp["ffn_conv"][l, int(n[-1])]
            else:
                a = inp[n][l]
            a = np.asarray(a, np.float32).reshape(-1)
            v[l, :, VEC_OFF[n]:VEC_OFF[n] + ln // 128] = a.reshape(ln // 128, 128).T
    g = np.zeros((128, NGVEC), np.float32)
    g[:, 0:8] = np.asarray(inp["norm_mem"], np.float32).reshape(8, 128).T
    g[:, 8:16] = np.asarray(inp["norm_final"], np.float32).reshape(8, 128).T
    pp = np.arange(128)
    g[:, 16] = ((pp // 16) % 2 == 0)
    g[:, 17] = ((pp // 16) % 2 == 1)
    g[:, 18] = np.pi / 2
    g[:, 19] = (pp >= 96)
    return np.ascontiguousarray(v.transpose(1, 0, 2)), g


S5X = 3124
oAC, oBC, oLC, oAP, oLP, oBP, oCP = 0, 512, 1024, 1028, 1060, 1076, 2100


def pack_s5(inp):
    out = np.zeros((128, DEPTH, S5X), np.float32)
    for l in range(DEPTH):
        a = np.stack([inp["s5_a_re"][l], inp["s5_a_im"][l]]).astype(np.float32)
        b = np.stack([inp["s5_b_re"][l], inp["s5_b_im"][l]]).astype(np.float32)
        c = np.stack([inp["s5_c_re"][l], inp["s5_c_im"][l]]).astype(np.float32)
        ls = np.asarray(inp["s5_log_step"][l], np.float32)
        aC = np.broadcast_to(a.reshape(2, 4, 8, 64).transpose(2, 1, 0, 3)[:, None], (8, 16, 4, 2, 64)).reshape(128, 512)
        bC = b.reshape(2, 4, 8, 64, 16).transpose(2, 4, 1, 0, 3).reshape(128, 512)
        lC = np.broadcast_to(ls.reshape(4, 8).T[:, None, :], (8, 16, 4)).reshape(128, 4)
        aP = a.reshape(2, 16, 2, 64).transpose(2, 3, 1, 0).reshape(128, 32)
        lP = np.broadcast_to(ls.reshape(16, 2).T[:, None, :], (2, 64, 16)).reshape(128, 16)
        bP = np.zeros((2, 64, 16, 2, 2, 16), np.float32)
        cP = np.zeros((2, 64, 16, 2, 2, 16), np.float32)
        b5 = b.reshape(2, 16, 2, 64, 16)
        c5 = c.reshape(2, 16, 2, 16, 64)
        for bb in range(2):
            bP[bb, :, :, :, bb, :] = b5[:, :, bb].transpose(2, 1, 0, 3)
            cP[bb, :, :, :, bb, :] = c5[:, :, bb].transpose(3, 1, 0, 2)
        out[:, l, oAC:oAC + 512] = aC
        out[:, l, oBC:oBC + 512] = bC
        out[:, l, oLC:oLC + 4] = lC
        out[:, l, oAP:oAP + 32] = aP
        out[:, l, oLP:oLP + 16] = lP
        out[:, l, oBP:oBP + 1024] = bP.reshape(128, 1024)
        out[:, l, oCP:oCP + 1024] = cP.reshape(128, 1024)
    return out


class K:
    def __init__(self, debug=None, n_layers=DEPTH):
        self.debug = debug or {}
        self.n_layers = n_layers
        nc = self.nc = bass.Bass("TRN2", target_bir_lowering=False)
        self.S = Sched(nc)
        self.uid = 0
        self.dram = {}
        self.dbuf = {}
        di = self.dram_in
        di("x", [T, D]); di("mem", [NSEQ * NMEM, D]); di("vecs", [128, DEPTH, NVEC]); di("gvec", [128, NGVEC])
        di("w_in", [DEPTH, D, N_IN])
        di("gla_gk_up", [DEPTH, 16, 256])
        di("s5w", [128, DEPTH, S5X]); di("s5_w_glu", [DEPTH, 512, 512])
        di("w_branch", [DEPTH, 3, 512, D]); di("w_out", [DEPTH, D, D])
        di("xa_wq", [DEPTH, D, D]); di("xa_wkv", [DEPTH, D, 2 * D]); di("xa_wo", [DEPTH, D, D])
        di("ffn_w_up", [DEPTH, D, 2 * DFF]); di("ffn_w_down", [DEPTH, DFF, D]); di("norm_final_bc", [128, D])
        di("rw_w_up", [DEPTH, 64, 512]); di("rw_a_up", [DEPTH, 64, 512]); di("rw_g_up", [DEPTH, 128, 512])
        self.out = nc.dram_tensor("out", [T, D], F32, kind="ExternalOutput").ap()
        self.Bout = Buf("out")
        self.scr("xres", [T, D], F32)
        self.scr("pT", [N_IN, T], BF16)
        self.scr("ymix", [3, 512, T], BF16)
        self.scr("actT", [DFF, T], BF16)
        self.ps = []
        for i in range(8):
            t = nc.alloc_psum_tensor("ps%d" % i, [128, 512], F32)
            self.ps.append((t, Buf("ps%d" % i)))
        self.ps_rr = 0
        self.ev_rr = 0
        self.consts()

    def dram_in(self, name, shape, dt=F32):
        self.dram[name] = self.nc.dram_tensor(name, shape, dt, kind="ExternalInput").ap()
        self.dbuf[name] = Buf(name)

    def scr(self, name, shape, dt):
        kind = "ExternalOutput" if name in self.debug else "Internal"
        self.dram[name] = self.nc.dram_tensor(name, shape, dt, kind=kind).ap()
        self.dbuf[name] = Buf(name)

    def psum(self):
        t, b = self.ps[self.ps_rr]
        self.ps_rr = (self.ps_rr + 1) % 8
        return t, b

    def ev_eng(self):
        self.ev_rr ^= 1
        return "act" if self.ev_rr else "dve"

    def consts(self):
        nc, S = self.nc, self.S
        self.vecs = nc.alloc_sbuf_tensor("vecs_sb", [128, DEPTH, NVEC], F32)
        self.Bvecs = Buf("vecs")
        if "delay" in self.debug:
            dj = nc.alloc_sbuf_tensor("dly", [128, 512], F32)
            Bd = Buf()
            for i in range(60):
                S.op("pool", lambda: nc.gpsimd.memset(dj[:], 0.0), writes=[Bd])
            S.barrier()
        S.dma("sp", self.vecs[:], self.dram["vecs"], writes=[self.Bvecs])
        self.gvec = nc.alloc_sbuf_tensor("gvec_sb", [128, NGVEC], F32)
        S.dma("sp", self.gvec[:], self.dram["gvec"], writes=[self.Bvecs])
        self.ident_f = nc.alloc_sbuf_tensor("ident_f", [128, 128], F32)
        self.ident_b = nc.alloc_sbuf_tensor("ident_b", [128, 128], BF16)
        self.Bconst = Buf("const")
        self.eps6 = nc.alloc_sbuf_tensor("eps6", [128, 1], F32)
        S.op("pool", lambda: nc.gpsimd.memset(self.ident_f[:], 0.0), writes=[self.Bconst])
        S.op("pool", lambda: nc.gpsimd.affine_select(out=self.ident_f[:], in_=self.ident_f[:], pattern=[[-1, 128]],
                                                     compare_op=ALU.not_equal, fill=1.0, base=0, channel_multiplier=1),
             reads=[self.Bconst], writes=[self.Bconst])
        S.op("dve", lambda: nc.vector.tensor_copy(out=self.ident_b[:], in_=self.ident_f[:]), reads=[self.Bconst], writes=[self.Bconst])
        S.op("dve", lambda: nc.vector.memset(self.eps6[:], 1e-6), writes=[self.Bconst])
        self.cst = nc.alloc_sbuf_tensor("cst", [128, 8], F32)
        for i, v in enumerate([1.0, 1e-5, 1e-12, 64e-5, 0.0]):
            S.op("dve", lambda: nc.vector.memset(self.cst[:, i:i + 1], v), writes=[self.Bconst])
        self.ones_f = nc.alloc_sbuf_tensor("ones_f", [128, 128], F32)
        S.op("dve", lambda: nc.vector.memset(self.ones_f[:], 1.0), writes=[self.Bconst])
        self.ones_bd = nc.alloc_sbuf_tensor("ones_bd", [128, 128], F32)
        S.op("dve", lambda: nc.vector.memset(self.ones_bd[:], 0.0), writes=[self.Bconst])
        S.op("dve", lambda: nc.vector.memset(self.ones_bd[0:64, 0:64], 1.0), writes=[self.Bconst])
        S.op("dve", lambda: nc.vector.memset(self.ones_bd[64:128, 64:128], 1.0), writes=[self.Bconst])
        self.mbd = nc.alloc_sbuf_tensor("mbd", [128, 3, 128], F32)
        S.op("pool", lambda: nc.gpsimd.memset(self.mbd[:], 1.0), writes=[self.Bconst])
        for i, (cm, st, op) in enumerate([(1, -1, ALU.is_gt), (-1, 1, ALU.is_gt), (-1, 1, ALU.is_ge)]):
            S.op("pool", lambda: nc.gpsimd.affine_select(out=self.mbd[:, i, :], in_=self.mbd[:, i, :], pattern=[[st, 128]],
                                                         compare_op=op, fill=0.0, base=0, channel_multiplier=cm),
                 reads=[self.Bconst], writes=[self.Bconst])
        S.op("pool", lambda: nc.gpsimd.memset(self.mbd[0:64, :, 64:128], 0.0), reads=[self.Bconst], writes=[self.Bconst])
        S.op("pool", lambda: nc.gpsimd.memset(self.mbd[64:128, :, 0:64], 0.0), reads=[self.Bconst], writes=[self.Bconst])
        self.mask128 = nc.alloc_sbuf_tensor("mask128", [128, 128], F32)
        S.op("pool", lambda: nc.gpsimd.memset(self.mask128[:], 1.0), writes=[self.Bconst])
        S.op("pool", lambda: nc.gpsimd.affine_select(out=self.mask128[:], in_=self.mask128[:], pattern=[[1, 128]],
                                                     compare_op=ALU.is_ge, fill=0.0, base=0, channel_multiplier=-1),
             reads=[self.Bconst], writes=[self.Bconst])

    def sbuf(self, name, shape, dt):
        self.uid += 1
        return self.nc.sbuf_tensor("%s_u%d" % (name, self.uid), shape, dt)

    def tiles(self, es, name, shape, dt, n):
        return [(es.enter_context(self.sbuf("%s%d" % (name, i), shape, dt)), Buf(name)) for i in range(n)]

    def V(self, fn, r=(), w=()):
        return self.S.op("dve", fn, r, w)

    def A(self, fn, r=(), w=()):
        return self.S.op("act", fn, r, w)

    def P(self, fn, r=(), w=()):
        return self.S.op("pe", fn, r, w)

    def G(self, fn, r=(), w=()):
        return self.S.op("pool", fn, r, w)

    def vcol(self, l, name, j=0, n=1):
        o = VEC_OFF[name] + j
        return self.vecs[:, l, o:o + n]

    def rmsnorm_fm(self, es, src_ap, Bsrc, ntok, gain_ap, hT, BhT, eps_ap):
        nc, S = self.nc, self.S
        TB = 4
        xts = [(es.enter_context(self.sbuf("nx%d" % i, [128, TB, D], F32)), [Buf() for _ in range(TB)]) for i in range(2)]
        xss = [(es.enter_context(self.sbuf("nxs%d" % i, [128, D], BF16)), Buf()) for i in range(4)]
        junk = es.enter_context(self.sbuf("njunk", [128, D], BF16)); Bjunk = Buf()
        st = [(es.enter_context(self.sbuf("nst%d" % i, [128, 4], F32)), Buf()) for i in range(4)]
        nblk = ntok // (128 * TB)
        cnt = 0
        for blk in range(nblk):
            xt, Bxs_ = xts[blk % 2]
            for j in range(TB):
                r0 = (blk * TB + j) * 128
                S.dma("sp", xt[:, j, :], src_ap[r0:r0 + 128, :], reads=[Bsrc], writes=[Bxs_[j]])
            for j in range(TB):
                Bx = Bxs_[j]
                s_, Bs = st[cnt % 4]
                xs, Bxs = xss[cnt % 4]
                cnt += 1
                S.op("act", lambda: nc.scalar.activation(out=junk[:], in_=xt[:, j, :], func=AF.Square, accum_out=s_[:, 0:1]),
                     reads=[Bx], writes=[Bjunk, Bs])
                S.op("act", lambda: nc.scalar.activation(out=s_[:, 1:2], in_=s_[:, 0:1], func=AF.Sqrt, scale=1.0 / D, bias=eps_ap),
                     reads=[Bs, self.Bconst], writes=[Bs])
                S.op("dve", lambda: nc.vector.reciprocal(out=s_[:, 2:3], in_=s_[:, 1:2]), reads=[Bs], writes=[Bs])
                S.op("dve", lambda: nc.vector.tensor_scalar(out=xs[:], in0=xt[:, j, :], scalar1=s_[:, 2:3], scalar2=None, op0=ALU.mult),
                     reads=[Bx, Bs], writes=[Bxs])
                pt, Bp = self.psum()
                pb = pt[:].bitcast(BF16)
                for kc in range(8):
                    S.op("pe", lambda: nc.tensor.transpose(out=pb[:, kc * 128:(kc + 1) * 128], in_=xs[:, kc * 128:(kc + 1) * 128], identity=self.ident_b[:]),
                         reads=[Bxs, self.Bconst], writes=[Bp])
                t0 = (blk * TB + j) * 128
                S.op("dve", lambda: nc.vector.tensor_tensor(out=hT[:, :, t0:t0 + 128], in0=pb.rearrange("p (k t) -> p k t", k=8),
                                                            in1=gain_ap.unsqueeze(2).to_broadcast([128, 8, 128]), op=ALU.mult),
                     reads=[Bp, self.Bvecs], writes=[BhT])

    def proj_fm(self, es, w_ap, Bw, segs, hT, BhT, ntok, dst_ap, Bdst, kc_n=8):
        nc, S = self.nc, self.S
        wts = [(es.enter_context(self.sbuf("pw%d" % i, [128, kc_n, 512], BF16)), [Buf() for _ in range((kc_n + 3) // 4)]) for i in range(2)]
        ots = [(es.enter_context(self.sbuf("po%d" % i, [128, 512], BF16)), Buf()) for i in range(4)]
        wi = 0
        oi = 0
        for (c0, ncol, func, r0) in segs:
            for g0 in range(0, ncol, 512):
                gn = min(512, ncol - g0)
                wt, Bwt = wts[wi % 2]; wi += 1
                src = w_ap[:, c0 + g0:c0 + g0 + gn].rearrange("(k p) c -> p k c", p=128)
                Bwt_l = Bwt
                for k0 in range(0, kc_n, 4):
                    S.dma("pool", wt[:, k0:k0 + 4, 0:gn], src[:, k0:k0 + 4, :], reads=[Bw], writes=[Bwt_l[k0 // 4]])
                for m0 in range(0, gn, 128):
                    mn = min(128, gn - m0)
                    for tb in range(ntok // 512):
                        pt, Bp = self.psum()
                        for kc in range(kc_n):
                            S.op("pe", lambda: nc.tensor.matmul(pt[0:mn, :], lhsT=wt[:, kc, m0:m0 + mn], rhs=hT[:, kc, tb * 512:(tb + 1) * 512],
                                                                start=(kc == 0), stop=(kc == kc_n - 1)),
                                 reads=[Bwt[kc // 4], BhT], writes=[Bp])
                        ot, Bo = ots[oi % 4]; oi += 1
                        if func is not None:
                            S.op("act", lambda: nc.scalar.activation(out=ot[0:mn, :], in_=pt[0:mn, :], func=func), reads=[Bp], writes=[Bo])
                        else:
                            e = self.ev_eng()
                            if e == "act":
                                S.op("act", lambda: nc.scalar.copy(out=ot[0:mn, :], in_=pt[0:mn, :]), reads=[Bp], writes=[Bo])
                            else:
                                S.op("dve", lambda: nc.vector.tensor_copy(out=ot[0:mn, :], in_=pt[0:mn, :]), reads=[Bp], writes=[Bo])
                        rr = r0 + g0 + m0
                        S.dma("sp", dst_ap[rr:rr + mn, tb * 512:(tb + 1) * 512], ot[0:mn, :], reads=[Bo], writes=[Bdst])

    def stage_mixproj(self, l):
        nc, S = self.nc, self.S
        with ExitStack() as es:
            hT = es.enter_context(self.sbuf("hT", [128, 8, T], BF16)); BhT = Buf("hT")
            xsrc, Bx = (self.dram["x"], self.dbuf["x"]) if l == 0 else (self.dram["xres"], self.dbuf["xres"])
            with ExitStack() as es2:
                self.rmsnorm_fm(es2, xsrc, Bx, T, self.vcol(l, "norm_mix", 0, 8), hT, BhT, self.eps6[:, 0:1])
                S.barrier()
            if "hT" in self.debug:
                self.dump("hT_dbg", hT[:], [BhT])
            segs = [(RW0, 1792, None, RW0), (S50, 512, None, S50), (GL0, 1552, None, GL0), (GT0, 3072, AF.Sigmoid, GT0)]
            if "segs" in self.debug:
                segs = self.debug["segs"]
            with ExitStack() as es2:
                self.proj_fm(es2, self.dram["w_in"][l], self.dbuf["w_in"], segs, hT, BhT, T, self.dram["pT"], self.dbuf["pT"])
                S.barrier()
        S.barrier()

    def dump(self, name, ap, bufs, dt=None):
        d = self.nc.dram_tensor(name, list(ap.shape), dt or ap.dtype, kind="ExternalOutput").ap()
        self.S.dma("sp", d, ap, reads=bufs, writes=[Buf()])

    def stage_gla(self, l, sq):
        nc, S = self.nc, self.S
        V, A, P = self.V, self.A, self.P
        pT, BpT = self.dram["pT"], self.dbuf["pT"]
        ym, Bym = self.dram["ymix"], self.dbuf["ymix"]
        t0 = sq * SEQ
        L = SEQ
        NCH = L // 128
        qr, kr, vr, gr, gor = GL0, GL0 + 256, GL0 + 512, GL0 + 1024, GL0 + 1040
        Bc = self.Bconst
        with ExitStack() as es:
            gkup = es.enter_context(self.sbuf("gkup", [16, 256], BF16)); Bgk = Buf()
            S.dma("pool", gkup[:], self.dram["gla_gk_up"][l], reads=[self.dbuf["gla_gk_up"]], writes=[Bgk])
            gkd = es.enter_context(self.sbuf("gkd", [16, L], BF16)); Bgkd = Buf()
            S.dma("sp", gkd[:], pT[gr:gr + 16, t0:t0 + L], reads=[BpT], writes=[Bgkd])
            negb = es.enter_context(self.sbuf("negb", [128, 2], F32)); Bnb = Buf()
            V(lambda: nc.vector.tensor_scalar(out=negb[:], in0=self.vcol(l, "gla_gk_b", 0, 2), scalar1=-1.0, scalar2=None, op0=ALU.mult),
              [self.Bvecs], [Bnb])
            qk = self.tiles(es, "gqk", [128, L], BF16, 2)
            qd = es.enter_context(self.sbuf("gqd", [128, L], BF16)); Bqd = Buf()
            ki = es.enter_context(self.sbuf("gki", [128, L], BF16)); Bki = Buf()
            csz = es.enter_context(self.sbuf("gcsz", [128, L + 1], F32)); Bcs = Buf()
            lt = es.enter_context(self.sbuf("glt", [128, L], F32)); Blt = Buf()
            csl = es.enter_context(self.sbuf("gcsl", [128, L], F32)); Bcsl = Buf()
            Eq = es.enter_context(self.sbuf("gEq", [128, L], F32)); BEq = Buf()
            Ek = es.enter_context(self.sbuf("gEk", [128, L], F32)); BEk = Buf()
            vts = self.tiles(es, "gv", [128, L], BF16, 2)
            ofm = self.tiles(es, "gofm", [128, L], F32, 2)
            S32 = self.tiles(es, "gS32", [128, 128], F32, 2)
            S16 = self.tiles(es, "gS16", [128, 128], BF16, 4)
            vtm = self.tiles(es, "gvtm", [128, 128], BF16, 4)
            ktm = self.tiles(es, "gktm", [128, 128], BF16, 2)
            att = self.tiles(es, "gatt", [128, 128], BF16, 4)
            gos = self.tiles(es, "ggo", [128, 512], BF16, 4)
            tmpa = self.tiles(es, "gta", [128, 512], F32, 4)
            tmpb = self.tiles(es, "gtb", [128, 512], F32, 4)
            tmpc = self.tiles(es, "gtc", [128, 512], F32, 4)
            yo = self.tiles(es, "gyo", [128, 512], BF16, 4)
            V(lambda: nc.vector.memset(csz[:, 0:1], 0.0), [], [Bcs])
            for j in range(2):
                qt, Bq = qk[0]; kt, Bk = qk[1]
                S.dma("sp", qt[:], pT[qr + 128 * j:qr + 128 * j + 128, t0:t0 + L], reads=[BpT], writes=[Bq])
                S.dma("sp", kt[:], pT[kr + 128 * j:kr + 128 * j + 128, t0:t0 + L], reads=[BpT], writes=[Bk])
                for tb in range(L // 512):
                    pt, Bp = self.psum()
                    P(lambda: nc.tensor.matmul(pt[:], lhsT=gkup[0:16, 128 * j:128 * j + 128], rhs=gkd[0:16, tb * 512:(tb + 1) * 512], start=True, stop=True),
                      [Bgk, Bgkd], [Bp])
                    A(lambda: nc.scalar.activation(out=lt[:, tb * 512:(tb + 1) * 512], in_=pt[:], func=AF.Exp, scale=-1.0, bias=negb[:, j:j + 1]),
                      [Bp, Bnb], [Blt])
                A(lambda: nc.scalar.activation(out=lt[:], in_=lt[:], func=AF.Ln, scale=1.0, bias=self.cst[:, 0:1]), [Blt, Bc], [Blt])
                V(lambda: nc.vector.tensor_tensor_scan(out=csz[:, 1:L + 1], data0=self.cst[:, 0:1].to_broadcast([128, L]), data1=lt[:],
                                                       initial=0.0, op0=ALU.mult, op1=ALU.add), [Blt, Bc], [Bcs])
                V(lambda: nc.vector.tensor_tensor(out=csl[:].rearrange("p (c t) -> p c t", t=128),
                                                  in0=csz[:, 1:L + 1].rearrange("p (c t) -> p c t", t=128),
                                                  in1=csz[:, 0:L].rearrange("p (c t) -> p c t", t=128)[:, :, 0:1].to_broadcast([128, NCH, 128]),
                                                  op=ALU.subtract), [Bcs], [Bcsl])
                A(lambda: nc.scalar.activation(out=Eq[:], in_=csl[:], func=AF.Exp, scale=-1.0 / 16), [Bcsl], [BEq])
                A(lambda: nc.scalar.activation(out=Ek[:], in_=csl[:], func=AF.Exp, scale=1.0 / 16), [Bcsl], [BEk])
                V(lambda: nc.vector.scalar_tensor_tensor(out=qd[:], in0=qt[:], scalar=0.125, in1=Eq[:], op0=ALU.mult, op1=ALU.mult), [Bq, BEq], [Bqd])
                V(lambda: nc.vector.tensor_tensor(out=ki[:], in0=kt[:], in1=Ek[:], op=ALU.mult), [Bk, BEk], [Bki])
                Eq3 = Eq[:].rearrange("p (c t) -> p c t", t=128)
                heads = [2 * j, 2 * j + 1]
                for hh in range(2):
                    h = heads[hh]
                    vt, Bv = vts[hh]
                    S.dma("sp", vt[:], pT[vr + 128 * h:vr + 128 * h + 128, t0:t0 + L], reads=[BpT], writes=[Bv])
                    V(lambda: nc.vector.memset(S32[hh][0][:], 0.0), [], [S32[hh][1]])
                    V(lambda: nc.vector.memset(S16[2 * hh][0][:], 0.0), [], [S16[2 * hh][1]])
                    V(lambda: nc.vector.memset(S16[2 * hh + 1][0][:], 0.0), [], [S16[2 * hh + 1][1]])
                cnt = 0
                for ci in range(NCH):
                    ts = slice(ci * 128, ci * 128 + 128)
                    ktmt, Bktm = ktm[ci % 2]
                    pt, Bp = self.psum()
                    pb = pt[:].bitcast(BF16)
                    P(lambda: nc.tensor.transpose(out=pb[:, 0:128], in_=ki[:, ts], identity=self.ident_b[:]), [Bki, Bc], [Bp])
                    A(lambda: nc.scalar.copy(out=ktmt[:], in_=pb[:, 0:128]), [Bp], [Bktm])
                    for hh in range(2):
                        h = heads[hh]
                        hp = slice(64 * hh, 64 * hh + 64)
                        vt, Bv = vts[hh]
                        vtmt, Bvtm = vtm[cnt % 4]
                        attt, Batt = att[cnt % 4]
                        cnt += 1
                        pt, Bp = self.psum()
                        pb = pt[:].bitcast(BF16)
                        P(lambda: nc.tensor.transpose(out=pb[:, 0:128], in_=vt[:, ts], identity=self.ident_b[:]), [Bv, Bc], [Bp])
                        A(lambda: nc.scalar.copy(out=vtmt[:], in_=pb[:, 0:128]), [Bp], [Bvtm])
                        pt2, Bp2 = self.psum()
                        P(lambda: nc.tensor.matmul(pt2[:, 0:128], lhsT=ki[hp, ts], rhs=qd[hp, ts], start=True, stop=True), [Bki, Bqd], [Bp2])
                        V(lambda: nc.vector.tensor_tensor(out=attt[:], in0=pt2[:, 0:128], in1=self.mask128[:], op=ALU.mult), [Bp2, Bc], [Batt])
                        s16c, Bs16c = S16[2 * hh + ci % 2]
                        s16n, Bs16n = S16[2 * hh + (ci + 1) % 2]
                        if ci < NCH - 1:
                            pt4, Bp4 = self.psum()
                            P(lambda: nc.tensor.matmul(pt4[:, 0:128], lhsT=ktmt[:], rhs=vtmt[:], start=True, stop=True), [Bktm, Bvtm], [Bp4])
                            s32, Bs32 = S32[hh]
                            V(lambda: nc.vector.tensor_tensor(out=s32[hp, :], in0=s32[hp, :], in1=pt4[hp, 0:128], op=ALU.add), [Bs32, Bp4], [Bs32])
                            V(lambda: nc.vector.tensor_scalar(out=s32[hp, :], in0=s32[hp, :], scalar1=Eq3[hp, ci, 127:128], scalar2=None, op0=ALU.mult),
                              [Bs32, BEq], [Bs32])
                            V(lambda: nc.vector.tensor_copy(out=s16n[hp, :], in_=s32[hp, :]), [Bs32], [Bs16n])
                        pt3, Bp3 = self.psum()
                        P(lambda: nc.tensor.matmul(pt3[:, 0:128], lhsT=vtmt[:], rhs=attt[:], start=True, stop=False), [Bvtm, Batt], [Bp3])
                        P(lambda: nc.tensor.matmul(pt3[:, 0:128], lhsT=s16c[:], rhs=qd[:, ts], start=False, stop=True), [Bs16c, Bqd], [Bp3])
                        A(lambda: nc.scalar.copy(out=ofm[hh][0][:, ts], in_=pt3[:, 0:128]), [Bp3], [ofm[hh][1]])
                NB = L // 512
                for hh in range(2):
                    h = heads[hh]
                    o, Bo = ofm[hh]
                    CS = [slice(tb * 512, tb * 512 + 512) for tb in range(NB)]
                    for tb in range(NB):
                        got, Bgo = gos[tb]
                        S.dma("sp", got[:], pT[gor + 128 * h:gor + 128 * h + 128, t0 + tb * 512:t0 + tb * 512 + 512], reads=[BpT], writes=[Bgo])
                    for tb in range(NB):
                        ta, Bta = tmpa[tb]
                        A(lambda: nc.scalar.activation(out=ta[:], in_=o[:, CS[tb]], func=AF.Square), [Bo], [Bta])
                    pts = {}
                    for tb in range(NB):
                        ta, Bta = tmpa[tb]
                        pts[tb] = self.psum()
                        pt, Bp = pts[tb]
                        P(lambda: nc.tensor.matmul(pt[:], lhsT=self.ones_f[:], rhs=ta[:], start=True, stop=True), [Bc, Bta], [Bp])
                    for tb in range(NB):
                        pt, Bp = pts[tb]; tb_, Btb = tmpb[tb]
                        A(lambda: nc.scalar.activation(out=tb_[:], in_=pt[:], func=AF.Sqrt, scale=1.0 / 128, bias=self.cst[:, 1:2]), [Bp, Bc], [Btb])
                    for tb in range(NB):
                        got, Bgo = gos[tb]; tc, Btc = tmpc[tb]
                        A(lambda: nc.scalar.activation(out=tc[:], in_=got[:], func=AF.Sigmoid), [Bgo], [Btc])
                    for tb in range(NB):
                        tb_, Btb = tmpb[tb]
                        V(lambda: nc.vector.reciprocal(out=tb_[:], in_=tb_[:]), [Btb], [Btb])
                    for tb in range(NB):
                        tb_, Btb = tmpb[tb]
                        V(lambda: nc.vector.scalar_tensor_tensor(out=tb_[:], in0=o[:, CS[tb]], scalar=self.vcol(l, "gla_norm", 0, 1), in1=tb_[:],
                                                                 op0=ALU.mult, op1=ALU.mult), [Bo, Btb, self.Bvecs], [Btb])
                    for tb in range(NB):
                        got, Bgo = gos[tb]; tc, Btc = tmpc[tb]
                        V(lambda: nc.vector.tensor_tensor(out=tc[:], in0=tc[:], in1=got[:], op=ALU.mult), [Btc, Bgo], [Btc])
                    for tb in range(NB):
                        tb_, Btb = tmpb[tb]; tc, Btc = tmpc[tb]; yt, By = yo[tb]
                        V(lambda: nc.vector.tensor_tensor(out=yt[:], in0=tb_[:], in1=tc[:], op=ALU.mult), [Btb, Btc], [By])
                        S.dma("sp", ym[2, 128 * h:128 * h + 128, t0 + tb * 512:t0 + tb * 512 + 512], yt[:], reads=[By], writes=[Bym])
            S.barrier()

    def s5_lambda(self, es, tag, are, aim, ls_bc, shp):
        nc = self.nc
        V, A = self.V, self.A
        F = int(np.prod(shp[1:]))
        def new(n):
            return es.enter_context(self.sbuf(tag + n, [128, F], F32))
        def vw(t):
            return t[:] if len(shp) == 2 else (t[:].rearrange("p (a b) -> p a b", b=shp[2]) if len(shp) == 3 else t[:])
        B = Buf()
        Bin = self.Bs5w
        dt, mag, ang, c, sn, t1, t2, lre, lim, fre, fim = [new(n) for n in ["dt", "mag", "ang", "c", "s", "t1", "t2", "lre", "lim", "fre", "fim"]]
        A(lambda: nc.scalar.activation(out=vw(dt), in_=ls_bc, func=AF.Exp), [Bin], [B])
        V(lambda: nc.vector.tensor_tensor(out=vw(mag), in0=vw(dt), in1=are, op=ALU.mult), [B, Bin], [B])
        A(lambda: nc.scalar.activation(out=mag[:], in_=mag[:], func=AF.Exp), [B], [B])
        V(lambda: nc.vector.tensor_tensor(out=vw(ang), in0=vw(dt), in1=aim, op=ALU.mult), [B, Bin], [B])
        A(lambda: nc.scalar.activation(out=sn[:], in_=ang[:], func=AF.Sin, scale=1.0 / 16), [B], [B])
        A(lambda: nc.scalar.activation(out=c[:], in_=ang[:], func=AF.Sin, scale=-1.0 / 16, bias=self.gvec[:, 18:19]), [B, self.Bvecs], [B])
        for _ in range(4):
            V(lambda: nc.vector.tensor_tensor(out=t1[:], in0=c[:], in1=sn[:], op=ALU.mult), [B], [B])
            V(lambda: nc.vector.tensor_tensor(out=c[:], in0=c[:], in1=c[:], op=ALU.mult), [B], [B])
            V(lambda: nc.vector.tensor_tensor(out=t2[:], in0=sn[:], in1=sn[:], op=ALU.mult), [B], [B])
            V(lambda: nc.vector.tensor_tensor(out=c[:], in0=c[:], in1=t2[:], op=ALU.subtract), [B], [B])
            V(lambda: nc.vector.tensor_scalar(out=sn[:], in0=t1[:], scalar1=2.0, scalar2=None, op0=ALU.mult), [B], [B])
        V(lambda: nc.vector.tensor_tensor(out=lre[:], in0=mag[:], in1=c[:], op=ALU.mult), [B], [B])
        V(lambda: nc.vector.tensor_tensor(out=lim[:], in0=mag[:], in1=sn[:], op=ALU.mult), [B], [B])
        V(lambda: nc.vector.tensor_tensor(out=vw(t1), in0=are, in1=are, op=ALU.mult), [Bin, B], [B])
        V(lambda: nc.vector.tensor_tensor(out=vw(t2), in0=aim, in1=aim, op=ALU.mult), [Bin, B], [B])
        V(lambda: nc.vector.tensor_tensor(out=t1[:], in0=t1[:], in1=t2[:], op=ALU.add), [B], [B])
        V(lambda: nc.vector.reciprocal(out=t1[:], in_=t1[:]), [B], [B])
        V(lambda: nc.vector.tensor_scalar(out=c[:], in0=lre[:], scalar1=-1.0, scalar2=None, op0=ALU.add), [B], [B])
        V(lambda: nc.vector.tensor_tensor(out=vw(fre), in0=vw(c), in1=are, op=ALU.mult), [B, Bin], [B])
        V(lambda: nc.vector.tensor_tensor(out=vw(t2), in0=vw(lim), in1=aim, op=ALU.mult), [B, Bin], [B])
        V(lambda: nc.vector.tensor_tensor(out=fre[:], in0=fre[:], in1=t2[:], op=ALU.add), [B], [B])
        V(lambda: nc.vector.tensor_tensor(out=fre[:], in0=fre[:], in1=t1[:], op=ALU.mult), [B], [B])
        V(lambda: nc.vector.tensor_tensor(out=vw(fim), in0=vw(lim), in1=are, op=ALU.mult), [B, Bin], [B])
        V(lambda: nc.vector.tensor_tensor(out=vw(t2), in0=vw(c), in1=aim, op=ALU.mult), [B, Bin], [B])
        V(lambda: nc.vector.tensor_tensor(out=fim[:], in0=fim[:], in1=t2[:], op=ALU.subtract), [B], [B])
        V(lambda: nc.vector.tensor_tensor(out=fim[:], in0=fim[:], in1=t1[:], op=ALU.mult), [B], [B])
        return lre, lim, fre, fim, B

    def cmul(self, ore, oim, are, aim, bre, bim, t1, r, w):
        nc, V = self.nc, self.V
        V(lambda: nc.vector.tensor_tensor(out=ore, in0=are, in1=bre, op=ALU.mult), r, w)
        V(lambda: nc.vector.tensor_tensor(out=t1, in0=aim, in1=bim, op=ALU.mult), r, w)
        V(lambda: nc.vector.tensor_tensor(out=ore, in0=ore, in1=t1, op=ALU.subtract), r, w)
        V(lambda: nc.vector.tensor_tensor(out=oim, in0=are, in1=bim, op=ALU.mult), r, w)
        V(lambda: nc.vector.tensor_tensor(out=t1, in0=aim, in1=bre, op=ALU.mult), r, w)
        V(lambda: nc.vector.tensor_tensor(out=oim, in0=oim, in1=t1, op=ALU.add), r, w)

    def stage_s5(self, l):
        nc, S = self.nc, self.S
        V, A, P = self.V, self.A, self.P
        pT, BpT = self.dram["pT"], self.dbuf["pT"]
        ym, Bym = self.dram["ymix"], self.dbuf["ymix"]
        Bc = self.Bconst
        with ExitStack() as es:
            M1re = es.enter_context(self.sbuf("M1re", [128, 4, 8, 128], BF16))
            M1im = es.enter_context(self.sbuf("M1im", [128, 4, 8, 128], BF16))
            M1hr = es.enter_context(self.sbuf("M1hr", [128, 4, 8, 128], BF16))
            M1hi = es.enter_context(self.sbuf("M1hi", [128, 4, 8, 128], BF16))
            M2hr = es.enter_context(self.sbuf("M2hr", [128, 4, 8, 64], BF16))
            M2hi = es.enter_context(self.sbuf("M2hi", [128, 4, 8, 64], BF16))
            M2re = es.enter_context(self.sbuf("M2re", [128, 16, 8, 32], BF16))
            M2im = es.enter_context(self.sbuf("M2im", [128, 16, 8, 32], BF16))
            Kbd = es.enter_context(self.sbuf("Kbd", [128, 4, 8, 128], BF16))
            LK = es.enter_context(self.sbuf("LK", [128, 16, 8, 3], F32))
            wglu = es.enter_context(self.sbuf("wglu", [128, 4, 512], BF16))
            BW = Buf("s5w")
            Bwg = Buf()
            S.dma("pool", wglu[:], self.dram["s5_w_glu"][l].rearrange("(k p) c -> p k c", p=128), reads=[self.dbuf["s5_w_glu"]], writes=[Bwg])
            with ExitStack() as e2:
                w = e2.enter_context(self.sbuf("s5w_sb", [128, S5X], F32)); self.Bs5w = Buf()
                S.dma("sp", w[:], self.dram["s5w"][:, l, :], reads=[self.dbuf["s5w"]], writes=[self.Bs5w])
                aC = w[:, oAC:oAC + 512].rearrange("p (g r n) -> p g r n", g=4, r=2)
                bC = w[:, oBC:oBC + 512].rearrange("p (g r n) -> p g r n", g=4, r=2)
                lC = w[:, oLC:oLC + 4].unsqueeze(2).to_broadcast([128, 4, 64])
                lre, lim, fre, fim, B1 = self.s5_lambda(e2, "c", aC[:, :, 0, :], aC[:, :, 1, :], lC, [128, 4, 64])
                v3 = lambda t: t[:].rearrange("p (a b) -> p a b", b=64)
                Bre = e2.enter_context(self.sbuf("cBre", [128, 256], F32)); Bim = e2.enter_context(self.sbuf("cBim", [128, 256], F32))
                tq = e2.enter_context(self.sbuf("ctq", [128, 256], F32))
                self.cmul(v3(Bre), v3(Bim), v3(fre), v3(fim), bC[:, :, 0, :], bC[:, :, 1, :], v3(tq), [B1, self.Bs5w], [B1])
                Pw = e2.enter_context(self.sbuf("cPw", [128, 8, 2, 256], F32))
                V(lambda: nc.vector.memset(Pw[:, 0, 0, :], 1.0), [], [B1])
                V(lambda: nc.vector.memset(Pw[:, 0, 1, :], 0.0), [], [B1])
                for m in range(1, 8):
                    self.cmul(Pw[:, m, 0, :], Pw[:, m, 1, :], Pw[:, m - 1, 0, :], Pw[:, m - 1, 1, :], lre[:], lim[:], tq[:], [B1], [B1])
                vre = e2.enter_context(self.sbuf("cvre", [128, 256], F32)); vim = e2.enter_context(self.sbuf("cvim", [128, 256], F32))
                for j in range(8):
                    m = 7 - j
                    self.cmul(vre[:], vim[:], Pw[:, m, 0, :], Pw[:, m, 1, :], Bre[:], Bim[:], tq[:], [B1], [B1])
                    for bb in range(2):
                        V(lambda: nc.vector.tensor_scalar(out=M1re[:, :, j, bb * 64:(bb + 1) * 64], in0=v3(vre), scalar1=self.gvec[:, 16 + bb:17 + bb], scalar2=None, op0=ALU.mult),
                          [B1, self.Bvecs], [BW])
                        V(lambda: nc.vector.tensor_scalar(out=M1im[:, :, j, bb * 64:(bb + 1) * 64], in0=v3(vim), scalar1=self.gvec[:, 16 + bb:17 + bb], scalar2=None, op0=ALU.mult),
                          [B1, self.Bvecs], [BW])
                        V(lambda: nc.vector.tensor_scalar(out=M1hr[:, :, j, bb * 64:(bb + 1) * 64], in0=v3(vre), scalar1=self.gvec[:, 16 + bb:17 + bb], scalar2=self.gvec[:, 19:20], op0=ALU.mult, op1=ALU.mult),
                          [B1, self.Bvecs], [BW])
                        V(lambda: nc.vector.tensor_scalar(out=M1hi[:, :, j, bb * 64:(bb + 1) * 64], in0=v3(vim), scalar1=self.gvec[:, 16 + bb:17 + bb], scalar2=self.gvec[:, 19:20], op0=ALU.mult, op1=ALU.mult),
                          [B1, self.Bvecs], [BW])
            S.barrier()
            with ExitStack() as e2:
                w = e2.enter_context(self.sbuf("s5w_sb2", [128, S5X], F32)); self.Bs5w = Buf()
                S.dma("sp", w[:], self.dram["s5w"][:, l, :], reads=[self.dbuf["s5w"]], writes=[self.Bs5w])
                aP = w[:, oAP:oAP + 32].rearrange("p (g r) -> p g r", r=2)
                lP = w[:, oLP:oLP + 16]
                bP = w[:, oBP:oBP + 1024].rearrange("p (g r c) -> p g r c", g=16, r=2)
                cP = w[:, oCP:oCP + 1024].rearrange("p (g r c) -> p g r c", g=16, r=2)
                lre, lim, fre, fim, B2 = self.s5_lambda(e2, "p", aP[:, :, 0], aP[:, :, 1], lP, [128, 16])
                tq = e2.enter_context(self.sbuf("ptq", [128, 16], F32))
                Pp = e2.enter_context(self.sbuf("pPw", [128, 9, 2, 16], F32))
                V(lambda: nc.vector.memset(Pp[:, 0, 0, :], 1.0), [], [B2])
                V(lambda: nc.vector.memset(Pp[:, 0, 1, :], 0.0), [], [B2])
                for m in range(1, 9):
                    self.cmul(Pp[:, m, 0, :], Pp[:, m, 1, :], Pp[:, m - 1, 0, :], Pp[:, m - 1, 1, :], lre[:], lim[:], tq[:], [B2], [B2])
                lkr = e2.enter_context(self.sbuf("lkr", [128, 8, 16], F32)); lki = e2.enter_context(self.sbuf("lki", [128, 8, 16], F32))
                V(lambda: nc.vector.tensor_copy(out=lkr[:, 0, :], in_=Pp[:, 8, 0, :]), [B2], [B2])
                V(lambda: nc.vector.tensor_copy(out=lki[:, 0, :], in_=Pp[:, 8, 1, :]), [B2], [B2])
                for lev in range(1, 8):
                    self.cmul(lkr[:, lev, :], lki[:, lev, :], lkr[:, lev - 1, :], lki[:, lev - 1, :], lkr[:, lev - 1, :], lki[:, lev - 1, :], tq[:], [B2], [B2])
                V(lambda: nc.vector.tensor_copy(out=LK[:, :, :, 0], in_=lkr[:].rearrange("p l g -> p g l")), [B2], [BW])
                V(lambda: nc.vector.tensor_copy(out=LK[:, :, :, 1], in_=lki[:].rearrange("p l g -> p g l")), [B2], [BW])
                V(lambda: nc.vector.tensor_scalar(out=LK[:, :, :, 2], in0=lki[:].rearrange("p l g -> p g l"), scalar1=-1.0, scalar2=None, op0=ALU.mult), [B2], [BW])
                Bpr = e2.enter_context(self.sbuf("pBre", [128, 16, 64], F32)); Bpi = e2.enter_context(self.sbuf("pBim", [128, 16, 64], F32))
                V(lambda: nc.vector.memset(Bpr[:], 0.0), [], [B2])
                V(lambda: nc.vector.memset(Bpi[:], 0.0), [], [B2])
                t3 = e2.enter_context(self.sbuf("pt3", [128, 16, 32], F32))
                bc = lambda t: t[:].unsqueeze(2).to_broadcast([128, 16, 32])
                self.cmul(Bpr[:, :, 32:64], Bpi[:, :, 32:64], bc(fre), bc(fim), bP[:, :, 0, :], bP[:, :, 1, :], t3[:], [B2, self.Bs5w], [B2])
                CLr = e2.enter_context(self.sbuf("CLr", [128, 16, 9, 32], F32)); CLn = e2.enter_context(self.sbuf("CLn", [128, 16, 9, 32], F32))
                for m in range(9):
                    pr = Pp[:, m, 0, :].unsqueeze(2).to_broadcast([128, 16, 32])
                    pi = Pp[:, m, 1, :].unsqueeze(2).to_broadcast([128, 16, 32])
                    V(lambda: nc.vector.tensor_tensor(out=CLr[:, :, m, :], in0=cP[:, :, 0, :], in1=pr, op=ALU.mult), [B2, self.Bs5w], [B2])
                    V(lambda: nc.vector.tensor_tensor(out=t3[:], in0=cP[:, :, 1, :], in1=pi, op=ALU.mult), [B2, self.Bs5w], [B2])
                    V(lambda: nc.vector.tensor_tensor(out=CLr[:, :, m, :], in0=CLr[:, :, m, :], in1=t3[:], op=ALU.subtract), [B2], [B2])
                    V(lambda: nc.vector.tensor_tensor(out=CLn[:, :, m, :], in0=cP[:, :, 0, :], in1=pi, op=ALU.mult), [B2, self.Bs5w], [B2])
                    V(lambda: nc.vector.tensor_tensor(out=t3[:], in0=cP[:, :, 1, :], in1=pr, op=ALU.mult), [B2, self.Bs5w], [B2])
                    V(lambda: nc.vector.scalar_tensor_tensor(out=CLn[:, :, m, :], in0=CLn[:, :, m, :], scalar=-1.0, in1=t3[:], op0=ALU.mult, op1=ALU.subtract), [B2], [B2])
                V(lambda: nc.vector.tensor_copy(out=M2re[:], in_=CLr[:, :, 1:9, :]), [B2], [BW])
                V(lambda: nc.vector.tensor_copy(out=M2im[:], in_=CLn[:, :, 1:9, :]), [B2], [BW])
                V(lambda: nc.vector.memset(M2hr[:], 0.0), [], [BW])
                V(lambda: nc.vector.memset(M2hi[:], 0.0), [], [BW])
                for gt in range(4):
                    V(lambda: nc.vector.tensor_copy(out=M2hr[:, gt, :, 32:64], in_=CLr[:, 4 * gt + 3, 1:9, :]), [B2], [BW])
                    V(lambda: nc.vector.tensor_copy(out=M2hi[:, gt, :, 32:64], in_=CLn[:, 4 * gt + 3, 1:9, :]), [B2], [BW])
                KbdF = e2.enter_context(self.sbuf("KbdF", [128, 4, 8, 128], F32))
                V(lambda: nc.vector.memset(KbdF[:], 0.0), [], [B2])
                for pair in range(16):
                    gt, gp = pair // 4, pair % 4
                    pt, Bp = self.psum()
                    rs = slice(32 * gp, 32 * gp + 32) if gp < 3 else slice(64, 128)
                    cs_ = slice(32, 64) if gp < 3 else slice(0, 64)
                    P(lambda: nc.tensor.matmul(pt[rs, 0:256], lhsT=Bpr[:, pair, cs_], rhs=CLr[:, pair, 0:8, :], start=True, stop=False), [B2], [Bp])
                    P(lambda: nc.tensor.matmul(pt[rs, 0:256], lhsT=Bpi[:, pair, cs_], rhs=CLn[:, pair, 0:8, :], start=False, stop=True), [B2], [Bp])
                    V(lambda: nc.vector.tensor_copy(out=KbdF[rs, gt, :, 32 * gp:32 * gp + 32], in_=pt[rs, 0:256].rearrange("p (m c) -> p m c", c=32)), [Bp, B2], [B2])
                for gt in range(4):
                    V(lambda: nc.vector.scalar_tensor_tensor(out=KbdF[:, gt, 0, :], in0=self.ident_f[:], scalar=self.vcol(l, "s5_d", gt, 1), in1=KbdF[:, gt, 0, :],
                                                             op0=ALU.mult, op1=ALU.add), [B2, Bc, self.Bvecs], [B2])
                V(lambda: nc.vector.tensor_copy(out=Kbd[:], in_=KbdF[:]), [B2], [BW])
            S.barrier()
            if "s5pre" in self.debug:
                self.dump("dbg_M1re", M1re[:], [BW]); self.dump("dbg_M1im", M1im[:], [BW]); self.dump("dbg_M2re", M2re[:], [BW])
                self.dump("dbg_M2im", M2im[:], [BW]); self.dump("dbg_Kbd", Kbd[:], [BW]); self.dump("dbg_LK", LK[:], [BW])
            for sq in range(NSEQ):
                with ExitStack() as e3:
                    self.s5_seq(e3, l, sq, (M1re, M1im, M1hr, M1hi), (M2re, M2im, M2hr, M2hi), Kbd, LK, wglu, BW, Bwg)
                    S.barrier()

    def s5_seq(self, es, l, sq, M1s, M2s, Kbd, LK, wglu, BW, Bwg):
        M1re, M1im, M1hr, M1hi = M1s
        M2re, M2im, M2hr, M2hi = M2s
        nc, S = self.nc, self.S
        V, A, P = self.V, self.A, self.P
        pT, BpT = self.dram["pT"], self.dbuf["pT"]
        ym, Bym = self.dram["ymix"], self.dbuf["ymix"]
        t0 = sq * SEQ
        L = SEQ
        NC8 = L // 8
        us = self.tiles(es, "s5u", [128, L], BF16, 4)
        for gt in range(4):
            S.dma("sp", us[gt][0][:], pT[S50 + 128 * gt:S50 + 128 * gt + 128, t0:t0 + L], reads=[BpT], writes=[us[gt][1]])
        S16r = es.enter_context(self.sbuf("s5Sr", [128, 16, NC8 + 1], BF16)); S16i = es.enter_context(self.sbuf("s5Si", [128, 16, NC8 + 1], BF16))
        BS16 = [Buf() for _ in range(16)]
        Bz = Buf()
        V(lambda: nc.vector.memset(S16r[:, :, 0:1], 0.0), [], [Bz])
        V(lambda: nc.vector.memset(S16i[:, :, 0:1], 0.0), [], [Bz])
        for b_ in BS16:
            b_.lw = Bz.lw
        sc = [[[self.tiles(es, "s5sc", [128, 2 * NC8], F32, 2) for _ri in range(2)] for _pp in range(1)] for _slot in range(2)]
        for slot in range(2):
            for ri in range(2):
                for pp in range(2):
                    tt_, Bt = sc[slot][0][ri][pp]
                    V(lambda: nc.vector.memset(tt_[:, 0:NC8], 0.0), [], [Bt])
        for p0 in range(0, 16, 2):
            prs = [p0, p0 + 1]
            for si, pair in enumerate(prs):
                gt, gp = pair // 4, pair % 4
                rs = slice(32 * gp, 32 * gp + 32) if gp < 3 else slice(64, 128)
                u, Bu = us[gt]
                u3 = u[:].rearrange("p (c j) -> p j c", j=8)
                for ri, M1 in enumerate((M1re, M1im) if gp < 3 else (M1hr, M1hi)):
                    pt, Bp = self.psum()
                    for j in range(8):
                        P(lambda: nc.tensor.matmul(pt[:, 0:NC8], lhsT=M1[rs, gt, j, :], rhs=u3[rs, j, :], start=(j == 0), stop=(j == 7)), [BW, Bu], [Bp])
                    tt_, Bt = sc[si][0][ri][0]
                    A(lambda: nc.scalar.copy(out=tt_[:, NC8:2 * NC8], in_=pt[:, 0:NC8]), [Bp], [Bt])
            for lev in range(8):
                sh = 1 << lev
                src, dst = lev % 2, (lev + 1) % 2
                for step in range(2):
                    for si, pair in enumerate(prs):
                        (re_s, Brs), (im_s, Bis) = sc[si][0][0][src], sc[si][0][1][src]
                        (re_d, Brd), (im_d, Bid) = sc[si][0][0][dst], sc[si][0][1][dst]
                        lr, li, nli = LK[:, pair, lev, 0:1], LK[:, pair, lev, 1:2], LK[:, pair, lev, 2:3]
                        cur = slice(NC8, 2 * NC8); shf = slice(NC8 - sh, 2 * NC8 - sh)
                        if step == 0:
                            V(lambda: nc.vector.scalar_tensor_tensor(out=re_d[:, cur], in0=re_s[:, shf], scalar=lr, in1=re_s[:, cur], op0=ALU.mult, op1=ALU.add), [Brs, BW], [Brd])
                            V(lambda: nc.vector.scalar_tensor_tensor(out=im_d[:, cur], in0=im_s[:, shf], scalar=lr, in1=im_s[:, cur], op0=ALU.mult, op1=ALU.add), [Bis, BW], [Bid])
                        else:
                            V(lambda: nc.vector.scalar_tensor_tensor(out=re_d[:, cur], in0=im_s[:, shf], scalar=nli, in1=re_d[:, cur], op0=ALU.mult, op1=ALU.add), [Bis, Brd, BW], [Brd])
                            V(lambda: nc.vector.scalar_tensor_tensor(out=im_d[:, cur], in0=re_s[:, shf], scalar=li, in1=im_d[:, cur], op0=ALU.mult, op1=ALU.add), [Brs, Bid, BW], [Bid])
            for si, pair in enumerate(prs):
                (re_f, Brf), (im_f, Bif) = sc[si][0][0][0], sc[si][0][1][0]
                A(lambda: nc.scalar.copy(out=S16r[:, pair, 1:NC8 + 1], in_=re_f[:, NC8:2 * NC8]), [Brf], [BS16[pair]])
                A(lambda: nc.scalar.copy(out=S16i[:, pair, 1:NC8 + 1], in_=im_f[:, NC8:2 * NC8]), [Bif], [BS16[pair]])
        ys = self.tiles(es, "s5y", [128, L], F32, 2)
        yg = self.tiles(es, "s5yg", [128, L], BF16, 4)
        ta = self.tiles(es, "s5ta", [128, 512], F32, 2)
        for gt in range(4):
            y, By = ys[gt % 2]
            u, Bu = us[gt]
            u3 = u[:].rearrange("p (c j) -> p j c", j=8)
            y3 = y[:].rearrange("p (c j) -> p j c", j=8)
            for j in range(8):
                pt, Bp = self.psum()
                for i in range(j + 1):
                    P(lambda: nc.tensor.matmul(pt[:, 0:NC8], lhsT=Kbd[:, gt, j - i, :], rhs=u3[:, i, :], start=(i == 0), stop=False), [BW, Bu], [Bp])
                for gp in range(4):
                    pair = 4 * gt + gp
                    if gp < 3:
                        rs = slice(32 * gp, 32 * gp + 32)
                        l_re, l_im = M2re[:, pair, j, :], M2im[:, pair, j, :]
                    else:
                        rs = slice(64, 128)
                        l_re, l_im = M2hr[:, gt, j, :], M2hi[:, gt, j, :]
                    P(lambda: nc.tensor.matmul(pt[rs, 0:NC8], lhsT=l_re, rhs=S16r[:, pair, 0:NC8], start=False, stop=False), [BW, BS16[pair]], [Bp])
                    P(lambda: nc.tensor.matmul(pt[rs, 0:NC8], lhsT=l_im, rhs=S16i[:, pair, 0:NC8], start=False, stop=(gp == 3)), [BW, BS16[pair]], [Bp])
                A(lambda: nc.scalar.copy(out=y3[:, j, :], in_=pt[:, 0:NC8]), [Bp], [By])
            g16, Bg = yg[gt]
            for tb in range(L // 512):
                cs = slice(tb * 512, tb * 512 + 512)
                t_, Bt = ta[tb % 2]
                A(lambda: nc.scalar.activation(out=t_[:], in_=y[:, cs], func=AF.Square), [By], [Bt])
                V(lambda: nc.vector.tensor_scalar(out=t_[:], in0=t_[:], scalar1=0.044715, scalar2=1.0, op0=ALU.mult, op1=ALU.add), [Bt], [Bt])
                V(lambda: nc.vector.tensor_tensor(out=t_[:], in0=t_[:], in1=y[:, cs], op=ALU.mult), [Bt, By], [Bt])
                A(lambda: nc.scalar.activation(out=t_[:], in_=t_[:], func=AF.Sigmoid, scale=1.5957691216057308), [Bt], [Bt])
                V(lambda: nc.vector.tensor_tensor(out=g16[:, cs], in0=t_[:], in1=y[:, cs], op=ALU.mult), [Bt, By], [Bg])
        yo = self.tiles(es, "s5yo", [128, 512], BF16, 2)
        sg = self.tiles(es, "s5sg", [128, 512], F32, 2)
        cnt = 0
        for m in range(4):
            for tb in range(L // 512):
                cs = slice(tb * 512, tb * 512 + 512)
                pt, Bp = self.psum()
                for kt in range(4):
                    P(lambda: nc.tensor.matmul(pt[:], lhsT=wglu[:, kt, m * 128:(m + 1) * 128], rhs=yg[kt][0][:, cs], start=(kt == 0), stop=(kt == 3)), [Bwg, yg[kt][1]], [Bp])
                s_, Bs = sg[cnt % 2]; o_, Bo = yo[cnt % 2]; cnt += 1
                A(lambda: nc.scalar.activation(out=s_[:], in_=pt[:], func=AF.Sigmoid, bias=self.vcol(l, "s5_b_glu", m, 1)), [Bp, self.Bvecs], [Bs])
                V(lambda: nc.vector.tensor_tensor(out=o_[:], in0=s_[:], in1=yg[m][0][:, cs], op=ALU.mult), [Bs, yg[m][1]], [Bo])
                S.dma("sp", ym[1, 128 * m:128 * m + 128, t0 + tb * 512:t0 + tb * 512 + 512], o_[:], reads=[Bo], writes=[Bym])

    def stage_rwkv(self, l, sq):
        nc, S = self.nc, self.S
        V, A, P = self.V, self.A, self.P
        pT, BpT = self.dram["pT"], self.dbuf["pT"]
        ym, Bym = self.dram["ymix"], self.dbuf["ymix"]
        Bc = self.Bconst
        Bv = self.Bvecs
        t0 = sq * SEQ
        L = SEQ
        NCH = L // 64
        ALPHA = float(np.exp(-0.5))
        with ExitStack() as es:
            WU = es.enter_context(self.sbuf("rwWU", [128, 512], BF16)); BWU = Buf()
            GU = es.enter_context(self.sbuf("rwGU", [128, 512], BF16)); BGU = Buf()
            S.dma("pool", WU[0:64, :], self.dram["rw_w_up"][l], reads=[self.dbuf["rw_w_up"]], writes=[BWU])
            S.dma("pool", WU[64:128, :], self.dram["rw_a_up"][l], reads=[self.dbuf["rw_a_up"]], writes=[BWU])
            S.dma("pool", GU[:], self.dram["rw_g_up"][l], reads=[self.dbuf["rw_g_up"]], writes=[BGU])
            TW = es.enter_context(self.sbuf("rwTW", [128, L], BF16)); BTW = Buf()
            SG = es.enter_context(self.sbuf("rwSG", [128, L], BF16)); BSG = Buf()
            bdn = ["Rt", "Kt", "Bt", "At", "Vb"]
            BD = {n: es.enter_context(self.sbuf("rw" + n, [128, NCH, 128], BF16)) for n in bdn}
            BBD = {n: Buf(n) for n in bdn}
            for n in bdn:
                V(lambda: nc.vector.memset(BD[n][:], 0.0), [], [BBD[n]])
            Dt = es.enter_context(self.sbuf("rwD", [128, NCH], F32)); BD_ = Buf()
            g16 = es.enter_context(self.sbuf("rwg16", [128, L], BF16)); Bg16 = Buf()
            bon = es.enter_context(self.sbuf("rwbon", [128, L], BF16)); Bbon = Buf()
            Yfm = es.enter_context(self.sbuf("rwY", [128, L], F32)); BY = Buf()
            Sall = es.enter_context(self.sbuf("rwSall", [128, NCH + 1, 128], BF16)); BSall = [Buf() for _ in range(NCH + 1)]
            with ExitStack() as e2:
                xin = e2.enter_context(self.sbuf("rwxin", [128, L + 1], BF16)); Bxin = Buf()
                tmp = e2.enter_context(self.sbuf("rwtmp", [128, L], F32)); Btmp = Buf()
                for (r0, mucol, which) in [(1536, 12, 0), (1664, 13, 1)]:
                    V(lambda: nc.vector.memset(xin[:, 0:1], 0.0), [], [Bxin])
                    S.dma("sp", xin[:, 1:L + 1], pT[RW0 + r0:RW0 + r0 + 128, t0:t0 + L], reads=[BpT], writes=[Bxin])
                    V(lambda: nc.vector.tensor_tensor(out=tmp[:], in0=xin[:, 0:L], in1=xin[:, 1:L + 1], op=ALU.subtract), [Bxin], [Btmp])
                    V(lambda: nc.vector.scalar_tensor_tensor(out=tmp[:], in0=tmp[:], scalar=self.vcol(l, "rw_mu", mucol, 1), in1=xin[:, 1:L + 1],
                                                             op0=ALU.mult, op1=ALU.add), [Btmp, Bxin, Bv], [Btmp])
                    if which == 0:
                        A(lambda: nc.scalar.activation(out=TW[0:64, :], in_=tmp[0:64, :], func=AF.Tanh), [Btmp], [BTW])
                        V(lambda: nc.vector.tensor_copy(out=TW[64:128, :], in_=tmp[64:128, :]), [Btmp], [BTW])
                    else:
                        A(lambda: nc.scalar.activation(out=SG[:], in_=tmp[:], func=AF.Sigmoid), [Btmp], [BSG])
                S.barrier()
            for hp in range(4):
                V(lambda: nc.vector.memset(Sall[:, 0, :], 0.0), [], [BSall[0]])
                with ExitStack() as e2:
                    self.rwkv_prologue(e2, l, sq, hp, WU, BWU, GU, BGU, TW, BTW, SG, BSG, BD, BBD, Dt, BD_, g16, Bg16, bon, Bbon)
                    S.barrier()
                with ExitStack() as e2:
                    self.rwkv_chunks(e2, BD, BBD, Dt, BD_, Yfm, BY, Sall, BSall)
                    S.barrier()
                with ExitStack() as e2:
                    self.rwkv_epilogue(e2, l, sq, hp, Yfm, BY, g16, Bg16, bon, Bbon)
                    S.barrier()

    def rwkv_prologue(self, es, l, sq, hp, WU, BWU, GU, BGU, TW, BTW, SG, BSG, BD, BBD, Dt, BDt, g16, Bg16, bon, Bbon):
        nc, S = self.nc, self.S
        V, A, P = self.V, self.A, self.P
        pT, BpT = self.dram["pT"], self.dbuf["pT"]
        Bc, Bv = self.Bconst, self.Bvecs
        t0 = sq * SEQ
        L = SEQ
        NCH = L // 64
        ALPHA = float(np.exp(-0.5))
        cs_hp = slice(128 * hp, 128 * hp + 128)
        def f32t(n):
            return es.enter_context(self.sbuf("rwp" + n, [128, L + 1], F32)), Buf(n)
        xin = [(es.enter_context(self.sbuf("rwpx%d" % i, [128, L + 1], BF16)), Buf()) for i in range(3)]
        (X1, B1), (X2, B2), (X3, B3) = [(es.enter_context(self.sbuf("rwpb%d" % i, [128, L], BF16 if i != 1 else F32)), Buf()) for i in range(3)]
        (X4, B4), (X5, B5), (X6, B6), (X7, B7), (X9, B9), (X10, B10) = [f32t(n) for n in ["4", "5", "6", "7", "9", "10"]]
        for i, (dst, Bd, r0) in enumerate([(X1, B1, 0), (X2, B2, 512), (X3, B3, 1024)]):
            xt, Bx = xin[i]
            V(lambda: nc.vector.memset(xt[:, 0:1], 0.0), [], [Bx])
            S.dma("sp", xt[:, 1:L + 1], pT[RW0 + r0 + 128 * hp:RW0 + r0 + 128 * hp + 128, t0:t0 + L], reads=[BpT], writes=[Bx])
            V(lambda: nc.vector.tensor_tensor(out=X7[:, 0:L], in0=xt[:, 0:L], in1=xt[:, 1:L + 1], op=ALU.subtract), [Bx], [B7])
            V(lambda: nc.vector.scalar_tensor_tensor(out=dst[:], in0=X7[:, 0:L], scalar=self.vcol(l, "rw_mu", 4 * i + hp, 1), in1=xt[:, 1:L + 1],
                                                     op0=ALU.mult, op1=ALU.add), [B7, Bx, Bv], [Bd])
        for tb in range(L // 512):
            cs = slice(tb * 512, tb * 512 + 512)
            pt, Bp = self.psum()
            P(lambda: nc.tensor.matmul(pt[:], lhsT=WU[0:64, cs_hp], rhs=TW[0:64, cs], start=True, stop=True), [BWU, BTW], [Bp])
            A(lambda: nc.scalar.activation(out=X4[:, cs], in_=pt[:], func=AF.Sigmoid, bias=self.vcol(l, "rw_w0", hp, 1)), [Bp, Bv], [B4])
            pt, Bp = self.psum()
            P(lambda: nc.tensor.matmul(pt[:], lhsT=WU[64:128, cs_hp], rhs=TW[64:128, cs], start=True, stop=True), [BWU, BTW], [Bp])
            A(lambda: nc.scalar.activation(out=X5[:, cs], in_=pt[:], func=AF.Sigmoid, bias=self.vcol(l, "rw_a0", hp, 1)), [Bp, Bv], [B5])
            pt, Bp = self.psum()
            P(lambda: nc.tensor.matmul(pt[:], lhsT=GU[:, cs_hp], rhs=SG[:, cs], start=True, stop=True), [BGU, BSG], [Bp])
            V(lambda: nc.vector.tensor_copy(out=g16[:, cs], in_=pt[:]), [Bp], [Bg16])
        V(lambda: nc.vector.memset(X9[:, 0:1], 0.0), [], [B9])
        V(lambda: nc.vector.tensor_tensor_scan(out=X9[:, 1:L + 1], data0=self.cst[:, 0:1].to_broadcast([128, L]), data1=X4[:, 0:L],
                                               initial=0.0, op0=ALU.mult, op1=ALU.add), [B4, Bc], [B9])
        V(lambda: nc.vector.tensor_scalar(out=X6[:, 0:L], in0=X2[:], scalar1=self.vcol(l, "rw_k_k", hp, 1), scalar2=None, op0=ALU.mult), [B2, Bv], [B6])
        A(lambda: nc.scalar.activation(out=X7[:, 0:L], in_=X6[:, 0:L], func=AF.Square), [B6], [B7])
        for tb in range(L // 512):
            cs = slice(tb * 512, tb * 512 + 512)
            pt, Bp = self.psum()
            P(lambda: nc.tensor.matmul(pt[:], lhsT=self.ones_bd[:], rhs=X7[:, cs], start=True, stop=True), [Bc, B7], [Bp])
            A(lambda: nc.scalar.activation(out=X4[:, cs], in_=pt[:], func=AF.Sqrt, bias=self.cst[:, 2:3]), [Bp, Bc, B9], [B4])
        V(lambda: nc.vector.reciprocal(out=X4[:, 0:L], in_=X4[:, 0:L]), [B4], [B4])
        V(lambda: nc.vector.tensor_tensor(out=X6[:, 0:L], in0=X6[:, 0:L], in1=X4[:, 0:L], op=ALU.mult), [B6, B4], [B6])
        V(lambda: nc.vector.tensor_scalar(out=X7[:, 0:L], in0=X5[:, 0:L], scalar1=-1.0, scalar2=self.vcol(l, "rw_k_a", hp, 1), op0=ALU.add, op1=ALU.mult), [B5, Bv], [B7])
        V(lambda: nc.vector.scalar_tensor_tensor(out=X2[:], in0=X7[:, 0:L], scalar=1.0, in1=X2[:], op0=ALU.add, op1=ALU.mult), [B7, B2], [B2])
        V(lambda: nc.vector.tensor_tensor(out=X5[:, 0:L], in0=X6[:, 0:L], in1=X5[:, 0:L], op=ALU.mult), [B6, B5], [B5])
        V(lambda: nc.vector.tensor_tensor(out=X7[:, 0:L], in0=X1[:], in1=X2[:], op=ALU.mult), [B1, B2], [B7])
        V(lambda: nc.vector.tensor_scalar(out=X7[:, 0:L], in0=X7[:, 0:L], scalar1=self.vcol(l, "rw_r_k", hp, 1), scalar2=None, op0=ALU.mult), [B7, Bv], [B7])
        for tb in range(L // 512):
            cs = slice(tb * 512, tb * 512 + 512)
            pt, Bp = self.psum()
            P(lambda: nc.tensor.matmul(pt[:], lhsT=self.ones_bd[:], rhs=X7[:, cs], start=True, stop=True), [Bc, B7], [Bp])
            V(lambda: nc.vector.tensor_tensor(out=bon[:, cs], in0=pt[:], in1=X3[:, cs], op=ALU.mult), [Bp, B3], [Bbon])
        c3 = lambda ap: ap.rearrange("p (c t) -> p c t", t=64)
        base = c3(X9[:, 0:L])[:, :, 0:1].to_broadcast([128, NCH, 64])
        V(lambda: nc.vector.tensor_tensor(out=c3(X7[:, 0:L]), in0=c3(X9[:, 1:L + 1]), in1=base, op=ALU.subtract), [B9, Bbon], [B7])
        A(lambda: nc.scalar.activation(out=X4[:, 0:L], in_=X7[:, 0:L], func=AF.Exp, scale=-ALPHA), [B7], [B4])
        A(lambda: nc.scalar.activation(out=X10[:, 0:L], in_=X7[:, 0:L], func=AF.Exp, scale=ALPHA), [B7], [B10])
        V(lambda: nc.vector.tensor_tensor(out=c3(X7[:, 0:L]), in0=c3(X9[:, 0:L]), in1=base, op=ALU.subtract), [B9, B4, B10], [B7])
        A(lambda: nc.scalar.activation(out=X7[:, 0:L], in_=X7[:, 0:L], func=AF.Exp, scale=-ALPHA), [B7], [B7])
        V(lambda: nc.vector.tensor_copy(out=Dt[:], in_=c3(X4[:, 0:L])[:, :, 63]), [B4], [BDt])
        for hf in range(2):
            ps_ = slice(64 * hf, 64 * hf + 64)
            fs_ = slice(64 * hf, 64 * hf + 64)
            V(lambda: nc.vector.tensor_tensor(out=BD["Rt"][ps_, :, fs_], in0=c3(X1[ps_, :]), in1=c3(X4[ps_, 0:L]), op=ALU.mult), [B1, B4], [BBD["Rt"]])
            V(lambda: nc.vector.tensor_tensor(out=BD["Kt"][ps_, :, fs_], in0=c3(X2[ps_, :]), in1=c3(X10[ps_, 0:L]), op=ALU.mult), [B2, B10], [BBD["Kt"]])
            V(lambda: nc.vector.tensor_tensor(out=BD["Bt"][ps_, :, fs_], in0=c3(X5[ps_, 0:L]), in1=c3(X10[ps_, 0:L]), op=ALU.mult), [B5, B10], [BBD["Bt"]])
            V(lambda: nc.vector.scalar_tensor_tensor(out=BD["At"][ps_, :, fs_], in0=c3(X6[ps_, 0:L]), scalar=-1.0, in1=c3(X7[ps_, 0:L]), op0=ALU.mult, op1=ALU.mult),
              [B6, B7], [BBD["At"]])
            A(lambda: nc.scalar.copy(out=BD["Vb"][ps_, :, fs_], in_=c3(X3[ps_, :])), [B3], [BBD["Vb"]])

    def rwkv_chunks(self, es, BD, BBD, Dt, BDt, Yfm, BY, Sall, BSall):
        nc, S = self.nc, self.S
        V, A, P = self.V, self.A, self.P
        Bc = self.Bconst
        NCH = SEQ // 64
        W = 4
        NU = NCH // 2
        Rt, Kt, Bt, At, Vb = [BD[n] for n in ["Rt", "Kt", "Bt", "At", "Vb"]]
        BRt, BKt, BBt, BAt, BVb = [BBD[n] for n in ["Rt", "Kt", "Bt", "At", "Vb"]]
        NR = 2 * W
        TM3 = self.tiles(es, "rcTM", [128, 2, 3, 128], BF16, NR)
        ZA = self.tiles(es, "rcZA", [128, 2, 256], BF16, NR)
        ZB = self.tiles(es, "rcZB", [128, 2, 256], BF16, NR)
        SC1 = self.tiles(es, "rcS1", [128, 2, 2, 128], BF16, NR)
        SC2 = self.tiles(es, "rcS2", [128, 2, 2, 128], BF16, NR)
        SC3 = self.tiles(es, "rcS3", [128, 2, 128], BF16, NR)
        PPA = self.tiles(es, "rcPA", [128, 2, 2, 128], BF16, NR)
        PPB = self.tiles(es, "rcPB", [128, 2, 2, 128], BF16, NR)
        IGQ = self.tiles(es, "rcIG", [128, 2, 2, 128], BF16, NR)
        HD = self.tiles(es, "rcHD", [128, 2, 128], F32, NR)
        m01 = self.mbd[:, 0:2, :].unsqueeze(1).to_broadcast([128, 2, 2, 128])
        m12 = self.mbd[:, 1:3, :].unsqueeze(1).to_broadcast([128, 2, 2, 128])
        m2 = self.mbd[:, 2:3, :].to_broadcast([128, 2, 128])
        identb = self.ident_f[:].unsqueeze(1).to_broadcast([128, 2, 128])

        pending = []
        post = []

        def drain(n):
            for _ in range(n):
                if pending:
                    pending.pop(0)()

        ngroups = NU // W
        for gi in range(ngroups):
            units = list(range(gi * W, gi * W + W))
            sl = {u: (u % NR) for u in units}
            for u in units:
                i = sl[u]; c0 = 2 * u
                pt, Bp = self.psum()
                pb = pt[:].bitcast(BF16).rearrange("p (k s t) -> p k s t", k=2, s=4)
                for k in range(2):
                    for si, (src, Bs) in enumerate([(Bt, BBt), (Kt, BKt), (Vb, BVb), (At, BAt)]):
                        P(lambda: nc.tensor.transpose(out=pb[:, k, si, :], in_=src[:, c0 + k, :], identity=self.ident_b[:]), [Bs, Bc], [Bp])
                A(lambda: nc.scalar.copy(out=TM3[i][0][:], in_=pb[:, :, 0:3, :]), [Bp], [TM3[i][1]])
                A(lambda: nc.scalar.copy(out=ZA[i][0][:, :, 0:128], in_=pb[:, :, 3, :]), [Bp], [ZA[i][1]])
            drain(1)
            for u in units:
                i = sl[u]; c0 = 2 * u
                pt, Bp = self.psum(); p4 = pt[:].rearrange("p (k s t) -> p k s t", k=2, s=2)
                for k in range(2):
                    c = c0 + k
                    P(lambda: nc.tensor.matmul(p4[:, k, 0, :], lhsT=At[:, c, :], rhs=Bt[:, c, :], start=True, stop=True), [BAt, BBt], [Bp])
                    P(lambda: nc.tensor.matmul(p4[:, k, 1, :], lhsT=Bt[:, c, :], rhs=At[:, c, :], start=True, stop=True), [BAt, BBt], [Bp])
                V(lambda: nc.vector.tensor_tensor(out=SC1[i][0][:], in0=p4, in1=m01, op=ALU.mult), [Bp, Bc], [SC1[i][1]])
                pt, Bp = self.psum(); p4 = pt[:].rearrange("p (k s t) -> p k s t", k=2, s=2)
                for k in range(2):
                    c = c0 + k
                    P(lambda: nc.tensor.matmul(p4[:, k, 0, :], lhsT=Kt[:, c, :], rhs=At[:, c, :], start=True, stop=True), [BAt, BKt], [Bp])
                    P(lambda: nc.tensor.matmul(p4[:, k, 1, :], lhsT=Bt[:, c, :], rhs=Rt[:, c, :], start=True, stop=True), [BRt, BBt], [Bp])
                V(lambda: nc.vector.tensor_tensor(out=SC2[i][0][:], in0=p4, in1=m12, op=ALU.mult), [Bp, Bc], [SC2[i][1]])
                pt, Bp = self.psum(); p3 = pt[:, 0:256].rearrange("p (k t) -> p k t", k=2)
                for k in range(2):
                    c = c0 + k
                    P(lambda: nc.tensor.matmul(p3[:, k, :], lhsT=Kt[:, c, :], rhs=Rt[:, c, :], start=True, stop=True), [BRt, BKt], [Bp])
                V(lambda: nc.vector.tensor_tensor(out=SC3[i][0][:], in0=p3, in1=m2, op=ALU.mult), [Bp, Bc], [SC3[i][1]])
            drain(1)
            for u in units:
                i = sl[u]
                pt, Bp = self.psum(); p3 = pt[:, 0:256].rearrange("p (k t) -> p k t", k=2)
                for k in range(2):
                    P(lambda: nc.tensor.matmul(p3[:, k, :], lhsT=SC2[i][0][:, k, 0, :], rhs=TM3[i][0][:, k, 2, :], start=True, stop=True), [SC2[i][1], TM3[i][1]], [Bp])
                A(lambda: nc.scalar.copy(out=ZA[i][0][:, :, 128:256], in_=p3), [Bp], [ZA[i][1]])
            drain(1)
            for lev in range(6):
                for u in units:
                    i = sl[u]
                    PPs = SC1[i] if lev == 0 else (PPA[i] if lev % 2 == 1 else PPB[i])
                    PPd = PPA[i] if lev % 2 == 0 else PPB[i]
                    Zs = ZA[i] if lev % 2 == 0 else ZB[i]
                    Zd = ZB[i] if lev % 2 == 0 else ZA[i]
                    pt, Bp = self.psum(); pz = pt[:].rearrange("p (k t) -> p k t", k=2)
                    if lev % 2 == 1:
                        for k in range(2):
                            P(lambda: nc.tensor.matmul(pz[:, k, :], lhsT=self.ident_b[:], rhs=Zs[0][:, k, :], start=True, stop=False), [Bc, Zs[1]], [Bp])
                            P(lambda: nc.tensor.matmul(pz[:, k, :], lhsT=PPs[0][:, k, 1, :], rhs=Zs[0][:, k, :], start=False, stop=True), [PPs[1], Zs[1]], [Bp])
                        A(lambda: nc.scalar.copy(out=Zd[0][:], in_=pz), [Bp], [Zd[1]])
                    else:
                        for k in range(2):
                            P(lambda: nc.tensor.matmul(pz[:, k, :], lhsT=PPs[0][:, k, 1, :], rhs=Zs[0][:, k, :], start=True, stop=True), [PPs[1], Zs[1]], [Bp])
                        V(lambda: nc.vector.tensor_tensor(out=Zd[0][:], in0=pz, in1=Zs[0][:], op=ALU.add), [Bp, Zs[1]], [Zd[1]])
                    if lev < 5:
                        pt, Bp = self.psum(); p4 = pt[:].rearrange("p (k s t) -> p k s t", k=2, s=2)
                        for k in range(2):
                            P(lambda: nc.tensor.matmul(p4[:, k, 0, :], lhsT=PPs[0][:, k, 1, :], rhs=PPs[0][:, k, 0, :], start=True, stop=True), [PPs[1]], [Bp])
                            P(lambda: nc.tensor.matmul(p4[:, k, 1, :], lhsT=PPs[0][:, k, 0, :], rhs=PPs[0][:, k, 1, :], start=True, stop=True), [PPs[1]], [Bp])
                        A(lambda: nc.scalar.copy(out=PPd[0][:], in_=p4), [Bp], [PPd[1]])
                drain(1)
            for u in units:
                i = sl[u]; c0 = 2 * u
                pt, Bp = self.psum(); p4 = pt[:].rearrange("p (k s t) -> p k s t", k=2, s=2)
                for k in range(2):
                    P(lambda: nc.tensor.matmul(p4[:, k, 0, :], lhsT=ZA[i][0][:, k, 0:128], rhs=TM3[i][0][:, k, 0, :], start=True, stop=True), [ZA[i][1], TM3[i][1]], [Bp])
                    P(lambda: nc.tensor.matmul(p4[:, k, 1, :], lhsT=ZA[i][0][:, k, 0:128], rhs=SC2[i][0][:, k, 1, :], start=True, stop=True), [ZA[i][1], SC2[i][1]], [Bp])
                V(lambda: nc.vector.tensor_tensor(out=IGQ[i][0][:, :, 0, :], in0=p4[:, :, 0, :], in1=identb, op=ALU.add), [Bp, Bc], [IGQ[i][1]])
                V(lambda: nc.vector.tensor_tensor(out=IGQ[i][0][:, :, 1, :], in0=p4[:, :, 1, :], in1=Rt[:, c0:c0 + 2, :], op=ALU.add), [Bp, BRt], [IGQ[i][1]])
            drain(1)
            for u in units:
                i = sl[u]; c0 = 2 * u
                pt, Bp = self.psum(); p3 = pt[:, 0:256].rearrange("p (k t) -> p k t", k=2)
                for k in range(2):
                    P(lambda: nc.tensor.matmul(p3[:, k, :], lhsT=TM3[i][0][:, k, 0, :], rhs=ZA[i][0][:, k, 128:256], start=True, stop=False), [ZA[i][1], TM3[i][1]], [Bp])
                    P(lambda: nc.tensor.matmul(p3[:, k, :], lhsT=TM3[i][0][:, k, 1, :], rhs=TM3[i][0][:, k, 2, :], start=False, stop=True), [TM3[i][1]], [Bp])
                for k in range(2):
                    A(lambda: nc.scalar.activation(out=HD[i][0][:, k, :], in_=p3[:, k, :], func=AF.Copy, scale=Dt[:, c0 + k:c0 + k + 1]), [Bp, BDt], [HD[i][1]])
                pt, Bp = self.psum(); p3 = pt[:, 0:256].rearrange("p (k t) -> p k t", k=2)
                for k in range(2):
                    P(lambda: nc.tensor.matmul(p3[:, k, :], lhsT=ZA[i][0][:, k, 128:256], rhs=SC2[i][0][:, k, 1, :], start=True, stop=False), [ZA[i][1], SC2[i][1]], [Bp])
                    P(lambda: nc.tensor.matmul(p3[:, k, :], lhsT=TM3[i][0][:, k, 2, :], rhs=SC3[i][0][:, k, :], start=False, stop=True), [TM3[i][1], SC3[i][1]], [Bp])
                for hf in range(2):
                    hs = slice(64 * hf, 64 * hf + 64)
                    A(lambda: nc.scalar.copy(out=Yfm[hs, 128 * u:128 * u + 128].rearrange("p (k t) -> p k t", k=2), in_=p3[hs, :, hs]), [Bp], [BY])
            drain(len(pending))
            for f in post:
                f()
            post = []
            for u in units:
                i = sl[u]; c0 = 2 * u
                for k in range(2):
                    def link(i=i, c=c0 + k, k=k):
                        pt, Bp = self.psum()
                        P(lambda: nc.tensor.matmul(pt[:, 0:128], lhsT=IGQ[i][0][:, k, 0, :], rhs=Sall[:, c, :], start=True, stop=True), [IGQ[i][1], BSall[c]], [Bp])
                        V(lambda: nc.vector.scalar_tensor_tensor(out=Sall[:, c + 1, :], in0=pt[:, 0:128], scalar=Dt[:, c:c + 1], in1=HD[i][0][:, k, :],
                                                                 op0=ALU.mult, op1=ALU.add), [Bp, BDt, HD[i][1]], [BSall[c + 1]])
                    pending.append(link)
                def ph8(i=i, u=u, c0=c0):
                    pt, Bp = self.psum(); p3 = pt[:, 0:256].rearrange("p (k t) -> p k t", k=2)
                    for k in range(2):
                        P(lambda: nc.tensor.matmul(p3[:, k, :], lhsT=Sall[:, c0 + k, :], rhs=IGQ[i][0][:, k, 1, :], start=True, stop=True), [BSall[c0 + k], IGQ[i][1]], [Bp])
                    for hf in range(2):
                        hs = slice(64 * hf, 64 * hf + 64)
                        yv = Yfm[hs, 128 * u:128 * u + 128].rearrange("p (k t) -> p k t", k=2)
                        V(lambda: nc.vector.tensor_tensor(out=yv, in0=p3[hs, :, hs], in1=yv, op=ALU.add), [Bp, BY], [BY])
                post.append(ph8)
        drain(len(pending))
        for f in post:
            f()

    def rwkv_epilogue(self, es, l, sq, hp, Yfm, BY, g16, Bg16, bon, Bbon):
        nc, S = self.nc, self.S
        V, A, P = self.V, self.A, self.P
        ym, Bym = self.dram["ymix"], self.dbuf["ymix"]
        Bc, Bv = self.Bconst, self.Bvecs
        t0 = sq * SEQ
        L = SEQ
        NB = L // 512
        ta = self.tiles(es, "reA", [128, 512], F32, NB)
        tb_ = self.tiles(es, "reB", [128, 512], F32, NB)
        yo = self.tiles(es, "reO", [128, 512], BF16, NB)
        CS = [slice(tb * 512, tb * 512 + 512) for tb in range(NB)]
        pts = {}
        for tb in range(NB):
            pts[tb] = self.psum()
            pt, Bp = pts[tb]
            P(lambda: nc.tensor.matmul(pt[:], lhsT=self.ones_bd[:], rhs=Yfm[:, CS[tb]], start=True, stop=True), [Bc, BY], [Bp])
        for tb in range(NB):
            pt, Bp = pts[tb]; a_, Ba = ta[tb]
            V(lambda: nc.vector.scalar_tensor_tensor(out=a_[:], in0=pt[:], scalar=-1.0 / 64, in1=Yfm[:, CS[tb]], op0=ALU.mult, op1=ALU.add), [Bp, BY], [Ba])
        for tb in range(NB):
            a_, Ba = ta[tb]; b_, Bb = tb_[tb]
            A(lambda: nc.scalar.activation(out=b_[:], in_=a_[:], func=AF.Square), [Ba], [Bb])
        for tb in range(NB):
            b_, Bb = tb_[tb]
            pts[tb] = self.psum()
            pt, Bp = pts[tb]
            P(lambda: nc.tensor.matmul(pt[:], lhsT=self.ones_bd[:], rhs=b_[:], start=True, stop=True), [Bc, Bb], [Bp])
        for tb in range(NB):
            pt, Bp = pts[tb]; b_, Bb = tb_[tb]
            A(lambda: nc.scalar.activation(out=b_[:], in_=pt[:], func=AF.Sqrt, scale=1.0 / 64, bias=self.cst[:, 3:4]), [Bp, Bc], [Bb])
        for tb in range(NB):
            b_, Bb = tb_[tb]
            V(lambda: nc.vector.reciprocal(out=b_[:], in_=b_[:]), [Bb], [Bb])
        for tb in range(NB):
            a_, Ba = ta[tb]; b_, Bb = tb_[tb]
            V(lambda: nc.vector.tensor_tensor(out=a_[:], in0=a_[:], in1=b_[:], op=ALU.mult), [Ba, Bb], [Ba])
        for tb in range(NB):
            a_, Ba = ta[tb]
            V(lambda: nc.vector.tensor_scalar(out=a_[:], in0=a_[:], scalar1=self.vcol(l, "rw_ln_w", hp, 1), scalar2=self.vcol(l, "rw_ln_b", hp, 1), op0=ALU.mult, op1=ALU.add),
              [Ba, Bv], [Ba])
        for tb in range(NB):
            a_, Ba = ta[tb]
            V(lambda: nc.vector.tensor_tensor(out=a_[:], in0=a_[:], in1=bon[:, CS[tb]], op=ALU.add), [Ba, Bbon], [Ba])
        for tb in range(NB):
            a_, Ba = ta[tb]; o_, Bo = yo[tb]
            V(lambda: nc.vector.tensor_tensor(out=o_[:], in0=a_[:], in1=g16[:, CS[tb]], op=ALU.mult), [Ba, Bg16], [Bo])
            S.dma("sp", ym[0, 128 * hp:128 * hp + 128, t0 + tb * 512:t0 + tb * 512 + 512], o_[:], reads=[Bo], writes=[Bym])

    def load_w(self, wt, Bw_list, src3, Bsrc, kc_n, step=4):
        for k0 in range(0, kc_n, step):
            k1 = min(kc_n, k0 + step)
            self.S.dma("pool", wt[:, k0:k1, :], src3[:, k0:k1, :], reads=[Bsrc], writes=[Bw_list[k0 // step]])

    def outproj_res(self, actf, Bact, kc_n, w, Bw_of, xt, Bx, ntt, dst_ap, Bdst, tok0, kstep=4):
        nc, S = self.nc, self.S
        for j in range(ntt):
            for fh in range(2):
                pt, Bp = self.psum()
                for kc in range(kc_n):
                    ba = Bact(kc) if callable(Bact) else Bact
                    self.P(lambda: nc.tensor.matmul(pt[:], lhsT=actf(kc, j), rhs=w[:, kc, fh * 512:(fh + 1) * 512], start=(kc == 0), stop=(kc == kc_n - 1)),
                           [ba, Bw_of(kc)], [Bp])
                self.V(lambda: nc.vector.tensor_tensor(out=xt[:, j, fh * 512:(fh + 1) * 512], in0=pt[:], in1=xt[:, j, fh * 512:(fh + 1) * 512], op=ALU.add),
                       [Bp, Bx[j]], [Bx[j]])
            S.dma("sp", dst_ap[tok0 + j * 128:tok0 + (j + 1) * 128, :], xt[:, j, :], reads=[Bx[j]], writes=[Bdst])

    def xsrc(self, l):
        return (self.dram["x"], self.dbuf["x"]) if l == 0 else (self.dram["xres"], self.dbuf["xres"])

    def stage_merge(self, l):
        nc, S = self.nc, self.S
        V, A, P = self.V, self.A, self.P
        pT, BpT = self.dram["pT"], self.dbuf["pT"]
        ym, Bym = self.dram["ymix"], self.dbuf["ymix"]
        xs_ap, Bxs = self.xsrc(l)
        xd_ap, Bxd = self.dram["xres"], self.dbuf["xres"]
        L = SEQ
        with ExitStack() as es:
            wb = es.enter_context(self.sbuf("mgwb", [128, 12, D], BF16)); Bwb = [Buf() for _ in range(3)]
            wo = es.enter_context(self.sbuf("mgwo", [128, 8, D], BF16)); Bwo = [Buf() for _ in range(2)]
            for i in range(3):
                S.dma("pool", wb[:, 4 * i:4 * i + 4, :], self.dram["w_branch"][l, i].rearrange("(k p) c -> p k c", p=128), reads=[self.dbuf["w_branch"]], writes=[Bwb[i]])
            self.load_w(wo, Bwo, self.dram["w_out"][l].rearrange("(k p) c -> p k c", p=128), self.dbuf["w_out"], 8)
            yt = es.enter_context(self.sbuf("mgy", [128, 12, L], BF16)); Byt = [Buf() for _ in range(12)]
            mg = es.enter_context(self.sbuf("mgm", [128, 8, L], BF16)); Bmg = Buf()
            gts = self.tiles(es, "mgg", [128, 3, 512], BF16, 2)
            acc = self.tiles(es, "mga", [128, 512], F32, 2)
            tmp = self.tiles(es, "mgt", [128, 512], F32, 2)
            xts = [(es.enter_context(self.sbuf("mgx%d" % i, [128, 4, D], F32)), [Buf() for _ in range(4)]) for i in range(2)]
            for sq in range(NSEQ):
                t0 = sq * L
                for i in range(3):
                    for kt in range(4):
                        S.dma("sp", yt[:, 4 * i + kt, :], ym[i, 128 * kt:128 * kt + 128, t0:t0 + L], reads=[Bym], writes=[Byt[4 * i + kt]])
                cnt = 0
                for ft in range(8):
                    for tb in range(L // 512):
                        cs = slice(tb * 512, tb * 512 + 512)
                        g_, Bg = gts[cnt % 2]; a_, Ba = acc[cnt % 2]; t_, Bt = tmp[cnt % 2]; cnt += 1
                        S.dma("sp", g_[:], pT[GT0 + 128 * ft:GT0 + 3072:1024, t0 + tb * 512:t0 + tb * 512 + 512].rearrange("(i p) t -> p i t", p=128) if False else
                              bass.AP(tensor=pT.tensor, offset=pT[GT0 + 128 * ft, t0 + tb * 512].offset, ap=[[T, 128], [1024 * T, 3], [1, 512]]),
                              reads=[BpT], writes=[Bg])
                        for i in range(3):
                            pt, Bp = self.psum()
                            for kt in range(4):
                                P(lambda: nc.tensor.matmul(pt[:], lhsT=wb[:, 4 * i + kt, ft * 128:(ft + 1) * 128], rhs=yt[:, 4 * i + kt, cs], start=(kt == 0), stop=(kt == 3)),
                                  [Bwb[i], Byt[4 * i + kt]], [Bp])
                            if i == 0:
                                V(lambda: nc.vector.tensor_tensor(out=a_[:], in0=pt[:], in1=g_[:, 0, :], op=ALU.mult), [Bp, Bg], [Ba])
                            else:
                                V(lambda: nc.vector.tensor_tensor(out=t_[:], in0=pt[:], in1=g_[:, i, :], op=ALU.mult), [Bp, Bg], [Bt])
                                if i == 1:
                                    V(lambda: nc.vector.tensor_tensor(out=a_[:], in0=a_[:], in1=t_[:], op=ALU.add), [Ba, Bt], [Ba])
                                else:
                                    V(lambda: nc.vector.tensor_tensor(out=mg[:, ft, cs], in0=a_[:], in1=t_[:], op=ALU.add), [Ba, Bt], [Bmg])
                def ldx(blk):
                    xt, Bx = xts[blk % 2]
                    tok0 = t0 + blk * 512
                    for j in range(4):
                        S.dma("sp", xt[:, j, :], xs_ap[tok0 + j * 128:tok0 + (j + 1) * 128, :], reads=[Bxs], writes=[Bx[j]])
                ldx(0)
                for blk in range(L // 512):
                    xt, Bx = xts[blk % 2]
                    tok0 = t0 + blk * 512
                    if blk + 1 < L // 512:
                        ldx(blk + 1)
                    self.outproj_res(lambda kc, j: mg[:, kc, blk * 512 + j * 128:blk * 512 + (j + 1) * 128], Bmg, 8, wo, lambda kc: Bwo[kc // 4], xt, Bx, 4, xd_ap, Bxd, tok0)
                S.barrier()

    def stage_memn(self):
        nc, S = self.nc, self.S
        self.memn = nc.alloc_sbuf_tensor("memn", [128, 8, NSEQ * NMEM], BF16); self.Bmemn = Buf("memn")
        with ExitStack() as es:
            self.rmsnorm_fm(es, self.dram["mem"], self.dbuf["mem"], NSEQ * NMEM, self.gvec[:, 0:8], self.memn, self.Bmemn, self.eps6[:, 0:1])
            S.barrier()

    def stage_xattn(self, l):
        nc, S = self.nc, self.S
        V, A, P = self.V, self.A, self.P
        Bc = self.Bconst
        xs_ap, Bxs = self.dram["xres"], self.dbuf["xres"]
        NM = NSEQ * NMEM
        with ExitStack() as es:
            kT = es.enter_context(self.sbuf("xakT", [128, 8, NM], BF16)); BkT = Buf()
            vtm = es.enter_context(self.sbuf("xavtm", [128, NM // 128, D], BF16)); Bvtm = Buf()
            wq = es.enter_context(self.sbuf("xawq", [128, 8, D], BF16)); Bwq = [Buf() for _ in range(2)]
            wo = es.enter_context(self.sbuf("xawo", [128, 8, D], BF16)); Bwo = [Buf() for _ in range(2)]
            ones_b = es.enter_context(self.sbuf("xaones", [128, 128], BF16)); Bob = Buf()
            V(lambda: nc.vector.memset(ones_b[:], 1.0), [], [Bob])
            with ExitStack() as e2:
                wts = [(e2.enter_context(self.sbuf("xawkv%d" % i, [128, 8, 512], BF16)), [Buf(), Buf()]) for i in range(2)]
                wkv = self.dram["xa_wkv"][l]
                for g in range(4):
                    wt, Bw = wts[g % 2]
                    self.load_w(wt, Bw, wkv[:, g * 512:(g + 1) * 512].rearrange("(k p) c -> p k c", p=128), self.dbuf["xa_wkv"], 8)
                    if g < 2:
                        for m in range(4):
                            pt, Bp = self.psum()
                            for kc in range(8):
                                P(lambda: nc.tensor.matmul(pt[:, 0:NM], lhsT=wt[:, kc, m * 128:(m + 1) * 128], rhs=self.memn[:, kc, :], start=(kc == 0), stop=(kc == 7)),
                                  [Bw[kc // 4], self.Bmemn], [Bp])
                            A(lambda: nc.scalar.copy(out=kT[:, g * 4 + m, :], in_=pt[:, 0:NM]), [Bp], [BkT])
                    else:
                        for mt in range(NM // 128):
                            pt, Bp = self.psum()
                            for kc in range(8):
                                P(lambda: nc.tensor.matmul(pt[:], lhsT=self.memn[:, kc, mt * 128:(mt + 1) * 128], rhs=wt[:, kc, :], start=(kc == 0), stop=(kc == 7)),
                                  [Bw[kc // 4], self.Bmemn], [Bp])
                            A(lambda: nc.scalar.copy(out=vtm[:, mt, (g - 2) * 512:(g - 1) * 512], in_=pt[:]), [Bp], [Bvtm])
                S.barrier()
            self.load_w(wq, Bwq, self.dram["xa_wq"][l].rearrange("(k p) c -> p k c", p=128), self.dbuf["xa_wq"], 8)
            self.load_w(wo, Bwo, self.dram["xa_wo"][l].rearrange("(k p) c -> p k c", p=128), self.dbuf["xa_wo"], 8)
            xts = [(es.enter_context(self.sbuf("xax%d" % i, [128, 4, D], F32)), [Buf() for _ in range(4)]) for i in range(2)]
            hTs = self.tiles(es, "xah", [128, 8, 512], BF16, 2)
            qTs = self.tiles(es, "xaq", [128, 2, 512], BF16, 2)
            Es = self.tiles(es, "xaE", [128, 2, 512], BF16, 2)
            rd = self.tiles(es, "xard", [128, 512], F32, 2)
            oTs = self.tiles(es, "xao", [128, 8, 512], BF16, 2)
            xss = self.tiles(es, "xaxs", [128, D], BF16, 2)
            junk = es.enter_context(self.sbuf("xajunk", [128, D], BF16)); Bjunk = Buf()
            st = self.tiles(es, "xast", [128, 4], F32, 2)
            cnt = 0
            hc = 0
            for blk in range(T // 512):
                sq = (blk * 512) // SEQ
                tok0 = blk * 512
                xt, Bx = xts[blk % 2]
                hT, BhT = hTs[blk % 2]
                oT, BoT = oTs[blk % 2]
                if blk == 0:
                    for j in range(4):
                        S.dma("sp", xt[:, j, :], xs_ap[tok0 + j * 128:tok0 + (j + 1) * 128, :], reads=[Bxs], writes=[Bx[j]])
                if blk + 1 < T // 512:
                    xtn, Bxn = xts[(blk + 1) % 2]
                    for j in range(4):
                        S.dma("sp", xtn[:, j, :], xs_ap[tok0 + 512 + j * 128:tok0 + 512 + (j + 1) * 128, :], reads=[Bxs], writes=[Bxn[j]])
                for j in range(4):
                    s_, Bs = st[cnt % 2]; xs, Bxs_ = xss[cnt % 2]; cnt += 1
                    A(lambda: nc.scalar.activation(out=junk[:], in_=xt[:, j, :], func=AF.Square, accum_out=s_[:, 0:1]), [Bx[j]], [Bjunk, Bs])
                    A(lambda: nc.scalar.activation(out=s_[:, 1:2], in_=s_[:, 0:1], func=AF.Sqrt, scale=1.0 / D, bias=self.eps6[:, 0:1]), [Bs, Bc], [Bs])
                    V(lambda: nc.vector.reciprocal(out=s_[:, 2:3], in_=s_[:, 1:2]), [Bs], [Bs])
                    V(lambda: nc.vector.tensor_scalar(out=xs[:], in0=xt[:, j, :], scalar1=s_[:, 2:3], scalar2=None, op0=ALU.mult), [Bx[j], Bs], [Bxs_])
                    pt, Bp = self.psum()
                    pb = pt[:].bitcast(BF16)
                    for kc in range(8):
                        P(lambda: nc.tensor.transpose(out=pb[:, kc * 128:(kc + 1) * 128], in_=xs[:, kc * 128:(kc + 1) * 128], identity=self.ident_b[:]), [Bxs_, Bc], [Bp])
                    V(lambda: nc.vector.tensor_tensor(out=hT[:, :, j * 128:(j + 1) * 128], in0=pb.rearrange("p (k t) -> p k t", k=8),
                                                      in1=self.vcol(l, "norm_xattn", 0, 8).unsqueeze(2).to_broadcast([128, 8, 128]), op=ALU.mult), [Bp, self.Bvecs], [BhT])
                for h in range(4):
                    qT, BqT = qTs[hc % 2]; E, BE = Es[hc % 2]; r_, Br = rd[hc % 2]; hc += 1
                    for dt in range(2):
                        pt, Bp = self.psum()
                        c0 = h * 256 + dt * 128
                        for kc in range(8):
                            P(lambda: nc.tensor.matmul(pt[:], lhsT=wq[:, kc, c0:c0 + 128], rhs=hT[:, kc, :], start=(kc == 0), stop=(kc == 7)), [Bwq[kc // 4], BhT], [Bp])
                        A(lambda: nc.scalar.copy(out=qT[:, dt, :], in_=pt[:]), [Bp], [BqT])
                    for mt in range(2):
                        ms = slice(sq * NMEM + mt * 128, sq * NMEM + (mt + 1) * 128)
                        pt, Bp = self.psum()
                        for dt in range(2):
                            P(lambda: nc.tensor.matmul(pt[:], lhsT=kT[:, h * 2 + dt, ms], rhs=qT[:, dt, :], start=(dt == 0), stop=(dt == 1)), [BkT, BqT], [Bp])
                        A(lambda: nc.scalar.activation(out=E[:, mt, :], in_=pt[:], func=AF.Exp, scale=1.0 / 16), [Bp], [BE])
                    pt, Bp = self.psum()
                    for mt in range(2):
                        P(lambda: nc.tensor.matmul(pt[:], lhsT=ones_b[:], rhs=E[:, mt, :], start=(mt == 0), stop=(mt == 1)), [Bob, BE], [Bp])
                    V(lambda: nc.vector.reciprocal(out=r_[:], in_=pt[:]), [Bp], [Br])
                    for dt in range(2):
                        pt, Bp = self.psum()
                        c0 = h * 256 + dt * 128
                        for mt in range(2):
                            P(lambda: nc.tensor.matmul(pt[:], lhsT=vtm[:, sq * 2 + mt, c0:c0 + 128], rhs=E[:, mt, :], start=(mt == 0), stop=(mt == 1)), [Bvtm, BE], [Bp])
                        V(lambda: nc.vector.tensor_tensor(out=oT[:, h * 2 + dt, :], in0=pt[:], in1=r_[:], op=ALU.mult), [Bp, Br], [BoT])
                self.outproj_res(lambda kc, j: oT[:, kc, j * 128:(j + 1) * 128], BoT, 8, wo, lambda kc: Bwo[kc // 4], xt, Bx, 4, xs_ap, Bxs, tok0)
            S.barrier()

    def stage_ffn(self, l):
        nc, S = self.nc, self.S
        V, A, P = self.V, self.A, self.P
        Bc, Bv = self.Bconst, self.Bvecs
        xs_ap, Bxs = self.dram["xres"], self.dbuf["xres"]
        aT, BaT = self.dram["actT"], self.dbuf["actT"]
        L = SEQ
        NP_ = DFF // 128
        with ExitStack() as es:
            hT = es.enter_context(self.sbuf("ffh", [128, 8, T], BF16)); BhT = Buf("ffh")
            with ExitStack() as e2:
                self.rmsnorm_fm(e2, xs_ap, Bxs, T, self.vcol(l, "norm_ffn", 0, 8), hT, BhT, self.eps6[:, 0:1])
                S.barrier()
            with ExitStack() as e2:
                wts = [(e2.enter_context(self.sbuf("ffw%d" % i, [128, 8, 256], BF16)), [Buf(), Buf()]) for i in range(2)]
                ug = self.tiles(e2, "ffug", [128, L + 2], BF16, 3)
                uv = self.tiles(e2, "ffuv", [128, L + 2], BF16, 3)
                dgs = self.tiles(e2, "ffdg", [128, 2, 3, 128], BF16, 2)
                sg = self.tiles(e2, "ffsg", [128, 512], F32, 4)
                ao = self.tiles(e2, "ffao", [128, L], BF16, 2)
                for t_, B_ in ug + uv:
                    V(lambda: nc.vector.memset(t_[:, 0:2], 0.0), [], [B_])
                wup = self.dram["ffn_w_up"][l]
                sc_box = [0]
                items = [(i, sq) for i in range(NP_) for sq in range(NSEQ)]

                def proj_part(n):
                    i, sq = items[n]
                    wt, Bw = wts[i % 2]
                    dg, Bdg = dgs[i % 2]
                    if sq == 0:
                        S.dma("pool", wt[:, :, 0:128], wup[:, 128 * i:128 * i + 128].rearrange("(k p) c -> p k c", p=128), reads=[self.dbuf["ffn_w_up"]], writes=[Bw[0]])
                        S.dma("pool", wt[:, :, 128:256], wup[:, DFF + 128 * i:DFF + 128 * i + 128].rearrange("(k p) c -> p k c", p=128), reads=[self.dbuf["ffn_w_up"]], writes=[Bw[1]])
                        for half in range(2):
                            col = i + half * NP_
                            for j in range(3):
                                V(lambda: nc.vector.tensor_scalar(out=dg[:, half, j, :], in0=self.ident_f[:], scalar1=self.vcol(l, "ffn_conv%d" % j, col, 1), scalar2=None, op0=ALU.mult),
                                  [Bc, Bv], [Bdg])
                    g_, Bg = ug[n % 3]; v_, Bvv = uv[n % 3]
                    for tb in range(L // 512):
                        ts = slice(sq * L + tb * 512, sq * L + tb * 512 + 512)
                        for half, (dst, Bd) in enumerate([(g_, Bg), (v_, Bvv)]):
                            pt, Bp = self.psum()
                            for kc in range(8):
                                P(lambda: nc.tensor.matmul(pt[:], lhsT=wt[:, kc, half * 128:(half + 1) * 128], rhs=hT[:, kc, ts], start=(kc == 0), stop=(kc == 7)),
                                  [Bw[half], BhT], [Bp])
                            A(lambda: nc.scalar.copy(out=dst[:, 2 + tb * 512:2 + tb * 512 + 512], in_=pt[:]), [Bp], [Bd])

                def conv_part(n):
                    i, sq = items[n]
                    dg, Bdg = dgs[i % 2]
                    g_, Bg = ug[n % 3]; v_, Bvv = uv[n % 3]; a_, Ba = ao[n % 2]
                    for tb in range(L // 512):
                        s_, Bs = sg[sc_box[0] % 4]; sc_box[0] += 1
                        ptg, Bpg = self.psum()
                        for j in range(3):
                            P(lambda: nc.tensor.matmul(ptg[:], lhsT=dg[:, 0, j, :], rhs=g_[:, tb * 512 + j:tb * 512 + j + 512], start=(j == 0), stop=(j == 2)), [Bdg, Bg], [Bpg])
                        ptv, Bpv = self.psum()
                        for j in range(3):
                            P(lambda: nc.tensor.matmul(ptv[:], lhsT=dg[:, 1, j, :], rhs=v_[:, tb * 512 + j:tb * 512 + j + 512], start=(j == 0), stop=(j == 2)), [Bdg, Bvv], [Bpv])
                        A(lambda: nc.scalar.activation(out=s_[:], in_=ptg[:], func=AF.Sigmoid, bias=self.vcol(l, "ffn_conv_b", i, 1)), [Bpg, Bv], [Bs])
                        V(lambda: nc.vector.scalar_tensor_tensor(out=s_[:], in0=ptg[:], scalar=self.vcol(l, "ffn_conv_b", i, 1), in1=s_[:], op0=ALU.add, op1=ALU.mult),
                          [Bpg, Bs, Bv], [Bs])
                        V(lambda: nc.vector.scalar_tensor_tensor(out=a_[:, tb * 512:tb * 512 + 512], in0=ptv[:], scalar=self.vcol(l, "ffn_conv_b", i + NP_, 1), in1=s_[:],
                                                                 op0=ALU.add, op1=ALU.mult), [Bpv, Bs, Bv], [Ba])
                    S.dma("sp", aT[128 * i:128 * i + 128, sq * L:(sq + 1) * L], a_[:], reads=[Ba], writes=[BaT])

                proj_part(0)
                for n in range(len(items)):
                    if n + 1 < len(items):
                        proj_part(n + 1)
                    conv_part(n)
                S.barrier()
        with ExitStack() as es:
            wd = es.enter_context(self.sbuf("ffwd", [128, NP_, D], BF16)); Bwd = [Buf() for _ in range((NP_ + 3) // 4)]
            self.load_w(wd, Bwd, self.dram["ffn_w_down"][l].rearrange("(k p) c -> p k c", p=128), self.dbuf["ffn_w_down"], NP_)
            xts = [(es.enter_context(self.sbuf("ffx%d" % i, [128, 4, D], F32)), [Buf() for _ in range(4)]) for i in range(2)]
            ats = [(es.enter_context(self.sbuf("ffat%d" % i, [128, NP_, 512], BF16)), [Buf(), Buf()]) for i in range(2)]
            def ldb(blk):
                tok0 = blk * 512
                xt, Bx = xts[blk % 2]
                at, Bat = ats[blk % 2]
                S.dma("sp", at[:, 0:11, :], aT[0:11 * 128, tok0:tok0 + 512].rearrange("(k p) t -> p k t", p=128), reads=[BaT], writes=[Bat[0]])
                S.dma("sp", at[:, 11:22, :], aT[11 * 128:22 * 128, tok0:tok0 + 512].rearrange("(k p) t -> p k t", p=128), reads=[BaT], writes=[Bat[1]])
                for j in range(4):
                    S.dma("sp", xt[:, j, :], xs_ap[tok0 + j * 128:tok0 + (j + 1) * 128, :], reads=[Bxs], writes=[Bx[j]])
            ldb(0)
            for blk in range(T // 512):
                tok0 = blk * 512
                xt, Bx = xts[blk % 2]
                at, Bat = ats[blk % 2]
                if blk + 1 < T // 512:
                    ldb(blk + 1)
                self.outproj_res(lambda kc, j: at[:, kc, j * 128:(j + 1) * 128], (lambda kc: Bat[0 if kc < 11 else 1]), NP_, wd, lambda kc: Bwd[kc // 4], xt, Bx, 4, xs_ap, Bxs, tok0)
            S.barrier()

    def stage_final(self):
        nc, S = self.nc, self.S
        V, A, P = self.V, self.A, self.P
        Bc = self.Bconst
        xs_ap, Bxs = self.dram["xres"], self.dbuf["xres"]
        with ExitStack() as es:
            gbc = es.enter_context(self.sbuf("fngb", [128, D], F32)); Bg = Buf()
            S.dma("sp", gbc[:], self.dram["norm_final_bc"], reads=[self.dbuf["norm_final_bc"]], writes=[Bg])
            xts = self.tiles(es, "fnx", [128, D], F32, 3)
            junk = es.enter_context(self.sbuf("fnjunk", [128, D], BF16)); Bjunk = Buf()
            st = self.tiles(es, "fnst", [128, 4], F32, 2)
            for tt in range(T // 128):
                xt, Bx = xts[tt % 3]
                s_, Bs = st[tt % 2]
                S.dma("sp", xt[:], xs_ap[tt * 128:(tt + 1) * 128, :], reads=[Bxs], writes=[Bx])
                A(lambda: nc.scalar.activation(out=junk[:], in_=xt[:], func=AF.Square, accum_out=s_[:, 0:1]), [Bx], [Bjunk, Bs])
                A(lambda: nc.scalar.activation(out=s_[:, 1:2], in_=s_[:, 0:1], func=AF.Sqrt, scale=1.0 / D, bias=self.eps6[:, 0:1]), [Bs, Bc], [Bs])
                V(lambda: nc.vector.reciprocal(out=s_[:, 2:3], in_=s_[:, 1:2]), [Bs], [Bs])
                V(lambda: nc.vector.scalar_tensor_tensor(out=xt[:], in0=xt[:], scalar=s_[:, 2:3], in1=gbc[:], op0=ALU.mult, op1=ALU.mult), [Bx, Bs, Bg], [Bx])
                S.dma("sp", self.out[tt * 128:(tt + 1) * 128, :], xt[:], reads=[Bx], writes=[self.Bout])
            S.barrier()

    def finish(self):
        S = self.S
        S.barrier()
        print("ops", S.n_ops, "waits", S.n_waits, "sems", S.nsem + S.n_dma_sems)


def build(debug=None, n_layers=DEPTH, final=True):
    k = K(debug, n_layers)
    k.stage_memn()
    for l in range(n_layers):
        k.stage_mixproj(l)
        for sq in range(NSEQ):
            k.stage_rwkv(l, sq)
        k.stage_s5(l)
        for sq in range(NSEQ):
            k.stage_gla(l, sq)
        k.stage_merge(l)
        k.stage_xattn(l)
        k.stage_ffn(l)
    if final:
        k.stage_final()
    k.finish()
    return k


def make_in_maps(inp):
    vecs, gvec = pack_vecs(inp)
    s5w = pack_s5(inp)
    maps = []
    for c in range(NCORES):
        m = {
            "x": np.ascontiguousarray(inp["x"][NSEQ * c:NSEQ * (c + 1)].reshape(T, D)),
            "mem": np.ascontiguousarray(inp["mem"][NSEQ * c:NSEQ * (c + 1)].reshape(NSEQ * NMEM, D)),
            "vecs": vecs, "gvec": gvec,
            "w_in": inp["w_in"],
            "gla_gk_up": inp["gla_gk_up"],
            "s5w": s5w, "s5_w_glu": inp["s5_w_glu"],
            "w_branch": inp["w_branch"], "w_out": inp["w_out"], "xa_wq": inp["xa_wq"], "xa_wkv": inp["xa_wkv"], "xa_wo": inp["xa_wo"],
            "ffn_w_up": inp["ffn_w_up"], "ffn_w_down": inp["ffn_w_down"],
            "norm_final_bc": np.ascontiguousarray(np.broadcast_to(np.asarray(inp["norm_final"], np.float32)[None, :], (128, D))),
            "rw_w_up": inp["rw_w_up"], "rw_a_up": inp["rw_a_up"], "rw_g_up": inp["rw_g_up"],
        }
        maps.append(m)
    return maps


def kernel(**inp):
    inp = {k: np.asarray(v) for k, v in inp.items()}
    k = build()
    res = run_bass_kernel_spmd(k.nc, make_in_maps(inp), core_ids=list(range(NCORES)))
    out = np.concatenate([r["out"].reshape(NSEQ, SEQ, D) for r in res.results], axis=0)
    return out.astype(np.float32)
```
